# Optimizing a Trainium2 kernel written in Bass

```python
import math
import jax
import jax.numpy as jnp
from jax import lax
import numpy as np

D_MODEL = 1024
BATCH = 2
SEQ = 8192
DEPTH = 2

CHUNK = 64
Q_BLOCK = 128
EPS = 1e-6
ROPE_THETA = 10000.0

A_HEADS = D_MODEL // 128
A_DK = 64
A_DV = 64
B_HEADS = D_MODEL // 128
B_DH = 64
C_DH = 64
C_DV = 2 * C_DH
C_HEADS = D_MODEL // C_DV
N_GROUPS = 4
EXPERTS_PER_GROUP = 8
TOP_K_IN_GROUP = 2
D_EXPERT = D_MODEL // 2

N_EVEN = (DEPTH + 1) // 2
N_ODD = DEPTH // 2

_EVEN_COLS = (A_HEADS * A_DK, A_HEADS * A_DK, A_HEADS * A_DV, A_HEADS * A_DV,
              B_HEADS * B_DH, B_HEADS * B_DH, B_HEADS * B_DH, B_HEADS)
EVEN_IN = sum(_EVEN_COLS)
EVEN_SPLITS = tuple(int(c) for c in np.cumsum(_EVEN_COLS)[:-1])
EVEN_OUT = A_HEADS * A_DV + B_HEADS * B_DH
ODD_IN = C_HEADS * (2 * C_DH + 2 * C_DH + C_DV)
ODD_OUT = C_HEADS * C_DV

kernel_name = 'hybrid_hgrn2_fox_diffattn_hmoe'


def rms_norm(x, gain):
    xf = x.astype(jnp.float32)
    y = xf * lax.rsqrt(jnp.mean(jnp.square(xf), axis=-1, keepdims=True) + EPS)
    return (y * gain.astype(jnp.float32)).astype(x.dtype)


def apply_rope(x, positions):
    half = x.shape[-1] // 2
    inv_freq = ROPE_THETA ** (-jnp.arange(half, dtype=jnp.float32) / half)
    ang = positions.astype(jnp.float32)[..., None] * inv_freq
    ang = ang.reshape(ang.shape[:2] + (1,) * (x.ndim - 3) + (half,))
    cos, sin = jnp.cos(ang), jnp.sin(ang)
    xf = x.astype(jnp.float32)
    x1, x2 = xf[..., :half], xf[..., half:]
    return jnp.concatenate([x1 * cos - x2 * sin, x2 * cos + x1 * sin], axis=-1).astype(x.dtype)


def hgrn2_recurrence(q, f_logit, i, g, lb, out_norm):
    B_, S_, H, DK = q.shape
    DV = i.shape[-1]
    nc = S_ // CHUNK
    qf = jax.nn.silu(q.astype(jnp.float32))
    f = lb + (1.0 - lb) * jax.nn.sigmoid(f_logit.astype(jnp.float32))
    log_f = jnp.log(f)
    k = 1.0 - f

    def to_chunks(t):
        return t.reshape(B_, nc, CHUNK, H, t.shape[-1]).transpose(1, 0, 3, 2, 4)

    qc, kc, vc, lfc = (to_chunks(t) for t in (qf, k, i.astype(jnp.float32), log_f))
    causal = jnp.tril(jnp.ones((CHUNK, CHUNK), dtype=bool))[:, :, None]

    def step(state, inp):
        qb, kb, vb, lf = inp
        cum = jnp.cumsum(lf, axis=2)
        rel = cum[:, :, :, None, :] - cum[:, :, None, :, :]
        decay = jnp.exp(jnp.where(causal, rel, -jnp.inf))
        scores = jnp.einsum('bhtsk,bhsk->bhts', qb[:, :, :, None, :] * decay, kb)
        o = (jnp.einsum('bhts,bhsv->bhtv', scores, vb)
             + jnp.einsum('bhtk,bhkv->bhtv', qb * jnp.exp(cum), state))
        last = cum[:, :, -1:, :]
        state = (jnp.exp(last[:, :, 0, :])[..., None] * state
                 + jnp.einsum('bhsk,bhsv->bhkv', kb * jnp.exp(last - cum), vb))
        return state, o

    state0 = jnp.zeros((B_, H, DK, DV), jnp.float32)
    _, o = lax.scan(step, state0, (qc, kc, vc, lfc))
    o = o.transpose(1, 0, 3, 2, 4).reshape(B_, S_, H, DV)
    o = rms_norm(o, out_norm) * jax.nn.silu(g.astype(jnp.float32))
    return o.astype(q.dtype)


def forgetting_attention(q, k, v, f_logit):
    B_, S_, H, Dh = q.shape
    scale = Dh ** -0.5
    log_f = jax.nn.log_sigmoid(f_logit.astype(jnp.float32))
    cum = jnp.cumsum(log_f, axis=1).transpose(0, 2, 1)
    qh, kh, vh = (t.transpose(0, 2, 1, 3) for t in (q, k, v))
    outs = []
    for q0 in range(0, S_, Q_BLOCK):
        q1 = q0 + Q_BLOCK
        logits = jnp.einsum('bhtd,bhsd->bhts', qh[:, :, q0:q1], kh[:, :, :q1]).astype(jnp.float32) * scale
        logits = logits + cum[:, :, q0:q1, None] - cum[:, :, None, :q1]
        t_pos = jnp.arange(q0, q1)[:, None]
        s_pos = jnp.arange(q1)[None, :]
        logits = jnp.where(s_pos <= t_pos, logits, -jnp.inf)
        p = jax.nn.softmax(logits, axis=-1).astype(v.dtype)
        outs.append(jnp.einsum('bhts,bhsd->bhtd', p, vh[:, :, :q1]))
    return jnp.concatenate(outs, axis=2).transpose(0, 2, 1, 3)


def differential_attention(q, k, v, lam, lambda_init, subln):
    B_, S_, H, _, Dh = q.shape
    scale = Dh ** -0.5
    qh = q.transpose(0, 2, 3, 1, 4)
    kh = k.transpose(0, 2, 3, 1, 4)
    vh = v.transpose(0, 2, 1, 3)
    outs = []
    for q0 in range(0, S_, Q_BLOCK):
        q1 = q0 + Q_BLOCK
        logits = jnp.einsum('bhmtd,bhmsd->bhmts', qh[:, :, :, q0:q1], kh[:, :, :, :q1]).astype(jnp.float32) * scale
        t_chunk = (jnp.arange(q0, q1) // CHUNK)[:, None]
        s_chunk = (jnp.arange(q1) // CHUNK)[None, :]
        logits = jnp.where(s_chunk <= t_chunk, logits, -jnp.inf)
        p = jax.nn.softmax(logits, axis=-1)
        p_diff = (p[:, :, 0] - lam * p[:, :, 1]).astype(v.dtype)
        outs.append(jnp.einsum('bhts,bhsv->bhtv', p_diff, vh[:, :, :q1]))
    o = jnp.concatenate(outs, axis=2).transpose(0, 2, 1, 3)
    return rms_norm(o, subln) * (1.0 - lambda_init)


def even_mixer(u, w_in, w_out, lb, fox_f_bias, hgrn_out_norm, fox_q_norm, fox_k_norm):
    B_, S_, _ = u.shape
    proj = u @ w_in
    a_q, a_f, a_i, a_g, b_q, b_k, b_v, b_f = jnp.split(proj, EVEN_SPLITS, axis=-1)
    heads = lambda t, h: t.reshape(B_, S_, h, t.shape[-1] // h)
    o_a = hgrn2_recurrence(heads(a_q, A_HEADS), heads(a_f, A_HEADS), heads(a_i, A_HEADS),
                           heads(a_g, A_HEADS), lb, hgrn_out_norm)
    o_b = forgetting_attention(rms_norm(heads(b_q, B_HEADS), fox_q_norm),
                               rms_norm(heads(b_k, B_HEADS), fox_k_norm),
                               heads(b_v, B_HEADS), b_f + fox_f_bias)
    o = jnp.concatenate([o_a.reshape(B_, S_, -1), o_b.reshape(B_, S_, -1)], axis=-1)
    return o @ w_out


def odd_mixer(u, positions, w_in, w_out, q_norm, k_norm, lq1, lk1, lq2, lk2, subln, lambda_init):
    B_, S_, _ = u.shape
    q, k, v = jnp.split(u @ w_in, 3, axis=-1)
    q = apply_rope(rms_norm(q.reshape(B_, S_, C_HEADS, 2, C_DH), q_norm), positions)
    k = apply_rope(rms_norm(k.reshape(B_, S_, C_HEADS, 2, C_DH), k_norm), positions)
    v = v.reshape(B_, S_, C_HEADS, C_DV)
    f32 = jnp.float32
    lam = (jnp.exp(jnp.sum(lq1.astype(f32) * lk1.astype(f32)))
           - jnp.exp(jnp.sum(lq2.astype(f32) * lk2.astype(f32))) + lambda_init)
    o = differential_attention(q, k, v, lam, lambda_init, subln)
    return o.reshape(B_, S_, ODD_OUT) @ w_out


def hierarchical_moe(u, w_router_group, w_router_expert, w_gate, w_up, w_down):
    B_, S_, D = u.shape
    x = u.reshape(B_ * S_, D)
    grp_logits = (x @ w_router_group).astype(jnp.float32)
    p_grp = jax.nn.softmax(grp_logits, axis=-1)
    g_onehot = jax.nn.one_hot(jnp.argmax(grp_logits, axis=-1), N_GROUPS, dtype=jnp.float32)
    p_g = jnp.sum(p_grp * g_onehot, axis=-1, keepdims=True)
    exp_logits = jnp.einsum('nd,gde->nge', x, w_router_expert).astype(jnp.float32)
    sel_logits = jnp.einsum('nge,ng->ne', exp_logits, g_onehot)
    p_exp = jax.nn.softmax(sel_logits, axis=-1)
    top_w, top_i = lax.top_k(p_exp, TOP_K_IN_GROUP)
    top_w = top_w / jnp.sum(top_w, axis=-1, keepdims=True)
    w_in_group = jnp.sum(jax.nn.one_hot(top_i, EXPERTS_PER_GROUP, dtype=jnp.float32) * top_w[..., None], axis=1)
    combine = (g_onehot[:, :, None] * (p_g[:, :, None] * w_in_group[:, None, :])).astype(x.dtype)
    y = jnp.zeros_like(x)
    for g in range(N_GROUPS):
        hid = jax.nn.silu(jnp.einsum('nd,edf->nef', x, w_gate[g])) * jnp.einsum('nd,edf->nef', x, w_up[g])
        y = y + jnp.einsum('nef,efd->nd', hid * combine[:, g, :, None], w_down[g])
    return y.reshape(B_, S_, D)


def setup_inputs(seed: int = 0) -> dict:
    key = jax.random.key(seed)
    ks = jax.random.split(key, 32)
    f32 = jnp.float32

    def nrm(k, shape, scale):
        return jax.random.normal(k, shape, f32) * scale

    def gain(k, shape):
        return 1.0 + 0.05 * jax.random.normal(k, shape, f32)

    res_scale = (2 * DEPTH) ** -0.5
    x = jax.random.normal(ks[0], (BATCH, SEQ, D_MODEL), f32)
    start = jax.random.randint(ks[1], (BATCH, 1), 0, 4096, dtype=jnp.int32)
    positions = start + jnp.arange(SEQ, dtype=jnp.int32)[None, :]
    return {
        'x': x,
        'positions': positions,
        'hgrn_lb_logits': nrm(ks[2], (N_EVEN + 1, A_HEADS * A_DK), 0.5),
        'norm_mix': gain(ks[3], (DEPTH, D_MODEL)),
        'norm_ffn': gain(ks[4], (DEPTH, D_MODEL)),
        'even_w_in': nrm(ks[5], (N_EVEN, D_MODEL, EVEN_IN), D_MODEL ** -0.5),
        'even_w_out': nrm(ks[6], (N_EVEN, EVEN_OUT, D_MODEL), EVEN_OUT ** -0.5 * res_scale),
        'fox_f_bias': jnp.linspace(1.0, 4.0, B_HEADS, dtype=f32)[None, :] + nrm(ks[7], (N_EVEN, B_HEADS), 0.1),
        'hgrn_out_norm': gain(ks[8], (N_EVEN, A_DV)),
        'fox_q_norm': gain(ks[9], (N_EVEN, B_DH)),
        'fox_k_norm': gain(ks[10], (N_EVEN, B_DH)),
        'odd_w_in': nrm(ks[11], (N_ODD, D_MODEL, ODD_IN), D_MODEL ** -0.5),
        'odd_w_out': nrm(ks[12], (N_ODD, ODD_OUT, D_MODEL), ODD_OUT ** -0.5 * res_scale),
        'diff_q_norm': gain(ks[13], (N_ODD, C_DH)),
        'diff_k_norm': gain(ks[14], (N_ODD, C_DH)),
        'diff_lambda_q1': nrm(ks[15], (N_ODD, C_DH), 0.1),
        'diff_lambda_k1': nrm(ks[16], (N_ODD, C_DH), 0.1),
        'diff_lambda_q2': nrm(ks[17], (N_ODD, C_DH), 0.1),
        'diff_lambda_k2': nrm(ks[18], (N_ODD, C_DH), 0.1),
        'diff_subln': gain(ks[19], (N_ODD, C_DV)),
        'moe_router_group': nrm(ks[20], (DEPTH, D_MODEL, N_GROUPS), D_MODEL ** -0.5),
        'moe_router_expert': nrm(ks[21], (DEPTH, N_GROUPS, D_MODEL, EXPERTS_PER_GROUP), D_MODEL ** -0.5),
        'moe_w_gate': nrm(ks[22], (DEPTH, N_GROUPS, EXPERTS_PER_GROUP, D_MODEL, D_EXPERT), D_MODEL ** -0.5),
        'moe_w_up': nrm(ks[23], (DEPTH, N_GROUPS, EXPERTS_PER_GROUP, D_MODEL, D_EXPERT), D_MODEL ** -0.5),
        'moe_w_down': nrm(ks[24], (DEPTH, N_GROUPS, EXPERTS_PER_GROUP, D_EXPERT, D_MODEL), D_EXPERT ** -0.5 * res_scale),
    }


def reference(x, positions, hgrn_lb_logits, norm_mix, norm_ffn, even_w_in, even_w_out, fox_f_bias,
              hgrn_out_norm, fox_q_norm, fox_k_norm, odd_w_in, odd_w_out, diff_q_norm, diff_k_norm,
              diff_lambda_q1, diff_lambda_k1, diff_lambda_q2, diff_lambda_k2, diff_subln,
              moe_router_group, moe_router_expert, moe_w_gate, moe_w_up, moe_w_down):
    lower_bounds = jnp.cumsum(jax.nn.softmax(hgrn_lb_logits.astype(jnp.float32), axis=0), axis=0)
    h = x
    for layer in range(DEPTH):
        j = layer // 2
        u = rms_norm(h, norm_mix[layer])
        if layer % 2 == 0:
            mix = even_mixer(u, even_w_in[j], even_w_out[j], lower_bounds[j].reshape(A_HEADS, A_DK),
                             fox_f_bias[j], hgrn_out_norm[j], fox_q_norm[j], fox_k_norm[j])
        else:
            lambda_init = 0.8 - 0.6 * math.exp(-0.3 * layer)
            mix = odd_mixer(u, positions, odd_w_in[j], odd_w_out[j], diff_q_norm[j], diff_k_norm[j],
                            diff_lambda_q1[j], diff_lambda_k1[j], diff_lambda_q2[j], diff_lambda_k2[j],
                            diff_subln[j], lambda_init)
        h = h + mix
        h = h + hierarchical_moe(rms_norm(h, norm_ffn[layer]), moe_router_group[layer], moe_router_expert[layer],
                                 moe_w_gate[layer], moe_w_up[layer], moe_w_down[layer])
    return h
```

```python
import ml_dtypes
import numpy as np
from contextlib import ExitStack
import concourse.bass as bass
import concourse.mybir as mybir
from concourse.bass_utils import run_bass_kernel_spmd

F32 = mybir.dt.float32
BF16 = mybir.dt.bfloat16
I32 = mybir.dt.int32
AF = mybir.ActivationFunctionType
ALU = mybir.AluOpType
AX = mybir.AxisListType

ENGS = ("pe", "act", "dve", "pool", "sp")


class Res:
    __slots__ = ("name", "last_w", "readers", "dsem")

    def __init__(self, name):
        self.name = name
        self.last_w = None
        self.readers = []
        self.dsem = None


class Op:
    __slots__ = ("eng", "fn", "deps", "is_dma", "sem", "semval", "signal", "count", "dma_waits", "idx")

    def __init__(self, eng, fn):
        self.eng = eng
        self.fn = fn
        self.deps = []
        self.is_dma = False
        self.sem = None
        self.semval = 0
        self.signal = False
        self.count = 0
        self.dma_waits = []
        self.idx = 0


class Prog:
    def __init__(self, nc):
        self.nc = nc
        self.ops = {e: [] for e in ENGS}
        self.all_res_reset = True
        self.esem = {e: nc.alloc_semaphore("es_" + e) for e in ENGS}
        self.dsems = []
        self.dma_cum = {}
        self.nops = 0
        self.ecount = {e: 0 for e in ENGS}
        self.waited = {e: {} for e in ENGS}

    def res(self, name, n=1):
        return [Res("%s.%d" % (name, i)) for i in range(n)]

    def new_dsem(self, name):
        s = self.nc.alloc_semaphore("ds_" + name)
        self.dma_cum[s] = 0
        return s

    def op(self, eng, fn, reads=(), writes=(), dma_sem=None):
        o = Op(eng, fn)
        o.idx = self.nops
        self.nops += 1
        deps = {}
        dma_waits = {}

        def add_dep(p):
            if p is None:
                return
            if p.is_dma:
                dma_waits[p.sem] = self.dma_cum[p.sem]
            else:
                deps[id(p)] = p

        for r in reads:
            add_dep(r.last_w)
        for r in writes:
            add_dep(r.last_w)
            for q in r.readers:
                add_dep(q)
        raw = set()
        for r in reads:
            if r.last_w is not None and not r.last_w.is_dma:
                raw.add(id(r.last_w))
        for k, p in list(deps.items()):
            if p.eng == eng and eng == "pe" and dma_sem is None:
                del deps[k]
        o.deps = list(deps.values())
        for p in o.deps:
            p.signal = True
        o.dma_waits = list(dma_waits.items())
        if dma_sem is not None:
            o.is_dma = True
            o.sem = dma_sem
            self.dma_cum[dma_sem] += 16
            o.semval = self.dma_cum[dma_sem]
        for r in reads:
            r.readers.append(o)
        for r in writes:
            r.last_w = o
            r.readers = []
        self.ops[eng].append(o)
        return o

    def finish_wait(self, eng="sp"):
        waits = [(s, v) for s, v in self.dma_cum.items() if v > 0]
        o = Op(eng, None)
        o.dma_waits = waits
        self.ops[eng].append(o)

    def replay(self):
        nc = self.nc
        for e in ENGS:
            last = None
            for o in self.ops[e]:
                if o.fn is not None and not o.is_dma:
                    last = o
            if last is not None:
                last.signal = True
            c = self.ecount[e]
            for o in self.ops[e]:
                if o.signal and not o.is_dma:
                    c += 1
                    o.count = c
            self.ecount[e] = c
        final = {self.esem[e]: self.ecount[e] for e in ENGS if self.ecount[e] > 0}
        for s_, v_ in self.dma_cum.items():
            if v_ > 0:
                final[s_] = v_
        handles = {"pe": "tensor", "act": "scalar", "dve": "vector", "pool": "gpsimd", "sp": "sync"}
        with nc.Block() as block:
            for e in ENGS:
                ops = self.ops[e]
                esem = self.esem
                my = esem[e]

                def body(engh, ops=ops, e=e, my=my):
                    waited = self.waited[e]
                    for o in ops:
                        need = {}
                        for p in o.deps:
                            s = esem[p.eng]
                            if need.get(s, 0) < p.count:
                                need[s] = p.count
                        for s, v in o.dma_waits:
                            if need.get(s, 0) < v:
                                need[s] = v
                        for s, v in need.items():
                            if waited.get(s, 0) < v:
                                engh.wait_ge(s, v)
                                waited[s] = v
                        if o.fn is None:
                            continue
                        ins = o.fn(engh)
                        if o.is_dma:
                            ins.then_inc(o.sem, 16)
                        elif o.signal:
                            ins.then_inc(my, 1)
                    for s, v in final.items():
                        if s is my:
                            continue
                        if waited.get(s, 0) < v:
                            engh.wait_ge(s, v)
                            waited[s] = v

                getattr(block, handles[e])(body)
        self.ops = {e: [] for e in ENGS}
        self.all_res_reset = True


from math import prod
import os
HG = int(os.environ.get('HG_STAGE', '9'))


class TT:
    def __init__(self, P, st, name, shape, dt, bs=None, psum=False, dsem=False):
        nc = P.nc
        self.h = st.enter_context(nc.psum_tensor(name, shape, dt) if psum else nc.sbuf_tensor(name, shape, dt))
        self.F = prod(shape[1:])
        self.bs = bs or self.F
        self.res = P.res(name, (self.F + self.bs - 1) // self.bs)
        self.dsem = P.new_dsem(name) if dsem else None

    def r(self, lo=0, hi=None):
        hi = self.F if hi is None else hi
        return self.res[lo // self.bs:(hi - 1) // self.bs + 1]

    def __getitem__(self, k):
        return self.h[k]


def mm(P, out, lhsT, rhs, start, stop, rd, wr):
    return P.op("pe", lambda e: e.matmul(out, lhsT=lhsT, rhs=rhs, start=start, stop=stop), rd, wr)


def act(P, out, in_, func, rd, wr, scale=1.0, bias=None):
    if bias is None:
        return P.op("act", lambda e: e.activation(out=out, in_=in_, func=func, scale=scale), rd, wr)
    return P.op("act", lambda e: e.activation(out=out, in_=in_, func=func, scale=scale, bias=bias), rd, wr)


def tt(P, eng, out, in0, in1, op, rd, wr):
    return P.op(eng, lambda e: e.tensor_tensor(out=out, in0=in0, in1=in1, op=op), rd, wr)


def ts(P, eng, out, in0, s1, op0, rd, wr, s2=None, op1=None):
    if op1 is None:
        return P.op(eng, lambda e: e.tensor_scalar(out=out, in0=in0, scalar1=s1, scalar2=None, op0=op0), rd, wr)
    return P.op(eng, lambda e: e.tensor_scalar(out=out, in0=in0, scalar1=s1, scalar2=s2, op0=op0, op1=op1), rd, wr)


def stt(P, out, in0, scalar, in1, op0, op1, rd, wr):
    return P.op("dve", lambda e: e.scalar_tensor_tensor(out=out, in0=in0, scalar=scalar, in1=in1, op0=op0, op1=op1), rd, wr)


def cp(P, eng, out, in_, rd, wr):
    if eng == "act":
        return P.op("act", lambda e: e.copy(out=out, in_=in_), rd, wr)
    return P.op(eng, lambda e: e.tensor_copy(out=out, in_=in_), rd, wr)


def recip(P, out, in_, rd, wr):
    return P.op("dve", lambda e: e.reciprocal(out=out, in_=in_), rd, wr)


def dma(P, q, out, in_, rd, wr, sem):
    return P.op(q, lambda e: e.dma_start(out=out, in_=in_), rd, wr, dma_sem=sem)


def memset(P, eng, ap, val, wr):
    return P.op(eng, lambda e: e.memset(ap, val), (), wr)


T = int(os.environ.get('KT', 8192))
TOK = 2 * T
NCOL_A = 449
EPS = 1e-6


def build_A(nbatch=2, do_hgrn=True, do_fox=True, debug=False):
    nc = bass.Bass("TRN2", target_bir_lowering=False)
    xT = nc.dram_tensor("xT", [1024, TOK], F32, kind="ExternalInput").ap()
    wA = nc.dram_tensor("wA", [1024, NCOL_A], F32, kind="ExternalInput").ap()
    gmix = nc.dram_tensor("gmix", [128, 8], F32, kind="ExternalInput").ap()
    pA = nc.dram_tensor("pA", [64, 8], F32, kind="ExternalInput").ap()
    fbias = nc.dram_tensor("fbias", [128, 1], F32, kind="ExternalInput").ap()
    oT = nc.dram_tensor("oT", [128, TOK], BF16, kind="ExternalOutput").ap()
    P = Prog(nc)
    xT_v = xT.rearrange("(kc p) t -> p kc t", p=128)
    with ExitStack() as st:
        def SB(name, shape, dt, bs=None, dsem=False):
            return TT(P, st, name, shape, dt, bs=bs, dsem=dsem)

        ones_bf = SB("ones_bf", [128, 128], BF16)
        ones_f = SB("ones_f", [128, 64], F32)
        eps_c = SB("eps_c", [128, 1], F32)
        one_c = SB("one_c", [128, 1], F32)
        ident = SB("ident", [128, 128], BF16)
        M2 = SB("M2", [128, 128], BF16)
        TBH = 256
        rmask = SB("rmask", [64, TBH], F32)
        par = SB("par", [64, 8], F32, dsem=True)
        gm = SB("gm", [128, 8], F32, dsem=True)
        nfb = SB("nfb", [128, 1], F32, dsem=True)
        lbc = SB("lbc", [64, 4], F32)
        Wf = None
        Wb = SB("Wb", [128, 8, NCOL_A], BF16, dsem=True)

        memset(P, "pool", ones_bf[:], 1.0, ones_bf.r())
        memset(P, "pool", ones_f[:], 1.0, ones_f.r())
        memset(P, "pool", eps_c[:], EPS, eps_c.r())
        memset(P, "pool", one_c[:], 1.0, one_c.r())
        memset(P, "pool", ident[:], 1.0, ident.r())
        P.op("pool", lambda e: e.affine_select(out=ident[:], in_=ident[:], pattern=[[-1, 128]], compare_op=ALU.is_equal,
                                               fill=0.0, base=0, channel_multiplier=1), ident.r(), ident.r())
        memset(P, "pool", M2[:], 1.0, M2.r())
        P.op("pool", lambda e: e.affine_select(out=M2[:], in_=M2[:], pattern=[[1, 128]], compare_op=ALU.is_ge,
                                               fill=0.0, base=0, channel_multiplier=-1), M2.r(), M2.r())
        memset(P, "pool", M2[0:64, 64:128], 0.0, M2.r())
        memset(P, "pool", rmask[:], 1.0, rmask.r())
        memset(P, "pool", rmask[:].rearrange("p (c l) -> p c l", l=64)[:, :, 0:1], 0.0, rmask.r())

        dma(P, "sp", par[:], pA, (), par.r(), par.dsem)
        dma(P, "sp", gm[:], gmix, (), gm.r(), gm.dsem)
        dma(P, "sp", nfb[:], fbias, (), nfb.r(), nfb.dsem)
        dma(P, "pool", Wb[:], wA.rearrange("(kc p) n -> p kc n", p=128), (), Wb.r(), Wb.dsem)
        for kc in range(8):
            ts(P, "dve", Wb[:, kc, :], Wb[:, kc, :], gm[:, kc:kc + 1], ALU.mult, Wb.r() + gm.r(), Wb.r())
        ts(P, "dve", nfb[:], nfb[:], -1.0, ALU.mult, nfb.r(), nfb.r())
        tt(P, "dve", lbc[:, 3:4], par[:, 1:2], par[:, 0:1], ALU.subtract, par.r(), lbc.r())
        act(P, lbc[:, 3:4], lbc[:, 3:4], AF.Exp, lbc.r(), lbc.r())
        ts(P, "dve", lbc[:, 3:4], lbc[:, 3:4], 1.0, ALU.add, lbc.r(), lbc.r())
        recip(P, lbc[:, 0:1], lbc[:, 3:4], lbc.r(), lbc.r())
        ts(P, "dve", lbc[:, 1:2], lbc[:, 0:1], -1.0, ALU.mult, lbc.r(), lbc.r(), 1.0, ALU.add)
        ts(P, "dve", lbc[:, 2:3], par[:, 3:4], 0.125, ALU.mult, par.r(), lbc.r())
        lb_ap, oml_ap, gq8_ap = lbc[:, 0:1], lbc[:, 1:2], lbc[:, 2:3]
        on_ap, gk_ap = par[:, 2:3], par[:, 4:5]

        Q = SB("Q", [64, T], BF16, bs=512)
        Fh = SB("Fh", [64, T], BF16, bs=512)
        G = SB("G", [64, T], BF16, bs=512)
        BQ = SB("BQ", [70, T], BF16, bs=512, dsem=True)
        BK = SB("BK", [70, T], BF16, bs=128, dsem=True)
        VAB = SB("VAB", [128, T // 128, 129], BF16, bs=129)
        XB = [SB("XB%d" % i, [128, 8, 512], BF16, dsem=True) for i in range(2)]
        SQ = SB("SQ", [128, 8, 512], BF16)
        LNV = SB("LNV", [128, 512], F32)
        RSTD = [SB("RSTD%d" % i, [128, 512], F32) for i in range(2)]
        ZF = SB("ZF", [128, 512], F32)
        CC = [SB("CC%d" % i, [128, 512], F32) for i in range(2)]
        R1 = SB("R1", [128, 512], F32)
        AUG = [SB("AUG0", [128, 6, 512], BF16)] * 2
        RCS = SB("RCS", [128, 4], F32)
        PSB = [TT(P, st, "ps%d" % i, [128, 512], F32, psum=True) for i in range(8)]

        HT2 = [[SB("HT%d_%d" % (k_, i), [64, TBH], F32) for i in range(5)] for k_ in range(2)]
        KHT2 = [SB("KHT%d" % k_, [64, TBH], BF16) for k_ in range(2)]
        ELC0 = SB("ELC0", [64, T // 64], F32)

        KH = SB("KH", [128, T // 128, 64], BF16, bs=64)
        ELC = SB("ELC", [64, T // 64], F32)
        SBF = SB("SBF", [64, T], BF16)
        Z64 = SB("Z64", [64, 64], BF16)
        memset(P, "pool", Z64[:], 0.0, Z64.r())
        ATS = [SB("ATS%d" % i, [128, 512], BF16) for i in range(2)]
        OAS = [SB("OAS%d" % i, [64, 512], BF16, dsem=True) for i in range(2)]
        PTB = [SB("PTB%d" % i, [128, 512], BF16) for i in range(4)]
        ACCS = SB("ACCS", [65, 512], F32)
        RDEN = SB("RDEN", [64, 512], F32)
        OBS = [SB("OBS%d" % i, [64, 512], BF16, dsem=True) for i in range(2)]
        RONE = SB("RONE", [128, 512], BF16)
        memset(P, "pool", RONE[:], 1.0, RONE.r())
        memset(P, "pool", VAB[:, :, 128:129], 1.0, VAB.r())
        memset(P, "pool", BQ[64:70, :], 1.0, BQ.r())
        memset(P, "pool", BK[64:70, :], 1.0, BK.r())

        groups = [(0, 64, Q), (64, 64, Fh), (128, 64, G), (192, 64, BQ), (256, 65, BK)]
        blk = 0
        for b in range(nbatch):
            for n in range(T // 512):
                tok0 = b * T + n * 512
                cols = (n * 512, (n + 1) * 512)
                xb = XB[blk % 2]
                rs = RSTD[blk % 2]
                dma(P, "pool", xb[:], xT_v[:, :, tok0:tok0 + 512], (), xb.r(), xb.dsem)
                act(P, SQ[:], xb[:], AF.Square, xb.r(), SQ.r())
                ss = PSB[0]
                for kc in range(8):
                    mm(P, ss[:, :], ones_bf[:, :], SQ[:, kc, :], kc == 0, kc == 7, SQ.r() + ones_bf.r(), ss.r())
                act(P, LNV[:], ss[:, :], AF.Ln, ss.r() + eps_c.r(), LNV.r(), scale=1.0 / 1024, bias=eps_c[:])
                act(P, rs[:], LNV[:], AF.Exp, LNV.r(), rs.r(), scale=-0.5)
                for gi, (c0, M, dest) in enumerate(groups):
                    ps = PSB[1 + gi]
                    for kc in range(8):
                        mm(P, ps[0:M, :], Wb[:, kc, c0:c0 + M], xb[:, kc, :], kc == 0, kc == 7, Wb.r() + xb.r(), ps.r())
                    tt(P, "dve", dest[0:64, cols[0]:cols[1]], ps[0:64, :], rs[0:64, :], ALU.mult,
                       ps.r() + rs.r(), dest.r(*cols))
                    if M == 65:
                        cc = CC[blk % 2]
                        ccp = CC[(blk + 1) % 2]
                        aug = AUG[blk % 2]
                        r64 = slice(64, 65)
                        tt(P, "dve", ZF[r64, :], ps[r64, :], rs[r64, :], ALU.mult, ps.r() + rs.r(), ZF.r())
                        act(P, ZF[r64, :], ZF[r64, :], AF.Exp, ZF.r() + nfb.r(), ZF.r(), scale=-1.0, bias=nfb[r64, :])
                        act(P, ZF[r64, :], ZF[r64, :], AF.Ln, ZF.r() + one_c.r(), ZF.r(), scale=1.0, bias=one_c[r64, :])
                        init = 0.0 if n == 0 else ccp[r64, 511:512]
                        P.op("dve", lambda e, cc=cc, init=init: e.tensor_tensor_scan(
                            out=cc[64:65, :], data0=RONE[64:65, :],
                            data1=ZF[64:65, :], initial=init, op0=ALU.mult, op1=ALU.add),
                            ZF.r() + ccp.r() + RONE.r(), cc.r())
                        cp(P, "dve", aug[r64, 0, :], cc[r64, :], cc.r(), aug.r())
                        tt(P, "dve", R1[r64, :], cc[r64, :], aug[r64, 0, :], ALU.subtract, cc.r() + aug.r(), R1.r())
                        cp(P, "dve", aug[r64, 1, :], R1[r64, :], R1.r(), aug.r())
                        tt(P, "dve", R1[r64, :], R1[r64, :], aug[r64, 1, :], ALU.subtract, R1.r() + aug.r(), R1.r())
                        cp(P, "dve", aug[r64, 2, :], R1[r64, :], R1.r(), aug.r())
                        ts(P, "dve", aug[r64, 3:6, :], aug[r64, 0:3, :], -1.0, ALU.mult, aug.r(), aug.r())
                        for i in range(3):
                            dma(P, "sp", BK[67 + i:68 + i, cols[0]:cols[1]], aug[r64, i, :], aug.r(), BK.r(*cols), BK.dsem)
                            dma(P, "sp", BQ[64 + i:65 + i, cols[0]:cols[1]], aug[r64, 3 + i, :], aug.r(), BQ.r(*cols), BQ.dsem)
                ptm = PSB[6]
                prc = PSB[7]
                for sub in range(4):
                    sc = slice(sub * 128, (sub + 1) * 128)
                    for kc in range(8):
                        mm(P, ptm[:, sc], xb[:, kc, sc], Wb[:, kc, 321:449], kc == 0, kc == 7, Wb.r() + xb.r(), ptm.r())
                    mm(P, prc[:, sub:sub + 1], rs[0:1, sc], ones_f[0:1, 0:1], True, True, rs.r() + ones_f.r(), prc.r())
                cp(P, "dve", RCS[:, 0:4], prc[:, 0:4], prc.r(), RCS.r())
                for sub in range(4):
                    ti = n * 4 + sub
                    sc = slice(sub * 128, (sub + 1) * 128)
                    ts(P, "dve", VAB[:, ti, 0:128], ptm[:, sc], RCS[:, sub:sub + 1], ALU.mult,
                       ptm.r() + RCS.r(), VAB.r(ti * 129, ti * 129 + 129))
                blk += 1
            if do_fox:
                for n in range(T // 512):
                    cs = slice(n * 512, (n + 1) * 512)
                    for (tl, gap, bank) in ((BQ, gq8_ap, 1), (BK, gk_ap, 2)):
                        ps = PSB[bank]
                        act(P, SQ[0:64, 0, :], tl[0:64, cs], AF.Square, tl.r(n * 512, n * 512 + 512), SQ.r())
                        mm(P, ps[0:64, :], ones_bf[0:64, 0:64], SQ[0:64, 0, :], True, True, SQ.r() + ones_bf.r(), ps.r())
                        act(P, LNV[0:64, :], ps[0:64, :], AF.Ln, ps.r() + eps_c.r(), LNV.r(), scale=1.0 / 64, bias=eps_c[0:64, :])
                        act(P, LNV[0:64, :], LNV[0:64, :], AF.Exp, LNV.r(), LNV.r(), scale=-0.5)
                        stt(P, tl[0:64, cs], tl[0:64, cs], gap, LNV[0:64, :], ALU.mult, ALU.mult,
                            tl.r(n * 512, n * 512 + 512) + LNV.r() + par.r() + lbc.r(), tl.r(n * 512, n * 512 + 512))
            if do_fox:
                k = 0
                for j in range(T // 512):
                    acc = PSB[4 + j % 2]
                    nt = 4 * j + 4
                    items = []
                    for i in range(nt):
                        m = i - 4 * j
                        c0 = 128 * m if m >= 0 else 0
                        items.append((i, m, c0))
                    def qk(it, k):
                        i, m, c0 = it
                        sc = PSB[1 + k % 3]
                        mm(P, sc[:, c0:512], BK[0:70, i * 128:(i + 1) * 128], BQ[0:70, j * 512 + c0:(j + 1) * 512], True, True,
                           BK.r(i * 128, i * 128 + 128) + BQ.r(j * 512, j * 512 + 512), sc.r())
                    def rest(it, k):
                        i, m, c0 = it
                        sc = PSB[1 + k % 3]
                        pt = PTB[k % 4]
                        act(P, pt[:, c0:512], sc[:, c0:512], AF.Exp, sc.r(), pt.r())
                        if m >= 0:
                            P.op("pool", lambda e, pt=pt, c0=c0: e.affine_select(out=pt[:, c0:c0 + 128], in_=pt[:, c0:c0 + 128], pattern=[[1, 128]],
                                 compare_op=ALU.is_ge, fill=0.0, base=0, channel_multiplier=-1), pt.r(), pt.r())
                        mm(P, acc[0:65, c0:512], VAB[:, i, 64:129], pt[:, c0:512], i == 0, i == nt - 1,
                           VAB.r(i * 129, i * 129 + 129) + pt.r(), acc.r())
                    LOOK = 2
                    for idx in range(nt + LOOK):
                        if idx < nt:
                            qk(items[idx], k + idx)
                        if idx >= LOOK:
                            rest(items[idx - LOOK], k + idx - LOOK)
                    k += nt
                    cp(P, "dve", ACCS[0:65, :], acc[0:65, :], acc.r(), ACCS.r())
                    pden = PSB[6]
                    mm(P, pden[0:64, :], ones_f[64:65, 0:64], ACCS[64:65, :], True, True, ones_f.r() + ACCS.r(), pden.r())
                    recip(P, RDEN[:], pden[0:64, :], pden.r(), RDEN.r())
                    obs = OBS[j % 2]
                    tt(P, "pool", obs[:], ACCS[0:64, :], RDEN[:], ALU.mult, ACCS.r() + RDEN.r(), obs.r())
                    dma(P, "sp", oT[64:128, b * T + j * 512: b * T + (j + 1) * 512], obs[:], obs.r(), (), obs.dsem)
            if do_hgrn:
                for sbk in range(T // TBH):
                    c0, c1 = sbk * TBH, (sbk + 1) * TBH
                    cs = slice(c0, c1)
                    t1, t2, t3, t4, t5 = HT2[sbk % 2]
                    kht = KHT2[sbk % 2]
                    act(P, t1[:], Fh[0:64, cs], AF.Exp, Fh.r(c0, c1), t1.r(), scale=-1.0)
                    act(P, t1[:], t1[:], AF.Identity, t1.r() + one_c.r(), t1.r(), scale=1.0, bias=one_c[0:64, :])
                    recip(P, t1[:], t1[:], t1.r(), t1.r())
                    ts(P, "dve", t1[:], t1[:], oml_ap, ALU.mult, t1.r() + lbc.r(), t1.r(), lb_ap, ALU.add)
                    act(P, t2[:], t1[:], AF.Ln, t1.r(), t2.r())
                    P.op("dve", lambda e, t3=t3, t2=t2: e.tensor_tensor_scan(out=t3[:], data0=rmask[:], data1=t2[:], initial=0.0,
                                                                             op0=ALU.mult, op1=ALU.add), t2.r() + rmask.r(), t3.r())
                    act(P, t1[:], t1[:], AF.Identity, t1.r() + one_c.r(), t1.r(), scale=-1.0, bias=one_c[0:64, :])
                    act(P, t4[:], t3[:], AF.Exp, t3.r(), t4.r())
                    act(P, t5[:], Q[0:64, cs], AF.Exp, Q.r(c0, c1), t5.r(), scale=-1.0)
                    act(P, t5[:], t5[:], AF.Identity, t5.r() + one_c.r(), t5.r(), scale=1.0, bias=one_c[0:64, :])
                    recip(P, t5[:], t5[:], t5.r(), t5.r())
                    tt(P, "dve", t5[:], Q[0:64, cs], t5[:], ALU.mult, Q.r(c0, c1) + t5.r(), t5.r())
                    tt(P, "dve", Q[0:64, cs], t5[:], t4[:], ALU.mult, t5.r() + t4.r(), Q.r(c0, c1))
                    act(P, t4[:], t3[:], AF.Exp, t3.r(), t4.r(), scale=-1.0)
                    tt(P, "dve", Fh[0:64, cs], t1[:], t4[:], ALU.mult, t1.r() + t4.r(), Fh.r(c0, c1))
                    nch = TBH // 64
                    ch0 = sbk * nch
                    act(P, ELC[0:64, ch0:ch0 + nch], t3[:].rearrange("p (c l) -> p c l", l=64)[:, :, 63],
                        AF.Exp, t3.r(), ELC.r())
                    tt(P, "dve", kht[:].rearrange("p (c l) -> p c l", l=64), Fh[0:64, cs].rearrange("p (c l) -> p c l", l=64),
                       ELC[0:64, ch0:ch0 + nch].unsqueeze(2).broadcast_to([64, nch, 64]), ALU.mult,
                       Fh.r(c0, c1) + ELC.r(), kht.r())
                    pst = PSB[3] if sbk % 2 == 0 else PSB[0]
                    pstb = pst.h.bitcast(BF16)
                    ntl = TBH // 128
                    for j in range(ntl):
                        P.op("pe", lambda e, j=j, pstb=pstb, kht=kht: e.transpose(out=pstb[:, j * 64:(j + 1) * 64], in_=kht[0:64, j * 128:(j + 1) * 128],
                                                              identity=ident[0:64, 0:64]), kht.r() + ident.r(), pst.r())
                    ti0 = sbk * ntl
                    cp(P, "act", KH[:, ti0:ti0 + ntl, :], pstb[:, 0:ntl * 64].rearrange("p (a b) -> p a b", b=64),
                       pst.r(), KH.r(ti0 * 64, (ti0 + ntl) * 64))
                    act(P, t2[:], G[0:64, cs], AF.Exp, G.r(c0, c1), t2.r(), scale=-1.0)
                    act(P, t2[:], t2[:], AF.Identity, t2.r() + one_c.r(), t2.r(), scale=1.0, bias=one_c[0:64, :])
                    recip(P, t2[:], t2[:], t2.r(), t2.r())
                    tt(P, "dve", G[0:64, cs], G[0:64, cs], t2[:], ALU.mult, G.r(c0, c1) + t2.r(), G.r(c0, c1))
                DSV = BQ
                NC_ = T // 64
                for g4 in range(T // 512):
                    for c in range(8 * g4, 8 * g4 + 8):
                        ti, hh = c // 2, c % 2
                        pds = PSB[4 + (g4 % 2) * 2 + hh]
                        slot = (c % 8) * 64
                        hs = slice(hh * 64, (hh + 1) * 64)
                        mm(P, pds[0:64, slot:slot + 64], KH[hs, ti, :], VAB[hs, ti, 0:64], True, True,
                           KH.r(ti * 64, ti * 64 + 64) + VAB.r(ti * 129, ti * 129 + 129), pds.r())
                    for hh in range(2):
                        pds = PSB[4 + (g4 % 2) * 2 + hh]
                        src = pds[0:64, :].rearrange("p (a b) -> p a b", b=128)[:, :, hh * 64:(hh + 1) * 64]
                        dst = DSV[0:64, :].rearrange("p (v c) -> p c v", c=NC_)[:, 8 * g4 + hh:8 * g4 + 8:2, :]
                        cp(P, "dve" if hh == 0 else "act", dst, src, pds.r(), DSV.r())
                cp(P, "pool", ELC0[:], ELC[:], ELC.r(), ELC0.r())
                memset(P, "pool", ELC0[:, 0:1], 0.0, ELC0.r())
                D0 = BK
                cp(P, "act", D0[0:64, :].rearrange("p (v c) -> p v c", c=NC_), ELC0[:, :].unsqueeze(1).broadcast_to([64, 64, NC_]),
                   ELC0.r(), D0.r())
                P.op("dve", lambda e: e.tensor_tensor_scan(out=SBF[:, :], data0=D0[0:64, :], data1=DSV[0:64, :],
                                                           initial=0.0, op0=ALU.mult, op1=ALU.add), D0.r() + DSV.r(), SBF.r())
                for g4 in range(T // 512):
                    pat = PSB[6 + g4 % 2]
                    for j in range(4):
                        ti = g4 * 4 + j
                        tcs = slice(ti * 128, (ti + 1) * 128)
                        mm(P, pat[:, j * 128:(j + 1) * 128], Fh[0:64, tcs], Q[0:64, tcs], True, True,
                           Fh.r(ti * 128, ti * 128 + 128) + Q.r(ti * 128, ti * 128 + 128), pat.r())
                    ats = ATS[g4 % 2]
                    tt(P, "dve", ats[:].rearrange("p (a b) -> p a b", b=128), pat[:, :].rearrange("p (a b) -> p a b", b=128),
                       M2[:].unsqueeze(1).broadcast_to([128, 4, 128]), ALU.mult, pat.r() + M2.r(), ats.r())
                    po = PSB[1 + g4 % 2]
                    for jj in range(4):
                        tj = g4 * 4 + jj
                        for hh in range(2):
                            c = 2 * tj + hh
                            ccs = slice(c * 64, (c + 1) * 64)
                            oc = slice(jj * 128 + hh * 64, jj * 128 + hh * 64 + 64)
                            mm(P, po[0:64, oc], VAB[:, tj, 0:64], ats[:, oc], True, False,
                               VAB.r(tj * 129, tj * 129 + 129) + ats.r(), po.r())
                            st_ap = Z64[:, :] if c == 0 else SBF[:, :].rearrange("p (v c) -> p v c", c=NC_)[:, :, c - 1]
                            mm(P, po[0:64, oc], st_ap, Q[0:64, ccs], False, True,
                               SBF.r() + Z64.r() + Q.r(c * 64, c * 64 + 64), po.r())
                    gcs = slice(g4 * 512, (g4 + 1) * 512)
                    pn = PSB[3]
                    sq64 = SQ
                    lnv = RSTD[g4 % 2]
                    oaf = (ZF, R1)[g4 % 2]
                    act(P, sq64[0:64, g4 % 2, :], po[0:64, :], AF.Square, po.r(), sq64.r())
                    mm(P, pn[0:64, :], ones_bf[0:64, 0:64], sq64[0:64, g4 % 2, :], True, True, sq64.r() + ones_bf.r(), pn.r())
                    act(P, lnv[0:64, :], pn[0:64, :], AF.Ln, pn.r() + eps_c.r(), lnv.r(), scale=1.0 / 64, bias=eps_c[0:64, :])
                    act(P, lnv[0:64, :], lnv[0:64, :], AF.Exp, lnv.r(), lnv.r(), scale=-0.5)
                    stt(P, oaf[0:64, :], po[0:64, :], on_ap, lnv[0:64, :], ALU.mult, ALU.mult, po.r() + lnv.r() + par.r(), oaf.r())
                    oas = OAS[g4 % 2]
                    tt(P, "dve", oas[:], oaf[0:64, :], G[0:64, gcs], ALU.mult, oaf.r() + G.r(g4 * 512, g4 * 512 + 512), oas.r())
                    dma(P, "sp", oT[0:64, b * T + g4 * 512: b * T + (g4 + 1) * 512], oas[:], oas.r(), (), oas.dsem)
        if os.environ.get('HG_DBG'):
            dsd2 = P.new_dsem("dbg2")
            for nm, tl, shp, dt_ in (("dKH", KH, [128, (T // 128) * 64], BF16), ("dKt", Fh, [64, T], BF16), ("dQt", Q, [64, T], BF16), ("dELC", ELC, [64, T // 64], F32), ("dSBF", SBF, [64, T], BF16)):
                dd = nc.dram_tensor(nm, shp, dt_, kind="ExternalOutput").ap()
                src = tl[:] if len(tl.h.shape) == 2 else tl[:].rearrange("p a b -> p (a b)")
                dma(P, "sp", dd, src, tl.r(), (), dsd2)
        if debug:
            dsd = P.new_dsem("dbg")
            for nm, tl, shp in (("dQ", Q, [64, T]), ("dF", Fh, [64, T]), ("dG", G, [64, T]), ("dBQ", BQ, [70, T]), ("dBK", BK, [70, T]), ("dV", VAB, [128, (T // 128) * 129])):
                dd = nc.dram_tensor(nm, shp, BF16, kind="ExternalOutput").ap()
                src = tl[:] if nm != "dV" else tl[:].rearrange("p a b -> p (a b)")
                dma(P, "sp", dd, src, tl.r(), (), dsd)
        P.replay()
    return nc


NT = 2048


def build_B():
    nc = bass.Bass("TRN2", target_bir_lowering=False)
    hT_in = nc.dram_tensor("hT_in", [1024, NT], F32, kind="ExternalInput").ap()
    oT_in = nc.dram_tensor("oT_in", [1024, NT], BF16, kind="ExternalInput").ap()
    w_out = nc.dram_tensor("w_out", [1024, 1024], F32, kind="ExternalInput").ap()
    gffn = nc.dram_tensor("gffn", [128, 8], F32, kind="ExternalInput").ap()
    w_r = nc.dram_tensor("w_r", [1024, 36], F32, kind="ExternalInput").ap()
    w_gate = nc.dram_tensor("w_gate", [32, 1024, 512], F32, kind="ExternalInput").ap()
    w_up = nc.dram_tensor("w_up", [32, 1024, 512], F32, kind="ExternalInput").ap()
    w_down = nc.dram_tensor("w_down", [32, 512, 1024], F32, kind="ExternalInput").ap()
    hT_out = nc.dram_tensor("hT_out", [1024, NT], F32, kind="ExternalOutput").ap()
    P = Prog(nc)
    hT_v = hT_in.rearrange("(kc p) t -> p kc t", p=128)
    oT_v = oT_in.rearrange("(kc p) t -> p kc t", p=128)
    ho_v = hT_out.rearrange("(kc p) t -> p kc t", p=128)
    with ExitStack() as st:
        def SB(name, shape, dt, bs=None, dsem=False):
            return TT(P, st, name, shape, dt, bs=bs, dsem=dsem)

        ones_bf = SB("ones_bf", [128, 128], BF16)
        eps_c = SB("eps_c", [128, 1], F32)
        identf = SB("identf", [128, 128], F32)
        SEL = SB("SEL", [32, 32, 128], BF16)
        gf = SB("gf", [128, 8], F32, dsem=True)
        WR = SB("WR", [128, 8, 36], F32, dsem=True)
        HM = SB("HM", [128, 8, NT], F32, bs=512, dsem=True)
        U = SB("U", [128, 8, NT], BF16, bs=512)
        WBUF = [SB("WB%d" % i, [128, 12288], BF16, dsem=True) for i in range(2)]
        OTB = [SB("OTB%d" % i, [128, 8, 512], BF16, dsem=True) for i in range(2)]
        HID = [SB("HID%d" % i, [128, 4, 512], BF16) for i in range(2)]
        SIL = [SB("SIL%d" % i, [128, 512], BF16) for i in range(2)]
        S2 = [SB("S2%d" % i, [128, 512], BF16) for i in range(2)]
        CREP = [SB("CREP%d" % i, [128, 512], BF16) for i in range(2)]
        CT = SB("CT", [32, NT], BF16, bs=128)
        LNV = SB("LNV", [128, 512], F32)
        RSTD = SB("RSTD", [128, 512], F32)
        RL = SB("RL", [128, 36], F32)
        RT = SB("RT", [128, 16], F32)
        OH = SB("OH", [128, 4], F32)
        EG = SB("EG", [128, 4], F32)
        SEL8 = SB("SEL8", [128, 8], F32)
        M8 = SB("M8", [128, 8], F32)
        WA8 = SB("WA8", [128, 8], F32)
        WB8 = SB("WB8", [128, 8], F32)
        COMB = SB("COMB", [128, 32], F32)
        PSB = [TT(P, st, "ps%d" % i, [128, 512], F32, psum=True) for i in range(8)]
        Wo = WBUF[1]
        Wo_v = Wo[:, 0:8192].rearrange("p (a b) -> p a b", b=1024)
        UFt = WBUF[0]
        UF_v = UFt.h.bitcast(F32)[:, 0:4096].rearrange("p (a b) -> p a b", b=512)
        SQt = HID[0]
        SQ = SB("SQ", [128, 8, 512], BF16)

        memset(P, "pool", ones_bf[:], 1.0, ones_bf.r())
        memset(P, "pool", eps_c[:], EPS, eps_c.r())
        memset(P, "pool", identf[:], 1.0, identf.r())
        P.op("pool", lambda e: e.affine_select(out=identf[:], in_=identf[:], pattern=[[-1, 128]], compare_op=ALU.is_equal,
                                               fill=0.0, base=0, channel_multiplier=1), identf.r(), identf.r())
        memset(P, "pool", SEL[:], 1.0, SEL.r())
        P.op("pool", lambda e: e.affine_select(out=SEL[:], in_=SEL[:], pattern=[[-1, 32], [0, 128]], compare_op=ALU.is_equal,
                                               fill=0.0, base=0, channel_multiplier=1), SEL.r(), SEL.r())
        dma(P, "sp", gf[:], gffn, (), gf.r(), gf.dsem)
        dma(P, "sp", WR[:], w_r.rearrange("(kc p) n -> p kc n", p=128), (), WR.r(), WR.dsem)
        dma(P, "pool", Wo_v, w_out.rearrange("(kc p) n -> p kc n", p=128), (), Wo.r(), Wo.dsem)
        for h2 in range(2):
            dma(P, "sp", HM[:, :, h2 * 1024:(h2 + 1) * 1024], hT_v[:, :, h2 * 1024:(h2 + 1) * 1024], (), HM.r(), HM.dsem)

        for blk in range(NT // 512):
            cs = slice(blk * 512, (blk + 1) * 512)
            ot = OTB[blk % 2]
            dma(P, "sp", ot[:], oT_v[:, :, cs], (), ot.r(), ot.dsem)
            for dc in range(8):
                ps = PSB[dc % 2]
                for kc in range(8):
                    mm(P, ps[:, :], Wo_v[:, kc, dc * 128:(dc + 1) * 128], ot[:, kc, :], kc == 0, kc == 7, Wo.r() + ot.r(), ps.r())
                lo = dc * NT + blk * 512
                tt(P, "dve", HM[:, dc, cs], HM[:, dc, cs], ps[:, :], ALU.add, HM.r(lo, lo + 512) + ps.r(), HM.r(lo, lo + 512))
            hm_blk = []
            for dc in range(8):
                hm_blk += HM.r(dc * NT + blk * 512, dc * NT + blk * 512 + 512)
            act(P, SQ[:], HM[:, :, cs], AF.Square, hm_blk, SQ.r())
            ss = PSB[2]
            for kc in range(8):
                mm(P, ss[:, :], ones_bf[:, :], SQ[:, kc, :], kc == 0, kc == 7, SQ.r() + ones_bf.r(), ss.r())
            act(P, LNV[:], ss[:, :], AF.Ln, ss.r() + eps_c.r(), LNV.r(), scale=1.0 / 1024, bias=eps_c[:])
            act(P, RSTD[:], LNV[:], AF.Exp, LNV.r(), RSTD.r(), scale=-0.5)
            u_blk = []
            for dc in range(8):
                lo = dc * NT + blk * 512
                stt(P, UF_v[:, dc, :], HM[:, dc, cs], gf[:, dc:dc + 1], RSTD[:], ALU.mult, ALU.mult,
                    HM.r(lo, lo + 512) + gf.r() + RSTD.r(), UFt.r())
                u_blk += U.r(lo, lo + 512)
            cp(P, "pool", U[:, :, cs], UF_v, UFt.r(), u_blk)
            for sub in range(4):
                scs = slice(sub * 128, (sub + 1) * 128)
                pr = PSB[3]
                for dc in range(8):
                    mm(P, pr[:, 0:36], UF_v[:, dc, scs], WR[:, dc, :], dc == 0, dc == 7, UFt.r() + WR.r(), pr.r())
                cp(P, "dve", RL[:], pr[:, 0:36], pr.r(), RL.r())
                P.op("dve", lambda e: e.reduce_max(out=RT[:, 0:1], in_=RL[:, 0:4], axis=AX.X), RL.r(), RT.r())
                ts(P, "dve", OH[:], RL[:, 0:4], RT[:, 0:1], ALU.is_equal, RL.r() + RT.r(), OH.r())
                ts(P, "dve", RT[:, 1:2], RT[:, 0:1], -1.0, ALU.mult, RT.r(), RT.r())
                act(P, EG[:], RL[:, 0:4], AF.Exp, RL.r() + RT.r(), EG.r(), scale=1.0, bias=RT[:, 1:2])
                P.op("dve", lambda e: e.reduce_sum(out=RT[:, 2:3], in_=EG[:], axis=AX.X), EG.r(), RT.r())
                recip(P, RT[:, 3:4], RT[:, 2:3], RT.r(), RT.r())
                ts(P, "dve", SEL8[:], RL[:, 4:12], OH[:, 0:1], ALU.mult, RL.r() + OH.r(), SEL8.r())
                for g in range(1, 4):
                    stt(P, SEL8[:], RL[:, 4 + 8 * g:12 + 8 * g], OH[:, g:g + 1], SEL8[:], ALU.mult, ALU.add, RL.r() + OH.r() + SEL8.r(), SEL8.r())
                P.op("dve", lambda e: e.max(out=M8[:], in_=SEL8[:]), SEL8.r(), M8.r())
                tt(P, "dve", RT[:, 4:5], M8[:, 1:2], M8[:, 0:1], ALU.subtract, M8.r(), RT.r())
                act(P, RT[:, 5:6], RT[:, 4:5], AF.Exp, RT.r(), RT.r())
                ts(P, "dve", RT[:, 6:7], RT[:, 5:6], 1.0, ALU.add, RT.r(), RT.r())
                recip(P, RT[:, 7:8], RT[:, 6:7], RT.r(), RT.r())
                ts(P, "dve", RT[:, 8:9], RT[:, 7:8], -1.0, ALU.mult, RT.r(), RT.r(), 1.0, ALU.add)
                tt(P, "dve", RT[:, 9:10], RT[:, 7:8], RT[:, 3:4], ALU.mult, RT.r(), RT.r())
                tt(P, "dve", RT[:, 10:11], RT[:, 8:9], RT[:, 3:4], ALU.mult, RT.r(), RT.r())
                ts(P, "dve", WA8[:], SEL8[:], M8[:, 0:1], ALU.is_equal, SEL8.r() + M8.r() + RT.r(), WA8.r(), RT[:, 9:10], ALU.mult)
                ts(P, "dve", WB8[:], SEL8[:], M8[:, 1:2], ALU.is_equal, SEL8.r() + M8.r() + RT.r(), WB8.r(), RT[:, 10:11], ALU.mult)
                tt(P, "dve", WA8[:], WA8[:], WB8[:], ALU.add, WA8.r() + WB8.r(), WA8.r())
                for g in range(4):
                    ts(P, "dve", COMB[:, g * 8:(g + 1) * 8], WA8[:], OH[:, g:g + 1], ALU.mult, WA8.r() + OH.r(), COMB.r())
                pt = PSB[4]
                P.op("pe", lambda e, pt=pt: e.transpose(out=pt[0:32, 0:128], in_=COMB[:, 0:32], identity=identf[:, :]),
                     COMB.r() + identf.r(), pt.r())
                t0 = blk * 512 + sub * 128
                cp(P, "dve", CT[0:32, t0:t0 + 128], pt[0:32, 0:128], pt.r(), CT.r(t0, t0 + 128))

        units = [(e, blk) for e in range(32) for blk in range(NT // 512)]
        wviews = []
        for i in range(2):
            wb = WBUF[i]
            wviews.append((wb[:, 0:4096].rearrange("p (a b) -> p a b", b=512),
                           wb[:, 4096:8192].rearrange("p (a b) -> p a b", b=512),
                           wb[:, 8192:12288].rearrange("p (a b) -> p a b", b=1024)))

        def load_w(e):
            wb = WBUF[e % 2]
            Wg, Wu, Wd = wviews[e % 2]
            dma(P, "pool", Wg, w_gate[e].rearrange("(kc p) n -> p kc n", p=128), (), wb.r(), wb.dsem)
            dma(P, "pool", Wu, w_up[e].rearrange("(kc p) n -> p kc n", p=128), (), wb.r(), wb.dsem)
            dma(P, "pool", Wd, w_down[e].rearrange("(fc p) n -> p fc n", p=128), (), wb.r(), wb.dsem)

        def GU(ui):
            e, blk = units[ui]
            cs = slice(blk * 512, (blk + 1) * 512)
            wb = WBUF[e % 2]
            Wg, Wu, Wd = wviews[e % 2]
            hid = HID[ui % 2]
            crep = CREP[ui % 2]
            pc = PSB[6]
            mm(P, pc[:, :], SEL[0:32, e, :], CT[0:32, cs], True, True, SEL.r() + CT.r(blk * 512, blk * 512 + 512), pc.r())
            cp(P, "dve", crep[:], pc[:, :], pc.r(), crep.r())
            u_blk = []
            for dc in range(8):
                u_blk += U.r(dc * NT + blk * 512, dc * NT + blk * 512 + 512)
            for fc in range(4):
                pg_ = PSB[(fc % 2) * 2]
                pu_ = PSB[(fc % 2) * 2 + 1]
                fs = slice(fc * 128, (fc + 1) * 128)
                for dc in range(8):
                    mm(P, pg_[:, :], Wg[:, dc, fs], U[:, dc, cs], dc == 0, dc == 7, wb.r() + u_blk, pg_.r())
                for dc in range(8):
                    mm(P, pu_[:, :], Wu[:, dc, fs], U[:, dc, cs], dc == 0, dc == 7, wb.r() + u_blk, pu_.r())
                sil = SIL[fc % 2]
                s2 = S2[fc % 2]
                act(P, sil[:], pg_[:, :], AF.Silu, pg_.r(), sil.r())
                tt(P, "pool", s2[:], sil[:], crep[:], ALU.mult, sil.r() + crep.r(), s2.r())
                tt(P, "dve", hid[:, fc, :], s2[:], pu_[:, :], ALU.mult, s2.r() + pu_.r(), hid.r())

        def DN(ui):
            e, blk = units[ui]
            cs = slice(blk * 512, (blk + 1) * 512)
            wb = WBUF[e % 2]
            Wg, Wu, Wd = wviews[e % 2]
            hid = HID[ui % 2]
            for dc in range(8):
                py = PSB[4 + dc % 2]
                for fc in range(4):
                    mm(P, py[:, :], Wd[:, fc, dc * 128:(dc + 1) * 128], hid[:, fc, :], fc == 0, fc == 3, wb.r() + hid.r(), py.r())
                lo = dc * NT + blk * 512
                tt(P, "dve", HM[:, dc, cs], HM[:, dc, cs], py[:, :], ALU.add, HM.r(lo, lo + 512) + py.r(), HM.r(lo, lo + 512))

        nE = int(os.environ.get("KB_NE", 32))
        units = [u_ for u_ in units if u_[0] < nE]
        load_w(0)
        for ui in range(len(units) + 1):
            if ui < len(units):
                GU(ui)
            if ui >= 1:
                DN(ui - 1)
            if ui < len(units):
                e, blk = units[ui]
                if blk == 0 and e + 1 < nE:
                    load_w(e + 1)
        for h2 in range(2):
            rr = []
            for dc in range(8):
                rr += HM.r(dc * NT + h2 * 1024, dc * NT + h2 * 1024 + 1024)
            dma(P, "sp", ho_v[:, :, h2 * 1024:(h2 + 1) * 1024], HM[:, :, h2 * 1024:(h2 + 1) * 1024], rr, (), HM.dsem)
        P.replay()
    return nc


import math

NCOL_C = 384
TWO_PI = 2.0 * math.pi
LAM_INIT = 0.8 - 0.6 * math.exp(-0.3 * 1)


def build_C(nbatch=2, debug=False):
    nc = bass.Bass("TRN2", target_bir_lowering=False)
    xT = nc.dram_tensor("xT", [1024, TOK], F32, kind="ExternalInput").ap()
    wC = nc.dram_tensor("wC", [1024, NCOL_C], F32, kind="ExternalInput").ap()
    gmix = nc.dram_tensor("gmix", [128, 8], F32, kind="ExternalInput").ap()
    pC = nc.dram_tensor("pC", [128, 8], F32, kind="ExternalInput").ap()
    posd = nc.dram_tensor("pos", [2, T], I32, kind="ExternalInput").ap()
    oT = nc.dram_tensor("oT", [128, TOK], BF16, kind="ExternalOutput").ap()
    P = Prog(nc)
    xT_v = xT.rearrange("(kc p) t -> p kc t", p=128)
    with ExitStack() as st:
        def SB(name, shape, dt, bs=None, dsem=False):
            return TT(P, st, name, shape, dt, bs=bs, dsem=dsem)

        ones_bf = SB("ones_bf", [128, 128], BF16)
        BD = SB("BD", [128, 128], BF16)
        ones_f = SB("ones_f", [128, 128], F32)
        eps_c = SB("eps_c", [128, 1], F32)
        pi_c = SB("pi_c", [128, 2], F32)
        pic2 = SB("pic2", [128, 2], F32)
        ident = SB("ident", [128, 128], BF16)
        PM = SB("PM", [128, 128], BF16)
        PM2 = SB("PM2", [128, 128], BF16)
        par = SB("par", [128, 8], F32, dsem=True)
        gm = SB("gm", [128, 8], F32, dsem=True)
        cst = SB("cst", [128, 8], F32)
        invi = SB("invi", [1, 128], I32)
        invf = SB("invf", [1, 128], F32)
        Wb = SB("Wb", [128, 8, NCOL_C], BF16, dsem=True)

        memset(P, "pool", ones_bf[:], 1.0, ones_bf.r())
        memset(P, "pool", ones_f[:], 1.0, ones_f.r())
        memset(P, "pool", eps_c[:], EPS, eps_c.r())
        memset(P, "pool", BD[:], 1.0, BD.r())
        memset(P, "pool", BD[0:64, 64:128], 0.0, BD.r())
        memset(P, "pool", BD[64:128, 0:64], 0.0, BD.r())
        memset(P, "pool", pi_c[:, 0:1], math.pi, pi_c.r())
        memset(P, "pool", pi_c[:, 1:2], -TWO_PI, pi_c.r())
        memset(P, "pool", pi_c[0:32, 0:1], -math.pi, pi_c.r())
        memset(P, "pool", pi_c[0:32, 1:2], TWO_PI, pi_c.r())
        memset(P, "pool", pi_c[64:96, 0:1], -math.pi, pi_c.r())
        memset(P, "pool", pi_c[64:96, 1:2], TWO_PI, pi_c.r())
        memset(P, "pool", pic2[:, 0:1], math.pi, pic2.r())
        memset(P, "pool", PM[:], 1.0, PM.r())
        P.op("pool", lambda e: e.affine_select(out=PM[:], in_=PM[:], pattern=[[1, 128]], compare_op=ALU.is_equal,
                                               fill=0.0, base=-32, channel_multiplier=-1), PM.r(), PM.r())
        memset(P, "pool", PM[:, 0:32], 0.0, PM.r())
        memset(P, "pool", PM[:, 64:96], 0.0, PM.r())
        memset(P, "pool", PM2[:], 1.0, PM2.r())
        P.op("pool", lambda e: e.affine_select(out=PM2[:], in_=PM2[:], pattern=[[1, 128]], compare_op=ALU.is_equal,
                                               fill=0.0, base=32, channel_multiplier=-1), PM2.r(), PM2.r())
        memset(P, "pool", PM2[:, 32:64], 0.0, PM2.r())
        memset(P, "pool", PM2[:, 96:128], 0.0, PM2.r())
        tt(P, "pool", PM[:], PM[:], PM2[:], ALU.add, PM.r() + PM2.r(), PM.r())
        P.op("pool", lambda e: e.iota(invi[:].rearrange("p (a b) -> p a b", b=32), pattern=[[0, 4], [1, 32]], base=0, channel_multiplier=0),
             (), invi.r())
        cp(P, "dve", invf[:], invi[:], invi.r(), invf.r())
        act(P, invf[:], invf[:], AF.Exp, invf.r(), invf.r(), scale=-math.log(10000.0) / 32.0)

        dma(P, "sp", par[:], pC, (), par.r(), par.dsem)
        dma(P, "sp", gm[:], gmix, (), gm.r(), gm.dsem)
        dma(P, "pool", Wb[:], wC.rearrange("(kc p) n -> p kc n", p=128), (), Wb.r(), Wb.dsem)
        for kc in range(8):
            ts(P, "dve", Wb[:, kc, :], Wb[:, kc, :], gm[:, kc:kc + 1], ALU.mult, Wb.r() + gm.r(), Wb.r())
        ts(P, "dve", cst[:, 0:1], par[:, 0:1], 0.125, ALU.mult, par.r(), cst.r())
        ts(P, "dve", cst[:, 3:4], par[:, 6:7], 1.0 - LAM_INIT, ALU.mult, par.r(), cst.r())
        PSB = [TT(P, st, "ps%d" % i, [128, 512], F32, psum=True) for i in range(8)]
        tt(P, "dve", cst[:, 4:5], par[:, 2:3], par[:, 3:4], ALU.mult, par.r(), cst.r())
        tt(P, "dve", cst[:, 5:6], par[:, 4:5], par[:, 5:6], ALU.mult, par.r(), cst.r())
        mm(P, PSB[0][:, 0:2], ones_f[:, :], cst[:, 4:6], True, True, ones_f.r() + cst.r(), PSB[0].r())
        act(P, cst[:, 6:8], PSB[0][:, 0:2], AF.Exp, PSB[0].r(), cst.r())
        tt(P, "dve", cst[:, 2:3], cst[:, 7:8], cst[:, 6:7], ALU.subtract, cst.r(), cst.r())
        ts(P, "dve", cst[:, 2:3], cst[:, 2:3], -LAM_INIT, ALU.add, cst.r(), cst.r())
        gq_ap, gk_ap, nlam_ap, gs_ap = cst[:, 0:1], par[:, 1:2], cst[:, 2:3], cst[:, 3:4]

        QT = SB("QT", [128, T], BF16, bs=512)
        KT = SB("KT", [128, T], BF16, bs=128)
        V = SB("V", [128, T // 128, 128], BF16, bs=128)
        SINT = SB("SINT", [128, T], BF16, bs=512)
        COST = SB("COST", [128, T], BF16, bs=512)
        XB = [SB("XB%d" % i, [128, 8, 512], BF16, dsem=True) for i in range(2)]
        SQ = SB("SQ", [128, 8, 512], BF16)
        LNV = SB("LNV", [128, 512], F32)
        RSTD = [SB("RSTD%d" % i, [128, 512], F32) for i in range(2)]
        RCS = SB("RCS", [128, 4], F32)
        POSI = SB("POSI", [1, 512], I32, dsem=True)
        POSF = SB("POSF", [1, 512], F32)
        RR = [SB("RR%d" % i, [128, 512], F32) for i in range(2)]
        RI = SB("RI", [128, 512], I32)
        RF = SB("RF", [128, 512], F32)
        RMK = SB("RMK", [128, 512], F32)
        TA = SB("TA", [128, 512], BF16)
        TB_ = SB("TB", [128, 512], BF16)
        PTB = [[SB("PT%d_%d" % (m, i), [128, 512], BF16) for i in range(3)] for m in range(2)]
        FT = [SB("FT%d" % i, [128, 512], F32) for i in range(4)]
        OUTS = [SB("OUTS%d" % i, [128, 512], BF16, dsem=True) for i in range(2)]

        groups = [(0, QT), (128, KT)]
        blk = 0
        for b in range(nbatch):
            for n in range(T // 512):
                tok0 = b * T + n * 512
                cols = (n * 512, (n + 1) * 512)
                xb = XB[blk % 2]
                rs = RSTD[blk % 2]
                dma(P, "pool", xb[:], xT_v[:, :, tok0:tok0 + 512], (), xb.r(), xb.dsem)
                act(P, SQ[:], xb[:], AF.Square, xb.r(), SQ.r())
                ss = PSB[0]
                for kc in range(8):
                    mm(P, ss[:, :], ones_bf[:, :], SQ[:, kc, :], kc == 0, kc == 7, SQ.r() + ones_bf.r(), ss.r())
                act(P, LNV[:], ss[:, :], AF.Ln, ss.r() + eps_c.r(), LNV.r(), scale=1.0 / 1024, bias=eps_c[:])
                act(P, rs[:], LNV[:], AF.Exp, LNV.r(), rs.r(), scale=-0.5)
                for gi, (c0, dest) in enumerate(groups):
                    ps = PSB[1 + gi]
                    for kc in range(8):
                        mm(P, ps[:, :], Wb[:, kc, c0:c0 + 128], xb[:, kc, :], kc == 0, kc == 7, Wb.r() + xb.r(), ps.r())
                    tt(P, "dve", dest[:, cols[0]:cols[1]], ps[:, :], rs[:, :], ALU.mult, ps.r() + rs.r(), dest.r(*cols))
                ptm = PSB[3]
                prc = PSB[4]
                for sub in range(4):
                    sc = slice(sub * 128, (sub + 1) * 128)
                    for kc in range(8):
                        mm(P, ptm[:, sc], xb[:, kc, sc], Wb[:, kc, 256:384], kc == 0, kc == 7, Wb.r() + xb.r(), ptm.r())
                    mm(P, prc[:, sub:sub + 1], rs[0:1, sc], ones_f[0:1, 0:1], True, True, rs.r() + ones_f.r(), prc.r())
                cp(P, "dve", RCS[:, 0:4], prc[:, 0:4], prc.r(), RCS.r())
                for sub in range(4):
                    ti = n * 4 + sub
                    sc = slice(sub * 128, (sub + 1) * 128)
                    ts(P, "dve", V[:, ti, :], ptm[:, sc], RCS[:, sub:sub + 1], ALU.mult, ptm.r() + RCS.r(), V.r(ti * 128, ti * 128 + 128))
                blk += 1
            for n in range(T // 512):
                cols = (n * 512, (n + 1) * 512)
                cs = slice(*cols)
                dma(P, "sp", POSI[:], posd[b:b + 1, cs], (), POSI.r(), POSI.dsem)
                cp(P, "dve", POSF[:], POSI[:], POSI.r(), POSF.r())
                pa = PSB[5]
                mm(P, pa[:, :], invf[0:1, :], POSF[0:1, :], True, True, invf.r() + POSF.r(), pa.r())
                for which, dest in ((0, SINT), (1, COST)):
                    rr = RR[which]
                    if which == 0:
                        ts(P, "dve", rr[:], pa[:, :], 1.0 / TWO_PI, ALU.mult, pa.r(), rr.r())
                    else:
                        ts(P, "dve", rr[:], pa[:, :], 1.0 / TWO_PI, ALU.mult, pa.r(), rr.r(), 0.25, ALU.add)
                    cp(P, "dve", RI[:], rr[:], rr.r(), RI.r())
                    cp(P, "act", RF[:], RI[:], RI.r(), RF.r())
                    tt(P, "dve", rr[:], rr[:], RF[:], ALU.subtract, rr.r() + RF.r(), rr.r())
                    ts(P, "dve", RMK[:], rr[:], 0.0, ALU.is_lt, rr.r(), RMK.r())
                    tt(P, "dve", rr[:], rr[:], RMK[:], ALU.add, rr.r() + RMK.r(), rr.r())
                    if which == 0:
                        P.op("act", lambda e, rr=rr, dest=dest, cs=cs: e.activation(out=dest[:, cs], in_=rr[:], func=AF.Sin, scale=pi_c[:, 1:2], bias=pi_c[:, 0:1]),
                             rr.r() + pi_c.r(), dest.r(*cols))
                    else:
                        P.op("act", lambda e, rr=rr, dest=dest, cs=cs: e.activation(out=dest[:, cs], in_=rr[:], func=AF.Sin, scale=-TWO_PI, bias=pic2[:, 0:1]),
                             rr.r() + pic2.r(), dest.r(*cols))
            for n in range(T // 512):
                cols = (n * 512, (n + 1) * 512)
                cs = slice(*cols)
                for (tl, gap, bank) in ((QT, gq_ap, 1), (KT, gk_ap, 2)):
                    ps = PSB[bank]
                    act(P, SQ[:, 0, :], tl[:, cs], AF.Square, tl.r(*cols), SQ.r())
                    mm(P, ps[:, :], BD[:, :], SQ[:, 0, :], True, True, SQ.r() + BD.r(), ps.r())
                    act(P, LNV[:], ps[:, :], AF.Ln, ps.r() + eps_c.r(), LNV.r(), scale=1.0 / 64, bias=eps_c[:])
                    act(P, LNV[:], LNV[:], AF.Exp, LNV.r(), LNV.r(), scale=-0.5)
                    stt(P, tl[:, cs], tl[:, cs], gap, LNV[:], ALU.mult, ALU.mult, tl.r(*cols) + LNV.r() + par.r() + cst.r(), tl.r(*cols))
                    pw = PSB[bank + 2]
                    mm(P, pw[:, :], PM[:, :], tl[:, cs], True, True, PM.r() + tl.r(*cols), pw.r())
                    tt(P, "dve", TA[:], tl[:, cs], COST[:, cs], ALU.mult, tl.r(*cols) + COST.r(*cols), TA.r())
                    tt(P, "dve", TB_[:], pw[:, :], SINT[:, cs], ALU.mult, pw.r() + SINT.r(*cols), TB_.r())
                    tt(P, "dve", tl[:, cs], TA[:], TB_[:], ALU.add, TA.r() + TB_.r(), tl.r(*cols))
            if debug:
                continue
            k = 0
            for j in range(T // 512):
                accs = (PSB[4], PSB[5])
                dens = (PSB[6], PSB[7])
                nt = 4 * j + 4
                items = []
                for i in range(nt):
                    m_ = i - 4 * j
                    c0 = 128 * m_ if m_ >= 0 else 0
                    items.append((i, m_, c0))

                def qk(it, k):
                    i, m_, c0 = it
                    for mp in range(2):
                        sc = PSB[(k % 2) * 2 + mp]
                        rows = slice(mp * 64, (mp + 1) * 64)
                        mm(P, sc[:, c0:512], KT[rows, i * 128:(i + 1) * 128], QT[rows, j * 512 + c0:(j + 1) * 512], True, True,
                           KT.r(i * 128, i * 128 + 128) + QT.r(j * 512, j * 512 + 512), sc.r())

                def rest(it, k):
                    i, m_, c0 = it
                    for mp in range(2):
                        sc = PSB[(k % 2) * 2 + mp]
                        pt = PTB[mp][k % 3]
                        act(P, pt[:, c0:512], sc[:, c0:512], AF.Exp, sc.r(), pt.r())
                        if m_ >= 0:
                            memset(P, "pool", pt[64:128, c0:c0 + 64], 0.0, pt.r())
                        mm(P, accs[mp][:, c0:512], V[:, i, :], pt[:, c0:512], i == 0, i == nt - 1, V.r(i * 128, i * 128 + 128) + pt.r(), accs[mp].r())
                        mm(P, dens[mp][:, c0:512], ones_bf[:, :], pt[:, c0:512], i == 0, i == nt - 1, ones_bf.r() + pt.r(), dens[mp].r())
                LOOK = 1
                for idx in range(nt + LOOK):
                    if idx < nt:
                        qk(items[idx], k + idx)
                    if idx >= LOOK:
                        rest(items[idx - LOOK], k + idx - LOOK)
                k += nt
                recip(P, FT[0][:], dens[0][:, :], dens[0].r(), FT[0].r())
                tt(P, "dve", FT[1][:], accs[0][:, :], FT[0][:], ALU.mult, accs[0].r() + FT[0].r(), FT[1].r())
                recip(P, FT[0][:], dens[1][:, :], dens[1].r(), FT[0].r())
                tt(P, "dve", FT[2][:], accs[1][:, :], FT[0][:], ALU.mult, accs[1].r() + FT[0].r(), FT[2].r())
                stt(P, FT[3][:], FT[2][:], nlam_ap, FT[1][:], ALU.mult, ALU.add, FT[2].r() + FT[1].r() + cst.r(), FT[3].r())
                act(P, SQ[:, 0, :], FT[3][:], AF.Square, FT[3].r(), SQ.r())
                pn = PSB[0]
                mm(P, pn[:, :], ones_bf[:, :], SQ[:, 0, :], True, True, SQ.r() + ones_bf.r(), pn.r())
                act(P, LNV[:], pn[:, :], AF.Ln, pn.r() + eps_c.r(), LNV.r(), scale=1.0 / 128, bias=eps_c[:])
                act(P, LNV[:], LNV[:], AF.Exp, LNV.r(), LNV.r(), scale=-0.5)
                outs = OUTS[j % 2]
                stt(P, outs[:], FT[3][:], gs_ap, LNV[:], ALU.mult, ALU.mult, FT[3].r() + LNV.r() + cst.r(), outs.r())
                dma(P, "sp", oT[:, b * T + j * 512: b * T + (j + 1) * 512], outs[:], outs.r(), (), outs.dsem)
        if debug:
            dsd = P.new_dsem("dbg")
            for nm, tl in (("dQ", QT), ("dK", KT), ("dS", SINT), ("dC", COST)):
                dd = nc.dram_tensor(nm, [128, T], BF16, kind="ExternalOutput").ap()
                dma(P, "sp", dd, tl[:], tl.r(), (), dsd)
            dd = nc.dram_tensor("dV", [128, T], BF16, kind="ExternalOutput").ap()
            dma(P, "sp", dd, V[:].rearrange("p a b -> p (a b)"), V.r(), (), dsd)
        P.replay()
    return nc


def _prep_A(inp, xT):
    w = inp["even_w_in"][0]
    gm = np.ascontiguousarray(inp["norm_mix"][0].reshape(8, 128).T)
    maps = []
    for c in range(8):
        sl = lambda base: w[:, base + c * 64: base + (c + 1) * 64]
        wA = np.concatenate([sl(0), sl(512), sl(1536), sl(2048), sl(2560), w[:, 3584 + c:3585 + c], sl(1024), sl(3072)], axis=1)
        pA = np.zeros((64, 8), np.float32)
        pA[:, 0] = inp["hgrn_lb_logits"][0, c * 64:(c + 1) * 64]
        pA[:, 1] = inp["hgrn_lb_logits"][1, c * 64:(c + 1) * 64]
        pA[:, 2] = inp["hgrn_out_norm"][0]
        pA[:, 3] = inp["fox_q_norm"][0]
        pA[:, 4] = inp["fox_k_norm"][0]
        fb = np.empty((128, 1), np.float32)
        fb[:, 0] = inp["fox_f_bias"][0, c]
        maps.append({"xT": xT, "wA": np.ascontiguousarray(wA), "gmix": gm, "pA": pA, "fbias": fb})
    return maps


def _prep_B(inp, layer, hT_full, oT_full, w_out):
    wr = np.concatenate([inp["moe_router_group"][layer]] + [inp["moe_router_expert"][layer, g] for g in range(4)], axis=1)
    wg = np.ascontiguousarray(inp["moe_w_gate"][layer].reshape(32, 1024, 512))
    wu = np.ascontiguousarray(inp["moe_w_up"][layer].reshape(32, 1024, 512))
    wd = np.ascontiguousarray(inp["moe_w_down"][layer].reshape(32, 512, 1024))
    gf = np.ascontiguousarray(inp["norm_ffn"][layer].reshape(8, 128).T)
    wr = np.ascontiguousarray(wr)
    w_out = np.ascontiguousarray(w_out)
    maps = []
    for c in range(8):
        cs = slice(c * NT, (c + 1) * NT)
        maps.append({"hT_in": np.ascontiguousarray(hT_full[:, cs]), "oT_in": np.ascontiguousarray(oT_full[:, cs]),
                     "w_out": w_out, "gffn": gf, "w_r": wr, "w_gate": wg, "w_up": wu, "w_down": wd})
    return maps


def _prep_C(inp, hT_full):
    w = inp["odd_w_in"][0]
    gm = np.ascontiguousarray(inp["norm_mix"][1].reshape(8, 128).T)
    pos = np.ascontiguousarray(inp["positions"].astype(np.int32))
    maps = []
    for c in range(8):
        wC = np.concatenate([w[:, c * 128:(c + 1) * 128], w[:, 1024 + c * 128:1024 + (c + 1) * 128],
                             w[:, 2048 + c * 128:2048 + (c + 1) * 128]], axis=1)
        pC = np.zeros((128, 8), np.float32)
        pC[0:64, 0] = inp["diff_q_norm"][0]; pC[64:128, 0] = inp["diff_q_norm"][0]
        pC[0:64, 1] = inp["diff_k_norm"][0]; pC[64:128, 1] = inp["diff_k_norm"][0]
        pC[0:64, 2] = inp["diff_lambda_q1"][0]; pC[0:64, 3] = inp["diff_lambda_k1"][0]
        pC[0:64, 4] = inp["diff_lambda_q2"][0]; pC[0:64, 5] = inp["diff_lambda_k2"][0]
        pC[:, 6] = inp["diff_subln"][0]
        maps.append({"xT": hT_full, "wC": np.ascontiguousarray(wC), "gmix": gm, "pC": pC, "pos": pos})
    return maps


def _run(nc, maps):
    res = run_bass_kernel_spmd(nc, maps, core_ids=list(range(8)))
    return res.results


def kernel(**inputs):
    inp = {k: np.asarray(v) for k, v in inputs.items()}
    x = inp["x"].astype(np.float32, copy=False).reshape(-1, 1024)
    xT = np.ascontiguousarray(x.T)
    rA = _run(build_A(), _prep_A(inp, xT))
    oT0 = np.empty((1024, TOK), ml_dtypes.bfloat16)
    for c in range(8):
        o = np.asarray(rA[c]["oT"])
        oT0[c * 64:(c + 1) * 64] = o[0:64]
        oT0[512 + c * 64:512 + (c + 1) * 64] = o[64:128]
    rB = _run(build_B(), _prep_B(inp, 0, xT, oT0, inp["even_w_out"][0]))
    h1T = np.ascontiguousarray(np.concatenate([np.asarray(r["hT_out"]) for r in rB], axis=1))
    rC = _run(build_C(), _prep_C(inp, h1T))
    oT1 = np.ascontiguousarray(np.concatenate([np.asarray(r["oT"]) for r in rC], axis=0))
    rD = _run(build_B(), _prep_B(inp, 1, h1T, oT1, inp["odd_w_out"][0]))
    outT = np.concatenate([np.asarray(r["hT_out"]) for r in rD], axis=1)
    return np.ascontiguousarray(outT.T).reshape(2, 8192, 1024).astype(np.float32, copy=False)
```

```python
import ml_dtypes
import numpy as np
from contextlib import ExitStack
import concourse.bass as bass
import concourse.mybir as mybir
from concourse.bass_utils import run_bass_kernel_spmd

F32 = mybir.dt.float32
BF16 = mybir.dt.bfloat16
I32 = mybir.dt.int32
AF = mybir.ActivationFunctionType
ALU = mybir.AluOpType
AX = mybir.AxisListType

ENGS = ("pe", "act", "dve", "pool", "sp")


class Res:
    __slots__ = ("name", "last_w", "readers", "dsem")

    def __init__(self, name):
        self.name = name
        self.last_w = None
        self.readers = []
        self.dsem = None


class Op:
    __slots__ = ("eng", "fn", "deps", "is_dma", "sem", "semval", "signal", "count", "dma_waits", "idx")

    def __init__(self, eng, fn):
        self.eng = eng
        self.fn = fn
        self.deps = []
        self.is_dma = False
        self.sem = None
        self.semval = 0
        self.signal = False
        self.count = 0
        self.dma_waits = []
        self.idx = 0


class Prog:
    def __init__(self, nc):
        self.nc = nc
        self.ops = {e: [] for e in ENGS}
        self.all_res_reset = True
        self.esem = {e: nc.alloc_semaphore("es_" + e) for e in ENGS}
        self.dsems = []
        self.dma_cum = {}
        self.nops = 0
        self.ecount = {e: 0 for e in ENGS}
        self.waited = {e: {} for e in ENGS}

    def res(self, name, n=1):
        return [Res("%s.%d" % (name, i)) for i in range(n)]

    def new_dsem(self, name):
        s = self.nc.alloc_semaphore("ds_" + name)
        self.dma_cum[s] = 0
        return s

    def op(self, eng, fn, reads=(), writes=(), dma_sem=None):
        o = Op(eng, fn)
        o.idx = self.nops
        self.nops += 1
        deps = {}
        dma_waits = {}

        def add_dep(p):
            if p is None:
                return
            if p.is_dma:
                dma_waits[p.sem] = self.dma_cum[p.sem]
            else:
                deps[id(p)] = p

        for r in reads:
            add_dep(r.last_w)
        for r in writes:
            add_dep(r.last_w)
            for q in r.readers:
                add_dep(q)
        raw = set()
        for r in reads:
            if r.last_w is not None and not r.last_w.is_dma:
                raw.add(id(r.last_w))
        for k, p in list(deps.items()):
            if p.eng == eng and eng == "pe" and dma_sem is None:
                del deps[k]
        o.deps = list(deps.values())
        for p in o.deps:
            p.signal = True
        o.dma_waits = list(dma_waits.items())
        if dma_sem is not None:
            o.is_dma = True
            o.sem = dma_sem
            self.dma_cum[dma_sem] += 16
            o.semval = self.dma_cum[dma_sem]
        for r in reads:
            r.readers.append(o)
        for r in writes:
            r.last_w = o
            r.readers = []
        self.ops[eng].append(o)
        return o

    def finish_wait(self, eng="sp"):
        waits = [(s, v) for s, v in self.dma_cum.items() if v > 0]
        o = Op(eng, None)
        o.dma_waits = waits
        self.ops[eng].append(o)

    def replay(self):
        nc = self.nc
        for e in ENGS:
            last = None
            for o in self.ops[e]:
                if o.fn is not None and not o.is_dma:
                    last = o
            if last is not None:
                last.signal = True
            c = self.ecount[e]
            for o in self.ops[e]:
                if o.signal and not o.is_dma:
                    c += 1
                    o.count = c
            self.ecount[e] = c
        final = {self.esem[e]: self.ecount[e] for e in ENGS if self.ecount[e] > 0}
        for s_, v_ in self.dma_cum.items():
            if v_ > 0:
                final[s_] = v_
        handles = {"pe": "tensor", "act": "scalar", "dve": "vector", "pool": "gpsimd", "sp": "sync"}
        with nc.Block() as block:
            for e in ENGS:
                ops = self.ops[e]
                esem = self.esem
                my = esem[e]

                def body(engh, ops=ops, e=e, my=my):
                    waited = self.waited[e]
                    for o in ops:
                        need = {}
                        for p in o.deps:
                            s = esem[p.eng]
                            if need.get(s, 0) < p.count:
                                need[s] = p.count
                        for s, v in o.dma_waits:
                            if need.get(s, 0) < v:
                                need[s] = v
                        for s, v in need.items():
                            if waited.get(s, 0) < v:
                                engh.wait_ge(s, v)
                                waited[s] = v
                        if o.fn is None:
                            continue
                        ins = o.fn(engh)
                        if o.is_dma:
                            ins.then_inc(o.sem, 16)
                        elif o.signal:
                            ins.then_inc(my, 1)
                    for s, v in final.items():
                        if s is my:
                            continue
                        if waited.get(s, 0) < v:
                            engh.wait_ge(s, v)
                            waited[s] = v

                getattr(block, handles[e])(body)
        self.ops = {e: [] for e in ENGS}
        self.all_res_reset = True


from math import prod
import os
HG = int(os.environ.get('HG_STAGE', '9'))


class TT:
    def __init__(self, P, st, name, shape, dt, bs=None, psum=False, dsem=False):
        nc = P.nc
        self.h = st.enter_context(nc.psum_tensor(name, shape, dt) if psum else nc.sbuf_tensor(name, shape, dt))
        self.F = prod(shape[1:])
        self.bs = bs or self.F
        self.res = P.res(name, (self.F + self.bs - 1) // self.bs)
        self.dsem = P.new_dsem(name) if dsem else None

    def r(self, lo=0, hi=None):
        hi = self.F if hi is None else hi
        return self.res[lo // self.bs:(hi - 1) // self.bs + 1]

    def __getitem__(self, k):
        return self.h[k]


def mm(P, out, lhsT, rhs, start, stop, rd, wr):
    return P.op("pe", lambda e: e.matmul(out, lhsT=lhsT, rhs=rhs, start=start, stop=stop), rd, wr)


def act(P, out, in_, func, rd, wr, scale=1.0, bias=None):
    if bias is None:
        return P.op("act", lambda e: e.activation(out=out, in_=in_, func=func, scale=scale), rd, wr)
    return P.op("act", lambda e: e.activation(out=out, in_=in_, func=func, scale=scale, bias=bias), rd, wr)


def tt(P, eng, out, in0, in1, op, rd, wr):
    return P.op(eng, lambda e: e.tensor_tensor(out=out, in0=in0, in1=in1, op=op), rd, wr)


def ts(P, eng, out, in0, s1, op0, rd, wr, s2=None, op1=None):
    if op1 is None:
        return P.op(eng, lambda e: e.tensor_scalar(out=out, in0=in0, scalar1=s1, scalar2=None, op0=op0), rd, wr)
    return P.op(eng, lambda e: e.tensor_scalar(out=out, in0=in0, scalar1=s1, scalar2=s2, op0=op0, op1=op1), rd, wr)


def stt(P, out, in0, scalar, in1, op0, op1, rd, wr):
    return P.op("dve", lambda e: e.scalar_tensor_tensor(out=out, in0=in0, scalar=scalar, in1=in1, op0=op0, op1=op1), rd, wr)


def cp(P, eng, out, in_, rd, wr):
    if eng == "act":
        return P.op("act", lambda e: e.copy(out=out, in_=in_), rd, wr)
    return P.op(eng, lambda e: e.tensor_copy(out=out, in_=in_), rd, wr)


def recip(P, out, in_, rd, wr):
    return P.op("dve", lambda e: e.reciprocal(out=out, in_=in_), rd, wr)


def dma(P, q, out, in_, rd, wr, sem):
    return P.op(q, lambda e: e.dma_start(out=out, in_=in_), rd, wr, dma_sem=sem)


def memset(P, eng, ap, val, wr):
    return P.op(eng, lambda e: e.memset(ap, val), (), wr)


T = int(os.environ.get('KT', 8192))
TOK = 2 * T
NCOL_A = 449
EPS = 1e-6


def build_A(nbatch=2, do_hgrn=True, do_fox=True, debug=False):
    nc = bass.Bass("TRN2", target_bir_lowering=False)
    xT = nc.dram_tensor("xT", [1024, TOK], F32, kind="ExternalInput").ap()
    wA = nc.dram_tensor("wA", [1024, NCOL_A], F32, kind="ExternalInput").ap()
    gmix = nc.dram_tensor("gmix", [128, 8], F32, kind="ExternalInput").ap()
    pA = nc.dram_tensor("pA", [64, 8], F32, kind="ExternalInput").ap()
    fbias = nc.dram_tensor("fbias", [128, 1], F32, kind="ExternalInput").ap()
    oT = nc.dram_tensor("oT", [128, TOK], BF16, kind="ExternalOutput").ap()
    P = Prog(nc)
    xT_v = xT.rearrange("(kc p) t -> p kc t", p=128)
    with ExitStack() as st:
        def SB(name, shape, dt, bs=None, dsem=False):
            return TT(P, st, name, shape, dt, bs=bs, dsem=dsem)

        ones_bf = SB("ones_bf", [128, 128], BF16)
        ones_f = SB("ones_f", [128, 64], F32)
        eps_c = SB("eps_c", [128, 1], F32)
        one_c = SB("one_c", [128, 1], F32)
        ident = SB("ident", [128, 128], BF16)
        M2 = SB("M2", [128, 128], BF16)
        TBH = 256
        rmask = SB("rmask", [64, TBH], F32)
        par = SB("par", [64, 8], F32, dsem=True)
        gm = SB("gm", [128, 8], F32, dsem=True)
        nfb = SB("nfb", [128, 1], F32, dsem=True)
        lbc = SB("lbc", [64, 4], F32)
        Wf = None
        Wb = SB("Wb", [128, 8, NCOL_A], BF16, dsem=True)

        memset(P, "pool", ones_bf[:], 1.0, ones_bf.r())
        memset(P, "pool", ones_f[:], 1.0, ones_f.r())
        memset(P, "pool", eps_c[:], EPS, eps_c.r())
        memset(P, "pool", one_c[:], 1.0, one_c.r())
        memset(P, "pool", ident[:], 1.0, ident.r())
        P.op("pool", lambda e: e.affine_select(out=ident[:], in_=ident[:], pattern=[[-1, 128]], compare_op=ALU.is_equal,
                                               fill=0.0, base=0, channel_multiplier=1), ident.r(), ident.r())
        memset(P, "pool", M2[:], 1.0, M2.r())
        P.op("pool", lambda e: e.affine_select(out=M2[:], in_=M2[:], pattern=[[1, 128]], compare_op=ALU.is_ge,
                                               fill=0.0, base=0, channel_multiplier=-1), M2.r(), M2.r())
        memset(P, "pool", M2[0:64, 64:128], 0.0, M2.r())
        memset(P, "pool", rmask[:], 1.0, rmask.r())
        memset(P, "pool", rmask[:].rearrange("p (c l) -> p c l", l=64)[:, :, 0:1], 0.0, rmask.r())

        dma(P, "sp", par[:], pA, (), par.r(), par.dsem)
        dma(P, "sp", gm[:], gmix, (), gm.r(), gm.dsem)
        dma(P, "sp", nfb[:], fbias, (), nfb.r(), nfb.dsem)
        dma(P, "pool", Wb[:], wA.rearrange("(kc p) n -> p kc n", p=128), (), Wb.r(), Wb.dsem)
        for kc in range(8):
            ts(P, "dve", Wb[:, kc, :], Wb[:, kc, :], gm[:, kc:kc + 1], ALU.mult, Wb.r() + gm.r(), Wb.r())
        ts(P, "dve", nfb[:], nfb[:], -1.0, ALU.mult, nfb.r(), nfb.r())
        tt(P, "dve", lbc[:, 3:4], par[:, 1:2], par[:, 0:1], ALU.subtract, par.r(), lbc.r())
        act(P, lbc[:, 3:4], lbc[:, 3:4], AF.Exp, lbc.r(), lbc.r())
        ts(P, "dve", lbc[:, 3:4], lbc[:, 3:4], 1.0, ALU.add, lbc.r(), lbc.r())
        recip(P, lbc[:, 0:1], lbc[:, 3:4], lbc.r(), lbc.r())
        ts(P, "dve", lbc[:, 1:2], lbc[:, 0:1], -1.0, ALU.mult, lbc.r(), lbc.r(), 1.0, ALU.add)
        ts(P, "dve", lbc[:, 2:3], par[:, 3:4], 0.125, ALU.mult, par.r(), lbc.r())
        lb_ap, oml_ap, gq8_ap = lbc[:, 0:1], lbc[:, 1:2], lbc[:, 2:3]
        on_ap, gk_ap = par[:, 2:3], par[:, 4:5]

        Q = SB("Q", [64, T], BF16, bs=512)
        Fh = SB("Fh", [64, T], BF16, bs=512)
        G = SB("G", [64, T], BF16, bs=512)
        BQ = SB("BQ", [70, T], BF16, bs=512, dsem=True)
        BK = SB("BK", [70, T], BF16, bs=128, dsem=True)
        VAB = SB("VAB", [128, T // 128, 129], BF16, bs=129)
        XB = [SB("XB%d" % i, [128, 8, 512], BF16, dsem=True) for i in range(2)]
        SQ = SB("SQ", [128, 8, 512], BF16)
        LNV = SB("LNV", [128, 512], F32)
        RSTD = [SB("RSTD%d" % i, [128, 512], F32) for i in range(2)]
        ZF = SB("ZF", [128, 512], F32)
        CC = [SB("CC%d" % i, [128, 512], F32) for i in range(2)]
        R1 = SB("R1", [128, 512], F32)
        AUG = [SB("AUG0", [128, 6, 512], BF16)] * 2
        RCS = SB("RCS", [128, 4], F32)
        PSB = [TT(P, st, "ps%d" % i, [128, 512], F32, psum=True) for i in range(8)]

        HT2 = [[SB("HT%d_%d" % (k_, i), [64, TBH], F32) for i in range(5)] for k_ in range(2)]
        KHT2 = [SB("KHT%d" % k_, [64, TBH], BF16) for k_ in range(2)]
        ELC0 = SB("ELC0", [64, T // 64], F32)

        KH = SB("KH", [128, T // 128, 64], BF16, bs=64)
        ELC = SB("ELC", [64, T // 64], F32)
        SBF = SB("SBF", [64, T], BF16)
        Z64 = SB("Z64", [64, 64], BF16)
        memset(P, "pool", Z64[:], 0.0, Z64.r())
        ATS = [SB("ATS%d" % i, [128, 512], BF16) for i in range(2)]
        OAS = [SB("OAS%d" % i, [64, 512], BF16, dsem=True) for i in range(2)]
        PTB = [SB("PTB%d" % i, [128, 512], BF16) for i in range(4)]
        ACCS = SB("ACCS", [65, 512], F32)
        RDEN = SB("RDEN", [64, 512], F32)
        OBS = [SB("OBS%d" % i, [64, 512], BF16, dsem=True) for i in range(2)]
        RONE = SB("RONE", [128, 512], BF16)
        memset(P, "pool", RONE[:], 1.0, RONE.r())
        memset(P, "pool", VAB[:, :, 128:129], 1.0, VAB.r())
        memset(P, "pool", BQ[64:70, :], 1.0, BQ.r())
        memset(P, "pool", BK[64:70, :], 1.0, BK.r())

        groups = [(0, 64, Q), (64, 64, Fh), (128, 64, G), (192, 64, BQ), (256, 65, BK)]
        blk = 0
        for b in range(nbatch):
            for n in range(T // 512):
                tok0 = b * T + n * 512
                cols = (n * 512, (n + 1) * 512)
                xb = XB[blk % 2]
                rs = RSTD[blk % 2]
                dma(P, "pool", xb[:], xT_v[:, :, tok0:tok0 + 512], (), xb.r(), xb.dsem)
                act(P, SQ[:], xb[:], AF.Square, xb.r(), SQ.r())
                ss = PSB[0]
                for kc in range(8):
                    mm(P, ss[:, :], ones_bf[:, :], SQ[:, kc, :], kc == 0, kc == 7, SQ.r() + ones_bf.r(), ss.r())
                act(P, LNV[:], ss[:, :], AF.Ln, ss.r() + eps_c.r(), LNV.r(), scale=1.0 / 1024, bias=eps_c[:])
                act(P, rs[:], LNV[:], AF.Exp, LNV.r(), rs.r(), scale=-0.5)
                for gi, (c0, M, dest) in enumerate(groups):
                    ps = PSB[1 + gi]
                    for kc in range(8):
                        mm(P, ps[0:M, :], Wb[:, kc, c0:c0 + M], xb[:, kc, :], kc == 0, kc == 7, Wb.r() + xb.r(), ps.r())
                    tt(P, "dve", dest[0:64, cols[0]:cols[1]], ps[0:64, :], rs[0:64, :], ALU.mult,
                       ps.r() + rs.r(), dest.r(*cols))
                    if M == 65:
                        cc = CC[blk % 2]
                        ccp = CC[(blk + 1) % 2]
                        aug = AUG[blk % 2]
                        r64 = slice(64, 65)
                        tt(P, "dve", ZF[r64, :], ps[r64, :], rs[r64, :], ALU.mult, ps.r() + rs.r(), ZF.r())
                        act(P, ZF[r64, :], ZF[r64, :], AF.Exp, ZF.r() + nfb.r(), ZF.r(), scale=-1.0, bias=nfb[r64, :])
                        act(P, ZF[r64, :], ZF[r64, :], AF.Ln, ZF.r() + one_c.r(), ZF.r(), scale=1.0, bias=one_c[r64, :])
                        init = 0.0 if n == 0 else ccp[r64, 511:512]
                        P.op("dve", lambda e, cc=cc, init=init: e.tensor_tensor_scan(
                            out=cc[64:65, :], data0=RONE[64:65, :],
                            data1=ZF[64:65, :], initial=init, op0=ALU.mult, op1=ALU.add),
                            ZF.r() + ccp.r() + RONE.r(), cc.r())
                        cp(P, "dve", aug[r64, 0, :], cc[r64, :], cc.r(), aug.r())
                        tt(P, "dve", R1[r64, :], cc[r64, :], aug[r64, 0, :], ALU.subtract, cc.r() + aug.r(), R1.r())
                        cp(P, "dve", aug[r64, 1, :], R1[r64, :], R1.r(), aug.r())
                        tt(P, "dve", R1[r64, :], R1[r64, :], aug[r64, 1, :], ALU.subtract, R1.r() + aug.r(), R1.r())
                        cp(P, "dve", aug[r64, 2, :], R1[r64, :], R1.r(), aug.r())
                        ts(P, "dve", aug[r64, 3:6, :], aug[r64, 0:3, :], -1.0, ALU.mult, aug.r(), aug.r())
                        for i in range(3):
                            dma(P, "sp", BK[67 + i:68 + i, cols[0]:cols[1]], aug[r64, i, :], aug.r(), BK.r(*cols), BK.dsem)
                            dma(P, "sp", BQ[64 + i:65 + i, cols[0]:cols[1]], aug[r64, 3 + i, :], aug.r(), BQ.r(*cols), BQ.dsem)
                ptm = PSB[6]
                prc = PSB[7]
                for sub in range(4):
                    sc = slice(sub * 128, (sub + 1) * 128)
                    for kc in range(8):
                        mm(P, ptm[:, sc], xb[:, kc, sc], Wb[:, kc, 321:449], kc == 0, kc == 7, Wb.r() + xb.r(), ptm.r())
                    mm(P, prc[:, sub:sub + 1], rs[0:1, sc], ones_f[0:1, 0:1], True, True, rs.r() + ones_f.r(), prc.r())
                cp(P, "dve", RCS[:, 0:4], prc[:, 0:4], prc.r(), RCS.r())
                for sub in range(4):
                    ti = n * 4 + sub
                    sc = slice(sub * 128, (sub + 1) * 128)
                    ts(P, "dve", VAB[:, ti, 0:128], ptm[:, sc], RCS[:, sub:sub + 1], ALU.mult,
                       ptm.r() + RCS.r(), VAB.r(ti * 129, ti * 129 + 129))
                blk += 1
            if do_fox:
                for n in range(T // 512):
                    cs = slice(n * 512, (n + 1) * 512)
                    for (tl, gap, bank) in ((BQ, gq8_ap, 1), (BK, gk_ap, 2)):
                        ps = PSB[bank]
                        act(P, SQ[0:64, 0, :], tl[0:64, cs], AF.Square, tl.r(n * 512, n * 512 + 512), SQ.r())
                        mm(P, ps[0:64, :], ones_bf[0:64, 0:64], SQ[0:64, 0, :], True, True, SQ.r() + ones_bf.r(), ps.r())
                        act(P, LNV[0:64, :], ps[0:64, :], AF.Ln, ps.r() + eps_c.r(), LNV.r(), scale=1.0 / 64, bias=eps_c[0:64, :])
                        act(P, LNV[0:64, :], LNV[0:64, :], AF.Exp, LNV.r(), LNV.r(), scale=-0.5)
                        stt(P, tl[0:64, cs], tl[0:64, cs], gap, LNV[0:64, :], ALU.mult, ALU.mult,
                            tl.r(n * 512, n * 512 + 512) + LNV.r() + par.r() + lbc.r(), tl.r(n * 512, n * 512 + 512))
            if do_hgrn:
                def hg_elem(par_):
                    for sbk in range(par_, T // TBH, 2):
                        c0, c1 = sbk * TBH, (sbk + 1) * TBH
                        cs = slice(c0, c1)
                        t1, t2, t3, t4, t5 = HT2[sbk % 2]
                        kht = KHT2[sbk % 2]
                        act(P, t1[:], Fh[0:64, cs], AF.Exp, Fh.r(c0, c1), t1.r(), scale=-1.0)
                        yield
                        act(P, t1[:], t1[:], AF.Identity, t1.r() + one_c.r(), t1.r(), scale=1.0, bias=one_c[0:64, :])
                        yield
                        recip(P, t1[:], t1[:], t1.r(), t1.r())
                        yield
                        ts(P, "dve", t1[:], t1[:], oml_ap, ALU.mult, t1.r() + lbc.r(), t1.r(), lb_ap, ALU.add)
                        yield
                        act(P, t2[:], t1[:], AF.Ln, t1.r(), t2.r())
                        yield
                        P.op("dve", lambda e, t3=t3, t2=t2: e.tensor_tensor_scan(out=t3[:], data0=rmask[:], data1=t2[:], initial=0.0,
                                                                                 op0=ALU.mult, op1=ALU.add), t2.r() + rmask.r(), t3.r())
                        yield
                        act(P, t1[:], t1[:], AF.Identity, t1.r() + one_c.r(), t1.r(), scale=-1.0, bias=one_c[0:64, :])
                        yield
                        act(P, t4[:], t3[:], AF.Exp, t3.r(), t4.r())
                        yield
                        act(P, t5[:], Q[0:64, cs], AF.Exp, Q.r(c0, c1), t5.r(), scale=-1.0)
                        yield
                        act(P, t5[:], t5[:], AF.Identity, t5.r() + one_c.r(), t5.r(), scale=1.0, bias=one_c[0:64, :])
                        yield
                        recip(P, t5[:], t5[:], t5.r(), t5.r())
                        yield
                        tt(P, "dve", t5[:], Q[0:64, cs], t5[:], ALU.mult, Q.r(c0, c1) + t5.r(), t5.r())
                        yield
                        tt(P, "dve", Q[0:64, cs], t5[:], t4[:], ALU.mult, t5.r() + t4.r(), Q.r(c0, c1))
                        yield
                        act(P, t4[:], t3[:], AF.Exp, t3.r(), t4.r(), scale=-1.0)
                        yield
                        tt(P, "dve", Fh[0:64, cs], t1[:], t4[:], ALU.mult, t1.r() + t4.r(), Fh.r(c0, c1))
                        yield
                        nch = TBH // 64
                        ch0 = sbk * nch
                        act(P, ELC[0:64, ch0:ch0 + nch], t3[:].rearrange("p (c l) -> p c l", l=64)[:, :, 63],
                            AF.Exp, t3.r(), ELC.r())
                        yield
                        tt(P, "dve", kht[:].rearrange("p (c l) -> p c l", l=64), Fh[0:64, cs].rearrange("p (c l) -> p c l", l=64),
                           ELC[0:64, ch0:ch0 + nch].unsqueeze(2).broadcast_to([64, nch, 64]), ALU.mult,
                           Fh.r(c0, c1) + ELC.r(), kht.r())
                        yield
                        pst = PSB[7] if sbk % 2 == 0 else PSB[0]
                        pstb = pst.h.bitcast(BF16)
                        ntl = TBH // 128
                        for j in range(ntl):
                            P.op("pe", lambda e, j=j, pstb=pstb, kht=kht: e.transpose(out=pstb[:, j * 64:(j + 1) * 64], in_=kht[0:64, j * 128:(j + 1) * 128],
                                                                  identity=ident[0:64, 0:64]), kht.r() + ident.r(), pst.r())
                            yield
                        ti0 = sbk * ntl
                        cp(P, "act", KH[:, ti0:ti0 + ntl, :], pstb[:, 0:ntl * 64].rearrange("p (a b) -> p a b", b=64),
                           pst.r(), KH.r(ti0 * 64, (ti0 + ntl) * 64))
                        yield
                        act(P, t2[:], G[0:64, cs], AF.Exp, G.r(c0, c1), t2.r(), scale=-1.0)
                        yield
                        act(P, t2[:], t2[:], AF.Identity, t2.r() + one_c.r(), t2.r(), scale=1.0, bias=one_c[0:64, :])
                        yield
                        recip(P, t2[:], t2[:], t2.r(), t2.r())
                        yield
                        tt(P, "dve", G[0:64, cs], G[0:64, cs], t2[:], ALU.mult, G.r(c0, c1) + t2.r(), G.r(c0, c1))
                        yield

                hg_gens = [hg_elem(0), hg_elem(1)]
            if do_fox:
                k = 0
                for j in range(T // 512):
                    acc = PSB[4 + j % 2]
                    nt = 4 * j + 4
                    items = []
                    for i in range(nt):
                        m = i - 4 * j
                        c0 = 128 * m if m >= 0 else 0
                        items.append((i, m, c0))
                    def qk(it, k):
                        i, m, c0 = it
                        sc = PSB[1 + k % 3]
                        mm(P, sc[:, c0:512], BK[0:70, i * 128:(i + 1) * 128], BQ[0:70, j * 512 + c0:(j + 1) * 512], True, True,
                           BK.r(i * 128, i * 128 + 128) + BQ.r(j * 512, j * 512 + 512), sc.r())
                    def rest(it, k):
                        i, m, c0 = it
                        sc = PSB[1 + k % 3]
                        pt = PTB[k % 4]
                        act(P, pt[:, c0:512], sc[:, c0:512], AF.Exp, sc.r(), pt.r())
                        if m >= 0:
                            P.op("pool", lambda e, pt=pt, c0=c0: e.affine_select(out=pt[:, c0:c0 + 128], in_=pt[:, c0:c0 + 128], pattern=[[1, 128]],
                                 compare_op=ALU.is_ge, fill=0.0, base=0, channel_multiplier=-1), pt.r(), pt.r())
                        mm(P, acc[0:65, c0:512], VAB[:, i, 64:129], pt[:, c0:512], i == 0, i == nt - 1,
                           VAB.r(i * 129, i * 129 + 129) + pt.r(), acc.r())
                    LOOK = 2
                    for idx in range(nt + LOOK):
                        if idx < nt:
                            qk(items[idx], k + idx)
                        if idx >= LOOK:
                            rest(items[idx - LOOK], k + idx - LOOK)
                            if do_hgrn and os.environ.get("HG_NOILV") is None:
                                for g_ in hg_gens:
                                    next(g_, None)
                    k += nt
                    cp(P, "dve", ACCS[0:65, :], acc[0:65, :], acc.r(), ACCS.r())
                    pden = PSB[6]
                    mm(P, pden[0:64, :], ones_f[64:65, 0:64], ACCS[64:65, :], True, True, ones_f.r() + ACCS.r(), pden.r())
                    recip(P, RDEN[:], pden[0:64, :], pden.r(), RDEN.r())
                    obs = OBS[j % 2]
                    tt(P, "pool", obs[:], ACCS[0:64, :], RDEN[:], ALU.mult, ACCS.r() + RDEN.r(), obs.r())
                    dma(P, "sp", oT[64:128, b * T + j * 512: b * T + (j + 1) * 512], obs[:], obs.r(), (), obs.dsem)
            if do_hgrn:
                for g_ in hg_gens:
                    for _ in g_:
                        pass
                DSV = BQ
                NC_ = T // 64
                for g4 in range(T // 512):
                    for c in range(8 * g4, 8 * g4 + 8):
                        ti, hh = c // 2, c % 2
                        pds = PSB[4 + (g4 % 2) * 2 + hh]
                        slot = (c % 8) * 64
                        hs = slice(hh * 64, (hh + 1) * 64)
                        mm(P, pds[0:64, slot:slot + 64], KH[hs, ti, :], VAB[hs, ti, 0:64], True, True,
                           KH.r(ti * 64, ti * 64 + 64) + VAB.r(ti * 129, ti * 129 + 129), pds.r())
                    for hh in range(2):
                        pds = PSB[4 + (g4 % 2) * 2 + hh]
                        src = pds[0:64, :].rearrange("p (a b) -> p a b", b=128)[:, :, hh * 64:(hh + 1) * 64]
                        dst = DSV[0:64, :].rearrange("p (v c) -> p c v", c=NC_)[:, 8 * g4 + hh:8 * g4 + 8:2, :]
                        cp(P, "dve" if hh == 0 else "act", dst, src, pds.r(), DSV.r())
                cp(P, "pool", ELC0[:], ELC[:], ELC.r(), ELC0.r())
                memset(P, "pool", ELC0[:, 0:1], 0.0, ELC0.r())
                D0 = BK
                cp(P, "act", D0[0:64, :].rearrange("p (v c) -> p v c", c=NC_), ELC0[:, :].unsqueeze(1).broadcast_to([64, 64, NC_]),
                   ELC0.r(), D0.r())
                P.op("dve", lambda e: e.tensor_tensor_scan(out=SBF[:, :], data0=D0[0:64, :], data1=DSV[0:64, :],
                                                           initial=0.0, op0=ALU.mult, op1=ALU.add), D0.r() + DSV.r(), SBF.r())
                for g4 in range(T // 512):
                    pat = PSB[6 + g4 % 2]
                    for j in range(4):
                        ti = g4 * 4 + j
                        tcs = slice(ti * 128, (ti + 1) * 128)
                        mm(P, pat[:, j * 128:(j + 1) * 128], Fh[0:64, tcs], Q[0:64, tcs], True, True,
                           Fh.r(ti * 128, ti * 128 + 128) + Q.r(ti * 128, ti * 128 + 128), pat.r())
                    ats = ATS[g4 % 2]
                    tt(P, "dve", ats[:].rearrange("p (a b) -> p a b", b=128), pat[:, :].rearrange("p (a b) -> p a b", b=128),
                       M2[:].unsqueeze(1).broadcast_to([128, 4, 128]), ALU.mult, pat.r() + M2.r(), ats.r())
                    po = PSB[1 + g4 % 2]
                    for jj in range(4):
                        tj = g4 * 4 + jj
                        for hh in range(2):
                            c = 2 * tj + hh
                            ccs = slice(c * 64, (c + 1) * 64)
                            oc = slice(jj * 128 + hh * 64, jj * 128 + hh * 64 + 64)
                            mm(P, po[0:64, oc], VAB[:, tj, 0:64], ats[:, oc], True, False,
                               VAB.r(tj * 129, tj * 129 + 129) + ats.r(), po.r())
                            st_ap = Z64[:, :] if c == 0 else SBF[:, :].rearrange("p (v c) -> p v c", c=NC_)[:, :, c - 1]
                            mm(P, po[0:64, oc], st_ap, Q[0:64, ccs], False, True,
                               SBF.r() + Z64.r() + Q.r(c * 64, c * 64 + 64), po.r())
                    gcs = slice(g4 * 512, (g4 + 1) * 512)
                    pn = PSB[3]
                    sq64 = SQ
                    lnv = RSTD[g4 % 2]
                    oaf = (ZF, R1)[g4 % 2]
                    act(P, sq64[0:64, g4 % 2, :], po[0:64, :], AF.Square, po.r(), sq64.r())
                    mm(P, pn[0:64, :], ones_bf[0:64, 0:64], sq64[0:64, g4 % 2, :], True, True, sq64.r() + ones_bf.r(), pn.r())
                    act(P, lnv[0:64, :], pn[0:64, :], AF.Ln, pn.r() + eps_c.r(), lnv.r(), scale=1.0 / 64, bias=eps_c[0:64, :])
                    act(P, lnv[0:64, :], lnv[0:64, :], AF.Exp, lnv.r(), lnv.r(), scale=-0.5)
                    stt(P, oaf[0:64, :], po[0:64, :], on_ap, lnv[0:64, :], ALU.mult, ALU.mult, po.r() + lnv.r() + par.r(), oaf.r())
                    oas = OAS[g4 % 2]
                    tt(P, "dve", oas[:], oaf[0:64, :], G[0:64, gcs], ALU.mult, oaf.r() + G.r(g4 * 512, g4 * 512 + 512), oas.r())
                    dma(P, "sp", oT[0:64, b * T + g4 * 512: b * T + (g4 + 1) * 512], oas[:], oas.r(), (), oas.dsem)
        if os.environ.get('HG_DBG'):
            dsd2 = P.new_dsem("dbg2")
            for nm, tl, shp, dt_ in (("dKH", KH, [128, (T // 128) * 64], BF16), ("dKt", Fh, [64, T], BF16), ("dQt", Q, [64, T], BF16), ("dELC", ELC, [64, T // 64], F32), ("dSBF", SBF, [64, T], BF16)):
                dd = nc.dram_tensor(nm, shp, dt_, kind="ExternalOutput").ap()
                src = tl[:] if len(tl.h.shape) == 2 else tl[:].rearrange("p a b -> p (a b)")
                dma(P, "sp", dd, src, tl.r(), (), dsd2)
        if debug:
            dsd = P.new_dsem("dbg")
            for nm, tl, shp in (("dQ", Q, [64, T]), ("dF", Fh, [64, T]), ("dG", G, [64, T]), ("dBQ", BQ, [70, T]), ("dBK", BK, [70, T]), ("dV", VAB, [128, (T // 128) * 129])):
                dd = nc.dram_tensor(nm, shp, BF16, kind="ExternalOutput").ap()
                src = tl[:] if nm != "dV" else tl[:].rearrange("p a b -> p (a b)")
                dma(P, "sp", dd, src, tl.r(), (), dsd)
        P.replay()
    return nc


NT = 2048


def build_B():
    nc = bass.Bass("TRN2", target_bir_lowering=False)
    hT_in = nc.dram_tensor("hT_in", [1024, NT], F32, kind="ExternalInput").ap()
    oT_in = nc.dram_tensor("oT_in", [1024, NT], BF16, kind="ExternalInput").ap()
    w_out = nc.dram_tensor("w_out", [1024, 1024], F32, kind="ExternalInput").ap()
    gffn = nc.dram_tensor("gffn", [128, 8], F32, kind="ExternalInput").ap()
    w_r = nc.dram_tensor("w_r", [1024, 36], F32, kind="ExternalInput").ap()
    w_gate = nc.dram_tensor("w_gate", [32, 1024, 512], F32, kind="ExternalInput").ap()
    w_up = nc.dram_tensor("w_up", [32, 1024, 512], F32, kind="ExternalInput").ap()
    w_down = nc.dram_tensor("w_down", [32, 512, 1024], F32, kind="ExternalInput").ap()
    hT_out = nc.dram_tensor("hT_out", [1024, NT], F32, kind="ExternalOutput").ap()
    P = Prog(nc)
    hT_v = hT_in.rearrange("(kc p) t -> p kc t", p=128)
    oT_v = oT_in.rearrange("(kc p) t -> p kc t", p=128)
    ho_v = hT_out.rearrange("(kc p) t -> p kc t", p=128)
    with ExitStack() as st:
        def SB(name, shape, dt, bs=None, dsem=False):
            return TT(P, st, name, shape, dt, bs=bs, dsem=dsem)

        ones_bf = SB("ones_bf", [128, 128], BF16)
        eps_c = SB("eps_c", [128, 1], F32)
        identf = SB("identf", [128, 128], F32)
        SEL = SB("SEL", [32, 32, 128], BF16)
        gf = SB("gf", [128, 8], F32, dsem=True)
        WR = SB("WR", [128, 8, 36], F32, dsem=True)
        HM = SB("HM", [128, 8, NT], F32, bs=512, dsem=True)
        U = SB("U", [128, 8, NT], BF16, bs=512)
        WBUF = [SB("WB%d" % i, [128, 12288], BF16, dsem=True) for i in range(2)]
        OTB = [SB("OTB%d" % i, [128, 8, 512], BF16, dsem=True) for i in range(2)]
        HID = [SB("HID%d" % i, [128, 4, 512], BF16) for i in range(2)]
        SIL = [SB("SIL%d" % i, [128, 512], BF16) for i in range(2)]
        S2 = [SB("S2%d" % i, [128, 512], BF16) for i in range(2)]
        CREP = [SB("CREP%d" % i, [128, 512], BF16) for i in range(2)]
        CT = SB("CT", [32, NT], BF16, bs=128)
        LNV = SB("LNV", [128, 512], F32)
        RSTD = SB("RSTD", [128, 512], F32)
        RLA = SB("RLA", [128, NT // 128, 36], F32)
        R16 = SB("R16", [128, 8, NT // 128], F32)
        OHA = SB("OHA", [128, NT // 128, 4], F32)
        EGA = SB("EGA", [128, NT // 128, 4], F32)
        SELA = SB("SELA", [128, NT // 128, 8], F32)
        TMP8 = SB("TMP8", [128, NT // 128, 8], F32)
        E1A = SB("E1A", [128, NT // 128, 8], F32)
        E2A = SB("E2A", [128, NT // 128, 8], F32)
        COMBA = SB("COMBA", [128, NT // 128, 32], F32)
        RL = SB("RL", [128, 36], F32)
        RT = SB("RT", [128, 16], F32)
        OH = SB("OH", [128, 4], F32)
        EG = SB("EG", [128, 4], F32)
        SEL8 = SB("SEL8", [128, 8], F32)
        M8 = SB("M8", [128, 8], F32)
        WA8 = SB("WA8", [128, 8], F32)
        WB8 = SB("WB8", [128, 8], F32)
        COMB = SB("COMB", [128, 32], F32)
        PSB = [TT(P, st, "ps%d" % i, [128, 512], F32, psum=True) for i in range(8)]
        Wo = WBUF[1]
        Wo_v = Wo[:, 0:8192].rearrange("p (a b) -> p a b", b=1024)
        UFt = WBUF[0]
        UF_v = UFt.h.bitcast(F32)[:, 0:4096].rearrange("p (a b) -> p a b", b=512)
        SQt = HID[0]
        SQ = SB("SQ", [128, 8, 512], BF16)

        memset(P, "pool", ones_bf[:], 1.0, ones_bf.r())
        memset(P, "pool", eps_c[:], EPS, eps_c.r())
        memset(P, "pool", identf[:], 1.0, identf.r())
        P.op("pool", lambda e: e.affine_select(out=identf[:], in_=identf[:], pattern=[[-1, 128]], compare_op=ALU.is_equal,
                                               fill=0.0, base=0, channel_multiplier=1), identf.r(), identf.r())
        memset(P, "pool", SEL[:], 1.0, SEL.r())
        P.op("pool", lambda e: e.affine_select(out=SEL[:], in_=SEL[:], pattern=[[-1, 32], [0, 128]], compare_op=ALU.is_equal,
                                               fill=0.0, base=0, channel_multiplier=1), SEL.r(), SEL.r())
        dma(P, "sp", gf[:], gffn, (), gf.r(), gf.dsem)
        dma(P, "sp", WR[:], w_r.rearrange("(kc p) n -> p kc n", p=128), (), WR.r(), WR.dsem)
        dma(P, "pool", Wo_v, w_out.rearrange("(kc p) n -> p kc n", p=128), (), Wo.r(), Wo.dsem)
        for h2 in range(2):
            dma(P, "sp", HM[:, :, h2 * 1024:(h2 + 1) * 1024], hT_v[:, :, h2 * 1024:(h2 + 1) * 1024], (), HM.r(), HM.dsem)

        for blk in range(NT // 512):
            cs = slice(blk * 512, (blk + 1) * 512)
            ot = OTB[blk % 2]
            dma(P, "sp", ot[:], oT_v[:, :, cs], (), ot.r(), ot.dsem)
            for dc in range(8):
                ps = PSB[dc % 2]
                for kc in range(8):
                    mm(P, ps[:, :], Wo_v[:, kc, dc * 128:(dc + 1) * 128], ot[:, kc, :], kc == 0, kc == 7, Wo.r() + ot.r(), ps.r())
                lo = dc * NT + blk * 512
                tt(P, "dve", HM[:, dc, cs], HM[:, dc, cs], ps[:, :], ALU.add, HM.r(lo, lo + 512) + ps.r(), HM.r(lo, lo + 512))
            hm_blk = []
            for dc in range(8):
                hm_blk += HM.r(dc * NT + blk * 512, dc * NT + blk * 512 + 512)
            act(P, SQ[:], HM[:, :, cs], AF.Square, hm_blk, SQ.r())
            ss = PSB[2]
            for kc in range(8):
                mm(P, ss[:, :], ones_bf[:, :], SQ[:, kc, :], kc == 0, kc == 7, SQ.r() + ones_bf.r(), ss.r())
            act(P, LNV[:], ss[:, :], AF.Ln, ss.r() + eps_c.r(), LNV.r(), scale=1.0 / 1024, bias=eps_c[:])
            act(P, RSTD[:], LNV[:], AF.Exp, LNV.r(), RSTD.r(), scale=-0.5)
            u_blk = []
            for dc in range(8):
                lo = dc * NT + blk * 512
                stt(P, UF_v[:, dc, :], HM[:, dc, cs], gf[:, dc:dc + 1], RSTD[:], ALU.mult, ALU.mult,
                    HM.r(lo, lo + 512) + gf.r() + RSTD.r(), UFt.r())
                u_blk += U.r(lo, lo + 512)
            cp(P, "pool", U[:, :, cs], UF_v, UFt.r(), u_blk)
            pr = PSB[3]
            for sub in range(4):
                scs = slice(sub * 128, (sub + 1) * 128)
                for dc in range(8):
                    mm(P, pr[:, sub * 36:(sub + 1) * 36], UF_v[:, dc, scs], WR[:, dc, :], dc == 0, dc == 7, UFt.r() + WR.r(), pr.r())
            cp(P, "dve", RLA[:, blk * 4:(blk + 1) * 4, :].rearrange("p a b -> p (a b)"), pr[:, 0:144], pr.r(), RLA.r())

        S_ = NT // 128
        def b3(ap2, n):
            return ap2.unsqueeze(2).broadcast_to([128, S_, n])
        GL = RLA[:, :, 0:4]
        P.op("dve", lambda e: e.tensor_reduce(out=R16[:, 0, :], in_=GL, axis=AX.X, op=ALU.max), RLA.r(), R16.r())
        tt(P, "dve", OHA[:], GL, b3(R16[:, 0, :], 4), ALU.is_equal, RLA.r() + R16.r(), OHA.r())
        tt(P, "dve", EGA[:], GL, b3(R16[:, 0, :], 4), ALU.subtract, RLA.r() + R16.r(), EGA.r())
        act(P, EGA[:], EGA[:], AF.Exp, EGA.r(), EGA.r())
        P.op("dve", lambda e: e.tensor_reduce(out=R16[:, 1, :], in_=EGA[:], axis=AX.X, op=ALU.add), EGA.r(), R16.r())
        recip(P, R16[:, 2, :], R16[:, 1, :], R16.r(), R16.r())
        tt(P, "dve", SELA[:], RLA[:, :, 4:12], b3(OHA[:, :, 0], 8), ALU.mult, RLA.r() + OHA.r(), SELA.r())
        for g in range(1, 4):
            tt(P, "dve", TMP8[:], RLA[:, :, 4 + 8 * g:12 + 8 * g], b3(OHA[:, :, g], 8), ALU.mult, RLA.r() + OHA.r(), TMP8.r())
            tt(P, "dve", SELA[:], SELA[:], TMP8[:], ALU.add, SELA.r() + TMP8.r(), SELA.r())
        P.op("dve", lambda e: e.tensor_reduce(out=R16[:, 3, :], in_=SELA[:], axis=AX.X, op=ALU.max), SELA.r(), R16.r())
        tt(P, "dve", E1A[:], SELA[:], b3(R16[:, 3, :], 8), ALU.is_equal, SELA.r() + R16.r(), E1A.r())
        ts(P, "dve", TMP8[:], E1A[:], -1.0e30, ALU.mult, E1A.r(), TMP8.r())
        tt(P, "dve", TMP8[:], SELA[:], TMP8[:], ALU.add, SELA.r() + TMP8.r(), TMP8.r())
        P.op("dve", lambda e: e.tensor_reduce(out=R16[:, 4, :], in_=TMP8[:], axis=AX.X, op=ALU.max), TMP8.r(), R16.r())
        tt(P, "dve", E2A[:], TMP8[:], b3(R16[:, 4, :], 8), ALU.is_equal, TMP8.r() + R16.r(), E2A.r())
        tt(P, "dve", R16[:, 5, :], R16[:, 4, :], R16[:, 3, :], ALU.subtract, R16.r(), R16.r())
        act(P, R16[:, 5, :], R16[:, 5, :], AF.Exp, R16.r(), R16.r())
        ts(P, "dve", R16[:, 5, :], R16[:, 5, :], 1.0, ALU.add, R16.r(), R16.r())
        recip(P, R16[:, 6, :], R16[:, 5, :], R16.r(), R16.r())
        ts(P, "dve", R16[:, 7, :], R16[:, 6, :], -1.0, ALU.mult, R16.r(), R16.r(), 1.0, ALU.add)
        tt(P, "dve", R16[:, 6, :], R16[:, 6, :], R16[:, 2, :], ALU.mult, R16.r(), R16.r())
        tt(P, "dve", R16[:, 7, :], R16[:, 7, :], R16[:, 2, :], ALU.mult, R16.r(), R16.r())
        tt(P, "dve", E1A[:], E1A[:], b3(R16[:, 6, :], 8), ALU.mult, E1A.r() + R16.r(), E1A.r())
        tt(P, "dve", E2A[:], E2A[:], b3(R16[:, 7, :], 8), ALU.mult, E2A.r() + R16.r(), E2A.r())
        tt(P, "dve", E1A[:], E1A[:], E2A[:], ALU.add, E1A.r() + E2A.r(), E1A.r())
        tt(P, "dve", COMBA[:].rearrange("p s (g e) -> p s g e", e=8),
           E1A[:].unsqueeze(2).broadcast_to([128, S_, 4, 8]), OHA[:].unsqueeze(3).broadcast_to([128, S_, 4, 8]), ALU.mult,
           E1A.r() + OHA.r(), COMBA.r())
        for q4 in range(S_ // 4):
            pt = PSB[4 + q4 % 2]
            for sub in range(4):
                sidx = q4 * 4 + sub
                P.op("pe", lambda e, pt=pt, sub=sub, sidx=sidx: e.transpose(out=pt[0:32, sub * 128:(sub + 1) * 128], in_=COMBA[:, sidx, :], identity=identf[:, :]),
                     COMBA.r() + identf.r(), pt.r())
            cp(P, "dve", CT[0:32, q4 * 512:(q4 + 1) * 512], pt[0:32, :], pt.r(), CT.r(q4 * 512, q4 * 512 + 512))

        units = [(e, blk) for e in range(32) for blk in range(NT // 512)]
        wviews = []
        for i in range(2):
            wb = WBUF[i]
            wviews.append((wb[:, 0:4096].rearrange("p (a b) -> p a b", b=512),
                           wb[:, 4096:8192].rearrange("p (a b) -> p a b", b=512),
                           wb[:, 8192:12288].rearrange("p (a b) -> p a b", b=1024)))

        def load_w(e):
            wb = WBUF[e % 2]
            Wg, Wu, Wd = wviews[e % 2]
            dma(P, "pool", Wg, w_gate[e].rearrange("(kc p) n -> p kc n", p=128), (), wb.r(), wb.dsem)
            dma(P, "pool", Wu, w_up[e].rearrange("(kc p) n -> p kc n", p=128), (), wb.r(), wb.dsem)
            dma(P, "pool", Wd, w_down[e].rearrange("(fc p) n -> p fc n", p=128), (), wb.r(), wb.dsem)

        def GU(ui):
            e, blk = units[ui]
            cs = slice(blk * 512, (blk + 1) * 512)
            wb = WBUF[e % 2]
            Wg, Wu, Wd = wviews[e % 2]
            hid = HID[ui % 2]
            crep = CREP[ui % 2]
            pc = PSB[6]
            mm(P, pc[:, :], SEL[0:32, e, :], CT[0:32, cs], True, True, SEL.r() + CT.r(blk * 512, blk * 512 + 512), pc.r())
            cp(P, "dve", crep[:], pc[:, :], pc.r(), crep.r())
            u_blk = []
            for dc in range(8):
                u_blk += U.r(dc * NT + blk * 512, dc * NT + blk * 512 + 512)
            for fc in range(4):
                pg_ = PSB[(fc % 2) * 2]
                pu_ = PSB[(fc % 2) * 2 + 1]
                fs = slice(fc * 128, (fc + 1) * 128)
                for dc in range(8):
                    mm(P, pg_[:, :], Wg[:, dc, fs], U[:, dc, cs], dc == 0, dc == 7, wb.r() + u_blk, pg_.r())
                for dc in range(8):
                    mm(P, pu_[:, :], Wu[:, dc, fs], U[:, dc, cs], dc == 0, dc == 7, wb.r() + u_blk, pu_.r())
                sil = SIL[fc % 2]
                s2 = S2[fc % 2]
                act(P, sil[:], pg_[:, :], AF.Silu, pg_.r(), sil.r())
                tt(P, "pool", s2[:], sil[:], crep[:], ALU.mult, sil.r() + crep.r(), s2.r())
                tt(P, "dve", hid[:, fc, :], s2[:], pu_[:, :], ALU.mult, s2.r() + pu_.r(), hid.r())

        def DN(ui):
            e, blk = units[ui]
            cs = slice(blk * 512, (blk + 1) * 512)
            wb = WBUF[e % 2]
            Wg, Wu, Wd = wviews[e % 2]
            hid = HID[ui % 2]
            for dc in range(8):
                py = PSB[4 + dc % 2]
                for fc in range(4):
                    mm(P, py[:, :], Wd[:, fc, dc * 128:(dc + 1) * 128], hid[:, fc, :], fc == 0, fc == 3, wb.r() + hid.r(), py.r())
                lo = dc * NT + blk * 512
                tt(P, "dve", HM[:, dc, cs], HM[:, dc, cs], py[:, :], ALU.add, HM.r(lo, lo + 512) + py.r(), HM.r(lo, lo + 512))

        nE = int(os.environ.get("KB_NE", 32))
        units = [u_ for u_ in units if u_[0] < nE]
        load_w(0)
        for ui in range(len(units) + 1):
            if ui < len(units):
                GU(ui)
            if ui >= 1:
                DN(ui - 1)
            if ui < len(units):
                e, blk = units[ui]
                if blk == 0 and e + 1 < nE:
                    load_w(e + 1)
        for h2 in range(2):
            rr = []
            for dc in range(8):
                rr += HM.r(dc * NT + h2 * 1024, dc * NT + h2 * 1024 + 1024)
            dma(P, "sp", ho_v[:, :, h2 * 1024:(h2 + 1) * 1024], HM[:, :, h2 * 1024:(h2 + 1) * 1024], rr, (), HM.dsem)
        P.replay()
    return nc


import math

NCOL_C = 384
TWO_PI = 2.0 * math.pi
LAM_INIT = 0.8 - 0.6 * math.exp(-0.3 * 1)


def build_C(nbatch=2, debug=False):
    nc = bass.Bass("TRN2", target_bir_lowering=False)
    xT = nc.dram_tensor("xT", [1024, TOK], F32, kind="ExternalInput").ap()
    wC = nc.dram_tensor("wC", [1024, NCOL_C], F32, kind="ExternalInput").ap()
    gmix = nc.dram_tensor("gmix", [128, 8], F32, kind="ExternalInput").ap()
    pC = nc.dram_tensor("pC", [128, 8], F32, kind="ExternalInput").ap()
    posd = nc.dram_tensor("pos", [2, T], I32, kind="ExternalInput").ap()
    oT = nc.dram_tensor("oT", [128, TOK], BF16, kind="ExternalOutput").ap()
    P = Prog(nc)
    xT_v = xT.rearrange("(kc p) t -> p kc t", p=128)
    with ExitStack() as st:
        def SB(name, shape, dt, bs=None, dsem=False):
            return TT(P, st, name, shape, dt, bs=bs, dsem=dsem)

        ones_bf = SB("ones_bf", [128, 128], BF16)
        BD = SB("BD", [128, 128], BF16)
        ones_f = SB("ones_f", [128, 128], F32)
        eps_c = SB("eps_c", [128, 1], F32)
        pi_c = SB("pi_c", [128, 2], F32)
        pic2 = SB("pic2", [128, 2], F32)
        ident = SB("ident", [128, 128], BF16)
        PM = SB("PM", [128, 128], BF16)
        PM2 = SB("PM2", [128, 128], BF16)
        par = SB("par", [128, 8], F32, dsem=True)
        gm = SB("gm", [128, 8], F32, dsem=True)
        cst = SB("cst", [128, 8], F32)
        invi = SB("invi", [1, 128], I32)
        invf = SB("invf", [1, 128], F32)
        Wb = SB("Wb", [128, 8, NCOL_C], BF16, dsem=True)

        memset(P, "pool", ones_bf[:], 1.0, ones_bf.r())
        memset(P, "pool", ones_f[:], 1.0, ones_f.r())
        memset(P, "pool", eps_c[:], EPS, eps_c.r())
        memset(P, "pool", BD[:], 1.0, BD.r())
        memset(P, "pool", BD[0:64, 64:128], 0.0, BD.r())
        memset(P, "pool", BD[64:128, 0:64], 0.0, BD.r())
        memset(P, "pool", pi_c[:, 0:1], math.pi, pi_c.r())
        memset(P, "pool", pi_c[:, 1:2], -TWO_PI, pi_c.r())
        memset(P, "pool", pi_c[0:32, 0:1], -math.pi, pi_c.r())
        memset(P, "pool", pi_c[0:32, 1:2], TWO_PI, pi_c.r())
        memset(P, "pool", pi_c[64:96, 0:1], -math.pi, pi_c.r())
        memset(P, "pool", pi_c[64:96, 1:2], TWO_PI, pi_c.r())
        memset(P, "pool", pic2[:, 0:1], math.pi, pic2.r())
        memset(P, "pool", PM[:], 1.0, PM.r())
        P.op("pool", lambda e: e.affine_select(out=PM[:], in_=PM[:], pattern=[[1, 128]], compare_op=ALU.is_equal,
                                               fill=0.0, base=-32, channel_multiplier=-1), PM.r(), PM.r())
        memset(P, "pool", PM[:, 0:32], 0.0, PM.r())
        memset(P, "pool", PM[:, 64:96], 0.0, PM.r())
        memset(P, "pool", PM2[:], 1.0, PM2.r())
        P.op("pool", lambda e: e.affine_select(out=PM2[:], in_=PM2[:], pattern=[[1, 128]], compare_op=ALU.is_equal,
                                               fill=0.0, base=32, channel_multiplier=-1), PM2.r(), PM2.r())
        memset(P, "pool", PM2[:, 32:64], 0.0, PM2.r())
        memset(P, "pool", PM2[:, 96:128], 0.0, PM2.r())
        tt(P, "pool", PM[:], PM[:], PM2[:], ALU.add, PM.r() + PM2.r(), PM.r())
        P.op("pool", lambda e: e.iota(invi[:].rearrange("p (a b) -> p a b", b=32), pattern=[[0, 4], [1, 32]], base=0, channel_multiplier=0),
             (), invi.r())
        cp(P, "dve", invf[:], invi[:], invi.r(), invf.r())
        act(P, invf[:], invf[:], AF.Exp, invf.r(), invf.r(), scale=-math.log(10000.0) / 32.0)

        dma(P, "sp", par[:], pC, (), par.r(), par.dsem)
        dma(P, "sp", gm[:], gmix, (), gm.r(), gm.dsem)
        dma(P, "pool", Wb[:], wC.rearrange("(kc p) n -> p kc n", p=128), (), Wb.r(), Wb.dsem)
        for kc in range(8):
            ts(P, "dve", Wb[:, kc, :], Wb[:, kc, :], gm[:, kc:kc + 1], ALU.mult, Wb.r() + gm.r(), Wb.r())
        ts(P, "dve", cst[:, 0:1], par[:, 0:1], 0.125, ALU.mult, par.r(), cst.r())
        ts(P, "dve", cst[:, 3:4], par[:, 6:7], 1.0 - LAM_INIT, ALU.mult, par.r(), cst.r())
        PSB = [TT(P, st, "ps%d" % i, [128, 512], F32, psum=True) for i in range(8)]
        tt(P, "dve", cst[:, 4:5], par[:, 2:3], par[:, 3:4], ALU.mult, par.r(), cst.r())
        tt(P, "dve", cst[:, 5:6], par[:, 4:5], par[:, 5:6], ALU.mult, par.r(), cst.r())
        mm(P, PSB[0][:, 0:2], ones_f[:, :], cst[:, 4:6], True, True, ones_f.r() + cst.r(), PSB[0].r())
        act(P, cst[:, 6:8], PSB[0][:, 0:2], AF.Exp, PSB[0].r(), cst.r())
        tt(P, "dve", cst[:, 2:3], cst[:, 7:8], cst[:, 6:7], ALU.subtract, cst.r(), cst.r())
        ts(P, "dve", cst[:, 2:3], cst[:, 2:3], -LAM_INIT, ALU.add, cst.r(), cst.r())
        gq_ap, gk_ap, nlam_ap, gs_ap = cst[:, 0:1], par[:, 1:2], cst[:, 2:3], cst[:, 3:4]

        QT = SB("QT", [128, T], BF16, bs=512)
        KT = SB("KT", [128, T], BF16, bs=128)
        V = SB("V", [128, T // 128, 128], BF16, bs=128)
        SINT = SB("SINT", [128, T], BF16, bs=512)
        COST = SB("COST", [128, T], BF16, bs=512)
        XB = [SB("XB%d" % i, [128, 8, 512], BF16, dsem=True) for i in range(2)]
        SQ = SB("SQ", [128, 8, 512], BF16)
        LNV = SB("LNV", [128, 512], F32)
        RSTD = [SB("RSTD%d" % i, [128, 512], F32) for i in range(2)]
        RCS = SB("RCS", [128, 4], F32)
        POSI = SB("POSI", [1, 512], I32, dsem=True)
        POSF = SB("POSF", [1, 512], F32)
        RR = [SB("RR%d" % i, [128, 512], F32) for i in range(2)]
        RI = SB("RI", [128, 512], I32)
        RF = SB("RF", [128, 512], F32)
        RMK = SB("RMK", [128, 512], F32)
        TA = SB("TA", [128, 512], BF16)
        TB_ = SB("TB", [128, 512], BF16)
        PTB = [[SB("PT%d_%d" % (m, i), [128, 512], BF16) for i in range(3)] for m in range(2)]
        FT = [SB("FT%d" % i, [128, 512], F32) for i in range(4)]
        OUTS = [SB("OUTS%d" % i, [128, 512], BF16, dsem=True) for i in range(2)]

        groups = [(0, QT), (128, KT)]
        blk = 0
        for b in range(nbatch):
            for n in range(T // 512):
                tok0 = b * T + n * 512
                cols = (n * 512, (n + 1) * 512)
                xb = XB[blk % 2]
                rs = RSTD[blk % 2]
                dma(P, "pool", xb[:], xT_v[:, :, tok0:tok0 + 512], (), xb.r(), xb.dsem)
                act(P, SQ[:], xb[:], AF.Square, xb.r(), SQ.r())
                ss = PSB[0]
                for kc in range(8):
                    mm(P, ss[:, :], ones_bf[:, :], SQ[:, kc, :], kc == 0, kc == 7, SQ.r() + ones_bf.r(), ss.r())
                act(P, LNV[:], ss[:, :], AF.Ln, ss.r() + eps_c.r(), LNV.r(), scale=1.0 / 1024, bias=eps_c[:])
                act(P, rs[:], LNV[:], AF.Exp, LNV.r(), rs.r(), scale=-0.5)
                for gi, (c0, dest) in enumerate(groups):
                    ps = PSB[1 + gi]
                    for kc in range(8):
                        mm(P, ps[:, :], Wb[:, kc, c0:c0 + 128], xb[:, kc, :], kc == 0, kc == 7, Wb.r() + xb.r(), ps.r())
                    tt(P, "dve", dest[:, cols[0]:cols[1]], ps[:, :], rs[:, :], ALU.mult, ps.r() + rs.r(), dest.r(*cols))
                ptm = PSB[3]
                prc = PSB[4]
                for sub in range(4):
                    sc = slice(sub * 128, (sub + 1) * 128)
                    for kc in range(8):
                        mm(P, ptm[:, sc], xb[:, kc, sc], Wb[:, kc, 256:384], kc == 0, kc == 7, Wb.r() + xb.r(), ptm.r())
                    mm(P, prc[:, sub:sub + 1], rs[0:1, sc], ones_f[0:1, 0:1], True, True, rs.r() + ones_f.r(), prc.r())
                cp(P, "dve", RCS[:, 0:4], prc[:, 0:4], prc.r(), RCS.r())
                for sub in range(4):
                    ti = n * 4 + sub
                    sc = slice(sub * 128, (sub + 1) * 128)
                    ts(P, "dve", V[:, ti, :], ptm[:, sc], RCS[:, sub:sub + 1], ALU.mult, ptm.r() + RCS.r(), V.r(ti * 128, ti * 128 + 128))
                blk += 1
            for n in range(T // 512):
                cols = (n * 512, (n + 1) * 512)
                cs = slice(*cols)
                dma(P, "sp", POSI[:], posd[b:b + 1, cs], (), POSI.r(), POSI.dsem)
                cp(P, "dve", POSF[:], POSI[:], POSI.r(), POSF.r())
                pa = PSB[5]
                mm(P, pa[:, :], invf[0:1, :], POSF[0:1, :], True, True, invf.r() + POSF.r(), pa.r())
                for which, dest in ((0, SINT), (1, COST)):
                    rr = RR[which]
                    if which == 0:
                        ts(P, "dve", rr[:], pa[:, :], 1.0 / TWO_PI, ALU.mult, pa.r(), rr.r())
                    else:
                        ts(P, "dve", rr[:], pa[:, :], 1.0 / TWO_PI, ALU.mult, pa.r(), rr.r(), 0.25, ALU.add)
                    cp(P, "dve", RI[:], rr[:], rr.r(), RI.r())
                    cp(P, "act", RF[:], RI[:], RI.r(), RF.r())
                    tt(P, "dve", rr[:], rr[:], RF[:], ALU.subtract, rr.r() + RF.r(), rr.r())
                    ts(P, "dve", RMK[:], rr[:], 0.0, ALU.is_lt, rr.r(), RMK.r())
                    tt(P, "dve", rr[:], rr[:], RMK[:], ALU.add, rr.r() + RMK.r(), rr.r())
                    if which == 0:
                        P.op("act", lambda e, rr=rr, dest=dest, cs=cs: e.activation(out=dest[:, cs], in_=rr[:], func=AF.Sin, scale=pi_c[:, 1:2], bias=pi_c[:, 0:1]),
                             rr.r() + pi_c.r(), dest.r(*cols))
                    else:
                        P.op("act", lambda e, rr=rr, dest=dest, cs=cs: e.activation(out=dest[:, cs], in_=rr[:], func=AF.Sin, scale=-TWO_PI, bias=pic2[:, 0:1]),
                             rr.r() + pic2.r(), dest.r(*cols))
            for n in range(T // 512):
                cols = (n * 512, (n + 1) * 512)
                cs = slice(*cols)
                for (tl, gap, bank) in ((QT, gq_ap, 1), (KT, gk_ap, 2)):
                    ps = PSB[bank]
                    act(P, SQ[:, 0, :], tl[:, cs], AF.Square, tl.r(*cols), SQ.r())
                    mm(P, ps[:, :], BD[:, :], SQ[:, 0, :], True, True, SQ.r() + BD.r(), ps.r())
                    act(P, LNV[:], ps[:, :], AF.Ln, ps.r() + eps_c.r(), LNV.r(), scale=1.0 / 64, bias=eps_c[:])
                    act(P, LNV[:], LNV[:], AF.Exp, LNV.r(), LNV.r(), scale=-0.5)
                    stt(P, tl[:, cs], tl[:, cs], gap, LNV[:], ALU.mult, ALU.mult, tl.r(*cols) + LNV.r() + par.r() + cst.r(), tl.r(*cols))
                    pw = PSB[bank + 2]
                    mm(P, pw[:, :], PM[:, :], tl[:, cs], True, True, PM.r() + tl.r(*cols), pw.r())
                    tt(P, "dve", TA[:], tl[:, cs], COST[:, cs], ALU.mult, tl.r(*cols) + COST.r(*cols), TA.r())
                    tt(P, "dve", TB_[:], pw[:, :], SINT[:, cs], ALU.mult, pw.r() + SINT.r(*cols), TB_.r())
                    tt(P, "dve", tl[:, cs], TA[:], TB_[:], ALU.add, TA.r() + TB_.r(), tl.r(*cols))
            if debug:
                continue
            k = 0
            for j in range(T // 512):
                accs = (PSB[4], PSB[5])
                dens = (PSB[6], PSB[7])
                nt = 4 * j + 4
                items = []
                for i in range(nt):
                    m_ = i - 4 * j
                    c0 = 128 * m_ if m_ >= 0 else 0
                    items.append((i, m_, c0))

                def qk(it, k):
                    i, m_, c0 = it
                    for mp in range(2):
                        sc = PSB[(k % 2) * 2 + mp]
                        rows = slice(mp * 64, (mp + 1) * 64)
                        mm(P, sc[:, c0:512], KT[rows, i * 128:(i + 1) * 128], QT[rows, j * 512 + c0:(j + 1) * 512], True, True,
                           KT.r(i * 128, i * 128 + 128) + QT.r(j * 512, j * 512 + 512), sc.r())

                def rest(it, k):
                    i, m_, c0 = it
                    for mp in range(2):
                        sc = PSB[(k % 2) * 2 + mp]
                        pt = PTB[mp][k % 3]
                        act(P, pt[:, c0:512], sc[:, c0:512], AF.Exp, sc.r(), pt.r())
                        if m_ >= 0:
                            memset(P, "pool", pt[64:128, c0:c0 + 64], 0.0, pt.r())
                        mm(P, accs[mp][:, c0:512], V[:, i, :], pt[:, c0:512], i == 0, i == nt - 1, V.r(i * 128, i * 128 + 128) + pt.r(), accs[mp].r())
                        mm(P, dens[mp][:, c0:512], ones_bf[:, :], pt[:, c0:512], i == 0, i == nt - 1, ones_bf.r() + pt.r(), dens[mp].r())
                LOOK = 1
                for idx in range(nt + LOOK):
                    if idx < nt:
                        qk(items[idx], k + idx)
                    if idx >= LOOK:
                        rest(items[idx - LOOK], k + idx - LOOK)
                k += nt
                recip(P, FT[0][:], dens[0][:, :], dens[0].r(), FT[0].r())
                tt(P, "dve", FT[1][:], accs[0][:, :], FT[0][:], ALU.mult, accs[0].r() + FT[0].r(), FT[1].r())
                recip(P, FT[0][:], dens[1][:, :], dens[1].r(), FT[0].r())
                tt(P, "dve", FT[2][:], accs[1][:, :], FT[0][:], ALU.mult, accs[1].r() + FT[0].r(), FT[2].r())
                stt(P, FT[3][:], FT[2][:], nlam_ap, FT[1][:], ALU.mult, ALU.add, FT[2].r() + FT[1].r() + cst.r(), FT[3].r())
                act(P, SQ[:, 0, :], FT[3][:], AF.Square, FT[3].r(), SQ.r())
                pn = PSB[0]
                mm(P, pn[:, :], ones_bf[:, :], SQ[:, 0, :], True, True, SQ.r() + ones_bf.r(), pn.r())
                act(P, LNV[:], pn[:, :], AF.Ln, pn.r() + eps_c.r(), LNV.r(), scale=1.0 / 128, bias=eps_c[:])
                act(P, LNV[:], LNV[:], AF.Exp, LNV.r(), LNV.r(), scale=-0.5)
                outs = OUTS[j % 2]
                stt(P, outs[:], FT[3][:], gs_ap, LNV[:], ALU.mult, ALU.mult, FT[3].r() + LNV.r() + cst.r(), outs.r())
                dma(P, "sp", oT[:, b * T + j * 512: b * T + (j + 1) * 512], outs[:], outs.r(), (), outs.dsem)
        if debug:
            dsd = P.new_dsem("dbg")
            for nm, tl in (("dQ", QT), ("dK", KT), ("dS", SINT), ("dC", COST)):
                dd = nc.dram_tensor(nm, [128, T], BF16, kind="ExternalOutput").ap()
                dma(P, "sp", dd, tl[:], tl.r(), (), dsd)
            dd = nc.dram_tensor("dV", [128, T], BF16, kind="ExternalOutput").ap()
            dma(P, "sp", dd, V[:].rearrange("p a b -> p (a b)"), V.r(), (), dsd)
        P.replay()
    return nc


def _prep_A(inp, xT):
    w = inp["even_w_in"][0]
    gm = np.ascontiguousarray(inp["norm_mix"][0].reshape(8, 128).T)
    maps = []
    for c in range(8):
        sl = lambda base: w[:, base + c * 64: base + (c + 1) * 64]
        wA = np.concatenate([sl(0), sl(512), sl(1536), sl(2048), sl(2560), w[:, 3584 + c:3585 + c], sl(1024), sl(3072)], axis=1)
        pA = np.zeros((64, 8), np.float32)
        pA[:, 0] = inp["hgrn_lb_logits"][0, c * 64:(c + 1) * 64]
        pA[:, 1] = inp["hgrn_lb_logits"][1, c * 64:(c + 1) * 64]
        pA[:, 2] = inp["hgrn_out_norm"][0]
        pA[:, 3] = inp["fox_q_norm"][0]
        pA[:, 4] = inp["fox_k_norm"][0]
        fb = np.empty((128, 1), np.float32)
        fb[:, 0] = inp["fox_f_bias"][0, c]
        maps.append({"xT": xT, "wA": np.ascontiguousarray(wA), "gmix": gm, "pA": pA, "fbias": fb})
    return maps


def _prep_B(inp, layer, hT_full, oT_full, w_out):
    wr = np.concatenate([inp["moe_router_group"][layer]] + [inp["moe_router_expert"][layer, g] for g in range(4)], axis=1)
    wg = np.ascontiguousarray(inp["moe_w_gate"][layer].reshape(32, 1024, 512))
    wu = np.ascontiguousarray(inp["moe_w_up"][layer].reshape(32, 1024, 512))
    wd = np.ascontiguousarray(inp["moe_w_down"][layer].reshape(32, 512, 1024))
    gf = np.ascontiguousarray(inp["norm_ffn"][layer].reshape(8, 128).T)
    wr = np.ascontiguousarray(wr)
    w_out = np.ascontiguousarray(w_out)
    maps = []
    for c in range(8):
        cs = slice(c * NT, (c + 1) * NT)
        maps.append({"hT_in": np.ascontiguousarray(hT_full[:, cs]), "oT_in": np.ascontiguousarray(oT_full[:, cs]),
                     "w_out": w_out, "gffn": gf, "w_r": wr, "w_gate": wg, "w_up": wu, "w_down": wd})
    return maps


def _prep_C(inp, hT_full):
    w = inp["odd_w_in"][0]
    gm = np.ascontiguousarray(inp["norm_mix"][1].reshape(8, 128).T)
    pos = np.ascontiguousarray(inp["positions"].astype(np.int32))
    maps = []
    for c in range(8):
        wC = np.concatenate([w[:, c * 128:(c + 1) * 128], w[:, 1024 + c * 128:1024 + (c + 1) * 128],
                             w[:, 2048 + c * 128:2048 + (c + 1) * 128]], axis=1)
        pC = np.zeros((128, 8), np.float32)
        pC[0:64, 0] = inp["diff_q_norm"][0]; pC[64:128, 0] = inp["diff_q_norm"][0]
        pC[0:64, 1] = inp["diff_k_norm"][0]; pC[64:128, 1] = inp["diff_k_norm"][0]
        pC[0:64, 2] = inp["diff_lambda_q1"][0]; pC[0:64, 3] = inp["diff_lambda_k1"][0]
        pC[0:64, 4] = inp["diff_lambda_q2"][0]; pC[0:64, 5] = inp["diff_lambda_k2"][0]
        pC[:, 6] = inp["diff_subln"][0]
        maps.append({"xT": hT_full, "wC": np.ascontiguousarray(wC), "gmix": gm, "pC": pC, "pos": pos})
    return maps


def _run(nc, maps):
    res = run_bass_kernel_spmd(nc, maps, core_ids=list(range(8)))
    return res.results


def kernel(**inputs):
    inp = {k: np.asarray(v) for k, v in inputs.items()}
    x = inp["x"].astype(np.float32, copy=False).reshape(-1, 1024)
    xT = np.ascontiguousarray(x.T)
    rA = _run(build_A(), _prep_A(inp, xT))
    oT0 = np.empty((1024, TOK), ml_dtypes.bfloat16)
    for c in range(8):
        o = np.asarray(rA[c]["oT"])
        oT0[c * 64:(c + 1) * 64] = o[0:64]
        oT0[512 + c * 64:512 + (c + 1) * 64] = o[64:128]
    rB = _run(build_B(), _prep_B(inp, 0, xT, oT0, inp["even_w_out"][0]))
    h1T = np.ascontiguousarray(np.concatenate([np.asarray(r["hT_out"]) for r in rB], axis=1))
    rC = _run(build_C(), _prep_C(inp, h1T))
    oT1 = np.ascontiguousarray(np.concatenate([np.asarray(r["oT"]) for r in rC], axis=0))
    rD = _run(build_B(), _prep_B(inp, 1, h1T, oT1, inp["odd_w_out"][0]))
    outT = np.concatenate([np.asarray(r["hT_out"]) for r in rD], axis=1)
    return np.ascontiguousarray(outT.T).reshape(2, 8192, 1024).astype(np.float32, copy=False)
```

```python
import ml_dtypes
import numpy as np
from contextlib import ExitStack
import concourse.bass as bass
import concourse.mybir as mybir
from concourse.bass_utils import run_bass_kernel_spmd

F32 = mybir.dt.float32
BF16 = mybir.dt.bfloat16
I32 = mybir.dt.int32
AF = mybir.ActivationFunctionType
ALU = mybir.AluOpType
AX = mybir.AxisListType

ENGS = ("pe", "act", "dve", "pool", "sp")


class Res:
    __slots__ = ("name", "last_w", "readers", "dsem")

    def __init__(self, name):
        self.name = name
        self.last_w = None
        self.readers = []
        self.dsem = None


class Op:
    __slots__ = ("eng", "fn", "deps", "is_dma", "sem", "semval", "signal", "count", "dma_waits", "idx")

    def __init__(self, eng, fn):
        self.eng = eng
        self.fn = fn
        self.deps = []
        self.is_dma = False
        self.sem = None
        self.semval = 0
        self.signal = False
        self.count = 0
        self.dma_waits = []
        self.idx = 0


class Prog:
    def __init__(self, nc):
        self.nc = nc
        self.ops = {e: [] for e in ENGS}
        self.all_res_reset = True
        self.esem = {e: nc.alloc_semaphore("es_" + e) for e in ENGS}
        self.dsems = []
        self.dma_cum = {}
        self.nops = 0
        self.ecount = {e: 0 for e in ENGS}
        self.waited = {e: {} for e in ENGS}

    def res(self, name, n=1):
        return [Res("%s.%d" % (name, i)) for i in range(n)]

    def new_dsem(self, name):
        s = self.nc.alloc_semaphore("ds_" + name)
        self.dma_cum[s] = 0
        return s

    def op(self, eng, fn, reads=(), writes=(), dma_sem=None):
        o = Op(eng, fn)
        o.idx = self.nops
        self.nops += 1
        deps = {}
        dma_waits = {}

        def add_dep(p):
            if p is None:
                return
            if p.is_dma:
                dma_waits[p.sem] = self.dma_cum[p.sem]
            else:
                deps[id(p)] = p

        for r in reads:
            add_dep(r.last_w)
        for r in writes:
            add_dep(r.last_w)
            for q in r.readers:
                add_dep(q)
        raw = set()
        for r in reads:
            if r.last_w is not None and not r.last_w.is_dma:
                raw.add(id(r.last_w))
        for k, p in list(deps.items()):
            if p.eng == eng and eng == "pe" and dma_sem is None:
                del deps[k]
        o.deps = list(deps.values())
        for p in o.deps:
            p.signal = True
        o.dma_waits = list(dma_waits.items())
        if dma_sem is not None:
            o.is_dma = True
            o.sem = dma_sem
            self.dma_cum[dma_sem] += 16
            o.semval = self.dma_cum[dma_sem]
        for r in reads:
            r.readers.append(o)
        for r in writes:
            r.last_w = o
            r.readers = []
        self.ops[eng].append(o)
        return o

    def finish_wait(self, eng="sp"):
        waits = [(s, v) for s, v in self.dma_cum.items() if v > 0]
        o = Op(eng, None)
        o.dma_waits = waits
        self.ops[eng].append(o)

    def replay(self):
        nc = self.nc
        for e in ENGS:
            last = None
            for o in self.ops[e]:
                if o.fn is not None and not o.is_dma:
                    last = o
            if last is not None:
                last.signal = True
            c = self.ecount[e]
            for o in self.ops[e]:
                if o.signal and not o.is_dma:
                    c += 1
                    o.count = c
            self.ecount[e] = c
        final = {self.esem[e]: self.ecount[e] for e in ENGS if self.ecount[e] > 0}
        for s_, v_ in self.dma_cum.items():
            if v_ > 0:
                final[s_] = v_
        handles = {"pe": "tensor", "act": "scalar", "dve": "vector", "pool": "gpsimd", "sp": "sync"}
        with nc.Block() as block:
            for e in ENGS:
                ops = self.ops[e]
                esem = self.esem
                my = esem[e]

                def body(engh, ops=ops, e=e, my=my):
                    waited = self.waited[e]
                    for o in ops:
                        need = {}
                        for p in o.deps:
                            s = esem[p.eng]
                            if need.get(s, 0) < p.count:
                                need[s] = p.count
                        for s, v in o.dma_waits:
                            if need.get(s, 0) < v:
                                need[s] = v
                        for s, v in need.items():
                            if waited.get(s, 0) < v:
                                engh.wait_ge(s, v)
                                waited[s] = v
                        if o.fn is None:
                            continue
                        ins = o.fn(engh)
                        if o.is_dma:
                            ins.then_inc(o.sem, 16)
                        elif o.signal:
                            ins.then_inc(my, 1)
                    for s, v in final.items():
                        if s is my:
                            continue
                        if waited.get(s, 0) < v:
                            engh.wait_ge(s, v)
                            waited[s] = v

                getattr(block, handles[e])(body)
        self.ops = {e: [] for e in ENGS}
        self.all_res_reset = True


from math import prod
import os
HG = int(os.environ.get('HG_STAGE', '9'))


class TT:
    def __init__(self, P, st, name, shape, dt, bs=None, psum=False, dsem=False):
        nc = P.nc
        self.h = st.enter_context(nc.psum_tensor(name, shape, dt) if psum else nc.sbuf_tensor(name, shape, dt))
        self.F = prod(shape[1:])
        self.bs = bs or self.F
        self.res = P.res(name, (self.F + self.bs - 1) // self.bs)
        self.dsem = P.new_dsem(name) if dsem else None

    def r(self, lo=0, hi=None):
        hi = self.F if hi is None else hi
        return self.res[lo // self.bs:(hi - 1) // self.bs + 1]

    def __getitem__(self, k):
        return self.h[k]


def mm(P, out, lhsT, rhs, start, stop, rd, wr):
    return P.op("pe", lambda e: e.matmul(out, lhsT=lhsT, rhs=rhs, start=start, stop=stop), rd, wr)


def act(P, out, in_, func, rd, wr, scale=1.0, bias=None):
    if bias is None:
        return P.op("act", lambda e: e.activation(out=out, in_=in_, func=func, scale=scale), rd, wr)
    return P.op("act", lambda e: e.activation(out=out, in_=in_, func=func, scale=scale, bias=bias), rd, wr)


def tt(P, eng, out, in0, in1, op, rd, wr):
    return P.op(eng, lambda e: e.tensor_tensor(out=out, in0=in0, in1=in1, op=op), rd, wr)


def ts(P, eng, out, in0, s1, op0, rd, wr, s2=None, op1=None):
    if op1 is None:
        return P.op(eng, lambda e: e.tensor_scalar(out=out, in0=in0, scalar1=s1, scalar2=None, op0=op0), rd, wr)
    return P.op(eng, lambda e: e.tensor_scalar(out=out, in0=in0, scalar1=s1, scalar2=s2, op0=op0, op1=op1), rd, wr)


def stt(P, out, in0, scalar, in1, op0, op1, rd, wr):
    return P.op("dve", lambda e: e.scalar_tensor_tensor(out=out, in0=in0, scalar=scalar, in1=in1, op0=op0, op1=op1), rd, wr)


def cp(P, eng, out, in_, rd, wr):
    if eng == "act":
        return P.op("act", lambda e: e.copy(out=out, in_=in_), rd, wr)
    return P.op(eng, lambda e: e.tensor_copy(out=out, in_=in_), rd, wr)


def recip(P, out, in_, rd, wr):
    return P.op("dve", lambda e: e.reciprocal(out=out, in_=in_), rd, wr)


def dma(P, q, out, in_, rd, wr, sem):
    return P.op(q, lambda e: e.dma_start(out=out, in_=in_), rd, wr, dma_sem=sem)


def memset(P, eng, ap, val, wr):
    return P.op(eng, lambda e: e.memset(ap, val), (), wr)


T = int(os.environ.get('KT', 8192))
TOK = 2 * T
NCOL_A = 449
EPS = 1e-6


def build_A(nbatch=2, do_hgrn=True, do_fox=True, debug=False):
    nc = bass.Bass("TRN2", target_bir_lowering=False)
    xT = nc.dram_tensor("xT", [1024, TOK], F32, kind="ExternalInput").ap()
    wA = nc.dram_tensor("wA", [1024, NCOL_A], F32, kind="ExternalInput").ap()
    gmix = nc.dram_tensor("gmix", [128, 8], F32, kind="ExternalInput").ap()
    pA = nc.dram_tensor("pA", [64, 8], F32, kind="ExternalInput").ap()
    fbias = nc.dram_tensor("fbias", [128, 1], F32, kind="ExternalInput").ap()
    oT = nc.dram_tensor("oT", [128, TOK], BF16, kind="ExternalOutput").ap()
    P = Prog(nc)
    xT_v = xT.rearrange("(kc p) t -> p kc t", p=128)
    with ExitStack() as st:
        def SB(name, shape, dt, bs=None, dsem=False):
            return TT(P, st, name, shape, dt, bs=bs, dsem=dsem)

        ones_bf = SB("ones_bf", [128, 128], BF16)
        ones_f = SB("ones_f", [128, 64], F32)
        eps_c = SB("eps_c", [128, 1], F32)
        one_c = SB("one_c", [128, 1], F32)
        ident = SB("ident", [128, 128], BF16)
        M2 = SB("M2", [128, 128], BF16)
        TBH = 256
        rmask = SB("rmask", [64, TBH], F32)
        par = SB("par", [64, 8], F32, dsem=True)
        gm = SB("gm", [128, 8], F32, dsem=True)
        nfb = SB("nfb", [128, 1], F32, dsem=True)
        lbc = SB("lbc", [64, 4], F32)
        Wf = None
        Wb = SB("Wb", [128, 8, NCOL_A], BF16, dsem=True)

        memset(P, "pool", ones_bf[:], 1.0, ones_bf.r())
        memset(P, "pool", ones_f[:], 1.0, ones_f.r())
        memset(P, "pool", eps_c[:], EPS, eps_c.r())
        memset(P, "pool", one_c[:], 1.0, one_c.r())
        memset(P, "pool", ident[:], 1.0, ident.r())
        P.op("pool", lambda e: e.affine_select(out=ident[:], in_=ident[:], pattern=[[-1, 128]], compare_op=ALU.is_equal,
                                               fill=0.0, base=0, channel_multiplier=1), ident.r(), ident.r())
        memset(P, "pool", M2[:], 1.0, M2.r())
        P.op("pool", lambda e: e.affine_select(out=M2[:], in_=M2[:], pattern=[[1, 128]], compare_op=ALU.is_ge,
                                               fill=0.0, base=0, channel_multiplier=-1), M2.r(), M2.r())
        memset(P, "pool", M2[0:64, 64:128], 0.0, M2.r())
        memset(P, "pool", rmask[:], 1.0, rmask.r())
        memset(P, "pool", rmask[:].rearrange("p (c l) -> p c l", l=64)[:, :, 0:1], 0.0, rmask.r())

        dma(P, "sp", par[:], pA, (), par.r(), par.dsem)
        dma(P, "sp", gm[:], gmix, (), gm.r(), gm.dsem)
        dma(P, "sp", nfb[:], fbias, (), nfb.r(), nfb.dsem)
        dma(P, "pool", Wb[:], wA.rearrange("(kc p) n -> p kc n", p=128), (), Wb.r(), Wb.dsem)
        for kc in range(8):
            ts(P, "dve", Wb[:, kc, :], Wb[:, kc, :], gm[:, kc:kc + 1], ALU.mult, Wb.r() + gm.r(), Wb.r())
        ts(P, "dve", nfb[:], nfb[:], -1.0, ALU.mult, nfb.r(), nfb.r())
        tt(P, "dve", lbc[:, 3:4], par[:, 1:2], par[:, 0:1], ALU.subtract, par.r(), lbc.r())
        act(P, lbc[:, 3:4], lbc[:, 3:4], AF.Exp, lbc.r(), lbc.r())
        ts(P, "dve", lbc[:, 3:4], lbc[:, 3:4], 1.0, ALU.add, lbc.r(), lbc.r())
        recip(P, lbc[:, 0:1], lbc[:, 3:4], lbc.r(), lbc.r())
        ts(P, "dve", lbc[:, 1:2], lbc[:, 0:1], -1.0, ALU.mult, lbc.r(), lbc.r(), 1.0, ALU.add)
        ts(P, "dve", lbc[:, 2:3], par[:, 3:4], 0.125, ALU.mult, par.r(), lbc.r())
        lb_ap, oml_ap, gq8_ap = lbc[:, 0:1], lbc[:, 1:2], lbc[:, 2:3]
        on_ap, gk_ap = par[:, 2:3], par[:, 4:5]

        Q = SB("Q", [64, T], BF16, bs=512)
        Fh = SB("Fh", [64, T], BF16, bs=512)
        G = SB("G", [64, T], BF16, bs=512)
        BQ = SB("BQ", [70, T], BF16, bs=512, dsem=True)
        BK = SB("BK", [70, T], BF16, bs=128, dsem=True)
        VAB = SB("VAB", [128, T // 128, 129], BF16, bs=129)
        XB = [SB("XB%d" % i, [128, 8, 512], BF16, dsem=True) for i in range(2)]
        SQ = SB("SQ", [128, 8, 512], BF16)
        LNV = SB("LNV", [128, 512], F32)
        RSTD = [SB("RSTD%d" % i, [128, 512], F32) for i in range(2)]
        ZF = SB("ZF", [128, 512], F32)
        CC = [SB("CC%d" % i, [128, 512], F32) for i in range(2)]
        R1 = SB("R1", [128, 512], F32)
        AUG = [SB("AUG0", [128, 6, 512], BF16)] * 2
        RCS = SB("RCS", [128, 4], F32)
        PSB = [TT(P, st, "ps%d" % i, [128, 512], F32, psum=True) for i in range(8)]

        HT2 = [[SB("HT%d_%d" % (k_, i), [64, TBH], F32) for i in range(5)] for k_ in range(2)]
        KHT2 = [SB("KHT%d" % k_, [64, TBH], BF16) for k_ in range(2)]
        ELC0 = SB("ELC0", [64, T // 64], F32)

        KH = SB("KH", [128, T // 128, 64], BF16, bs=64)
        ELC = SB("ELC", [64, T // 64], F32)
        SBF = SB("SBF", [64, T], BF16)
        Z64 = SB("Z64", [64, 64], BF16)
        memset(P, "pool", Z64[:], 0.0, Z64.r())
        ATS = [SB("ATS%d" % i, [128, 512], BF16) for i in range(2)]
        OAS = [SB("OAS%d" % i, [64, 512], BF16, dsem=True) for i in range(2)]
        PTB = [SB("PTB%d" % i, [128, 512], BF16) for i in range(4)]
        ACCS = SB("ACCS", [65, 512], F32)
        RDEN = SB("RDEN", [64, 512], F32)
        OBS = [SB("OBS%d" % i, [64, 512], BF16, dsem=True) for i in range(2)]
        RONE = SB("RONE", [128, 512], BF16)
        memset(P, "pool", RONE[:], 1.0, RONE.r())
        memset(P, "pool", VAB[:, :, 128:129], 1.0, VAB.r())
        memset(P, "pool", BQ[64:70, :], 1.0, BQ.r())
        memset(P, "pool", BK[64:70, :], 1.0, BK.r())

        groups = [(0, 64, Q), (64, 64, Fh), (128, 64, G), (192, 64, BQ), (256, 65, BK)]
        blk = 0
        for b in range(nbatch):
            for n in range(T // 512):
                tok0 = b * T + n * 512
                cols = (n * 512, (n + 1) * 512)
                xb = XB[blk % 2]
                rs = RSTD[blk % 2]
                dma(P, "pool", xb[:], xT_v[:, :, tok0:tok0 + 512], (), xb.r(), xb.dsem)
                act(P, SQ[:], xb[:], AF.Square, xb.r(), SQ.r())
                ss = PSB[0]
                for kc in range(8):
                    mm(P, ss[:, :], ones_bf[:, :], SQ[:, kc, :], kc == 0, kc == 7, SQ.r() + ones_bf.r(), ss.r())
                act(P, LNV[:], ss[:, :], AF.Ln, ss.r() + eps_c.r(), LNV.r(), scale=1.0 / 1024, bias=eps_c[:])
                act(P, rs[:], LNV[:], AF.Exp, LNV.r(), rs.r(), scale=-0.5)
                for gi, (c0, M, dest) in enumerate(groups):
                    ps = PSB[1 + gi]
                    for kc in range(8):
                        mm(P, ps[0:M, :], Wb[:, kc, c0:c0 + M], xb[:, kc, :], kc == 0, kc == 7, Wb.r() + xb.r(), ps.r())
                    tt(P, "dve", dest[0:64, cols[0]:cols[1]], ps[0:64, :], rs[0:64, :], ALU.mult,
                       ps.r() + rs.r(), dest.r(*cols))
                    if M == 65:
                        cc = CC[blk % 2]
                        ccp = CC[(blk + 1) % 2]
                        aug = AUG[blk % 2]
                        r64 = slice(64, 65)
                        tt(P, "dve", ZF[r64, :], ps[r64, :], rs[r64, :], ALU.mult, ps.r() + rs.r(), ZF.r())
                        act(P, ZF[r64, :], ZF[r64, :], AF.Exp, ZF.r() + nfb.r(), ZF.r(), scale=-1.0, bias=nfb[r64, :])
                        act(P, ZF[r64, :], ZF[r64, :], AF.Ln, ZF.r() + one_c.r(), ZF.r(), scale=1.0, bias=one_c[r64, :])
                        init = 0.0 if n == 0 else ccp[r64, 511:512]
                        P.op("dve", lambda e, cc=cc, init=init: e.tensor_tensor_scan(
                            out=cc[64:65, :], data0=RONE[64:65, :],
                            data1=ZF[64:65, :], initial=init, op0=ALU.mult, op1=ALU.add),
                            ZF.r() + ccp.r() + RONE.r(), cc.r())
                        cp(P, "dve", aug[r64, 0, :], cc[r64, :], cc.r(), aug.r())
                        tt(P, "dve", R1[r64, :], cc[r64, :], aug[r64, 0, :], ALU.subtract, cc.r() + aug.r(), R1.r())
                        cp(P, "dve", aug[r64, 1, :], R1[r64, :], R1.r(), aug.r())
                        tt(P, "dve", R1[r64, :], R1[r64, :], aug[r64, 1, :], ALU.subtract, R1.r() + aug.r(), R1.r())
                        cp(P, "dve", aug[r64, 2, :], R1[r64, :], R1.r(), aug.r())
                        ts(P, "dve", aug[r64, 3:6, :], aug[r64, 0:3, :], -1.0, ALU.mult, aug.r(), aug.r())
                        for i in range(3):
                            dma(P, "sp", BK[67 + i:68 + i, cols[0]:cols[1]], aug[r64, i, :], aug.r(), BK.r(*cols), BK.dsem)
                            dma(P, "sp", BQ[64 + i:65 + i, cols[0]:cols[1]], aug[r64, 3 + i, :], aug.r(), BQ.r(*cols), BQ.dsem)
                ptm = PSB[6]
                prc = PSB[7]
                for sub in range(4):
                    sc = slice(sub * 128, (sub + 1) * 128)
                    for kc in range(8):
                        mm(P, ptm[:, sc], xb[:, kc, sc], Wb[:, kc, 321:449], kc == 0, kc == 7, Wb.r() + xb.r(), ptm.r())
                    mm(P, prc[:, sub:sub + 1], rs[0:1, sc], ones_f[0:1, 0:1], True, True, rs.r() + ones_f.r(), prc.r())
                cp(P, "dve", RCS[:, 0:4], prc[:, 0:4], prc.r(), RCS.r())
                for sub in range(4):
                    ti = n * 4 + sub
                    sc = slice(sub * 128, (sub + 1) * 128)
                    ts(P, "dve", VAB[:, ti, 0:128], ptm[:, sc], RCS[:, sub:sub + 1], ALU.mult,
                       ptm.r() + RCS.r(), VAB.r(ti * 129, ti * 129 + 129))
                blk += 1
            if do_fox:
                for n in range(T // 512):
                    cs = slice(n * 512, (n + 1) * 512)
                    for (tl, gap, bank) in ((BQ, gq8_ap, 1), (BK, gk_ap, 2)):
                        ps = PSB[bank]
                        act(P, SQ[0:64, 0, :], tl[0:64, cs], AF.Square, tl.r(n * 512, n * 512 + 512), SQ.r())
                        mm(P, ps[0:64, :], ones_bf[0:64, 0:64], SQ[0:64, 0, :], True, True, SQ.r() + ones_bf.r(), ps.r())
                        act(P, LNV[0:64, :], ps[0:64, :], AF.Ln, ps.r() + eps_c.r(), LNV.r(), scale=1.0 / 64, bias=eps_c[0:64, :])
                        act(P, LNV[0:64, :], LNV[0:64, :], AF.Exp, LNV.r(), LNV.r(), scale=-0.5)
                        stt(P, tl[0:64, cs], tl[0:64, cs], gap, LNV[0:64, :], ALU.mult, ALU.mult,
                            tl.r(n * 512, n * 512 + 512) + LNV.r() + par.r() + lbc.r(), tl.r(n * 512, n * 512 + 512))
            if do_hgrn:
                def hg_elem(par_):
                    for sbk in range(par_, T // TBH, 2):
                        c0, c1 = sbk * TBH, (sbk + 1) * TBH
                        cs = slice(c0, c1)
                        t1, t2, t3, t4, t5 = HT2[sbk % 2]
                        kht = KHT2[sbk % 2]
                        act(P, t1[:], Fh[0:64, cs], AF.Exp, Fh.r(c0, c1), t1.r(), scale=-1.0)
                        yield
                        act(P, t1[:], t1[:], AF.Identity, t1.r() + one_c.r(), t1.r(), scale=1.0, bias=one_c[0:64, :])
                        yield
                        recip(P, t1[:], t1[:], t1.r(), t1.r())
                        yield
                        ts(P, "dve", t1[:], t1[:], oml_ap, ALU.mult, t1.r() + lbc.r(), t1.r(), lb_ap, ALU.add)
                        yield
                        act(P, t2[:], t1[:], AF.Ln, t1.r(), t2.r())
                        yield
                        P.op("dve", lambda e, t3=t3, t2=t2: e.tensor_tensor_scan(out=t3[:], data0=rmask[:], data1=t2[:], initial=0.0,
                                                                                 op0=ALU.mult, op1=ALU.add), t2.r() + rmask.r(), t3.r())
                        yield
                        act(P, t1[:], t1[:], AF.Identity, t1.r() + one_c.r(), t1.r(), scale=-1.0, bias=one_c[0:64, :])
                        yield
                        act(P, t4[:], t3[:], AF.Exp, t3.r(), t4.r())
                        yield
                        act(P, t5[:], Q[0:64, cs], AF.Exp, Q.r(c0, c1), t5.r(), scale=-1.0)
                        yield
                        act(P, t5[:], t5[:], AF.Identity, t5.r() + one_c.r(), t5.r(), scale=1.0, bias=one_c[0:64, :])
                        yield
                        recip(P, t5[:], t5[:], t5.r(), t5.r())
                        yield
                        tt(P, "dve", t5[:], Q[0:64, cs], t5[:], ALU.mult, Q.r(c0, c1) + t5.r(), t5.r())
                        yield
                        tt(P, "dve", Q[0:64, cs], t5[:], t4[:], ALU.mult, t5.r() + t4.r(), Q.r(c0, c1))
                        yield
                        act(P, t4[:], t3[:], AF.Exp, t3.r(), t4.r(), scale=-1.0)
                        yield
                        tt(P, "dve", Fh[0:64, cs], t1[:], t4[:], ALU.mult, t1.r() + t4.r(), Fh.r(c0, c1))
                        yield
                        nch = TBH // 64
                        ch0 = sbk * nch
                        act(P, ELC[0:64, ch0:ch0 + nch], t3[:].rearrange("p (c l) -> p c l", l=64)[:, :, 63],
                            AF.Exp, t3.r(), ELC.r())
                        yield
                        tt(P, "dve", kht[:].rearrange("p (c l) -> p c l", l=64), Fh[0:64, cs].rearrange("p (c l) -> p c l", l=64),
                           ELC[0:64, ch0:ch0 + nch].unsqueeze(2).broadcast_to([64, nch, 64]), ALU.mult,
                           Fh.r(c0, c1) + ELC.r(), kht.r())
                        yield
                        pst = PSB[7] if sbk % 2 == 0 else PSB[0]
                        pstb = pst.h.bitcast(BF16)
                        ntl = TBH // 128
                        for j in range(ntl):
                            P.op("pe", lambda e, j=j, pstb=pstb, kht=kht: e.transpose(out=pstb[:, j * 64:(j + 1) * 64], in_=kht[0:64, j * 128:(j + 1) * 128],
                                                                  identity=ident[0:64, 0:64]), kht.r() + ident.r(), pst.r())
                            yield
                        ti0 = sbk * ntl
                        cp(P, "act", KH[:, ti0:ti0 + ntl, :], pstb[:, 0:ntl * 64].rearrange("p (a b) -> p a b", b=64),
                           pst.r(), KH.r(ti0 * 64, (ti0 + ntl) * 64))
                        yield
                        act(P, t2[:], G[0:64, cs], AF.Exp, G.r(c0, c1), t2.r(), scale=-1.0)
                        yield
                        act(P, t2[:], t2[:], AF.Identity, t2.r() + one_c.r(), t2.r(), scale=1.0, bias=one_c[0:64, :])
                        yield
                        recip(P, t2[:], t2[:], t2.r(), t2.r())
                        yield
                        tt(P, "dve", G[0:64, cs], G[0:64, cs], t2[:], ALU.mult, G.r(c0, c1) + t2.r(), G.r(c0, c1))
                        yield

                hg_gens = [hg_elem(0), hg_elem(1)]
            if do_fox:
                k = 0
                for j in range(T // 512):
                    acc = PSB[4 + j % 2]
                    nt = 4 * j + 4
                    items = []
                    for i in range(nt):
                        m = i - 4 * j
                        c0 = 128 * m if m >= 0 else 0
                        items.append((i, m, c0))
                    def qk(it, k):
                        i, m, c0 = it
                        sc = PSB[1 + k % 3]
                        mm(P, sc[:, c0:512], BK[0:70, i * 128:(i + 1) * 128], BQ[0:70, j * 512 + c0:(j + 1) * 512], True, True,
                           BK.r(i * 128, i * 128 + 128) + BQ.r(j * 512, j * 512 + 512), sc.r())
                    def rest(it, k):
                        i, m, c0 = it
                        sc = PSB[1 + k % 3]
                        pt = PTB[k % 4]
                        act(P, pt[:, c0:512], sc[:, c0:512], AF.Exp, sc.r(), pt.r())
                        if m >= 0:
                            P.op("pool", lambda e, pt=pt, c0=c0: e.affine_select(out=pt[:, c0:c0 + 128], in_=pt[:, c0:c0 + 128], pattern=[[1, 128]],
                                 compare_op=ALU.is_ge, fill=0.0, base=0, channel_multiplier=-1), pt.r(), pt.r())
                        mm(P, acc[0:65, c0:512], VAB[:, i, 64:129], pt[:, c0:512], i == 0, i == nt - 1,
                           VAB.r(i * 129, i * 129 + 129) + pt.r(), acc.r())
                    LOOK = 2
                    for idx in range(nt + LOOK):
                        if idx < nt:
                            qk(items[idx], k + idx)
                        if idx >= LOOK:
                            rest(items[idx - LOOK], k + idx - LOOK)
                            if do_hgrn and os.environ.get("HG_NOILV") is None:
                                for g_ in hg_gens:
                                    next(g_, None)
                    k += nt
                    cp(P, "dve", ACCS[0:65, :], acc[0:65, :], acc.r(), ACCS.r())
                    pden = PSB[6]
                    mm(P, pden[0:64, :], ones_f[64:65, 0:64], ACCS[64:65, :], True, True, ones_f.r() + ACCS.r(), pden.r())
                    recip(P, RDEN[:], pden[0:64, :], pden.r(), RDEN.r())
                    obs = OBS[j % 2]
                    tt(P, "pool", obs[:], ACCS[0:64, :], RDEN[:], ALU.mult, ACCS.r() + RDEN.r(), obs.r())
                    dma(P, "sp", oT[64:128, b * T + j * 512: b * T + (j + 1) * 512], obs[:], obs.r(), (), obs.dsem)
            if do_hgrn:
                for g_ in hg_gens:
                    for _ in g_:
                        pass
                DSV = BQ
                NC_ = T // 64
                for g4 in range(T // 512):
                    for c in range(8 * g4, 8 * g4 + 8):
                        ti, hh = c // 2, c % 2
                        pds = PSB[4 + (g4 % 2) * 2 + hh]
                        slot = (c % 8) * 64
                        hs = slice(hh * 64, (hh + 1) * 64)
                        mm(P, pds[0:64, slot:slot + 64], KH[hs, ti, :], VAB[hs, ti, 0:64], True, True,
                           KH.r(ti * 64, ti * 64 + 64) + VAB.r(ti * 129, ti * 129 + 129), pds.r())
                    for hh in range(2):
                        pds = PSB[4 + (g4 % 2) * 2 + hh]
                        src = pds[0:64, :].rearrange("p (a b) -> p a b", b=128)[:, :, hh * 64:(hh + 1) * 64]
                        dst = DSV[0:64, :].rearrange("p (v c) -> p c v", c=NC_)[:, 8 * g4 + hh:8 * g4 + 8:2, :]
                        cp(P, "dve" if hh == 0 else "act", dst, src, pds.r(), DSV.r())
                cp(P, "pool", ELC0[:], ELC[:], ELC.r(), ELC0.r())
                memset(P, "pool", ELC0[:, 0:1], 0.0, ELC0.r())
                D0 = BK
                cp(P, "act", D0[0:64, :].rearrange("p (v c) -> p v c", c=NC_), ELC0[:, :].unsqueeze(1).broadcast_to([64, 64, NC_]),
                   ELC0.r(), D0.r())
                P.op("dve", lambda e: e.tensor_tensor_scan(out=SBF[:, :], data0=D0[0:64, :], data1=DSV[0:64, :],
                                                           initial=0.0, op0=ALU.mult, op1=ALU.add), D0.r() + DSV.r(), SBF.r())
                for g4 in range(T // 512):
                    pat = PSB[6 + g4 % 2]
                    for j in range(4):
                        ti = g4 * 4 + j
                        tcs = slice(ti * 128, (ti + 1) * 128)
                        mm(P, pat[:, j * 128:(j + 1) * 128], Fh[0:64, tcs], Q[0:64, tcs], True, True,
                           Fh.r(ti * 128, ti * 128 + 128) + Q.r(ti * 128, ti * 128 + 128), pat.r())
                    ats = ATS[g4 % 2]
                    tt(P, "dve", ats[:].rearrange("p (a b) -> p a b", b=128), pat[:, :].rearrange("p (a b) -> p a b", b=128),
                       M2[:].unsqueeze(1).broadcast_to([128, 4, 128]), ALU.mult, pat.r() + M2.r(), ats.r())
                    po = PSB[1 + g4 % 2]
                    for jj in range(4):
                        tj = g4 * 4 + jj
                        for hh in range(2):
                            c = 2 * tj + hh
                            ccs = slice(c * 64, (c + 1) * 64)
                            oc = slice(jj * 128 + hh * 64, jj * 128 + hh * 64 + 64)
                            mm(P, po[0:64, oc], VAB[:, tj, 0:64], ats[:, oc], True, False,
                               VAB.r(tj * 129, tj * 129 + 129) + ats.r(), po.r())
                            st_ap = Z64[:, :] if c == 0 else SBF[:, :].rearrange("p (v c) -> p v c", c=NC_)[:, :, c - 1]
                            mm(P, po[0:64, oc], st_ap, Q[0:64, ccs], False, True,
                               SBF.r() + Z64.r() + Q.r(c * 64, c * 64 + 64), po.r())
                    gcs = slice(g4 * 512, (g4 + 1) * 512)
                    pn = PSB[3]
                    sq64 = SQ
                    lnv = RSTD[g4 % 2]
                    oaf = (ZF, R1)[g4 % 2]
                    act(P, sq64[0:64, g4 % 2, :], po[0:64, :], AF.Square, po.r(), sq64.r())
                    mm(P, pn[0:64, :], ones_bf[0:64, 0:64], sq64[0:64, g4 % 2, :], True, True, sq64.r() + ones_bf.r(), pn.r())
                    act(P, lnv[0:64, :], pn[0:64, :], AF.Ln, pn.r() + eps_c.r(), lnv.r(), scale=1.0 / 64, bias=eps_c[0:64, :])
                    act(P, lnv[0:64, :], lnv[0:64, :], AF.Exp, lnv.r(), lnv.r(), scale=-0.5)
                    stt(P, oaf[0:64, :], po[0:64, :], on_ap, lnv[0:64, :], ALU.mult, ALU.mult, po.r() + lnv.r() + par.r(), oaf.r())
                    oas = OAS[g4 % 2]
                    tt(P, "dve", oas[:], oaf[0:64, :], G[0:64, gcs], ALU.mult, oaf.r() + G.r(g4 * 512, g4 * 512 + 512), oas.r())
                    dma(P, "sp", oT[0:64, b * T + g4 * 512: b * T + (g4 + 1) * 512], oas[:], oas.r(), (), oas.dsem)
        if os.environ.get('HG_DBG'):
            dsd2 = P.new_dsem("dbg2")
            for nm, tl, shp, dt_ in (("dKH", KH, [128, (T // 128) * 64], BF16), ("dKt", Fh, [64, T], BF16), ("dQt", Q, [64, T], BF16), ("dELC", ELC, [64, T // 64], F32), ("dSBF", SBF, [64, T], BF16)):
                dd = nc.dram_tensor(nm, shp, dt_, kind="ExternalOutput").ap()
                src = tl[:] if len(tl.h.shape) == 2 else tl[:].rearrange("p a b -> p (a b)")
                dma(P, "sp", dd, src, tl.r(), (), dsd2)
        if debug:
            dsd = P.new_dsem("dbg")
            for nm, tl, shp in (("dQ", Q, [64, T]), ("dF", Fh, [64, T]), ("dG", G, [64, T]), ("dBQ", BQ, [70, T]), ("dBK", BK, [70, T]), ("dV", VAB, [128, (T // 128) * 129])):
                dd = nc.dram_tensor(nm, shp, BF16, kind="ExternalOutput").ap()
                src = tl[:] if nm != "dV" else tl[:].rearrange("p a b -> p (a b)")
                dma(P, "sp", dd, src, tl.r(), (), dsd)
        P.replay()
    return nc


NT = 2048


def build_B():
    nc = bass.Bass("TRN2", target_bir_lowering=False)
    hT_in = nc.dram_tensor("hT_in", [1024, NT], F32, kind="ExternalInput").ap()
    oT_in = nc.dram_tensor("oT_in", [1024, NT], BF16, kind="ExternalInput").ap()
    w_out = nc.dram_tensor("w_out", [1024, 1024], F32, kind="ExternalInput").ap()
    gffn = nc.dram_tensor("gffn", [128, 8], F32, kind="ExternalInput").ap()
    w_r = nc.dram_tensor("w_r", [1024, 36], F32, kind="ExternalInput").ap()
    w_gate = nc.dram_tensor("w_gate", [32, 1024, 512], F32, kind="ExternalInput").ap()
    w_up = nc.dram_tensor("w_up", [32, 1024, 512], F32, kind="ExternalInput").ap()
    w_down = nc.dram_tensor("w_down", [32, 512, 1024], F32, kind="ExternalInput").ap()
    hT_out = nc.dram_tensor("hT_out", [1024, NT], F32, kind="ExternalOutput").ap()
    P = Prog(nc)
    hT_v = hT_in.rearrange("(kc p) t -> p kc t", p=128)
    oT_v = oT_in.rearrange("(kc p) t -> p kc t", p=128)
    ho_v = hT_out.rearrange("(kc p) t -> p kc t", p=128)
    with ExitStack() as st:
        def SB(name, shape, dt, bs=None, dsem=False):
            return TT(P, st, name, shape, dt, bs=bs, dsem=dsem)

        ones_bf = SB("ones_bf", [128, 128], BF16)
        eps_c = SB("eps_c", [128, 1], F32)
        identf = SB("identf", [128, 128], F32)
        SEL = SB("SEL", [32, 32, 128], BF16)
        gf = SB("gf", [128, 8], F32, dsem=True)
        WR = SB("WR", [128, 8, 36], F32, dsem=True)
        HM = SB("HM", [128, 8, NT], F32, bs=512, dsem=True)
        U = SB("U", [128, 8, NT], BF16, bs=512)
        WBUF = [SB("WB%d" % i, [128, 12288], BF16, dsem=True) for i in range(2)]
        OTB = [SB("OTB%d" % i, [128, 8, 512], BF16, dsem=True) for i in range(2)]
        HID = [SB("HID%d" % i, [128, 4, 512], BF16) for i in range(2)]
        SIL = [SB("SIL%d" % i, [128, 512], BF16) for i in range(2)]
        S2 = [SB("S2%d" % i, [128, 512], BF16) for i in range(2)]
        CREP = [SB("CREP%d" % i, [128, 512], BF16) for i in range(2)]
        CT = SB("CT", [32, NT], BF16, bs=128)
        LNV = SB("LNV", [128, 512], F32)
        RSTD = SB("RSTD", [128, 512], F32)
        RLA = SB("RLA", [128, NT // 128, 36], F32)
        R16 = SB("R16", [128, 8, NT // 128], F32)
        OHA = SB("OHA", [128, NT // 128, 4], F32)
        EGA = SB("EGA", [128, NT // 128, 4], F32)
        SELA = SB("SELA", [128, NT // 128, 8], F32)
        TMP8 = SB("TMP8", [128, NT // 128, 8], F32)
        E1A = SB("E1A", [128, NT // 128, 8], F32)
        E2A = SB("E2A", [128, NT // 128, 8], F32)
        COMBA = SB("COMBA", [128, NT // 128, 32], F32)
        RL = SB("RL", [128, 36], F32)
        RT = SB("RT", [128, 16], F32)
        OH = SB("OH", [128, 4], F32)
        EG = SB("EG", [128, 4], F32)
        SEL8 = SB("SEL8", [128, 8], F32)
        M8 = SB("M8", [128, 8], F32)
        WA8 = SB("WA8", [128, 8], F32)
        WB8 = SB("WB8", [128, 8], F32)
        COMB = SB("COMB", [128, 32], F32)
        PSB = [TT(P, st, "ps%d" % i, [128, 512], F32, psum=True) for i in range(8)]
        Wo = WBUF[1]
        Wo_v = Wo[:, 0:8192].rearrange("p (a b) -> p a b", b=1024)
        UFt = WBUF[0]
        UF_v = UFt.h.bitcast(F32)[:, 0:4096].rearrange("p (a b) -> p a b", b=512)
        SQt = HID[0]
        SQ = SB("SQ", [128, 8, 512], BF16)

        memset(P, "pool", ones_bf[:], 1.0, ones_bf.r())
        memset(P, "pool", eps_c[:], EPS, eps_c.r())
        memset(P, "pool", identf[:], 1.0, identf.r())
        P.op("pool", lambda e: e.affine_select(out=identf[:], in_=identf[:], pattern=[[-1, 128]], compare_op=ALU.is_equal,
                                               fill=0.0, base=0, channel_multiplier=1), identf.r(), identf.r())
        memset(P, "pool", SEL[:], 1.0, SEL.r())
        P.op("pool", lambda e: e.affine_select(out=SEL[:], in_=SEL[:], pattern=[[-1, 32], [0, 128]], compare_op=ALU.is_equal,
                                               fill=0.0, base=0, channel_multiplier=1), SEL.r(), SEL.r())
        dma(P, "sp", gf[:], gffn, (), gf.r(), gf.dsem)
        dma(P, "sp", WR[:], w_r.rearrange("(kc p) n -> p kc n", p=128), (), WR.r(), WR.dsem)
        dma(P, "pool", Wo_v, w_out.rearrange("(kc p) n -> p kc n", p=128), (), Wo.r(), Wo.dsem)
        for h2 in range(2):
            dma(P, "sp", HM[:, :, h2 * 1024:(h2 + 1) * 1024], hT_v[:, :, h2 * 1024:(h2 + 1) * 1024], (), HM.r(), HM.dsem)

        for blk in range(NT // 512):
            cs = slice(blk * 512, (blk + 1) * 512)
            ot = OTB[blk % 2]
            dma(P, "sp", ot[:], oT_v[:, :, cs], (), ot.r(), ot.dsem)
            for dc in range(8):
                ps = PSB[dc % 2]
                for kc in range(8):
                    mm(P, ps[:, :], Wo_v[:, kc, dc * 128:(dc + 1) * 128], ot[:, kc, :], kc == 0, kc == 7, Wo.r() + ot.r(), ps.r())
                lo = dc * NT + blk * 512
                tt(P, "dve", HM[:, dc, cs], HM[:, dc, cs], ps[:, :], ALU.add, HM.r(lo, lo + 512) + ps.r(), HM.r(lo, lo + 512))
            hm_blk = []
            for dc in range(8):
                hm_blk += HM.r(dc * NT + blk * 512, dc * NT + blk * 512 + 512)
            act(P, SQ[:], HM[:, :, cs], AF.Square, hm_blk, SQ.r())
            ss = PSB[2]
            for kc in range(8):
                mm(P, ss[:, :], ones_bf[:, :], SQ[:, kc, :], kc == 0, kc == 7, SQ.r() + ones_bf.r(), ss.r())
            act(P, LNV[:], ss[:, :], AF.Ln, ss.r() + eps_c.r(), LNV.r(), scale=1.0 / 1024, bias=eps_c[:])
            act(P, RSTD[:], LNV[:], AF.Exp, LNV.r(), RSTD.r(), scale=-0.5)
            u_blk = []
            for dc in range(8):
                lo = dc * NT + blk * 512
                stt(P, UF_v[:, dc, :], HM[:, dc, cs], gf[:, dc:dc + 1], RSTD[:], ALU.mult, ALU.mult,
                    HM.r(lo, lo + 512) + gf.r() + RSTD.r(), UFt.r())
                u_blk += U.r(lo, lo + 512)
            cp(P, "pool", U[:, :, cs], UF_v, UFt.r(), u_blk)
            pr = PSB[3]
            for sub in range(4):
                scs = slice(sub * 128, (sub + 1) * 128)
                for dc in range(8):
                    mm(P, pr[:, sub * 36:(sub + 1) * 36], UF_v[:, dc, scs], WR[:, dc, :], dc == 0, dc == 7, UFt.r() + WR.r(), pr.r())
            cp(P, "dve", RLA[:, blk * 4:(blk + 1) * 4, :].rearrange("p a b -> p (a b)"), pr[:, 0:144], pr.r(), RLA.r())

        S_ = NT // 128
        def b3(ap2, n):
            return ap2.unsqueeze(2).broadcast_to([128, S_, n])
        GL = RLA[:, :, 0:4]
        P.op("dve", lambda e: e.tensor_reduce(out=R16[:, 0, :], in_=GL, axis=AX.X, op=ALU.max), RLA.r(), R16.r())
        tt(P, "dve", OHA[:], GL, b3(R16[:, 0, :], 4), ALU.is_equal, RLA.r() + R16.r(), OHA.r())
        tt(P, "dve", EGA[:], GL, b3(R16[:, 0, :], 4), ALU.subtract, RLA.r() + R16.r(), EGA.r())
        act(P, EGA[:], EGA[:], AF.Exp, EGA.r(), EGA.r())
        P.op("dve", lambda e: e.tensor_reduce(out=R16[:, 1, :], in_=EGA[:], axis=AX.X, op=ALU.add), EGA.r(), R16.r())
        recip(P, R16[:, 2, :], R16[:, 1, :], R16.r(), R16.r())
        tt(P, "dve", SELA[:], RLA[:, :, 4:12], b3(OHA[:, :, 0], 8), ALU.mult, RLA.r() + OHA.r(), SELA.r())
        for g in range(1, 4):
            tt(P, "dve", TMP8[:], RLA[:, :, 4 + 8 * g:12 + 8 * g], b3(OHA[:, :, g], 8), ALU.mult, RLA.r() + OHA.r(), TMP8.r())
            tt(P, "dve", SELA[:], SELA[:], TMP8[:], ALU.add, SELA.r() + TMP8.r(), SELA.r())
        P.op("dve", lambda e: e.tensor_reduce(out=R16[:, 3, :], in_=SELA[:], axis=AX.X, op=ALU.max), SELA.r(), R16.r())
        tt(P, "dve", E1A[:], SELA[:], b3(R16[:, 3, :], 8), ALU.is_equal, SELA.r() + R16.r(), E1A.r())
        ts(P, "dve", TMP8[:], E1A[:], -1.0e30, ALU.mult, E1A.r(), TMP8.r())
        tt(P, "dve", TMP8[:], SELA[:], TMP8[:], ALU.add, SELA.r() + TMP8.r(), TMP8.r())
        P.op("dve", lambda e: e.tensor_reduce(out=R16[:, 4, :], in_=TMP8[:], axis=AX.X, op=ALU.max), TMP8.r(), R16.r())
        tt(P, "dve", E2A[:], TMP8[:], b3(R16[:, 4, :], 8), ALU.is_equal, TMP8.r() + R16.r(), E2A.r())
        tt(P, "dve", R16[:, 5, :], R16[:, 4, :], R16[:, 3, :], ALU.subtract, R16.r(), R16.r())
        act(P, R16[:, 5, :], R16[:, 5, :], AF.Exp, R16.r(), R16.r())
        ts(P, "dve", R16[:, 5, :], R16[:, 5, :], 1.0, ALU.add, R16.r(), R16.r())
        recip(P, R16[:, 6, :], R16[:, 5, :], R16.r(), R16.r())
        ts(P, "dve", R16[:, 7, :], R16[:, 6, :], -1.0, ALU.mult, R16.r(), R16.r(), 1.0, ALU.add)
        tt(P, "dve", R16[:, 6, :], R16[:, 6, :], R16[:, 2, :], ALU.mult, R16.r(), R16.r())
        tt(P, "dve", R16[:, 7, :], R16[:, 7, :], R16[:, 2, :], ALU.mult, R16.r(), R16.r())
        tt(P, "dve", E1A[:], E1A[:], b3(R16[:, 6, :], 8), ALU.mult, E1A.r() + R16.r(), E1A.r())
        tt(P, "dve", E2A[:], E2A[:], b3(R16[:, 7, :], 8), ALU.mult, E2A.r() + R16.r(), E2A.r())
        tt(P, "dve", E1A[:], E1A[:], E2A[:], ALU.add, E1A.r() + E2A.r(), E1A.r())
        tt(P, "dve", COMBA[:].rearrange("p s (g e) -> p s g e", e=8),
           E1A[:].unsqueeze(2).broadcast_to([128, S_, 4, 8]), OHA[:].unsqueeze(3).broadcast_to([128, S_, 4, 8]), ALU.mult,
           E1A.r() + OHA.r(), COMBA.r())
        for q4 in range(S_ // 4):
            pt = PSB[4 + q4 % 2]
            for sub in range(4):
                sidx = q4 * 4 + sub
                P.op("pe", lambda e, pt=pt, sub=sub, sidx=sidx: e.transpose(out=pt[0:32, sub * 128:(sub + 1) * 128], in_=COMBA[:, sidx, :], identity=identf[:, :]),
                     COMBA.r() + identf.r(), pt.r())
            cp(P, "dve", CT[0:32, q4 * 512:(q4 + 1) * 512], pt[0:32, :], pt.r(), CT.r(q4 * 512, q4 * 512 + 512))

        units = [(e, blk) for e in range(32) for blk in range(NT // 512)]
        wviews = []
        for i in range(2):
            wb = WBUF[i]
            wviews.append((wb[:, 0:4096].rearrange("p (a b) -> p a b", b=512),
                           wb[:, 4096:8192].rearrange("p (a b) -> p a b", b=512),
                           wb[:, 8192:12288].rearrange("p (a b) -> p a b", b=1024)))

        def load_w(e):
            wb = WBUF[e % 2]
            Wg, Wu, Wd = wviews[e % 2]
            dma(P, "pool", Wg, w_gate[e].rearrange("(kc p) n -> p kc n", p=128), (), wb.r(), wb.dsem)
            dma(P, "pool", Wu, w_up[e].rearrange("(kc p) n -> p kc n", p=128), (), wb.r(), wb.dsem)
            dma(P, "pool", Wd, w_down[e].rearrange("(fc p) n -> p fc n", p=128), (), wb.r(), wb.dsem)

        def GU(ui):
            e, blk = units[ui]
            cs = slice(blk * 512, (blk + 1) * 512)
            wb = WBUF[e % 2]
            Wg, Wu, Wd = wviews[e % 2]
            hid = HID[ui % 2]
            crep = CREP[ui % 2]
            pc = PSB[6]
            mm(P, pc[:, :], SEL[0:32, e, :], CT[0:32, cs], True, True, SEL.r() + CT.r(blk * 512, blk * 512 + 512), pc.r())
            cp(P, "dve", crep[:], pc[:, :], pc.r(), crep.r())
            u_blk = []
            for dc in range(8):
                u_blk += U.r(dc * NT + blk * 512, dc * NT + blk * 512 + 512)
            for fc in range(4):
                pg_ = PSB[(fc % 2) * 2]
                pu_ = PSB[(fc % 2) * 2 + 1]
                fs = slice(fc * 128, (fc + 1) * 128)
                for dc in range(8):
                    mm(P, pg_[:, :], Wg[:, dc, fs], U[:, dc, cs], dc == 0, dc == 7, wb.r() + u_blk, pg_.r())
                for dc in range(8):
                    mm(P, pu_[:, :], Wu[:, dc, fs], U[:, dc, cs], dc == 0, dc == 7, wb.r() + u_blk, pu_.r())
                sil = SIL[fc % 2]
                s2 = S2[fc % 2]
                act(P, sil[:], pg_[:, :], AF.Silu, pg_.r(), sil.r())
                tt(P, "dve", s2[:], sil[:], crep[:], ALU.mult, sil.r() + crep.r(), s2.r())
                tt(P, "dve", hid[:, fc, :], s2[:], pu_[:, :], ALU.mult, s2.r() + pu_.r(), hid.r())

        def DN(ui):
            e, blk = units[ui]
            cs = slice(blk * 512, (blk + 1) * 512)
            wb = WBUF[e % 2]
            Wg, Wu, Wd = wviews[e % 2]
            hid = HID[ui % 2]
            for dc in range(8):
                py = PSB[4 + dc % 2]
                for fc in range(4):
                    mm(P, py[:, :], Wd[:, fc, dc * 128:(dc + 1) * 128], hid[:, fc, :], fc == 0, fc == 3, wb.r() + hid.r(), py.r())
                lo = dc * NT + blk * 512
                tt(P, "dve", HM[:, dc, cs], HM[:, dc, cs], py[:, :], ALU.add, HM.r(lo, lo + 512) + py.r(), HM.r(lo, lo + 512))

        nE = int(os.environ.get("KB_NE", 32))
        units = [u_ for u_ in units if u_[0] < nE]
        load_w(0)
        for ui in range(len(units) + 1):
            if ui < len(units):
                GU(ui)
            if ui >= 1:
                DN(ui - 1)
            if ui < len(units):
                e, blk = units[ui]
                if blk == 0 and e + 1 < nE:
                    load_w(e + 1)
        for h2 in range(2):
            rr = []
            for dc in range(8):
                rr += HM.r(dc * NT + h2 * 1024, dc * NT + h2 * 1024 + 1024)
            dma(P, "sp", ho_v[:, :, h2 * 1024:(h2 + 1) * 1024], HM[:, :, h2 * 1024:(h2 + 1) * 1024], rr, (), HM.dsem)
        P.replay()
    return nc


import math

NCOL_C = 384
TWO_PI = 2.0 * math.pi
LAM_INIT = 0.8 - 0.6 * math.exp(-0.3 * 1)


def build_C(nbatch=2, debug=False):
    nc = bass.Bass("TRN2", target_bir_lowering=False)
    xT = nc.dram_tensor("xT", [1024, TOK], F32, kind="ExternalInput").ap()
    wC = nc.dram_tensor("wC", [1024, NCOL_C], F32, kind="ExternalInput").ap()
    gmix = nc.dram_tensor("gmix", [128, 8], F32, kind="ExternalInput").ap()
    pC = nc.dram_tensor("pC", [128, 8], F32, kind="ExternalInput").ap()
    posd = nc.dram_tensor("pos", [2, T], I32, kind="ExternalInput").ap()
    oT = nc.dram_tensor("oT", [128, TOK], BF16, kind="ExternalOutput").ap()
    P = Prog(nc)
    xT_v = xT.rearrange("(kc p) t -> p kc t", p=128)
    with ExitStack() as st:
        def SB(name, shape, dt, bs=None, dsem=False):
            return TT(P, st, name, shape, dt, bs=bs, dsem=dsem)

        ones_bf = SB("ones_bf", [128, 128], BF16)
        BD = SB("BD", [128, 128], BF16)
        ones_f = SB("ones_f", [128, 128], F32)
        eps_c = SB("eps_c", [128, 1], F32)
        pi_c = SB("pi_c", [128, 2], F32)
        pic2 = SB("pic2", [128, 2], F32)
        ident = SB("ident", [128, 128], BF16)
        PM = SB("PM", [128, 128], BF16)
        PM2 = SB("PM2", [128, 128], BF16)
        par = SB("par", [128, 8], F32, dsem=True)
        gm = SB("gm", [128, 8], F32, dsem=True)
        cst = SB("cst", [128, 8], F32)
        invi = SB("invi", [1, 128], I32)
        invf = SB("invf", [1, 128], F32)
        Wb = SB("Wb", [128, 8, NCOL_C], BF16, dsem=True)

        memset(P, "pool", ones_bf[:], 1.0, ones_bf.r())
        memset(P, "pool", ones_f[:], 1.0, ones_f.r())
        memset(P, "pool", eps_c[:], EPS, eps_c.r())
        memset(P, "pool", BD[:], 1.0, BD.r())
        memset(P, "pool", BD[0:64, 64:128], 0.0, BD.r())
        memset(P, "pool", BD[64:128, 0:64], 0.0, BD.r())
        memset(P, "pool", pi_c[:, 0:1], math.pi, pi_c.r())
        memset(P, "pool", pi_c[:, 1:2], -TWO_PI, pi_c.r())
        memset(P, "pool", pi_c[0:32, 0:1], -math.pi, pi_c.r())
        memset(P, "pool", pi_c[0:32, 1:2], TWO_PI, pi_c.r())
        memset(P, "pool", pi_c[64:96, 0:1], -math.pi, pi_c.r())
        memset(P, "pool", pi_c[64:96, 1:2], TWO_PI, pi_c.r())
        memset(P, "pool", pic2[:, 0:1], math.pi, pic2.r())
        memset(P, "pool", PM[:], 1.0, PM.r())
        P.op("pool", lambda e: e.affine_select(out=PM[:], in_=PM[:], pattern=[[1, 128]], compare_op=ALU.is_equal,
                                               fill=0.0, base=-32, channel_multiplier=-1), PM.r(), PM.r())
        memset(P, "pool", PM[:, 0:32], 0.0, PM.r())
        memset(P, "pool", PM[:, 64:96], 0.0, PM.r())
        memset(P, "pool", PM2[:], 1.0, PM2.r())
        P.op("pool", lambda e: e.affine_select(out=PM2[:], in_=PM2[:], pattern=[[1, 128]], compare_op=ALU.is_equal,
                                               fill=0.0, base=32, channel_multiplier=-1), PM2.r(), PM2.r())
        memset(P, "pool", PM2[:, 32:64], 0.0, PM2.r())
        memset(P, "pool", PM2[:, 96:128], 0.0, PM2.r())
        tt(P, "pool", PM[:], PM[:], PM2[:], ALU.add, PM.r() + PM2.r(), PM.r())
        P.op("pool", lambda e: e.iota(invi[:].rearrange("p (a b) -> p a b", b=32), pattern=[[0, 4], [1, 32]], base=0, channel_multiplier=0),
             (), invi.r())
        cp(P, "dve", invf[:], invi[:], invi.r(), invf.r())
        act(P, invf[:], invf[:], AF.Exp, invf.r(), invf.r(), scale=-math.log(10000.0) / 32.0)

        dma(P, "sp", par[:], pC, (), par.r(), par.dsem)
        dma(P, "sp", gm[:], gmix, (), gm.r(), gm.dsem)
        dma(P, "pool", Wb[:], wC.rearrange("(kc p) n -> p kc n", p=128), (), Wb.r(), Wb.dsem)
        for kc in range(8):
            ts(P, "dve", Wb[:, kc, :], Wb[:, kc, :], gm[:, kc:kc + 1], ALU.mult, Wb.r() + gm.r(), Wb.r())
        ts(P, "dve", cst[:, 0:1], par[:, 0:1], 0.125, ALU.mult, par.r(), cst.r())
        ts(P, "dve", cst[:, 3:4], par[:, 6:7], 1.0 - LAM_INIT, ALU.mult, par.r(), cst.r())
        PSB = [TT(P, st, "ps%d" % i, [128, 512], F32, psum=True) for i in range(8)]
        tt(P, "dve", cst[:, 4:5], par[:, 2:3], par[:, 3:4], ALU.mult, par.r(), cst.r())
        tt(P, "dve", cst[:, 5:6], par[:, 4:5], par[:, 5:6], ALU.mult, par.r(), cst.r())
        mm(P, PSB[0][:, 0:2], ones_f[:, :], cst[:, 4:6], True, True, ones_f.r() + cst.r(), PSB[0].r())
        act(P, cst[:, 6:8], PSB[0][:, 0:2], AF.Exp, PSB[0].r(), cst.r())
        tt(P, "dve", cst[:, 2:3], cst[:, 7:8], cst[:, 6:7], ALU.subtract, cst.r(), cst.r())
        ts(P, "dve", cst[:, 2:3], cst[:, 2:3], -LAM_INIT, ALU.add, cst.r(), cst.r())
        gq_ap, gk_ap, nlam_ap, gs_ap = cst[:, 0:1], par[:, 1:2], cst[:, 2:3], cst[:, 3:4]

        QT = SB("QT", [128, T], BF16, bs=512)
        KT = SB("KT", [128, T], BF16, bs=128)
        V = SB("V", [128, T // 128, 128], BF16, bs=128)
        SINT = SB("SINT", [128, T], BF16, bs=512)
        COST = SB("COST", [128, T], BF16, bs=512)
        XB = [SB("XB%d" % i, [128, 8, 512], BF16, dsem=True) for i in range(2)]
        SQ = SB("SQ", [128, 8, 512], BF16)
        LNV = SB("LNV", [128, 512], F32)
        RSTD = [SB("RSTD%d" % i, [128, 512], F32) for i in range(2)]
        RCS = SB("RCS", [128, 4], F32)
        POSI = SB("POSI", [1, 512], I32, dsem=True)
        POSF = SB("POSF", [1, 512], F32)
        RR = [SB("RR%d" % i, [128, 512], F32) for i in range(2)]
        RI = SB("RI", [128, 512], I32)
        RF = SB("RF", [128, 512], F32)
        RMK = SB("RMK", [128, 512], F32)
        TA = SB("TA", [128, 512], BF16)
        TB_ = SB("TB", [128, 512], BF16)
        PTB = [[SB("PT%d_%d" % (m, i), [128, 512], BF16) for i in range(3)] for m in range(2)]
        FT = [SB("FT%d" % i, [128, 512], F32) for i in range(4)]
        OUTS = [SB("OUTS%d" % i, [128, 512], BF16, dsem=True) for i in range(2)]
        DACC = [SB("DACC%d" % i, [128, 512], F32) for i in range(2)]
        TMPP = [[SB("TMPP%d_%d" % (m, i), [128, 512], BF16) for i in range(2)] for m in range(2)]

        groups = [(0, QT), (128, KT)]
        blk = 0
        for b in range(nbatch):
            for n in range(T // 512):
                tok0 = b * T + n * 512
                cols = (n * 512, (n + 1) * 512)
                xb = XB[blk % 2]
                rs = RSTD[blk % 2]
                dma(P, "pool", xb[:], xT_v[:, :, tok0:tok0 + 512], (), xb.r(), xb.dsem)
                act(P, SQ[:], xb[:], AF.Square, xb.r(), SQ.r())
                ss = PSB[0]
                for kc in range(8):
                    mm(P, ss[:, :], ones_bf[:, :], SQ[:, kc, :], kc == 0, kc == 7, SQ.r() + ones_bf.r(), ss.r())
                act(P, LNV[:], ss[:, :], AF.Ln, ss.r() + eps_c.r(), LNV.r(), scale=1.0 / 1024, bias=eps_c[:])
                act(P, rs[:], LNV[:], AF.Exp, LNV.r(), rs.r(), scale=-0.5)
                for gi, (c0, dest) in enumerate(groups):
                    ps = PSB[1 + gi]
                    for kc in range(8):
                        mm(P, ps[:, :], Wb[:, kc, c0:c0 + 128], xb[:, kc, :], kc == 0, kc == 7, Wb.r() + xb.r(), ps.r())
                    tt(P, "dve", dest[:, cols[0]:cols[1]], ps[:, :], rs[:, :], ALU.mult, ps.r() + rs.r(), dest.r(*cols))
                ptm = PSB[3]
                prc = PSB[4]
                for sub in range(4):
                    sc = slice(sub * 128, (sub + 1) * 128)
                    for kc in range(8):
                        mm(P, ptm[:, sc], xb[:, kc, sc], Wb[:, kc, 256:384], kc == 0, kc == 7, Wb.r() + xb.r(), ptm.r())
                    mm(P, prc[:, sub:sub + 1], rs[0:1, sc], ones_f[0:1, 0:1], True, True, rs.r() + ones_f.r(), prc.r())
                cp(P, "dve", RCS[:, 0:4], prc[:, 0:4], prc.r(), RCS.r())
                for sub in range(4):
                    ti = n * 4 + sub
                    sc = slice(sub * 128, (sub + 1) * 128)
                    ts(P, "dve", V[:, ti, :], ptm[:, sc], RCS[:, sub:sub + 1], ALU.mult, ptm.r() + RCS.r(), V.r(ti * 128, ti * 128 + 128))
                blk += 1
            for n in range(T // 512):
                cols = (n * 512, (n + 1) * 512)
                cs = slice(*cols)
                dma(P, "sp", POSI[:], posd[b:b + 1, cs], (), POSI.r(), POSI.dsem)
                cp(P, "dve", POSF[:], POSI[:], POSI.r(), POSF.r())
                pa = PSB[5]
                mm(P, pa[:, :], invf[0:1, :], POSF[0:1, :], True, True, invf.r() + POSF.r(), pa.r())
                for which, dest in ((0, SINT), (1, COST)):
                    rr = RR[which]
                    if which == 0:
                        ts(P, "dve", rr[:], pa[:, :], 1.0 / TWO_PI, ALU.mult, pa.r(), rr.r())
                    else:
                        ts(P, "dve", rr[:], pa[:, :], 1.0 / TWO_PI, ALU.mult, pa.r(), rr.r(), 0.25, ALU.add)
                    cp(P, "dve", RI[:], rr[:], rr.r(), RI.r())
                    cp(P, "act", RF[:], RI[:], RI.r(), RF.r())
                    tt(P, "dve", rr[:], rr[:], RF[:], ALU.subtract, rr.r() + RF.r(), rr.r())
                    ts(P, "dve", RMK[:], rr[:], 0.0, ALU.is_lt, rr.r(), RMK.r())
                    tt(P, "dve", rr[:], rr[:], RMK[:], ALU.add, rr.r() + RMK.r(), rr.r())
                    if which == 0:
                        P.op("act", lambda e, rr=rr, dest=dest, cs=cs: e.activation(out=dest[:, cs], in_=rr[:], func=AF.Sin, scale=pi_c[:, 1:2], bias=pi_c[:, 0:1]),
                             rr.r() + pi_c.r(), dest.r(*cols))
                    else:
                        P.op("act", lambda e, rr=rr, dest=dest, cs=cs: e.activation(out=dest[:, cs], in_=rr[:], func=AF.Sin, scale=-TWO_PI, bias=pic2[:, 0:1]),
                             rr.r() + pic2.r(), dest.r(*cols))
            for n in range(T // 512):
                cols = (n * 512, (n + 1) * 512)
                cs = slice(*cols)
                for (tl, gap, bank) in ((QT, gq_ap, 1), (KT, gk_ap, 2)):
                    ps = PSB[bank]
                    act(P, SQ[:, 0, :], tl[:, cs], AF.Square, tl.r(*cols), SQ.r())
                    mm(P, ps[:, :], BD[:, :], SQ[:, 0, :], True, True, SQ.r() + BD.r(), ps.r())
                    act(P, LNV[:], ps[:, :], AF.Ln, ps.r() + eps_c.r(), LNV.r(), scale=1.0 / 64, bias=eps_c[:])
                    act(P, LNV[:], LNV[:], AF.Exp, LNV.r(), LNV.r(), scale=-0.5)
                    stt(P, tl[:, cs], tl[:, cs], gap, LNV[:], ALU.mult, ALU.mult, tl.r(*cols) + LNV.r() + par.r() + cst.r(), tl.r(*cols))
                    pw = PSB[bank + 2]
                    mm(P, pw[:, :], PM[:, :], tl[:, cs], True, True, PM.r() + tl.r(*cols), pw.r())
                    tt(P, "dve", TA[:], tl[:, cs], COST[:, cs], ALU.mult, tl.r(*cols) + COST.r(*cols), TA.r())
                    tt(P, "dve", TB_[:], pw[:, :], SINT[:, cs], ALU.mult, pw.r() + SINT.r(*cols), TB_.r())
                    tt(P, "dve", tl[:, cs], TA[:], TB_[:], ALU.add, TA.r() + TB_.r(), tl.r(*cols))
            if debug:
                continue
            k = 0
            for j in range(T // 512):
                accs = (PSB[4], PSB[5])
                dens = (PSB[6], PSB[7])
                nt = 4 * j + 4
                items = []
                for i in range(nt):
                    m_ = i - 4 * j
                    c0 = 128 * m_ if m_ >= 0 else 0
                    items.append((i, m_, c0))

                def qk(it, k):
                    i, m_, c0 = it
                    for mp in range(2):
                        sc = PSB[(k % 2) * 2 + mp]
                        rows = slice(mp * 64, (mp + 1) * 64)
                        mm(P, sc[:, c0:512], KT[rows, i * 128:(i + 1) * 128], QT[rows, j * 512 + c0:(j + 1) * 512], True, True,
                           KT.r(i * 128, i * 128 + 128) + QT.r(j * 512, j * 512 + 512), sc.r())

                def rest(it, k):
                    i, m_, c0 = it
                    for mp in range(2):
                        sc = PSB[(k % 2) * 2 + mp]
                        pt = PTB[mp][k % 3]
                        act(P, pt[:, c0:512], sc[:, c0:512], AF.Exp, sc.r(), pt.r())
                        if m_ >= 0:
                            memset(P, "pool", pt[64:128, c0:c0 + 64], 0.0, pt.r())
                        mm(P, accs[mp][:, c0:512], V[:, i, :], pt[:, c0:512], i == 0, i == nt - 1, V.r(i * 128, i * 128 + 128) + pt.r(), accs[mp].r())
                        def flush(src, c0_, mp=mp):
                            if not dinit[mp]:
                                cp(P, "dve", DACC[mp][:, :], src[:, :], src.r(), DACC[mp].r())
                                dinit[mp] = True
                            else:
                                tt(P, "dve", DACC[mp][:, c0_:512], DACC[mp][:, c0_:512], src[:, c0_:512], ALU.add, DACC[mp].r() + src.r(), DACC[mp].r())
                        if m_ < 0 and i < 4 * j - 1 and pend[mp] is None:
                            pend[mp] = pt
                        elif m_ < 0 and pend[mp] is not None:
                            tp = TMPP[mp][npair[mp] % 2]
                            npair[mp] += 1
                            tt(P, "dve", tp[:], pend[mp][:], pt[:], ALU.add, pend[mp].r() + pt.r(), tp.r())
                            pend[mp] = None
                            flush(tp, 0)
                        else:
                            flush(pt, c0)
                LOOK = 1
                dinit = [False, False]
                pend = [None, None]
                npair = [0, 0]
                for idx in range(nt + LOOK):
                    if idx < nt:
                        qk(items[idx], k + idx)
                    if idx >= LOOK:
                        rest(items[idx - LOOK], k + idx - LOOK)
                k += nt
                for mp in range(2):
                    mm(P, dens[mp][:, :], ones_f[:, :], DACC[mp][:, :], True, True, ones_f.r() + DACC[mp].r(), dens[mp].r())
                recip(P, FT[0][:], dens[0][:, :], dens[0].r(), FT[0].r())
                tt(P, "dve", FT[1][:], accs[0][:, :], FT[0][:], ALU.mult, accs[0].r() + FT[0].r(), FT[1].r())
                recip(P, FT[0][:], dens[1][:, :], dens[1].r(), FT[0].r())
                tt(P, "dve", FT[2][:], accs[1][:, :], FT[0][:], ALU.mult, accs[1].r() + FT[0].r(), FT[2].r())
                stt(P, FT[3][:], FT[2][:], nlam_ap, FT[1][:], ALU.mult, ALU.add, FT[2].r() + FT[1].r() + cst.r(), FT[3].r())
                act(P, SQ[:, 0, :], FT[3][:], AF.Square, FT[3].r(), SQ.r())
                pn = PSB[0]
                mm(P, pn[:, :], ones_bf[:, :], SQ[:, 0, :], True, True, SQ.r() + ones_bf.r(), pn.r())
                act(P, LNV[:], pn[:, :], AF.Ln, pn.r() + eps_c.r(), LNV.r(), scale=1.0 / 128, bias=eps_c[:])
                act(P, LNV[:], LNV[:], AF.Exp, LNV.r(), LNV.r(), scale=-0.5)
                outs = OUTS[j % 2]
                stt(P, outs[:], FT[3][:], gs_ap, LNV[:], ALU.mult, ALU.mult, FT[3].r() + LNV.r() + cst.r(), outs.r())
                dma(P, "sp", oT[:, b * T + j * 512: b * T + (j + 1) * 512], outs[:], outs.r(), (), outs.dsem)
        if debug:
            dsd = P.new_dsem("dbg")
            for nm, tl in (("dQ", QT), ("dK", KT), ("dS", SINT), ("dC", COST)):
                dd = nc.dram_tensor(nm, [128, T], BF16, kind="ExternalOutput").ap()
                dma(P, "sp", dd, tl[:], tl.r(), (), dsd)
            dd = nc.dram_tensor("dV", [128, T], BF16, kind="ExternalOutput").ap()
            dma(P, "sp", dd, V[:].rearrange("p a b -> p (a b)"), V.r(), (), dsd)
        P.replay()
    return nc


def _prep_A(inp, xT):
    w = inp["even_w_in"][0]
    gm = np.ascontiguousarray(inp["norm_mix"][0].reshape(8, 128).T)
    maps = []
    for c in range(8):
        sl = lambda base: w[:, base + c * 64: base + (c + 1) * 64]
        wA = np.concatenate([sl(0), sl(512), sl(1536), sl(2048), sl(2560), w[:, 3584 + c:3585 + c], sl(1024), sl(3072)], axis=1)
        pA = np.zeros((64, 8), np.float32)
        pA[:, 0] = inp["hgrn_lb_logits"][0, c * 64:(c + 1) * 64]
        pA[:, 1] = inp["hgrn_lb_logits"][1, c * 64:(c + 1) * 64]
        pA[:, 2] = inp["hgrn_out_norm"][0]
        pA[:, 3] = inp["fox_q_norm"][0]
        pA[:, 4] = inp["fox_k_norm"][0]
        fb = np.empty((128, 1), np.float32)
        fb[:, 0] = inp["fox_f_bias"][0, c]
        maps.append({"xT": xT, "wA": np.ascontiguousarray(wA), "gmix": gm, "pA": pA, "fbias": fb})
    return maps


def _prep_B(inp, layer, hT_full, oT_full, w_out):
    wr = np.concatenate([inp["moe_router_group"][layer]] + [inp["moe_router_expert"][layer, g] for g in range(4)], axis=1)
    wg = np.ascontiguousarray(inp["moe_w_gate"][layer].reshape(32, 1024, 512))
    wu = np.ascontiguousarray(inp["moe_w_up"][layer].reshape(32, 1024, 512))
    wd = np.ascontiguousarray(inp["moe_w_down"][layer].reshape(32, 512, 1024))
    gf = np.ascontiguousarray(inp["norm_ffn"][layer].reshape(8, 128).T)
    wr = np.ascontiguousarray(wr)
    w_out = np.ascontiguousarray(w_out)
    maps = []
    for c in range(8):
        cs = slice(c * NT, (c + 1) * NT)
        maps.append({"hT_in": np.ascontiguousarray(hT_full[:, cs]), "oT_in": np.ascontiguousarray(oT_full[:, cs]),
                     "w_out": w_out, "gffn": gf, "w_r": wr, "w_gate": wg, "w_up": wu, "w_down": wd})
    return maps


def _prep_C(inp, hT_full):
    w = inp["odd_w_in"][0]
    gm = np.ascontiguousarray(inp["norm_mix"][1].reshape(8, 128).T)
    pos = np.ascontiguousarray(inp["positions"].astype(np.int32))
    maps = []
    for c in range(8):
        wC = np.concatenate([w[:, c * 128:(c + 1) * 128], w[:, 1024 + c * 128:1024 + (c + 1) * 128],
                             w[:, 2048 + c * 128:2048 + (c + 1) * 128]], axis=1)
        pC = np.zeros((128, 8), np.float32)
        pC[0:64, 0] = inp["diff_q_norm"][0]; pC[64:128, 0] = inp["diff_q_norm"][0]
        pC[0:64, 1] = inp["diff_k_norm"][0]; pC[64:128, 1] = inp["diff_k_norm"][0]
        pC[0:64, 2] = inp["diff_lambda_q1"][0]; pC[0:64, 3] = inp["diff_lambda_k1"][0]
        pC[0:64, 4] = inp["diff_lambda_q2"][0]; pC[0:64, 5] = inp["diff_lambda_k2"][0]
        pC[:, 6] = inp["diff_subln"][0]
        maps.append({"xT": hT_full, "wC": np.ascontiguousarray(wC), "gmix": gm, "pC": pC, "pos": pos})
    return maps


def _run(nc, maps):
    res = run_bass_kernel_spmd(nc, maps, core_ids=list(range(8)))
    return res.results


def kernel(**inputs):
    inp = {k: np.asarray(v) for k, v in inputs.items()}
    x = inp["x"].astype(np.float32, copy=False).reshape(-1, 1024)
    xT = np.ascontiguousarray(x.T)
    rA = _run(build_A(), _prep_A(inp, xT))
    oT0 = np.empty((1024, TOK), ml_dtypes.bfloat16)
    for c in range(8):
        o = np.asarray(rA[c]["oT"])
        oT0[c * 64:(c + 1) * 64] = o[0:64]
        oT0[512 + c * 64:512 + (c + 1) * 64] = o[64:128]
    rB = _run(build_B(), _prep_B(inp, 0, xT, oT0, inp["even_w_out"][0]))
    h1T = np.ascontiguousarray(np.concatenate([np.asarray(r["hT_out"]) for r in rB], axis=1))
    rC = _run(build_C(), _prep_C(inp, h1T))
    oT1 = np.ascontiguousarray(np.concatenate([np.asarray(r["oT"]) for r in rC], axis=0))
    rD = _run(build_B(), _prep_B(inp, 1, h1T, oT1, inp["odd_w_out"][0]))
    outT = np.concatenate([np.asarray(r["hT_out"]) for r in rD], axis=1)
    return np.ascontiguousarray(outT.T).reshape(2, 8192, 1024).astype(np.float32, copy=False)
```

```python
import ml_dtypes
import numpy as np
from contextlib import ExitStack
import concourse.bass as bass
import concourse.mybir as mybir
from concourse.bass_utils import run_bass_kernel_spmd

F32 = mybir.dt.float32
BF16 = mybir.dt.bfloat16
I32 = mybir.dt.int32
AF = mybir.ActivationFunctionType
ALU = mybir.AluOpType
AX = mybir.AxisListType

ENGS = ("pe", "act", "dve", "pool", "sp")


class Res:
    __slots__ = ("name", "last_w", "readers", "dsem")

    def __init__(self, name):
        self.name = name
        self.last_w = None
        self.readers = []
        self.dsem = None


class Op:
    __slots__ = ("eng", "fn", "deps", "is_dma", "sem", "semval", "signal", "count", "dma_waits", "idx")

    def __init__(self, eng, fn):
        self.eng = eng
        self.fn = fn
        self.deps = []
        self.is_dma = False
        self.sem = None
        self.semval = 0
        self.signal = False
        self.count = 0
        self.dma_waits = []
        self.idx = 0


class Prog:
    def __init__(self, nc):
        self.nc = nc
        self.ops = {e: [] for e in ENGS}
        self.all_res_reset = True
        self.esem = {e: nc.alloc_semaphore("es_" + e) for e in ENGS}
        self.dsems = []
        self.dma_cum = {}
        self.nops = 0
        self.ecount = {e: 0 for e in ENGS}
        self.waited = {e: {} for e in ENGS}

    def res(self, name, n=1):
        return [Res("%s.%d" % (name, i)) for i in range(n)]

    def new_dsem(self, name):
        s = self.nc.alloc_semaphore("ds_" + name)
        self.dma_cum[s] = 0
        return s

    def op(self, eng, fn, reads=(), writes=(), dma_sem=None):
        o = Op(eng, fn)
        o.idx = self.nops
        self.nops += 1
        deps = {}
        dma_waits = {}

        def add_dep(p):
            if p is None:
                return
            if p.is_dma:
                dma_waits[p.sem] = self.dma_cum[p.sem]
            else:
                deps[id(p)] = p

        for r in reads:
            add_dep(r.last_w)
        for r in writes:
            add_dep(r.last_w)
            for q in r.readers:
                add_dep(q)
        raw = set()
        for r in reads:
            if r.last_w is not None and not r.last_w.is_dma:
                raw.add(id(r.last_w))
        for k, p in list(deps.items()):
            if p.eng == eng and eng == "pe" and dma_sem is None:
                del deps[k]
        o.deps = list(deps.values())
        for p in o.deps:
            p.signal = True
        o.dma_waits = list(dma_waits.items())
        if dma_sem is not None:
            o.is_dma = True
            o.sem = dma_sem
            self.dma_cum[dma_sem] += 16
            o.semval = self.dma_cum[dma_sem]
        for r in reads:
            r.readers.append(o)
        for r in writes:
            r.last_w = o
            r.readers = []
        self.ops[eng].append(o)
        return o

    def finish_wait(self, eng="sp"):
        waits = [(s, v) for s, v in self.dma_cum.items() if v > 0]
        o = Op(eng, None)
        o.dma_waits = waits
        self.ops[eng].append(o)

    def replay(self):
        nc = self.nc
        for e in ENGS:
            last = None
            for o in self.ops[e]:
                if o.fn is not None and not o.is_dma:
                    last = o
            if last is not None:
                last.signal = True
            c = self.ecount[e]
            for o in self.ops[e]:
                if o.signal and not o.is_dma:
                    c += 1
                    o.count = c
            self.ecount[e] = c
        final = {self.esem[e]: self.ecount[e] for e in ENGS if self.ecount[e] > 0}
        for s_, v_ in self.dma_cum.items():
            if v_ > 0:
                final[s_] = v_
        handles = {"pe": "tensor", "act": "scalar", "dve": "vector", "pool": "gpsimd", "sp": "sync"}
        with nc.Block() as block:
            for e in ENGS:
                ops = self.ops[e]
                esem = self.esem
                my = esem[e]

                def body(engh, ops=ops, e=e, my=my):
                    waited = self.waited[e]
                    for o in ops:
                        need = {}
                        for p in o.deps:
                            s = esem[p.eng]
                            if need.get(s, 0) < p.count:
                                need[s] = p.count
                        for s, v in o.dma_waits:
                            if need.get(s, 0) < v:
                                need[s] = v
                        for s, v in need.items():
                            if waited.get(s, 0) < v:
                                engh.wait_ge(s, v)
                                waited[s] = v
                        if o.fn is None:
                            continue
                        ins = o.fn(engh)
                        if o.is_dma:
                            ins.then_inc(o.sem, 16)
                        elif o.signal:
                            ins.then_inc(my, 1)
                    for s, v in final.items():
                        if s is my:
                            continue
                        if waited.get(s, 0) < v:
                            engh.wait_ge(s, v)
                            waited[s] = v

                getattr(block, handles[e])(body)
        self.ops = {e: [] for e in ENGS}
        self.all_res_reset = True


from math import prod
import os
HG = int(os.environ.get('HG_STAGE', '9'))


class TT:
    def __init__(self, P, st, name, shape, dt, bs=None, psum=False, dsem=False):
        nc = P.nc
        self.h = st.enter_context(nc.psum_tensor(name, shape, dt) if psum else nc.sbuf_tensor(name, shape, dt))
        self.F = prod(shape[1:])
        self.bs = bs or self.F
        self.res = P.res(name, (self.F + self.bs - 1) // self.bs)
        self.dsem = P.new_dsem(name) if dsem else None

    def r(self, lo=0, hi=None):
        hi = self.F if hi is None else hi
        return self.res[lo // self.bs:(hi - 1) // self.bs + 1]

    def __getitem__(self, k):
        return self.h[k]


def mm(P, out, lhsT, rhs, start, stop, rd, wr):
    return P.op("pe", lambda e: e.matmul(out, lhsT=lhsT, rhs=rhs, start=start, stop=stop), rd, wr)


def act(P, out, in_, func, rd, wr, scale=1.0, bias=None):
    if bias is None:
        return P.op("act", lambda e: e.activation(out=out, in_=in_, func=func, scale=scale), rd, wr)
    return P.op("act", lambda e: e.activation(out=out, in_=in_, func=func, scale=scale, bias=bias), rd, wr)


def tt(P, eng, out, in0, in1, op, rd, wr):
    return P.op(eng, lambda e: e.tensor_tensor(out=out, in0=in0, in1=in1, op=op), rd, wr)


def ts(P, eng, out, in0, s1, op0, rd, wr, s2=None, op1=None):
    if op1 is None:
        return P.op(eng, lambda e: e.tensor_scalar(out=out, in0=in0, scalar1=s1, scalar2=None, op0=op0), rd, wr)
    return P.op(eng, lambda e: e.tensor_scalar(out=out, in0=in0, scalar1=s1, scalar2=s2, op0=op0, op1=op1), rd, wr)


def stt(P, out, in0, scalar, in1, op0, op1, rd, wr):
    return P.op("dve", lambda e: e.scalar_tensor_tensor(out=out, in0=in0, scalar=scalar, in1=in1, op0=op0, op1=op1), rd, wr)


def cp(P, eng, out, in_, rd, wr):
    if eng == "act":
        return P.op("act", lambda e: e.copy(out=out, in_=in_), rd, wr)
    return P.op(eng, lambda e: e.tensor_copy(out=out, in_=in_), rd, wr)


def recip(P, out, in_, rd, wr):
    return P.op("dve", lambda e: e.reciprocal(out=out, in_=in_), rd, wr)


def dma(P, q, out, in_, rd, wr, sem):
    return P.op(q, lambda e: e.dma_start(out=out, in_=in_), rd, wr, dma_sem=sem)


def memset(P, eng, ap, val, wr):
    return P.op(eng, lambda e: e.memset(ap, val), (), wr)


T = int(os.environ.get('KT', 8192))
TOK = 2 * T
NCOL_A = 449
EPS = 1e-6


def build_A(nbatch=2, do_hgrn=True, do_fox=True, debug=False):
    nc = bass.Bass("TRN2", target_bir_lowering=False)
    xT = nc.dram_tensor("xT", [1024, TOK], F32, kind="ExternalInput").ap()
    wA = nc.dram_tensor("wA", [1024, NCOL_A], F32, kind="ExternalInput").ap()
    gmix = nc.dram_tensor("gmix", [128, 8], F32, kind="ExternalInput").ap()
    pA = nc.dram_tensor("pA", [64, 8], F32, kind="ExternalInput").ap()
    fbias = nc.dram_tensor("fbias", [128, 1], F32, kind="ExternalInput").ap()
    oT = nc.dram_tensor("oT", [128, TOK], BF16, kind="ExternalOutput").ap()
    P = Prog(nc)
    xT_v = xT.rearrange("(kc p) t -> p kc t", p=128)
    with ExitStack() as st:
        def SB(name, shape, dt, bs=None, dsem=False):
            return TT(P, st, name, shape, dt, bs=bs, dsem=dsem)

        ones_bf = SB("ones_bf", [128, 128], BF16)
        ones_f = SB("ones_f", [128, 64], F32)
        eps_c = SB("eps_c", [128, 1], F32)
        one_c = SB("one_c", [128, 1], F32)
        ident = SB("ident", [128, 128], BF16)
        M2 = SB("M2", [128, 128], BF16)
        TBH = 256
        rmask = SB("rmask", [64, TBH], F32)
        par = SB("par", [64, 8], F32, dsem=True)
        gm = SB("gm", [128, 8], F32, dsem=True)
        nfb = SB("nfb", [128, 1], F32, dsem=True)
        lbc = SB("lbc", [64, 4], F32)
        Wf = None
        Wb = SB("Wb", [128, 8, NCOL_A], BF16, dsem=True)

        memset(P, "pool", ones_bf[:], 1.0, ones_bf.r())
        memset(P, "pool", ones_f[:], 1.0, ones_f.r())
        memset(P, "pool", eps_c[:], EPS, eps_c.r())
        memset(P, "pool", one_c[:], 1.0, one_c.r())
        memset(P, "pool", ident[:], 1.0, ident.r())
        P.op("pool", lambda e: e.affine_select(out=ident[:], in_=ident[:], pattern=[[-1, 128]], compare_op=ALU.is_equal,
                                               fill=0.0, base=0, channel_multiplier=1), ident.r(), ident.r())
        memset(P, "pool", M2[:], 1.0, M2.r())
        P.op("pool", lambda e: e.affine_select(out=M2[:], in_=M2[:], pattern=[[1, 128]], compare_op=ALU.is_ge,
                                               fill=0.0, base=0, channel_multiplier=-1), M2.r(), M2.r())
        memset(P, "pool", M2[0:64, 64:128], 0.0, M2.r())
        memset(P, "pool", rmask[:], 1.0, rmask.r())
        memset(P, "pool", rmask[:].rearrange("p (c l) -> p c l", l=64)[:, :, 0:1], 0.0, rmask.r())

        dma(P, "sp", par[:], pA, (), par.r(), par.dsem)
        dma(P, "sp", gm[:], gmix, (), gm.r(), gm.dsem)
        dma(P, "sp", nfb[:], fbias, (), nfb.r(), nfb.dsem)
        dma(P, "pool", Wb[:], wA.rearrange("(kc p) n -> p kc n", p=128), (), Wb.r(), Wb.dsem)
        for kc in range(8):
            ts(P, "dve", Wb[:, kc, :], Wb[:, kc, :], gm[:, kc:kc + 1], ALU.mult, Wb.r() + gm.r(), Wb.r())
        ts(P, "dve", nfb[:], nfb[:], -1.0, ALU.mult, nfb.r(), nfb.r())
        tt(P, "dve", lbc[:, 3:4], par[:, 1:2], par[:, 0:1], ALU.subtract, par.r(), lbc.r())
        act(P, lbc[:, 3:4], lbc[:, 3:4], AF.Exp, lbc.r(), lbc.r())
        ts(P, "dve", lbc[:, 3:4], lbc[:, 3:4], 1.0, ALU.add, lbc.r(), lbc.r())
        recip(P, lbc[:, 0:1], lbc[:, 3:4], lbc.r(), lbc.r())
        ts(P, "dve", lbc[:, 1:2], lbc[:, 0:1], -1.0, ALU.mult, lbc.r(), lbc.r(), 1.0, ALU.add)
        ts(P, "dve", lbc[:, 2:3], par[:, 3:4], 0.125, ALU.mult, par.r(), lbc.r())
        lb_ap, oml_ap, gq8_ap = lbc[:, 0:1], lbc[:, 1:2], lbc[:, 2:3]
        on_ap, gk_ap = par[:, 2:3], par[:, 4:5]

        Q = SB("Q", [64, T], BF16, bs=512)
        Fh = SB("Fh", [64, T], BF16, bs=512)
        G = SB("G", [64, T], BF16, bs=512)
        BQ = SB("BQ", [70, T], BF16, bs=512, dsem=True)
        BK = SB("BK", [70, T], BF16, bs=128, dsem=True)
        VAB = SB("VAB", [128, T // 128, 129], BF16, bs=129)
        XB = [SB("XB%d" % i, [128, 8, 512], BF16, dsem=True) for i in range(2)]
        SQ = SB("SQ", [128, 8, 512], BF16)
        LNV = SB("LNV", [128, 512], F32)
        RSTD = [SB("RSTD%d" % i, [128, 512], F32) for i in range(2)]
        ZF = SB("ZF", [128, 512], F32)
        CC = [SB("CC%d" % i, [128, 512], F32) for i in range(2)]
        R1 = SB("R1", [128, 512], F32)
        AUG = [SB("AUG0", [128, 6, 512], BF16)] * 2
        RCS = SB("RCS", [128, 4], F32)
        PSB = [TT(P, st, "ps%d" % i, [128, 512], F32, psum=True) for i in range(8)]

        HT2 = [[SB("HT%d_%d" % (k_, i), [64, TBH], F32) for i in range(5)] for k_ in range(2)]
        KHT2 = [SB("KHT%d" % k_, [64, TBH], BF16) for k_ in range(2)]
        ELC0 = SB("ELC0", [64, T // 64], F32)

        KH = SB("KH", [128, T // 128, 64], BF16, bs=64)
        ELC = SB("ELC", [64, T // 64], F32)
        SBF = SB("SBF", [64, T], BF16)
        Z64 = SB("Z64", [64, 64], BF16)
        memset(P, "pool", Z64[:], 0.0, Z64.r())
        ATS = [SB("ATS%d" % i, [128, 512], BF16) for i in range(2)]
        OAS = [SB("OAS%d" % i, [64, 512], BF16, dsem=True) for i in range(2)]
        PTB = [SB("PTB%d" % i, [128, 512], BF16) for i in range(4)]
        ACCS = SB("ACCS", [65, 512], F32)
        RDEN = SB("RDEN", [64, 512], F32)
        OBS = [SB("OBS%d" % i, [64, 512], BF16, dsem=True) for i in range(2)]
        RONE = SB("RONE", [128, 512], BF16)
        memset(P, "pool", RONE[:], 1.0, RONE.r())
        memset(P, "pool", VAB[:, :, 128:129], 1.0, VAB.r())
        memset(P, "pool", BQ[64:70, :], 1.0, BQ.r())
        memset(P, "pool", BK[64:70, :], 1.0, BK.r())

        groups = [(0, 64, Q), (64, 64, Fh), (128, 64, G), (192, 64, BQ), (256, 65, BK)]
        blk = 0
        for b in range(nbatch):
            for n in range(T // 512):
                tok0 = b * T + n * 512
                cols = (n * 512, (n + 1) * 512)
                xb = XB[blk % 2]
                rs = RSTD[blk % 2]
                dma(P, "pool", xb[:], xT_v[:, :, tok0:tok0 + 512], (), xb.r(), xb.dsem)
                act(P, SQ[:], xb[:], AF.Square, xb.r(), SQ.r())
                ss = PSB[0]
                for kc in range(8):
                    mm(P, ss[:, :], ones_bf[:, :], SQ[:, kc, :], kc == 0, kc == 7, SQ.r() + ones_bf.r(), ss.r())
                act(P, LNV[:], ss[:, :], AF.Ln, ss.r() + eps_c.r(), LNV.r(), scale=1.0 / 1024, bias=eps_c[:])
                act(P, rs[:], LNV[:], AF.Exp, LNV.r(), rs.r(), scale=-0.5)
                for gi, (c0, M, dest) in enumerate(groups):
                    ps = PSB[1 + gi]
                    for kc in range(8):
                        mm(P, ps[0:M, :], Wb[:, kc, c0:c0 + M], xb[:, kc, :], kc == 0, kc == 7, Wb.r() + xb.r(), ps.r())
                    tt(P, "dve", dest[0:64, cols[0]:cols[1]], ps[0:64, :], rs[0:64, :], ALU.mult,
                       ps.r() + rs.r(), dest.r(*cols))
                    if M == 65:
                        cc = CC[blk % 2]
                        ccp = CC[(blk + 1) % 2]
                        aug = AUG[blk % 2]
                        r64 = slice(64, 65)
                        tt(P, "dve", ZF[r64, :], ps[r64, :], rs[r64, :], ALU.mult, ps.r() + rs.r(), ZF.r())
                        act(P, ZF[r64, :], ZF[r64, :], AF.Exp, ZF.r() + nfb.r(), ZF.r(), scale=-1.0, bias=nfb[r64, :])
                        act(P, ZF[r64, :], ZF[r64, :], AF.Ln, ZF.r() + one_c.r(), ZF.r(), scale=1.0, bias=one_c[r64, :])
                        init = 0.0 if n == 0 else ccp[r64, 511:512]
                        P.op("dve", lambda e, cc=cc, init=init: e.tensor_tensor_scan(
                            out=cc[64:65, :], data0=RONE[64:65, :],
                            data1=ZF[64:65, :], initial=init, op0=ALU.mult, op1=ALU.add),
                            ZF.r() + ccp.r() + RONE.r(), cc.r())
                        cp(P, "dve", aug[r64, 0, :], cc[r64, :], cc.r(), aug.r())
                        tt(P, "dve", R1[r64, :], cc[r64, :], aug[r64, 0, :], ALU.subtract, cc.r() + aug.r(), R1.r())
                        cp(P, "dve", aug[r64, 1, :], R1[r64, :], R1.r(), aug.r())
                        tt(P, "dve", R1[r64, :], R1[r64, :], aug[r64, 1, :], ALU.subtract, R1.r() + aug.r(), R1.r())
                        cp(P, "dve", aug[r64, 2, :], R1[r64, :], R1.r(), aug.r())
                        ts(P, "dve", aug[r64, 3:6, :], aug[r64, 0:3, :], -1.0, ALU.mult, aug.r(), aug.r())
                        for i in range(3):
                            dma(P, "sp", BK[67 + i:68 + i, cols[0]:cols[1]], aug[r64, i, :], aug.r(), BK.r(*cols), BK.dsem)
                            dma(P, "sp", BQ[64 + i:65 + i, cols[0]:cols[1]], aug[r64, 3 + i, :], aug.r(), BQ.r(*cols), BQ.dsem)
                ptm = PSB[6]
                prc = PSB[7]
                for sub in range(4):
                    sc = slice(sub * 128, (sub + 1) * 128)
                    for kc in range(8):
                        mm(P, ptm[:, sc], xb[:, kc, sc], Wb[:, kc, 321:449], kc == 0, kc == 7, Wb.r() + xb.r(), ptm.r())
                    mm(P, prc[:, sub:sub + 1], rs[0:1, sc], ones_f[0:1, 0:1], True, True, rs.r() + ones_f.r(), prc.r())
                cp(P, "dve", RCS[:, 0:4], prc[:, 0:4], prc.r(), RCS.r())
                for sub in range(4):
                    ti = n * 4 + sub
                    sc = slice(sub * 128, (sub + 1) * 128)
                    ts(P, "dve", VAB[:, ti, 0:128], ptm[:, sc], RCS[:, sub:sub + 1], ALU.mult,
                       ptm.r() + RCS.r(), VAB.r(ti * 129, ti * 129 + 129))
                blk += 1
            if do_fox:
                for n in range(T // 512):
                    cs = slice(n * 512, (n + 1) * 512)
                    for (tl, gap, bank) in ((BQ, gq8_ap, 1), (BK, gk_ap, 2)):
                        ps = PSB[bank]
                        act(P, SQ[0:64, 0, :], tl[0:64, cs], AF.Square, tl.r(n * 512, n * 512 + 512), SQ.r())
                        mm(P, ps[0:64, :], ones_bf[0:64, 0:64], SQ[0:64, 0, :], True, True, SQ.r() + ones_bf.r(), ps.r())
                        act(P, LNV[0:64, :], ps[0:64, :], AF.Ln, ps.r() + eps_c.r(), LNV.r(), scale=1.0 / 64, bias=eps_c[0:64, :])
                        act(P, LNV[0:64, :], LNV[0:64, :], AF.Exp, LNV.r(), LNV.r(), scale=-0.5)
                        stt(P, tl[0:64, cs], tl[0:64, cs], gap, LNV[0:64, :], ALU.mult, ALU.mult,
                            tl.r(n * 512, n * 512 + 512) + LNV.r() + par.r() + lbc.r(), tl.r(n * 512, n * 512 + 512))
            if do_hgrn:
                def hg_elem(par_):
                    for sbk in range(par_, T // TBH, 2):
                        c0, c1 = sbk * TBH, (sbk + 1) * TBH
                        cs = slice(c0, c1)
                        t1, t2, t3, t4, t5 = HT2[sbk % 2]
                        kht = KHT2[sbk % 2]
                        act(P, t1[:], Fh[0:64, cs], AF.Exp, Fh.r(c0, c1), t1.r(), scale=-1.0)
                        yield
                        act(P, t1[:], t1[:], AF.Identity, t1.r() + one_c.r(), t1.r(), scale=1.0, bias=one_c[0:64, :])
                        yield
                        recip(P, t1[:], t1[:], t1.r(), t1.r())
                        yield
                        ts(P, "dve", t1[:], t1[:], oml_ap, ALU.mult, t1.r() + lbc.r(), t1.r(), lb_ap, ALU.add)
                        yield
                        act(P, t2[:], t1[:], AF.Ln, t1.r(), t2.r())
                        yield
                        P.op("dve", lambda e, t3=t3, t2=t2: e.tensor_tensor_scan(out=t3[:], data0=rmask[:], data1=t2[:], initial=0.0,
                                                                                 op0=ALU.mult, op1=ALU.add), t2.r() + rmask.r(), t3.r())
                        yield
                        act(P, t1[:], t1[:], AF.Identity, t1.r() + one_c.r(), t1.r(), scale=-1.0, bias=one_c[0:64, :])
                        yield
                        act(P, t4[:], t3[:], AF.Exp, t3.r(), t4.r())
                        yield
                        act(P, t5[:], Q[0:64, cs], AF.Exp, Q.r(c0, c1), t5.r(), scale=-1.0)
                        yield
                        act(P, t5[:], t5[:], AF.Identity, t5.r() + one_c.r(), t5.r(), scale=1.0, bias=one_c[0:64, :])
                        yield
                        recip(P, t5[:], t5[:], t5.r(), t5.r())
                        yield
                        tt(P, "dve", t5[:], Q[0:64, cs], t5[:], ALU.mult, Q.r(c0, c1) + t5.r(), t5.r())
                        yield
                        tt(P, "dve", Q[0:64, cs], t5[:], t4[:], ALU.mult, t5.r() + t4.r(), Q.r(c0, c1))
                        yield
                        act(P, t4[:], t3[:], AF.Exp, t3.r(), t4.r(), scale=-1.0)
                        yield
                        tt(P, "dve", Fh[0:64, cs], t1[:], t4[:], ALU.mult, t1.r() + t4.r(), Fh.r(c0, c1))
                        yield
                        nch = TBH // 64
                        ch0 = sbk * nch
                        act(P, ELC[0:64, ch0:ch0 + nch], t3[:].rearrange("p (c l) -> p c l", l=64)[:, :, 63],
                            AF.Exp, t3.r(), ELC.r())
                        yield
                        tt(P, "dve", kht[:].rearrange("p (c l) -> p c l", l=64), Fh[0:64, cs].rearrange("p (c l) -> p c l", l=64),
                           ELC[0:64, ch0:ch0 + nch].unsqueeze(2).broadcast_to([64, nch, 64]), ALU.mult,
                           Fh.r(c0, c1) + ELC.r(), kht.r())
                        yield
                        pst = PSB[7] if sbk % 2 == 0 else PSB[0]
                        pstb = pst.h.bitcast(BF16)
                        ntl = TBH // 128
                        for j in range(ntl):
                            P.op("pe", lambda e, j=j, pstb=pstb, kht=kht: e.transpose(out=pstb[:, j * 64:(j + 1) * 64], in_=kht[0:64, j * 128:(j + 1) * 128],
                                                                  identity=ident[0:64, 0:64]), kht.r() + ident.r(), pst.r())
                            yield
                        ti0 = sbk * ntl
                        cp(P, "act", KH[:, ti0:ti0 + ntl, :], pstb[:, 0:ntl * 64].rearrange("p (a b) -> p a b", b=64),
                           pst.r(), KH.r(ti0 * 64, (ti0 + ntl) * 64))
                        yield
                        act(P, t2[:], G[0:64, cs], AF.Exp, G.r(c0, c1), t2.r(), scale=-1.0)
                        yield
                        act(P, t2[:], t2[:], AF.Identity, t2.r() + one_c.r(), t2.r(), scale=1.0, bias=one_c[0:64, :])
                        yield
                        recip(P, t2[:], t2[:], t2.r(), t2.r())
                        yield
                        tt(P, "dve", G[0:64, cs], G[0:64, cs], t2[:], ALU.mult, G.r(c0, c1) + t2.r(), G.r(c0, c1))
                        yield

                hg_gens = [hg_elem(0), hg_elem(1)]
            if do_fox:
                k = 0
                for j in range(T // 512):
                    acc = PSB[4 + j % 2]
                    nt = 4 * j + 4
                    items = []
                    for i in range(nt):
                        m = i - 4 * j
                        c0 = 128 * m if m >= 0 else 0
                        items.append((i, m, c0))
                    def qk(it, k):
                        i, m, c0 = it
                        sc = PSB[1 + k % 3]
                        mm(P, sc[:, c0:512], BK[0:70, i * 128:(i + 1) * 128], BQ[0:70, j * 512 + c0:(j + 1) * 512], True, True,
                           BK.r(i * 128, i * 128 + 128) + BQ.r(j * 512, j * 512 + 512), sc.r())
                    def rest(it, k):
                        i, m, c0 = it
                        sc = PSB[1 + k % 3]
                        pt = PTB[k % 4]
                        act(P, pt[:, c0:512], sc[:, c0:512], AF.Exp, sc.r(), pt.r())
                        if m >= 0:
                            P.op("pool", lambda e, pt=pt, c0=c0: e.affine_select(out=pt[:, c0:c0 + 128], in_=pt[:, c0:c0 + 128], pattern=[[1, 128]],
                                 compare_op=ALU.is_ge, fill=0.0, base=0, channel_multiplier=-1), pt.r(), pt.r())
                        mm(P, acc[0:65, c0:512], VAB[:, i, 64:129], pt[:, c0:512], i == 0, i == nt - 1,
                           VAB.r(i * 129, i * 129 + 129) + pt.r(), acc.r())
                    LOOK = 2
                    for idx in range(nt + LOOK):
                        if idx < nt:
                            qk(items[idx], k + idx)
                        if idx >= LOOK:
                            rest(items[idx - LOOK], k + idx - LOOK)
                            if do_hgrn and os.environ.get("HG_NOILV") is None:
                                for g_ in hg_gens:
                                    next(g_, None)
                    k += nt
                    cp(P, "dve", ACCS[0:65, :], acc[0:65, :], acc.r(), ACCS.r())
                    pden = PSB[6]
                    mm(P, pden[0:64, :], ones_f[64:65, 0:64], ACCS[64:65, :], True, True, ones_f.r() + ACCS.r(), pden.r())
                    recip(P, RDEN[:], pden[0:64, :], pden.r(), RDEN.r())
                    obs = OBS[j % 2]
                    tt(P, "pool", obs[:], ACCS[0:64, :], RDEN[:], ALU.mult, ACCS.r() + RDEN.r(), obs.r())
                    dma(P, "sp", oT[64:128, b * T + j * 512: b * T + (j + 1) * 512], obs[:], obs.r(), (), obs.dsem)
            if do_hgrn:
                for g_ in hg_gens:
                    for _ in g_:
                        pass
                DSV = BQ
                NC_ = T // 64
                for g4 in range(T // 512):
                    for c in range(8 * g4, 8 * g4 + 8):
                        ti, hh = c // 2, c % 2
                        pds = PSB[4 + (g4 % 2) * 2 + hh]
                        slot = (c % 8) * 64
                        hs = slice(hh * 64, (hh + 1) * 64)
                        mm(P, pds[0:64, slot:slot + 64], KH[hs, ti, :], VAB[hs, ti, 0:64], True, True,
                           KH.r(ti * 64, ti * 64 + 64) + VAB.r(ti * 129, ti * 129 + 129), pds.r())
                    for hh in range(2):
                        pds = PSB[4 + (g4 % 2) * 2 + hh]
                        src = pds[0:64, :].rearrange("p (a b) -> p a b", b=128)[:, :, hh * 64:(hh + 1) * 64]
                        dst = DSV[0:64, :].rearrange("p (v c) -> p c v", c=NC_)[:, 8 * g4 + hh:8 * g4 + 8:2, :]
                        cp(P, "dve" if hh == 0 else "act", dst, src, pds.r(), DSV.r())
                cp(P, "pool", ELC0[:], ELC[:], ELC.r(), ELC0.r())
                memset(P, "pool", ELC0[:, 0:1], 0.0, ELC0.r())
                D0 = BK
                cp(P, "act", D0[0:64, :].rearrange("p (v c) -> p v c", c=NC_), ELC0[:, :].unsqueeze(1).broadcast_to([64, 64, NC_]),
                   ELC0.r(), D0.r())
                P.op("dve", lambda e: e.tensor_tensor_scan(out=SBF[:, :], data0=D0[0:64, :], data1=DSV[0:64, :],
                                                           initial=0.0, op0=ALU.mult, op1=ALU.add), D0.r() + DSV.r(), SBF.r())
                for g4 in range(T // 512):
                    pat = PSB[6 + g4 % 2]
                    for j in range(4):
                        ti = g4 * 4 + j
                        tcs = slice(ti * 128, (ti + 1) * 128)
                        mm(P, pat[:, j * 128:(j + 1) * 128], Fh[0:64, tcs], Q[0:64, tcs], True, True,
                           Fh.r(ti * 128, ti * 128 + 128) + Q.r(ti * 128, ti * 128 + 128), pat.r())
                    ats = ATS[g4 % 2]
                    tt(P, "dve", ats[:].rearrange("p (a b) -> p a b", b=128), pat[:, :].rearrange("p (a b) -> p a b", b=128),
                       M2[:].unsqueeze(1).broadcast_to([128, 4, 128]), ALU.mult, pat.r() + M2.r(), ats.r())
                    po = PSB[1 + g4 % 2]
                    for jj in range(4):
                        tj = g4 * 4 + jj
                        for hh in range(2):
                            c = 2 * tj + hh
                            ccs = slice(c * 64, (c + 1) * 64)
                            oc = slice(jj * 128 + hh * 64, jj * 128 + hh * 64 + 64)
                            mm(P, po[0:64, oc], VAB[:, tj, 0:64], ats[:, oc], True, False,
                               VAB.r(tj * 129, tj * 129 + 129) + ats.r(), po.r())
                            st_ap = Z64[:, :] if c == 0 else SBF[:, :].rearrange("p (v c) -> p v c", c=NC_)[:, :, c - 1]
                            mm(P, po[0:64, oc], st_ap, Q[0:64, ccs], False, True,
                               SBF.r() + Z64.r() + Q.r(c * 64, c * 64 + 64), po.r())
                    gcs = slice(g4 * 512, (g4 + 1) * 512)
                    pn = PSB[3]
                    sq64 = SQ
                    lnv = RSTD[g4 % 2]
                    oaf = (ZF, R1)[g4 % 2]
                    act(P, sq64[0:64, g4 % 2, :], po[0:64, :], AF.Square, po.r(), sq64.r())
                    mm(P, pn[0:64, :], ones_bf[0:64, 0:64], sq64[0:64, g4 % 2, :], True, True, sq64.r() + ones_bf.r(), pn.r())
                    act(P, lnv[0:64, :], pn[0:64, :], AF.Ln, pn.r() + eps_c.r(), lnv.r(), scale=1.0 / 64, bias=eps_c[0:64, :])
                    act(P, lnv[0:64, :], lnv[0:64, :], AF.Exp, lnv.r(), lnv.r(), scale=-0.5)
                    stt(P, oaf[0:64, :], po[0:64, :], on_ap, lnv[0:64, :], ALU.mult, ALU.mult, po.r() + lnv.r() + par.r(), oaf.r())
                    oas = OAS[g4 % 2]
                    tt(P, "dve", oas[:], oaf[0:64, :], G[0:64, gcs], ALU.mult, oaf.r() + G.r(g4 * 512, g4 * 512 + 512), oas.r())
                    dma(P, "sp", oT[0:64, b * T + g4 * 512: b * T + (g4 + 1) * 512], oas[:], oas.r(), (), oas.dsem)
        if os.environ.get('HG_DBG'):
            dsd2 = P.new_dsem("dbg2")
            for nm, tl, shp, dt_ in (("dKH", KH, [128, (T // 128) * 64], BF16), ("dKt", Fh, [64, T], BF16), ("dQt", Q, [64, T], BF16), ("dELC", ELC, [64, T // 64], F32), ("dSBF", SBF, [64, T], BF16)):
                dd = nc.dram_tensor(nm, shp, dt_, kind="ExternalOutput").ap()
                src = tl[:] if len(tl.h.shape) == 2 else tl[:].rearrange("p a b -> p (a b)")
                dma(P, "sp", dd, src, tl.r(), (), dsd2)
        if debug:
            dsd = P.new_dsem("dbg")
            for nm, tl, shp in (("dQ", Q, [64, T]), ("dF", Fh, [64, T]), ("dG", G, [64, T]), ("dBQ", BQ, [70, T]), ("dBK", BK, [70, T]), ("dV", VAB, [128, (T // 128) * 129])):
                dd = nc.dram_tensor(nm, shp, BF16, kind="ExternalOutput").ap()
                src = tl[:] if nm != "dV" else tl[:].rearrange("p a b -> p (a b)")
                dma(P, "sp", dd, src, tl.r(), (), dsd)
        P.replay()
    return nc


NT = 2048


def build_B():
    nc = bass.Bass("TRN2", target_bir_lowering=False)
    hT_in = nc.dram_tensor("hT_in", [1024, NT], F32, kind="ExternalInput").ap()
    oT_in = nc.dram_tensor("oT_in", [1024, NT], BF16, kind="ExternalInput").ap()
    w_out = nc.dram_tensor("w_out", [1024, 1024], F32, kind="ExternalInput").ap()
    gffn = nc.dram_tensor("gffn", [128, 8], F32, kind="ExternalInput").ap()
    w_r = nc.dram_tensor("w_r", [1024, 36], F32, kind="ExternalInput").ap()
    w_gate = nc.dram_tensor("w_gate", [32, 1024, 512], F32, kind="ExternalInput").ap()
    w_up = nc.dram_tensor("w_up", [32, 1024, 512], F32, kind="ExternalInput").ap()
    w_down = nc.dram_tensor("w_down", [32, 512, 1024], F32, kind="ExternalInput").ap()
    hT_out = nc.dram_tensor("hT_out", [1024, NT], F32, kind="ExternalOutput").ap()
    P = Prog(nc)
    hT_v = hT_in.rearrange("(kc p) t -> p kc t", p=128)
    oT_v = oT_in.rearrange("(kc p) t -> p kc t", p=128)
    ho_v = hT_out.rearrange("(kc p) t -> p kc t", p=128)
    with ExitStack() as st:
        def SB(name, shape, dt, bs=None, dsem=False):
            return TT(P, st, name, shape, dt, bs=bs, dsem=dsem)

        ones_bf = SB("ones_bf", [128, 128], BF16)
        eps_c = SB("eps_c", [128, 1], F32)
        identf = SB("identf", [128, 128], F32)
        SEL = SB("SEL", [32, 32, 128], BF16)
        gf = SB("gf", [128, 8], F32, dsem=True)
        WR = SB("WR", [128, 8, 36], F32, dsem=True)
        HM = SB("HM", [128, 8, NT], F32, bs=512, dsem=True)
        U = SB("U", [128, 8, NT], BF16, bs=512)
        WBUF = [SB("WB%d" % i, [128, 12288], BF16, dsem=True) for i in range(2)]
        OTB = [SB("OTB%d" % i, [128, 8, 512], BF16, dsem=True) for i in range(2)]
        HID = [SB("HID%d" % i, [128, 4, 512], BF16) for i in range(2)]
        SIL = [SB("SIL%d" % i, [128, 512], BF16) for i in range(2)]
        S2 = [SB("S2%d" % i, [128, 512], BF16) for i in range(2)]
        CREP = [SB("CREP%d" % i, [128, 512], BF16) for i in range(2)]
        CT = SB("CT", [32, NT], BF16, bs=128)
        LNV = SB("LNV", [128, 512], F32)
        RSTD = SB("RSTD", [128, 512], F32)
        RLA = SB("RLA", [128, NT // 128, 36], F32)
        R16 = SB("R16", [128, 8, NT // 128], F32)
        OHA = SB("OHA", [128, NT // 128, 4], F32)
        EGA = SB("EGA", [128, NT // 128, 4], F32)
        SELA = SB("SELA", [128, NT // 128, 8], F32)
        TMP8 = SB("TMP8", [128, NT // 128, 8], F32)
        E1A = SB("E1A", [128, NT // 128, 8], F32)
        E2A = SB("E2A", [128, NT // 128, 8], F32)
        COMBA = SB("COMBA", [128, NT // 128, 32], F32)
        RL = SB("RL", [128, 36], F32)
        RT = SB("RT", [128, 16], F32)
        OH = SB("OH", [128, 4], F32)
        EG = SB("EG", [128, 4], F32)
        SEL8 = SB("SEL8", [128, 8], F32)
        M8 = SB("M8", [128, 8], F32)
        WA8 = SB("WA8", [128, 8], F32)
        WB8 = SB("WB8", [128, 8], F32)
        COMB = SB("COMB", [128, 32], F32)
        PSB = [TT(P, st, "ps%d" % i, [128, 512], F32, psum=True) for i in range(8)]
        Wo = WBUF[1]
        Wo_v = Wo[:, 0:8192].rearrange("p (a b) -> p a b", b=1024)
        UFt = WBUF[0]
        UF_v = UFt.h.bitcast(F32)[:, 0:4096].rearrange("p (a b) -> p a b", b=512)
        SQt = HID[0]
        SQ = SB("SQ", [128, 8, 512], BF16)

        memset(P, "pool", ones_bf[:], 1.0, ones_bf.r())
        memset(P, "pool", eps_c[:], EPS, eps_c.r())
        memset(P, "pool", identf[:], 1.0, identf.r())
        P.op("pool", lambda e: e.affine_select(out=identf[:], in_=identf[:], pattern=[[-1, 128]], compare_op=ALU.is_equal,
                                               fill=0.0, base=0, channel_multiplier=1), identf.r(), identf.r())
        memset(P, "pool", SEL[:], 1.0, SEL.r())
        P.op("pool", lambda e: e.affine_select(out=SEL[:], in_=SEL[:], pattern=[[-1, 32], [0, 128]], compare_op=ALU.is_equal,
                                               fill=0.0, base=0, channel_multiplier=1), SEL.r(), SEL.r())
        dma(P, "sp", gf[:], gffn, (), gf.r(), gf.dsem)
        dma(P, "sp", WR[:], w_r.rearrange("(kc p) n -> p kc n", p=128), (), WR.r(), WR.dsem)
        dma(P, "pool", Wo_v, w_out.rearrange("(kc p) n -> p kc n", p=128), (), Wo.r(), Wo.dsem)
        for h2 in range(2):
            dma(P, "sp", HM[:, :, h2 * 1024:(h2 + 1) * 1024], hT_v[:, :, h2 * 1024:(h2 + 1) * 1024], (), HM.r(), HM.dsem)

        for blk in range(NT // 512):
            cs = slice(blk * 512, (blk + 1) * 512)
            ot = OTB[blk % 2]
            dma(P, "sp", ot[:], oT_v[:, :, cs], (), ot.r(), ot.dsem)
            for dc in range(8):
                ps = PSB[dc % 2]
                for kc in range(8):
                    mm(P, ps[:, :], Wo_v[:, kc, dc * 128:(dc + 1) * 128], ot[:, kc, :], kc == 0, kc == 7, Wo.r() + ot.r(), ps.r())
                lo = dc * NT + blk * 512
                tt(P, "dve", HM[:, dc, cs], HM[:, dc, cs], ps[:, :], ALU.add, HM.r(lo, lo + 512) + ps.r(), HM.r(lo, lo + 512))
            hm_blk = []
            for dc in range(8):
                hm_blk += HM.r(dc * NT + blk * 512, dc * NT + blk * 512 + 512)
            act(P, SQ[:], HM[:, :, cs], AF.Square, hm_blk, SQ.r())
            ss = PSB[2]
            for kc in range(8):
                mm(P, ss[:, :], ones_bf[:, :], SQ[:, kc, :], kc == 0, kc == 7, SQ.r() + ones_bf.r(), ss.r())
            act(P, LNV[:], ss[:, :], AF.Ln, ss.r() + eps_c.r(), LNV.r(), scale=1.0 / 1024, bias=eps_c[:])
            act(P, RSTD[:], LNV[:], AF.Exp, LNV.r(), RSTD.r(), scale=-0.5)
            u_blk = []
            for dc in range(8):
                lo = dc * NT + blk * 512
                stt(P, UF_v[:, dc, :], HM[:, dc, cs], gf[:, dc:dc + 1], RSTD[:], ALU.mult, ALU.mult,
                    HM.r(lo, lo + 512) + gf.r() + RSTD.r(), UFt.r())
                u_blk += U.r(lo, lo + 512)
            cp(P, "pool", U[:, :, cs], UF_v, UFt.r(), u_blk)
            pr = PSB[3]
            for sub in range(4):
                scs = slice(sub * 128, (sub + 1) * 128)
                for dc in range(8):
                    mm(P, pr[:, sub * 36:(sub + 1) * 36], UF_v[:, dc, scs], WR[:, dc, :], dc == 0, dc == 7, UFt.r() + WR.r(), pr.r())
            cp(P, "dve", RLA[:, blk * 4:(blk + 1) * 4, :].rearrange("p a b -> p (a b)"), pr[:, 0:144], pr.r(), RLA.r())

        S_ = NT // 128
        def b3(ap2, n):
            return ap2.unsqueeze(2).broadcast_to([128, S_, n])
        GL = RLA[:, :, 0:4]
        P.op("dve", lambda e: e.tensor_reduce(out=R16[:, 0, :], in_=GL, axis=AX.X, op=ALU.max), RLA.r(), R16.r())
        tt(P, "dve", OHA[:], GL, b3(R16[:, 0, :], 4), ALU.is_equal, RLA.r() + R16.r(), OHA.r())
        tt(P, "dve", EGA[:], GL, b3(R16[:, 0, :], 4), ALU.subtract, RLA.r() + R16.r(), EGA.r())
        act(P, EGA[:], EGA[:], AF.Exp, EGA.r(), EGA.r())
        P.op("dve", lambda e: e.tensor_reduce(out=R16[:, 1, :], in_=EGA[:], axis=AX.X, op=ALU.add), EGA.r(), R16.r())
        recip(P, R16[:, 2, :], R16[:, 1, :], R16.r(), R16.r())
        tt(P, "dve", SELA[:], RLA[:, :, 4:12], b3(OHA[:, :, 0], 8), ALU.mult, RLA.r() + OHA.r(), SELA.r())
        for g in range(1, 4):
            tt(P, "dve", TMP8[:], RLA[:, :, 4 + 8 * g:12 + 8 * g], b3(OHA[:, :, g], 8), ALU.mult, RLA.r() + OHA.r(), TMP8.r())
            tt(P, "dve", SELA[:], SELA[:], TMP8[:], ALU.add, SELA.r() + TMP8.r(), SELA.r())
        P.op("dve", lambda e: e.tensor_reduce(out=R16[:, 3, :], in_=SELA[:], axis=AX.X, op=ALU.max), SELA.r(), R16.r())
        tt(P, "dve", E1A[:], SELA[:], b3(R16[:, 3, :], 8), ALU.is_equal, SELA.r() + R16.r(), E1A.r())
        ts(P, "dve", TMP8[:], E1A[:], -1.0e30, ALU.mult, E1A.r(), TMP8.r())
        tt(P, "dve", TMP8[:], SELA[:], TMP8[:], ALU.add, SELA.r() + TMP8.r(), TMP8.r())
        P.op("dve", lambda e: e.tensor_reduce(out=R16[:, 4, :], in_=TMP8[:], axis=AX.X, op=ALU.max), TMP8.r(), R16.r())
        tt(P, "dve", E2A[:], TMP8[:], b3(R16[:, 4, :], 8), ALU.is_equal, TMP8.r() + R16.r(), E2A.r())
        tt(P, "dve", R16[:, 5, :], R16[:, 4, :], R16[:, 3, :], ALU.subtract, R16.r(), R16.r())
        act(P, R16[:, 5, :], R16[:, 5, :], AF.Exp, R16.r(), R16.r())
        ts(P, "dve", R16[:, 5, :], R16[:, 5, :], 1.0, ALU.add, R16.r(), R16.r())
        recip(P, R16[:, 6, :], R16[:, 5, :], R16.r(), R16.r())
        ts(P, "dve", R16[:, 7, :], R16[:, 6, :], -1.0, ALU.mult, R16.r(), R16.r(), 1.0, ALU.add)
        tt(P, "dve", R16[:, 6, :], R16[:, 6, :], R16[:, 2, :], ALU.mult, R16.r(), R16.r())
        tt(P, "dve", R16[:, 7, :], R16[:, 7, :], R16[:, 2, :], ALU.mult, R16.r(), R16.r())
        tt(P, "dve", E1A[:], E1A[:], b3(R16[:, 6, :], 8), ALU.mult, E1A.r() + R16.r(), E1A.r())
        tt(P, "dve", E2A[:], E2A[:], b3(R16[:, 7, :], 8), ALU.mult, E2A.r() + R16.r(), E2A.r())
        tt(P, "dve", E1A[:], E1A[:], E2A[:], ALU.add, E1A.r() + E2A.r(), E1A.r())
        tt(P, "dve", COMBA[:].rearrange("p s (g e) -> p s g e", e=8),
           E1A[:].unsqueeze(2).broadcast_to([128, S_, 4, 8]), OHA[:].unsqueeze(3).broadcast_to([128, S_, 4, 8]), ALU.mult,
           E1A.r() + OHA.r(), COMBA.r())
        for q4 in range(S_ // 4):
            pt = PSB[4 + q4 % 2]
            for sub in range(4):
                sidx = q4 * 4 + sub
                P.op("pe", lambda e, pt=pt, sub=sub, sidx=sidx: e.transpose(out=pt[0:32, sub * 128:(sub + 1) * 128], in_=COMBA[:, sidx, :], identity=identf[:, :]),
                     COMBA.r() + identf.r(), pt.r())
            cp(P, "dve", CT[0:32, q4 * 512:(q4 + 1) * 512], pt[0:32, :], pt.r(), CT.r(q4 * 512, q4 * 512 + 512))

        units = [(e, blk) for e in range(32) for blk in range(NT // 512)]
        wviews = []
        for i in range(2):
            wb = WBUF[i]
            wviews.append((wb[:, 0:4096].rearrange("p (a b) -> p a b", b=512),
                           wb[:, 4096:8192].rearrange("p (a b) -> p a b", b=512),
                           wb[:, 8192:12288].rearrange("p (a b) -> p a b", b=1024)))

        def load_w(e):
            wb = WBUF[e % 2]
            Wg, Wu, Wd = wviews[e % 2]
            dma(P, "pool", Wg, w_gate[e].rearrange("(kc p) n -> p kc n", p=128), (), wb.r(), wb.dsem)
            dma(P, "pool", Wu, w_up[e].rearrange("(kc p) n -> p kc n", p=128), (), wb.r(), wb.dsem)
            dma(P, "pool", Wd, w_down[e].rearrange("(fc p) n -> p fc n", p=128), (), wb.r(), wb.dsem)

        def GU(ui):
            e, blk = units[ui]
            cs = slice(blk * 512, (blk + 1) * 512)
            wb = WBUF[e % 2]
            Wg, Wu, Wd = wviews[e % 2]
            hid = HID[ui % 2]
            crep = CREP[ui % 2]
            pc = PSB[6]
            mm(P, pc[:, :], SEL[0:32, e, :], CT[0:32, cs], True, True, SEL.r() + CT.r(blk * 512, blk * 512 + 512), pc.r())
            cp(P, "dve", crep[:], pc[:, :], pc.r(), crep.r())
            u_blk = []
            for dc in range(8):
                u_blk += U.r(dc * NT + blk * 512, dc * NT + blk * 512 + 512)
            for fc in range(4):
                pg_ = PSB[(fc % 2) * 2]
                pu_ = PSB[(fc % 2) * 2 + 1]
                fs = slice(fc * 128, (fc + 1) * 128)
                for dc in range(8):
                    mm(P, pg_[:, :], Wg[:, dc, fs], U[:, dc, cs], dc == 0, dc == 7, wb.r() + u_blk, pg_.r())
                for dc in range(8):
                    mm(P, pu_[:, :], Wu[:, dc, fs], U[:, dc, cs], dc == 0, dc == 7, wb.r() + u_blk, pu_.r())
                sil = SIL[fc % 2]
                s2 = S2[fc % 2]
                act(P, sil[:], pg_[:, :], AF.Silu, pg_.r(), sil.r())
                tt(P, "dve", s2[:], sil[:], crep[:], ALU.mult, sil.r() + crep.r(), s2.r())
                tt(P, "dve", hid[:, fc, :], s2[:], pu_[:, :], ALU.mult, s2.r() + pu_.r(), hid.r())

        def DN(ui):
            e, blk = units[ui]
            cs = slice(blk * 512, (blk + 1) * 512)
            wb = WBUF[e % 2]
            Wg, Wu, Wd = wviews[e % 2]
            hid = HID[ui % 2]
            for dc in range(8):
                py = PSB[4 + dc % 2]
                for fc in range(4):
                    mm(P, py[:, :], Wd[:, fc, dc * 128:(dc + 1) * 128], hid[:, fc, :], fc == 0, fc == 3, wb.r() + hid.r(), py.r())
                lo = dc * NT + blk * 512
                tt(P, "dve", HM[:, dc, cs], HM[:, dc, cs], py[:, :], ALU.add, HM.r(lo, lo + 512) + py.r(), HM.r(lo, lo + 512))

        nE = int(os.environ.get("KB_NE", 32))
        units = [u_ for u_ in units if u_[0] < nE]
        load_w(0)
        for ui in range(len(units) + 1):
            if ui < len(units):
                GU(ui)
            if ui >= 1:
                DN(ui - 1)
            if ui < len(units):
                e, blk = units[ui]
                if blk == 0 and e + 1 < nE:
                    load_w(e + 1)
        for h2 in range(2):
            rr = []
            for dc in range(8):
                rr += HM.r(dc * NT + h2 * 1024, dc * NT + h2 * 1024 + 1024)
            dma(P, "sp", ho_v[:, :, h2 * 1024:(h2 + 1) * 1024], HM[:, :, h2 * 1024:(h2 + 1) * 1024], rr, (), HM.dsem)
        P.replay()
    return nc


import math

NCOL_C = 384
TWO_PI = 2.0 * math.pi
LAM_INIT = 0.8 - 0.6 * math.exp(-0.3 * 1)


def build_C(nbatch=2, debug=False):
    nc = bass.Bass("TRN2", target_bir_lowering=False)
    xT = nc.dram_tensor("xT", [1024, TOK], F32, kind="ExternalInput").ap()
    wC = nc.dram_tensor("wC", [1024, NCOL_C], F32, kind="ExternalInput").ap()
    gmix = nc.dram_tensor("gmix", [128, 8], F32, kind="ExternalInput").ap()
    pC = nc.dram_tensor("pC", [128, 8], F32, kind="ExternalInput").ap()
    posd = nc.dram_tensor("pos", [2, T], I32, kind="ExternalInput").ap()
    oT = nc.dram_tensor("oT", [128, TOK], BF16, kind="ExternalOutput").ap()
    P = Prog(nc)
    xT_v = xT.rearrange("(kc p) t -> p kc t", p=128)
    with ExitStack() as st:
        def SB(name, shape, dt, bs=None, dsem=False):
            return TT(P, st, name, shape, dt, bs=bs, dsem=dsem)

        ones_bf = SB("ones_bf", [128, 128], BF16)
        BD = SB("BD", [128, 128], BF16)
        ones_f = SB("ones_f", [128, 128], F32)
        eps_c = SB("eps_c", [128, 1], F32)
        pi_c = SB("pi_c", [128, 2], F32)
        pic2 = SB("pic2", [128, 2], F32)
        ident = SB("ident", [128, 128], BF16)
        PM = SB("PM", [128, 128], BF16)
        PM2 = SB("PM2", [128, 128], BF16)
        par = SB("par", [128, 8], F32, dsem=True)
        gm = SB("gm", [128, 8], F32, dsem=True)
        cst = SB("cst", [128, 8], F32)
        invi = SB("invi", [1, 128], I32)
        invf = SB("invf", [1, 128], F32)
        Wb = SB("Wb", [128, 8, NCOL_C], BF16, dsem=True)

        memset(P, "pool", ones_bf[:], 1.0, ones_bf.r())
        memset(P, "pool", ones_f[:], 1.0, ones_f.r())
        memset(P, "pool", eps_c[:], EPS, eps_c.r())
        memset(P, "pool", BD[:], 1.0, BD.r())
        memset(P, "pool", BD[0:64, 64:128], 0.0, BD.r())
        memset(P, "pool", BD[64:128, 0:64], 0.0, BD.r())
        memset(P, "pool", pi_c[:, 0:1], math.pi, pi_c.r())
        memset(P, "pool", pi_c[:, 1:2], -TWO_PI, pi_c.r())
        memset(P, "pool", pi_c[0:32, 0:1], -math.pi, pi_c.r())
        memset(P, "pool", pi_c[0:32, 1:2], TWO_PI, pi_c.r())
        memset(P, "pool", pi_c[64:96, 0:1], -math.pi, pi_c.r())
        memset(P, "pool", pi_c[64:96, 1:2], TWO_PI, pi_c.r())
        memset(P, "pool", pic2[:, 0:1], math.pi, pic2.r())
        memset(P, "pool", PM[:], 1.0, PM.r())
        P.op("pool", lambda e: e.affine_select(out=PM[:], in_=PM[:], pattern=[[1, 128]], compare_op=ALU.is_equal,
                                               fill=0.0, base=-32, channel_multiplier=-1), PM.r(), PM.r())
        memset(P, "pool", PM[:, 0:32], 0.0, PM.r())
        memset(P, "pool", PM[:, 64:96], 0.0, PM.r())
        memset(P, "pool", PM2[:], 1.0, PM2.r())
        P.op("pool", lambda e: e.affine_select(out=PM2[:], in_=PM2[:], pattern=[[1, 128]], compare_op=ALU.is_equal,
                                               fill=0.0, base=32, channel_multiplier=-1), PM2.r(), PM2.r())
        memset(P, "pool", PM2[:, 32:64], 0.0, PM2.r())
        memset(P, "pool", PM2[:, 96:128], 0.0, PM2.r())
        tt(P, "pool", PM[:], PM[:], PM2[:], ALU.add, PM.r() + PM2.r(), PM.r())
        P.op("pool", lambda e: e.iota(invi[:].rearrange("p (a b) -> p a b", b=32), pattern=[[0, 4], [1, 32]], base=0, channel_multiplier=0),
             (), invi.r())
        cp(P, "dve", invf[:], invi[:], invi.r(), invf.r())
        act(P, invf[:], invf[:], AF.Exp, invf.r(), invf.r(), scale=-math.log(10000.0) / 32.0)

        dma(P, "sp", par[:], pC, (), par.r(), par.dsem)
        dma(P, "sp", gm[:], gmix, (), gm.r(), gm.dsem)
        dma(P, "pool", Wb[:], wC.rearrange("(kc p) n -> p kc n", p=128), (), Wb.r(), Wb.dsem)
        for kc in range(8):
            ts(P, "dve", Wb[:, kc, :], Wb[:, kc, :], gm[:, kc:kc + 1], ALU.mult, Wb.r() + gm.r(), Wb.r())
        ts(P, "dve", cst[:, 0:1], par[:, 0:1], 0.125, ALU.mult, par.r(), cst.r())
        ts(P, "dve", cst[:, 3:4], par[:, 6:7], 1.0 - LAM_INIT, ALU.mult, par.r(), cst.r())
        PSB = [TT(P, st, "ps%d" % i, [128, 512], F32, psum=True) for i in range(8)]
        tt(P, "dve", cst[:, 4:5], par[:, 2:3], par[:, 3:4], ALU.mult, par.r(), cst.r())
        tt(P, "dve", cst[:, 5:6], par[:, 4:5], par[:, 5:6], ALU.mult, par.r(), cst.r())
        mm(P, PSB[0][:, 0:2], ones_f[:, :], cst[:, 4:6], True, True, ones_f.r() + cst.r(), PSB[0].r())
        act(P, cst[:, 6:8], PSB[0][:, 0:2], AF.Exp, PSB[0].r(), cst.r())
        tt(P, "dve", cst[:, 2:3], cst[:, 7:8], cst[:, 6:7], ALU.subtract, cst.r(), cst.r())
        ts(P, "dve", cst[:, 2:3], cst[:, 2:3], -LAM_INIT, ALU.add, cst.r(), cst.r())
        gq_ap, gk_ap, nlam_ap, gs_ap = cst[:, 0:1], par[:, 1:2], cst[:, 2:3], cst[:, 3:4]

        QT = SB("QT", [128, T], BF16, bs=512)
        KT = SB("KT", [128, T], BF16, bs=128)
        V = SB("V", [128, T // 128, 128], BF16, bs=128)
        SINT = SB("SINT", [128, T], BF16, bs=512)
        COST = SB("COST", [128, T], BF16, bs=512)
        XB = [SB("XB%d" % i, [128, 8, 512], BF16, dsem=True) for i in range(2)]
        SQ = SB("SQ", [128, 8, 512], BF16)
        LNV = SB("LNV", [128, 512], F32)
        RSTD = [SB("RSTD%d" % i, [128, 512], F32) for i in range(2)]
        RCS = SB("RCS", [128, 4], F32)
        POSI = SB("POSI", [1, 512], I32, dsem=True)
        POSF = SB("POSF", [1, 512], F32)
        RR = [SB("RR%d" % i, [128, 512], F32) for i in range(2)]
        RI = SB("RI", [128, 512], I32)
        RF = SB("RF", [128, 512], F32)
        RMK = SB("RMK", [128, 512], F32)
        TA = SB("TA", [128, 512], BF16)
        TB_ = SB("TB", [128, 512], BF16)
        PTB = [[SB("PT%d_%d" % (m, i), [128, 512], BF16) for i in range(3)] for m in range(2)]
        FT = [SB("FT%d" % i, [128, 512], F32) for i in range(4)]
        OUTS = [SB("OUTS%d" % i, [128, 512], BF16, dsem=True) for i in range(2)]
        DACC = [SB("DACC%d" % i, [128, 512], F32) for i in range(2)]
        TMPP = [[SB("TMPP%d_%d" % (m, i), [128, 512], BF16) for i in range(2)] for m in range(2)]

        groups = [(0, QT), (128, KT)]
        blk = 0
        for b in range(nbatch):
            for n in range(T // 512):
                tok0 = b * T + n * 512
                cols = (n * 512, (n + 1) * 512)
                xb = XB[blk % 2]
                rs = RSTD[blk % 2]
                dma(P, "pool", xb[:], xT_v[:, :, tok0:tok0 + 512], (), xb.r(), xb.dsem)
                act(P, SQ[:], xb[:], AF.Square, xb.r(), SQ.r())
                ss = PSB[0]
                for kc in range(8):
                    mm(P, ss[:, :], ones_bf[:, :], SQ[:, kc, :], kc == 0, kc == 7, SQ.r() + ones_bf.r(), ss.r())
                act(P, LNV[:], ss[:, :], AF.Ln, ss.r() + eps_c.r(), LNV.r(), scale=1.0 / 1024, bias=eps_c[:])
                act(P, rs[:], LNV[:], AF.Exp, LNV.r(), rs.r(), scale=-0.5)
                for gi, (c0, dest) in enumerate(groups):
                    ps = PSB[1 + gi]
                    for kc in range(8):
                        mm(P, ps[:, :], Wb[:, kc, c0:c0 + 128], xb[:, kc, :], kc == 0, kc == 7, Wb.r() + xb.r(), ps.r())
                    tt(P, "dve", dest[:, cols[0]:cols[1]], ps[:, :], rs[:, :], ALU.mult, ps.r() + rs.r(), dest.r(*cols))
                ptm = PSB[3]
                prc = PSB[4]
                for sub in range(4):
                    sc = slice(sub * 128, (sub + 1) * 128)
                    for kc in range(8):
                        mm(P, ptm[:, sc], xb[:, kc, sc], Wb[:, kc, 256:384], kc == 0, kc == 7, Wb.r() + xb.r(), ptm.r())
                    mm(P, prc[:, sub:sub + 1], rs[0:1, sc], ones_f[0:1, 0:1], True, True, rs.r() + ones_f.r(), prc.r())
                cp(P, "dve", RCS[:, 0:4], prc[:, 0:4], prc.r(), RCS.r())
                for sub in range(4):
                    ti = n * 4 + sub
                    sc = slice(sub * 128, (sub + 1) * 128)
                    ts(P, "dve", V[:, ti, :], ptm[:, sc], RCS[:, sub:sub + 1], ALU.mult, ptm.r() + RCS.r(), V.r(ti * 128, ti * 128 + 128))
                blk += 1
            for n in range(T // 512):
                cols = (n * 512, (n + 1) * 512)
                cs = slice(*cols)
                dma(P, "sp", POSI[:], posd[b:b + 1, cs], (), POSI.r(), POSI.dsem)
                cp(P, "dve", POSF[:], POSI[:], POSI.r(), POSF.r())
                pa = PSB[5]
                mm(P, pa[:, :], invf[0:1, :], POSF[0:1, :], True, True, invf.r() + POSF.r(), pa.r())
                for which, dest in ((0, SINT), (1, COST)):
                    rr = RR[which]
                    if which == 0:
                        ts(P, "dve", rr[:], pa[:, :], 1.0 / TWO_PI, ALU.mult, pa.r(), rr.r())
                    else:
                        ts(P, "dve", rr[:], pa[:, :], 1.0 / TWO_PI, ALU.mult, pa.r(), rr.r(), 0.25, ALU.add)
                    cp(P, "dve", RI[:], rr[:], rr.r(), RI.r())
                    cp(P, "act", RF[:], RI[:], RI.r(), RF.r())
                    tt(P, "dve", rr[:], rr[:], RF[:], ALU.subtract, rr.r() + RF.r(), rr.r())
                    ts(P, "dve", RMK[:], rr[:], 0.0, ALU.is_lt, rr.r(), RMK.r())
                    tt(P, "dve", rr[:], rr[:], RMK[:], ALU.add, rr.r() + RMK.r(), rr.r())
                    if which == 0:
                        P.op("act", lambda e, rr=rr, dest=dest, cs=cs: e.activation(out=dest[:, cs], in_=rr[:], func=AF.Sin, scale=pi_c[:, 1:2], bias=pi_c[:, 0:1]),
                             rr.r() + pi_c.r(), dest.r(*cols))
                    else:
                        P.op("act", lambda e, rr=rr, dest=dest, cs=cs: e.activation(out=dest[:, cs], in_=rr[:], func=AF.Sin, scale=-TWO_PI, bias=pic2[:, 0:1]),
                             rr.r() + pic2.r(), dest.r(*cols))
            for n in range(T // 512):
                cols = (n * 512, (n + 1) * 512)
                cs = slice(*cols)
                for (tl, gap, bank) in ((QT, gq_ap, 1), (KT, gk_ap, 2)):
                    ps = PSB[bank]
                    act(P, SQ[:, 0, :], tl[:, cs], AF.Square, tl.r(*cols), SQ.r())
                    mm(P, ps[:, :], BD[:, :], SQ[:, 0, :], True, True, SQ.r() + BD.r(), ps.r())
                    act(P, LNV[:], ps[:, :], AF.Ln, ps.r() + eps_c.r(), LNV.r(), scale=1.0 / 64, bias=eps_c[:])
                    act(P, LNV[:], LNV[:], AF.Exp, LNV.r(), LNV.r(), scale=-0.5)
                    stt(P, tl[:, cs], tl[:, cs], gap, LNV[:], ALU.mult, ALU.mult, tl.r(*cols) + LNV.r() + par.r() + cst.r(), tl.r(*cols))
                    pw = PSB[bank + 2]
                    mm(P, pw[:, :], PM[:, :], tl[:, cs], True, True, PM.r() + tl.r(*cols), pw.r())
                    tt(P, "dve", TA[:], tl[:, cs], COST[:, cs], ALU.mult, tl.r(*cols) + COST.r(*cols), TA.r())
                    tt(P, "dve", TB_[:], pw[:, :], SINT[:, cs], ALU.mult, pw.r() + SINT.r(*cols), TB_.r())
                    tt(P, "dve", tl[:, cs], TA[:], TB_[:], ALU.add, TA.r() + TB_.r(), tl.r(*cols))
            if debug:
                continue
            k = 0
            for j in range(T // 512):
                accs = (PSB[4], PSB[5])
                dens = (PSB[6], PSB[7])
                nt = 4 * j + 4
                items = []
                for i in range(nt):
                    m_ = i - 4 * j
                    c0 = 128 * m_ if m_ >= 0 else 0
                    items.append((i, m_, c0))

                def qk(it, k):
                    i, m_, c0 = it
                    for mp in range(2):
                        sc = PSB[(k % 2) * 2 + mp]
                        rows = slice(mp * 64, (mp + 1) * 64)
                        mm(P, sc[:, c0:512], KT[rows, i * 128:(i + 1) * 128], QT[rows, j * 512 + c0:(j + 1) * 512], True, True,
                           KT.r(i * 128, i * 128 + 128) + QT.r(j * 512, j * 512 + 512), sc.r())

                def rest(it, k):
                    i, m_, c0 = it
                    for mp in range(2):
                        sc = PSB[(k % 2) * 2 + mp]
                        pt = PTB[mp][k % 3]
                        act(P, pt[:, c0:512], sc[:, c0:512], AF.Exp, sc.r(), pt.r())
                        if m_ >= 0:
                            memset(P, "pool", pt[64:128, c0:c0 + 64], 0.0, pt.r())
                        mm(P, accs[mp][:, c0:512], V[:, i, :], pt[:, c0:512], i == 0, i == nt - 1, V.r(i * 128, i * 128 + 128) + pt.r(), accs[mp].r())
                        if mp == 0:
                            mm(P, dens[0][:, c0:512], ones_bf[:, :], pt[:, c0:512], i == 0, i == nt - 1, ones_bf.r() + pt.r(), dens[0].r())
                        elif not dinit[mp]:
                            assert c0 == 0
                            cp(P, "dve", DACC[mp][:, :], pt[:, :], pt.r(), DACC[mp].r())
                            dinit[mp] = True
                        else:
                            tt(P, "dve", DACC[mp][:, c0:512], DACC[mp][:, c0:512], pt[:, c0:512], ALU.add, DACC[mp].r() + pt.r(), DACC[mp].r())
                LOOK = 1
                dinit = [False, False]
                pend = [None, None]
                npair = [0, 0]
                for idx in range(nt + LOOK):
                    if idx < nt:
                        qk(items[idx], k + idx)
                    if idx >= LOOK:
                        rest(items[idx - LOOK], k + idx - LOOK)
                k += nt
                for mp in range(1, 2):
                    mm(P, dens[mp][:, :], ones_f[:, :], DACC[mp][:, :], True, True, ones_f.r() + DACC[mp].r(), dens[mp].r())
                recip(P, FT[0][:], dens[0][:, :], dens[0].r(), FT[0].r())
                tt(P, "dve", FT[1][:], accs[0][:, :], FT[0][:], ALU.mult, accs[0].r() + FT[0].r(), FT[1].r())
                recip(P, FT[0][:], dens[1][:, :], dens[1].r(), FT[0].r())
                tt(P, "dve", FT[2][:], accs[1][:, :], FT[0][:], ALU.mult, accs[1].r() + FT[0].r(), FT[2].r())
                stt(P, FT[3][:], FT[2][:], nlam_ap, FT[1][:], ALU.mult, ALU.add, FT[2].r() + FT[1].r() + cst.r(), FT[3].r())
                act(P, SQ[:, 0, :], FT[3][:], AF.Square, FT[3].r(), SQ.r())
                pn = PSB[0]
                mm(P, pn[:, :], ones_bf[:, :], SQ[:, 0, :], True, True, SQ.r() + ones_bf.r(), pn.r())
                act(P, LNV[:], pn[:, :], AF.Ln, pn.r() + eps_c.r(), LNV.r(), scale=1.0 / 128, bias=eps_c[:])
                act(P, LNV[:], LNV[:], AF.Exp, LNV.r(), LNV.r(), scale=-0.5)
                outs = OUTS[j % 2]
                stt(P, outs[:], FT[3][:], gs_ap, LNV[:], ALU.mult, ALU.mult, FT[3].r() + LNV.r() + cst.r(), outs.r())
                dma(P, "sp", oT[:, b * T + j * 512: b * T + (j + 1) * 512], outs[:], outs.r(), (), outs.dsem)
        if debug:
            dsd = P.new_dsem("dbg")
            for nm, tl in (("dQ", QT), ("dK", KT), ("dS", SINT), ("dC", COST)):
                dd = nc.dram_tensor(nm, [128, T], BF16, kind="ExternalOutput").ap()
                dma(P, "sp", dd, tl[:], tl.r(), (), dsd)
            dd = nc.dram_tensor("dV", [128, T], BF16, kind="ExternalOutput").ap()
            dma(P, "sp", dd, V[:].rearrange("p a b -> p (a b)"), V.r(), (), dsd)
        P.replay()
    return nc


def _prep_A(inp, xT):
    w = inp["even_w_in"][0]
    gm = np.ascontiguousarray(inp["norm_mix"][0].reshape(8, 128).T)
    maps = []
    for c in range(8):
        sl = lambda base: w[:, base + c * 64: base + (c + 1) * 64]
        wA = np.concatenate([sl(0), sl(512), sl(1536), sl(2048), sl(2560), w[:, 3584 + c:3585 + c], sl(1024), sl(3072)], axis=1)
        pA = np.zeros((64, 8), np.float32)
        pA[:, 0] = inp["hgrn_lb_logits"][0, c * 64:(c + 1) * 64]
        pA[:, 1] = inp["hgrn_lb_logits"][1, c * 64:(c + 1) * 64]
        pA[:, 2] = inp["hgrn_out_norm"][0]
        pA[:, 3] = inp["fox_q_norm"][0]
        pA[:, 4] = inp["fox_k_norm"][0]
        fb = np.empty((128, 1), np.float32)
        fb[:, 0] = inp["fox_f_bias"][0, c]
        maps.append({"xT": xT, "wA": np.ascontiguousarray(wA), "gmix": gm, "pA": pA, "fbias": fb})
    return maps


def _prep_B(inp, layer, hT_full, oT_full, w_out):
    wr = np.concatenate([inp["moe_router_group"][layer]] + [inp["moe_router_expert"][layer, g] for g in range(4)], axis=1)
    wg = np.ascontiguousarray(inp["moe_w_gate"][layer].reshape(32, 1024, 512))
    wu = np.ascontiguousarray(inp["moe_w_up"][layer].reshape(32, 1024, 512))
    wd = np.ascontiguousarray(inp["moe_w_down"][layer].reshape(32, 512, 1024))
    gf = np.ascontiguousarray(inp["norm_ffn"][layer].reshape(8, 128).T)
    wr = np.ascontiguousarray(wr)
    w_out = np.ascontiguousarray(w_out)
    maps = []
    for c in range(8):
        cs = slice(c * NT, (c + 1) * NT)
        maps.append({"hT_in": np.ascontiguousarray(hT_full[:, cs]), "oT_in": np.ascontiguousarray(oT_full[:, cs]),
                     "w_out": w_out, "gffn": gf, "w_r": wr, "w_gate": wg, "w_up": wu, "w_down": wd})
    return maps


def _prep_C(inp, hT_full):
    w = inp["odd_w_in"][0]
    gm = np.ascontiguousarray(inp["norm_mix"][1].reshape(8, 128).T)
    pos = np.ascontiguousarray(inp["positions"].astype(np.int32))
    maps = []
    for c in range(8):
        wC = np.concatenate([w[:, c * 128:(c + 1) * 128], w[:, 1024 + c * 128:1024 + (c + 1) * 128],
                             w[:, 2048 + c * 128:2048 + (c + 1) * 128]], axis=1)
        pC = np.zeros((128, 8), np.float32)
        pC[0:64, 0] = inp["diff_q_norm"][0]; pC[64:128, 0] = inp["diff_q_norm"][0]
        pC[0:64, 1] = inp["diff_k_norm"][0]; pC[64:128, 1] = inp["diff_k_norm"][0]
        pC[0:64, 2] = inp["diff_lambda_q1"][0]; pC[0:64, 3] = inp["diff_lambda_k1"][0]
        pC[0:64, 4] = inp["diff_lambda_q2"][0]; pC[0:64, 5] = inp["diff_lambda_k2"][0]
        pC[:, 6] = inp["diff_subln"][0]
        maps.append({"xT": hT_full, "wC": np.ascontiguousarray(wC), "gmix": gm, "pC": pC, "pos": pos})
    return maps


def _run(nc, maps):
    res = run_bass_kernel_spmd(nc, maps, core_ids=list(range(8)))
    return res.results


def kernel(**inputs):
    inp = {k: np.asarray(v) for k, v in inputs.items()}
    x = inp["x"].astype(np.float32, copy=False).reshape(-1, 1024)
    xT = np.ascontiguousarray(x.T)
    rA = _run(build_A(), _prep_A(inp, xT))
    oT0 = np.empty((1024, TOK), ml_dtypes.bfloat16)
    for c in range(8):
        o = np.asarray(rA[c]["oT"])
        oT0[c * 64:(c + 1) * 64] = o[0:64]
        oT0[512 + c * 64:512 + (c + 1) * 64] = o[64:128]
    rB = _run(build_B(), _prep_B(inp, 0, xT, oT0, inp["even_w_out"][0]))
    h1T = np.ascontiguousarray(np.concatenate([np.asarray(r["hT_out"]) for r in rB], axis=1))
    rC = _run(build_C(), _prep_C(inp, h1T))
    oT1 = np.ascontiguousarray(np.concatenate([np.asarray(r["oT"]) for r in rC], axis=0))
    rD = _run(build_B(), _prep_B(inp, 1, h1T, oT1, inp["odd_w_out"][0]))
    outT = np.concatenate([np.asarray(r["hT_out"]) for r in rD], axis=1)
    return np.ascontiguousarray(outT.T).reshape(2, 8192, 1024).astype(np.float32, copy=False)
```

```python
import ml_dtypes
import numpy as np
from contextlib import ExitStack
import concourse.bass as bass
import concourse.mybir as mybir
from concourse.bass_utils import run_bass_kernel_spmd

F32 = mybir.dt.float32
BF16 = mybir.dt.bfloat16
I32 = mybir.dt.int32
AF = mybir.ActivationFunctionType
ALU = mybir.AluOpType
AX = mybir.AxisListType

ENGS = ("pe", "act", "dve", "pool", "sp")


class Res:
    __slots__ = ("name", "last_w", "readers", "dsem")

    def __init__(self, name):
        self.name = name
        self.last_w = None
        self.readers = []
        self.dsem = None


class Op:
    __slots__ = ("eng", "fn", "deps", "is_dma", "sem", "semval", "signal", "count", "dma_waits", "idx")

    def __init__(self, eng, fn):
        self.eng = eng
        self.fn = fn
        self.deps = []
        self.is_dma = False
        self.sem = None
        self.semval = 0
        self.signal = False
        self.count = 0
        self.dma_waits = []
        self.idx = 0


class Prog:
    def __init__(self, nc):
        self.nc = nc
        self.ops = {e: [] for e in ENGS}
        self.all_res_reset = True
        self.esem = {e: nc.alloc_semaphore("es_" + e) for e in ENGS}
        self.dsems = []
        self.dma_cum = {}
        self.nops = 0
        self.ecount = {e: 0 for e in ENGS}
        self.waited = {e: {} for e in ENGS}

    def res(self, name, n=1):
        return [Res("%s.%d" % (name, i)) for i in range(n)]

    def new_dsem(self, name):
        s = self.nc.alloc_semaphore("ds_" + name)
        self.dma_cum[s] = 0
        return s

    def op(self, eng, fn, reads=(), writes=(), dma_sem=None):
        o = Op(eng, fn)
        o.idx = self.nops
        self.nops += 1
        deps = {}
        dma_waits = {}

        def add_dep(p):
            if p is None:
                return
            if p.is_dma:
                dma_waits[p.sem] = self.dma_cum[p.sem]
            else:
                deps[id(p)] = p

        for r in reads:
            add_dep(r.last_w)
        for r in writes:
            add_dep(r.last_w)
            for q in r.readers:
                add_dep(q)
        raw = set()
        for r in reads:
            if r.last_w is not None and not r.last_w.is_dma:
                raw.add(id(r.last_w))
        for k, p in list(deps.items()):
            if p.eng == eng and eng == "pe" and dma_sem is None:
                del deps[k]
        o.deps = list(deps.values())
        for p in o.deps:
            p.signal = True
        o.dma_waits = list(dma_waits.items())
        if dma_sem is not None:
            o.is_dma = True
            o.sem = dma_sem
            self.dma_cum[dma_sem] += 16
            o.semval = self.dma_cum[dma_sem]
        for r in reads:
            r.readers.append(o)
        for r in writes:
            r.last_w = o
            r.readers = []
        self.ops[eng].append(o)
        return o

    def finish_wait(self, eng="sp"):
        waits = [(s, v) for s, v in self.dma_cum.items() if v > 0]
        o = Op(eng, None)
        o.dma_waits = waits
        self.ops[eng].append(o)

    def replay(self):
        nc = self.nc
        for e in ENGS:
            last = None
            for o in self.ops[e]:
                if o.fn is not None and not o.is_dma:
                    last = o
            if last is not None:
                last.signal = True
            c = self.ecount[e]
            for o in self.ops[e]:
                if o.signal and not o.is_dma:
                    c += 1
                    o.count = c
            self.ecount[e] = c
        final = {self.esem[e]: self.ecount[e] for e in ENGS if self.ecount[e] > 0}
        for s_, v_ in self.dma_cum.items():
            if v_ > 0:
                final[s_] = v_
        handles = {"pe": "tensor", "act": "scalar", "dve": "vector", "pool": "gpsimd", "sp": "sync"}
        with nc.Block() as block:
            for e in ENGS:
                ops = self.ops[e]
                esem = self.esem
                my = esem[e]

                def body(engh, ops=ops, e=e, my=my):
                    waited = self.waited[e]
                    for o in ops:
                        need = {}
                        for p in o.deps:
                            s = esem[p.eng]
                            if need.get(s, 0) < p.count:
                                need[s] = p.count
                        for s, v in o.dma_waits:
                            if need.get(s, 0) < v:
                                need[s] = v
                        for s, v in need.items():
                            if waited.get(s, 0) < v:
                                engh.wait_ge(s, v)
                                waited[s] = v
                        if o.fn is None:
                            continue
                        ins = o.fn(engh)
                        if o.is_dma:
                            ins.then_inc(o.sem, 16)
                        elif o.signal:
                            ins.then_inc(my, 1)
                    for s, v in final.items():
                        if s is my:
                            continue
                        if waited.get(s, 0) < v:
                            engh.wait_ge(s, v)
                            waited[s] = v

                getattr(block, handles[e])(body)
        self.ops = {e: [] for e in ENGS}
        self.all_res_reset = True


from math import prod
import os
HG = int(os.environ.get('HG_STAGE', '9'))


class TT:
    def __init__(self, P, st, name, shape, dt, bs=None, psum=False, dsem=False):
        nc = P.nc
        self.h = st.enter_context(nc.psum_tensor(name, shape, dt) if psum else nc.sbuf_tensor(name, shape, dt))
        self.F = prod(shape[1:])
        self.bs = bs or self.F
        self.res = P.res(name, (self.F + self.bs - 1) // self.bs)
        self.dsem = P.new_dsem(name) if dsem else None

    def r(self, lo=0, hi=None):
        hi = self.F if hi is None else hi
        return self.res[lo // self.bs:(hi - 1) // self.bs + 1]

    def __getitem__(self, k):
        return self.h[k]


def mm(P, out, lhsT, rhs, start, stop, rd, wr):
    return P.op("pe", lambda e: e.matmul(out, lhsT=lhsT, rhs=rhs, start=start, stop=stop), rd, wr)


def act(P, out, in_, func, rd, wr, scale=1.0, bias=None):
    if bias is None:
        return P.op("act", lambda e: e.activation(out=out, in_=in_, func=func, scale=scale), rd, wr)
    return P.op("act", lambda e: e.activation(out=out, in_=in_, func=func, scale=scale, bias=bias), rd, wr)


def tt(P, eng, out, in0, in1, op, rd, wr):
    return P.op(eng, lambda e: e.tensor_tensor(out=out, in0=in0, in1=in1, op=op), rd, wr)


def ts(P, eng, out, in0, s1, op0, rd, wr, s2=None, op1=None):
    if op1 is None:
        return P.op(eng, lambda e: e.tensor_scalar(out=out, in0=in0, scalar1=s1, scalar2=None, op0=op0), rd, wr)
    return P.op(eng, lambda e: e.tensor_scalar(out=out, in0=in0, scalar1=s1, scalar2=s2, op0=op0, op1=op1), rd, wr)


def stt(P, out, in0, scalar, in1, op0, op1, rd, wr):
    return P.op("dve", lambda e: e.scalar_tensor_tensor(out=out, in0=in0, scalar=scalar, in1=in1, op0=op0, op1=op1), rd, wr)


def cp(P, eng, out, in_, rd, wr):
    if eng == "act":
        return P.op("act", lambda e: e.copy(out=out, in_=in_), rd, wr)
    return P.op(eng, lambda e: e.tensor_copy(out=out, in_=in_), rd, wr)


def recip(P, out, in_, rd, wr):
    return P.op("dve", lambda e: e.reciprocal(out=out, in_=in_), rd, wr)


def dma(P, q, out, in_, rd, wr, sem):
    return P.op(q, lambda e: e.dma_start(out=out, in_=in_), rd, wr, dma_sem=sem)


def memset(P, eng, ap, val, wr):
    return P.op(eng, lambda e: e.memset(ap, val), (), wr)


T = int(os.environ.get('KT', 8192))
TOK = 2 * T
NCOL_A = 449
EPS = 1e-6


def build_A(nbatch=2, do_hgrn=True, do_fox=True, debug=False):
    nc = bass.Bass("TRN2", target_bir_lowering=False)
    xT = nc.dram_tensor("xT", [1024, TOK], F32, kind="ExternalInput").ap()
    wA = nc.dram_tensor("wA", [1024, NCOL_A], F32, kind="ExternalInput").ap()
    gmix = nc.dram_tensor("gmix", [128, 8], F32, kind="ExternalInput").ap()
    pA = nc.dram_tensor("pA", [64, 8], F32, kind="ExternalInput").ap()
    fbias = nc.dram_tensor("fbias", [128, 1], F32, kind="ExternalInput").ap()
    oT = nc.dram_tensor("oT", [128, TOK], BF16, kind="ExternalOutput").ap()
    P = Prog(nc)
    xT_v = xT.rearrange("(kc p) t -> p kc t", p=128)
    with ExitStack() as st:
        def SB(name, shape, dt, bs=None, dsem=False):
            return TT(P, st, name, shape, dt, bs=bs, dsem=dsem)

        ones_bf = SB("ones_bf", [128, 128], BF16)
        ones_f = SB("ones_f", [128, 64], F32)
        eps_c = SB("eps_c", [128, 1], F32)
        one_c = SB("one_c", [128, 1], F32)
        ident = SB("ident", [128, 128], BF16)
        M2 = SB("M2", [128, 128], BF16)
        TBH = 256
        rmask = SB("rmask", [64, TBH], F32)
        par = SB("par", [64, 8], F32, dsem=True)
        gm = SB("gm", [128, 8], F32, dsem=True)
        nfb = SB("nfb", [128, 1], F32, dsem=True)
        lbc = SB("lbc", [64, 4], F32)
        Wf = None
        Wb = SB("Wb", [128, 8, NCOL_A], BF16, dsem=True)

        memset(P, "pool", ones_bf[:], 1.0, ones_bf.r())
        memset(P, "pool", ones_f[:], 1.0, ones_f.r())
        memset(P, "pool", eps_c[:], EPS, eps_c.r())
        memset(P, "pool", one_c[:], 1.0, one_c.r())
        memset(P, "pool", ident[:], 1.0, ident.r())
        P.op("pool", lambda e: e.affine_select(out=ident[:], in_=ident[:], pattern=[[-1, 128]], compare_op=ALU.is_equal,
                                               fill=0.0, base=0, channel_multiplier=1), ident.r(), ident.r())
        memset(P, "pool", M2[:], 1.0, M2.r())
        P.op("pool", lambda e: e.affine_select(out=M2[:], in_=M2[:], pattern=[[1, 128]], compare_op=ALU.is_ge,
                                               fill=0.0, base=0, channel_multiplier=-1), M2.r(), M2.r())
        memset(P, "pool", M2[0:64, 64:128], 0.0, M2.r())
        memset(P, "pool", rmask[:], 1.0, rmask.r())
        memset(P, "pool", rmask[:].rearrange("p (c l) -> p c l", l=64)[:, :, 0:1], 0.0, rmask.r())

        dma(P, "sp", par[:], pA, (), par.r(), par.dsem)
        dma(P, "sp", gm[:], gmix, (), gm.r(), gm.dsem)
        dma(P, "sp", nfb[:], fbias, (), nfb.r(), nfb.dsem)
        dma(P, "pool", Wb[:], wA.rearrange("(kc p) n -> p kc n", p=128), (), Wb.r(), Wb.dsem)
        for kc in range(8):
            ts(P, "dve", Wb[:, kc, :], Wb[:, kc, :], gm[:, kc:kc + 1], ALU.mult, Wb.r() + gm.r(), Wb.r())
        ts(P, "dve", nfb[:], nfb[:], -1.0, ALU.mult, nfb.r(), nfb.r())
        tt(P, "dve", lbc[:, 3:4], par[:, 1:2], par[:, 0:1], ALU.subtract, par.r(), lbc.r())
        act(P, lbc[:, 3:4], lbc[:, 3:4], AF.Exp, lbc.r(), lbc.r())
        ts(P, "dve", lbc[:, 3:4], lbc[:, 3:4], 1.0, ALU.add, lbc.r(), lbc.r())
        recip(P, lbc[:, 0:1], lbc[:, 3:4], lbc.r(), lbc.r())
        ts(P, "dve", lbc[:, 1:2], lbc[:, 0:1], -1.0, ALU.mult, lbc.r(), lbc.r(), 1.0, ALU.add)
        ts(P, "dve", lbc[:, 2:3], par[:, 3:4], 0.125, ALU.mult, par.r(), lbc.r())
        lb_ap, oml_ap, gq8_ap = lbc[:, 0:1], lbc[:, 1:2], lbc[:, 2:3]
        on_ap, gk_ap = par[:, 2:3], par[:, 4:5]

        Q = SB("Q", [64, T], BF16, bs=512)
        Fh = SB("Fh", [64, T], BF16, bs=512, dsem=True)
        G = SB("G", [64, T], BF16, bs=512)
        BQ = SB("BQ", [70, T], BF16, bs=512, dsem=True)
        BK = SB("BK", [70, T], BF16, bs=128, dsem=True)
        VAB = SB("VAB", [128, T // 128, 129], BF16, bs=129)
        XB = [SB("XB%d" % i, [128, 8, 512], BF16, dsem=True) for i in range(2)]
        SQ = SB("SQ", [128, 8, 512], BF16)
        LNV = SB("LNV", [128, 512], F32)
        RSTD = [SB("RSTD%d" % i, [128, 512], F32) for i in range(2)]
        ZF = SB("ZF", [128, 512], F32)
        CC = [SB("CC%d" % i, [128, 512], F32) for i in range(2)]
        R1 = SB("R1", [128, 512], F32)
        AUG = [SB("AUG0", [128, 6, 512], BF16)] * 2
        RCS = SB("RCS", [128, 4], F32)
        PSB = [TT(P, st, "ps%d" % i, [128, 512], F32, psum=True) for i in range(8)]

        HT2 = [[SB("HT%d_%d" % (k_, i), [64, TBH], F32) for i in range(5)] for k_ in range(2)]
        KHT2 = [SB("KHT%d" % k_, [64, TBH], BF16) for k_ in range(2)]
        ELC0 = SB("ELC0", [64, T // 64], F32)

        KH = SB("KH", [128, T // 128, 64], BF16, bs=64)
        ELC = SB("ELC", [64, T // 64], F32)
        SBF = SB("SBF", [64, T], BF16)
        Z64 = SB("Z64", [64, 64], BF16)
        memset(P, "pool", Z64[:], 0.0, Z64.r())
        ATS = [SB("ATS%d" % i, [128, 512], BF16) for i in range(2)]
        OAS = [SB("OAS%d" % i, [64, 512], BF16, dsem=True) for i in range(2)]
        ACCS = SB("ACCS", [65, 512], F32)
        RDEN = SB("RDEN", [64, 512], F32)
        OBS = [SB("OBS%d" % i, [64, 512], BF16, dsem=True) for i in range(2)]
        RONE = SB("RONE", [128, 512], BF16)
        memset(P, "pool", RONE[:], 1.0, RONE.r())
        memset(P, "pool", VAB[:, :, 128:129], 1.0, VAB.r())
        memset(P, "pool", BQ[64:70, :], 1.0, BQ.r())
        memset(P, "pool", BK[64:70, :], 1.0, BK.r())

        PTB = [SB("PTB%d" % i, [128, 512], BF16) for i in range(4)]
        groups = [(0, 128, Q, Fh), (128, 128, G, BQ), (256, 65, BK, None)]
        blk = 0
        for b in range(nbatch):
            for n in range(T // 512):
                tok0 = b * T + n * 512
                cols = (n * 512, (n + 1) * 512)
                xb = XB[blk % 2]
                rs = RSTD[blk % 2]
                dma(P, "pool", xb[:], xT_v[:, :, tok0:tok0 + 512], (), xb.r(), xb.dsem)
                act(P, SQ[:], xb[:], AF.Square, xb.r(), SQ.r())
                ss = PSB[0]
                for kc in range(8):
                    mm(P, ss[:, :], ones_bf[:, :], SQ[:, kc, :], kc == 0, kc == 7, SQ.r() + ones_bf.r(), ss.r())
                act(P, LNV[:], ss[:, :], AF.Ln, ss.r() + eps_c.r(), LNV.r(), scale=1.0 / 1024, bias=eps_c[:])
                act(P, rs[:], LNV[:], AF.Exp, LNV.r(), rs.r(), scale=-0.5)
                for gi, (c0, M, dest, dest2) in enumerate(groups):
                    ps = PSB[[[1, 4], [2, 2], [3, 5]][gi][blk % 2]]
                    for kc in range(8):
                        mm(P, ps[0:M, :], Wb[:, kc, c0:c0 + M], xb[:, kc, :], kc == 0, kc == 7, Wb.r() + xb.r(), ps.r())
                    tt(P, "dve", dest[0:64, cols[0]:cols[1]], ps[0:64, :], rs[0:64, :], ALU.mult,
                       ps.r() + rs.r(), dest.r(*cols))
                    if dest2 is not None:
                        stg = PTB[gi * 2 + blk % 2]
                        tt(P, "dve", stg[64:128, :], ps[64:128, :], rs[64:128, :], ALU.mult, ps.r() + rs.r(), stg.r())
                        dma(P, "sp", dest2[0:64, cols[0]:cols[1]], stg[64:128, :], stg.r(), dest2.r(*cols), dest2.dsem)
                    if M == 65:
                        cc = CC[blk % 2]
                        ccp = CC[(blk + 1) % 2]
                        aug = AUG[blk % 2]
                        r64 = slice(64, 65)
                        tt(P, "dve", ZF[r64, :], ps[r64, :], rs[r64, :], ALU.mult, ps.r() + rs.r(), ZF.r())
                        act(P, ZF[r64, :], ZF[r64, :], AF.Exp, ZF.r() + nfb.r(), ZF.r(), scale=-1.0, bias=nfb[r64, :])
                        act(P, ZF[r64, :], ZF[r64, :], AF.Ln, ZF.r() + one_c.r(), ZF.r(), scale=1.0, bias=one_c[r64, :])
                        init = 0.0 if n == 0 else ccp[r64, 511:512]
                        P.op("dve", lambda e, cc=cc, init=init: e.tensor_tensor_scan(
                            out=cc[64:65, :], data0=RONE[64:65, :],
                            data1=ZF[64:65, :], initial=init, op0=ALU.mult, op1=ALU.add),
                            ZF.r() + ccp.r() + RONE.r(), cc.r())
                        cp(P, "dve", aug[r64, 0, :], cc[r64, :], cc.r(), aug.r())
                        tt(P, "dve", R1[r64, :], cc[r64, :], aug[r64, 0, :], ALU.subtract, cc.r() + aug.r(), R1.r())
                        cp(P, "dve", aug[r64, 1, :], R1[r64, :], R1.r(), aug.r())
                        tt(P, "dve", R1[r64, :], R1[r64, :], aug[r64, 1, :], ALU.subtract, R1.r() + aug.r(), R1.r())
                        cp(P, "dve", aug[r64, 2, :], R1[r64, :], R1.r(), aug.r())
                        ts(P, "dve", aug[r64, 3:6, :], aug[r64, 0:3, :], -1.0, ALU.mult, aug.r(), aug.r())
                        for i in range(3):
                            dma(P, "sp", BK[67 + i:68 + i, cols[0]:cols[1]], aug[r64, i, :], aug.r(), BK.r(*cols), BK.dsem)
                            dma(P, "sp", BQ[64 + i:65 + i, cols[0]:cols[1]], aug[r64, 3 + i, :], aug.r(), BQ.r(*cols), BQ.dsem)
                ptm = PSB[6]
                prc = PSB[7]
                for sub in range(4):
                    sc = slice(sub * 128, (sub + 1) * 128)
                    for kc in range(8):
                        mm(P, ptm[:, sc], xb[:, kc, sc], Wb[:, kc, 321:449], kc == 0, kc == 7, Wb.r() + xb.r(), ptm.r())
                    mm(P, prc[:, sub:sub + 1], rs[0:1, sc], ones_f[0:1, 0:1], True, True, rs.r() + ones_f.r(), prc.r())
                cp(P, "dve", RCS[:, 0:4], prc[:, 0:4], prc.r(), RCS.r())
                for sub in range(4):
                    ti = n * 4 + sub
                    sc = slice(sub * 128, (sub + 1) * 128)
                    ts(P, "dve", VAB[:, ti, 0:128], ptm[:, sc], RCS[:, sub:sub + 1], ALU.mult,
                       ptm.r() + RCS.r(), VAB.r(ti * 129, ti * 129 + 129))
                blk += 1
            if do_fox:
                for n in range(T // 512):
                    cs = slice(n * 512, (n + 1) * 512)
                    for (tl, gap, bank) in ((BQ, gq8_ap, 1), (BK, gk_ap, 2)):
                        ps = PSB[bank]
                        act(P, SQ[0:64, 0, :], tl[0:64, cs], AF.Square, tl.r(n * 512, n * 512 + 512), SQ.r())
                        mm(P, ps[0:64, :], ones_bf[0:64, 0:64], SQ[0:64, 0, :], True, True, SQ.r() + ones_bf.r(), ps.r())
                        act(P, LNV[0:64, :], ps[0:64, :], AF.Ln, ps.r() + eps_c.r(), LNV.r(), scale=1.0 / 64, bias=eps_c[0:64, :])
                        act(P, LNV[0:64, :], LNV[0:64, :], AF.Exp, LNV.r(), LNV.r(), scale=-0.5)
                        stt(P, tl[0:64, cs], tl[0:64, cs], gap, LNV[0:64, :], ALU.mult, ALU.mult,
                            tl.r(n * 512, n * 512 + 512) + LNV.r() + par.r() + lbc.r(), tl.r(n * 512, n * 512 + 512))
            if do_hgrn:
                def hg_elem(par_):
                    for sbk in range(par_, T // TBH, 2):
                        c0, c1 = sbk * TBH, (sbk + 1) * TBH
                        cs = slice(c0, c1)
                        t1, t2, t3, t4, t5 = HT2[sbk % 2]
                        kht = KHT2[sbk % 2]
                        act(P, t1[:], Fh[0:64, cs], AF.Exp, Fh.r(c0, c1), t1.r(), scale=-1.0)
                        yield
                        act(P, t1[:], t1[:], AF.Identity, t1.r() + one_c.r(), t1.r(), scale=1.0, bias=one_c[0:64, :])
                        yield
                        recip(P, t1[:], t1[:], t1.r(), t1.r())
                        yield
                        ts(P, "dve", t1[:], t1[:], oml_ap, ALU.mult, t1.r() + lbc.r(), t1.r(), lb_ap, ALU.add)
                        yield
                        act(P, t2[:], t1[:], AF.Ln, t1.r(), t2.r())
                        yield
                        P.op("dve", lambda e, t3=t3, t2=t2: e.tensor_tensor_scan(out=t3[:], data0=rmask[:], data1=t2[:], initial=0.0,
                                                                                 op0=ALU.mult, op1=ALU.add), t2.r() + rmask.r(), t3.r())
                        yield
                        act(P, t1[:], t1[:], AF.Identity, t1.r() + one_c.r(), t1.r(), scale=-1.0, bias=one_c[0:64, :])
                        yield
                        act(P, t4[:], t3[:], AF.Exp, t3.r(), t4.r())
                        yield
                        act(P, t5[:], Q[0:64, cs], AF.Exp, Q.r(c0, c1), t5.r(), scale=-1.0)
                        yield
                        act(P, t5[:], t5[:], AF.Identity, t5.r() + one_c.r(), t5.r(), scale=1.0, bias=one_c[0:64, :])
                        yield
                        recip(P, t5[:], t5[:], t5.r(), t5.r())
                        yield
                        tt(P, "dve", t5[:], Q[0:64, cs], t5[:], ALU.mult, Q.r(c0, c1) + t5.r(), t5.r())
                        yield
                        tt(P, "dve", Q[0:64, cs], t5[:], t4[:], ALU.mult, t5.r() + t4.r(), Q.r(c0, c1))
                        yield
                        act(P, t4[:], t3[:], AF.Exp, t3.r(), t4.r(), scale=-1.0)
                        yield
                        tt(P, "dve", Fh[0:64, cs], t1[:], t4[:], ALU.mult, t1.r() + t4.r(), Fh.r(c0, c1))
                        yield
                        nch = TBH // 64
                        ch0 = sbk * nch
                        act(P, ELC[0:64, ch0:ch0 + nch], t3[:].rearrange("p (c l) -> p c l", l=64)[:, :, 63],
                            AF.Exp, t3.r(), ELC.r())
                        yield
                        tt(P, "dve", kht[:].rearrange("p (c l) -> p c l", l=64), Fh[0:64, cs].rearrange("p (c l) -> p c l", l=64),
                           ELC[0:64, ch0:ch0 + nch].unsqueeze(2).broadcast_to([64, nch, 64]), ALU.mult,
                           Fh.r(c0, c1) + ELC.r(), kht.r())
                        yield
                        pst = PSB[7] if sbk % 2 == 0 else PSB[0]
                        pstb = pst.h.bitcast(BF16)
                        ntl = TBH // 128
                        for j in range(ntl):
                            P.op("pe", lambda e, j=j, pstb=pstb, kht=kht: e.transpose(out=pstb[:, j * 64:(j + 1) * 64], in_=kht[0:64, j * 128:(j + 1) * 128],
                                                                  identity=ident[0:64, 0:64]), kht.r() + ident.r(), pst.r())
                            yield
                        ti0 = sbk * ntl
                        cp(P, "act", KH[:, ti0:ti0 + ntl, :], pstb[:, 0:ntl * 64].rearrange("p (a b) -> p a b", b=64),
                           pst.r(), KH.r(ti0 * 64, (ti0 + ntl) * 64))
                        yield
                        act(P, t2[:], G[0:64, cs], AF.Exp, G.r(c0, c1), t2.r(), scale=-1.0)
                        yield
                        act(P, t2[:], t2[:], AF.Identity, t2.r() + one_c.r(), t2.r(), scale=1.0, bias=one_c[0:64, :])
                        yield
                        recip(P, t2[:], t2[:], t2.r(), t2.r())
                        yield
                        tt(P, "dve", G[0:64, cs], G[0:64, cs], t2[:], ALU.mult, G.r(c0, c1) + t2.r(), G.r(c0, c1))
                        yield

                hg_gens = [hg_elem(0), hg_elem(1)]
            if do_fox:
                k = 0
                for j in range(T // 512):
                    acc = PSB[4 + j % 2]
                    nt = 4 * j + 4
                    items = []
                    for i in range(nt):
                        m = i - 4 * j
                        c0 = 128 * m if m >= 0 else 0
                        items.append((i, m, c0))
                    def qk(it, k):
                        i, m, c0 = it
                        sc = PSB[1 + k % 3]
                        mm(P, sc[:, c0:512], BK[0:70, i * 128:(i + 1) * 128], BQ[0:70, j * 512 + c0:(j + 1) * 512], True, True,
                           BK.r(i * 128, i * 128 + 128) + BQ.r(j * 512, j * 512 + 512), sc.r())
                    def rest(it, k):
                        i, m, c0 = it
                        sc = PSB[1 + k % 3]
                        pt = PTB[k % 4]
                        act(P, pt[:, c0:512], sc[:, c0:512], AF.Exp, sc.r(), pt.r())
                        if m >= 0:
                            P.op("pool", lambda e, pt=pt, c0=c0: e.affine_select(out=pt[:, c0:c0 + 128], in_=pt[:, c0:c0 + 128], pattern=[[1, 128]],
                                 compare_op=ALU.is_ge, fill=0.0, base=0, channel_multiplier=-1), pt.r(), pt.r())
                        mm(P, acc[0:65, c0:512], VAB[:, i, 64:129], pt[:, c0:512], i == 0, i == nt - 1,
                           VAB.r(i * 129, i * 129 + 129) + pt.r(), acc.r())
                    LOOK = 2
                    for idx in range(nt + LOOK):
                        if idx < nt:
                            qk(items[idx], k + idx)
                        if idx >= LOOK:
                            rest(items[idx - LOOK], k + idx - LOOK)
                            if do_hgrn and os.environ.get("HG_NOILV") is None:
                                for g_ in hg_gens:
                                    next(g_, None)
                    k += nt
                    cp(P, "dve", ACCS[0:65, :], acc[0:65, :], acc.r(), ACCS.r())
                    pden = PSB[6]
                    mm(P, pden[0:64, :], ones_f[64:65, 0:64], ACCS[64:65, :], True, True, ones_f.r() + ACCS.r(), pden.r())
                    recip(P, RDEN[:], pden[0:64, :], pden.r(), RDEN.r())
                    obs = OBS[j % 2]
                    tt(P, "pool", obs[:], ACCS[0:64, :], RDEN[:], ALU.mult, ACCS.r() + RDEN.r(), obs.r())
                    dma(P, "sp", oT[64:128, b * T + j * 512: b * T + (j + 1) * 512], obs[:], obs.r(), (), obs.dsem)
            if do_hgrn:
                for g_ in hg_gens:
                    for _ in g_:
                        pass
                DSV = BQ
                NC_ = T // 64
                for g4 in range(T // 512):
                    for c in range(8 * g4, 8 * g4 + 8):
                        ti, hh = c // 2, c % 2
                        pds = PSB[4 + (g4 % 2) * 2 + hh]
                        slot = (c % 8) * 64
                        hs = slice(hh * 64, (hh + 1) * 64)
                        mm(P, pds[0:64, slot:slot + 64], KH[hs, ti, :], VAB[hs, ti, 0:64], True, True,
                           KH.r(ti * 64, ti * 64 + 64) + VAB.r(ti * 129, ti * 129 + 129), pds.r())
                    for hh in range(2):
                        pds = PSB[4 + (g4 % 2) * 2 + hh]
                        src = pds[0:64, :].rearrange("p (a b) -> p a b", b=128)[:, :, hh * 64:(hh + 1) * 64]
                        dst = DSV[0:64, :].rearrange("p (v c) -> p c v", c=NC_)[:, 8 * g4 + hh:8 * g4 + 8:2, :]
                        cp(P, "dve" if hh == 0 else "act", dst, src, pds.r(), DSV.r())
                cp(P, "pool", ELC0[:], ELC[:], ELC.r(), ELC0.r())
                memset(P, "pool", ELC0[:, 0:1], 0.0, ELC0.r())
                D0 = BK
                cp(P, "act", D0[0:64, :].rearrange("p (v c) -> p v c", c=NC_), ELC0[:, :].unsqueeze(1).broadcast_to([64, 64, NC_]),
                   ELC0.r(), D0.r())
                P.op("dve", lambda e: e.tensor_tensor_scan(out=SBF[:, :], data0=D0[0:64, :], data1=DSV[0:64, :],
                                                           initial=0.0, op0=ALU.mult, op1=ALU.add), D0.r() + DSV.r(), SBF.r())
                for g4 in range(T // 512):
                    pat = PSB[6 + g4 % 2]
                    for j in range(4):
                        ti = g4 * 4 + j
                        tcs = slice(ti * 128, (ti + 1) * 128)
                        mm(P, pat[:, j * 128:(j + 1) * 128], Fh[0:64, tcs], Q[0:64, tcs], True, True,
                           Fh.r(ti * 128, ti * 128 + 128) + Q.r(ti * 128, ti * 128 + 128), pat.r())
                    ats = ATS[g4 % 2]
                    tt(P, "dve", ats[:].rearrange("p (a b) -> p a b", b=128), pat[:, :].rearrange("p (a b) -> p a b", b=128),
                       M2[:].unsqueeze(1).broadcast_to([128, 4, 128]), ALU.mult, pat.r() + M2.r(), ats.r())
                    po = PSB[1 + g4 % 2]
                    for jj in range(4):
                        tj = g4 * 4 + jj
                        for hh in range(2):
                            c = 2 * tj + hh
                            ccs = slice(c * 64, (c + 1) * 64)
                            oc = slice(jj * 128 + hh * 64, jj * 128 + hh * 64 + 64)
                            mm(P, po[0:64, oc], VAB[:, tj, 0:64], ats[:, oc], True, False,
                               VAB.r(tj * 129, tj * 129 + 129) + ats.r(), po.r())
                            st_ap = Z64[:, :] if c == 0 else SBF[:, :].rearrange("p (v c) -> p v c", c=NC_)[:, :, c - 1]
                            mm(P, po[0:64, oc], st_ap, Q[0:64, ccs], False, True,
                               SBF.r() + Z64.r() + Q.r(c * 64, c * 64 + 64), po.r())
                    gcs = slice(g4 * 512, (g4 + 1) * 512)
                    pn = PSB[3]
                    sq64 = SQ
                    lnv = RSTD[g4 % 2]
                    oaf = (ZF, R1)[g4 % 2]
                    act(P, sq64[0:64, g4 % 2, :], po[0:64, :], AF.Square, po.r(), sq64.r())
                    mm(P, pn[0:64, :], ones_bf[0:64, 0:64], sq64[0:64, g4 % 2, :], True, True, sq64.r() + ones_bf.r(), pn.r())
                    act(P, lnv[0:64, :], pn[0:64, :], AF.Ln, pn.r() + eps_c.r(), lnv.r(), scale=1.0 / 64, bias=eps_c[0:64, :])
                    act(P, lnv[0:64, :], lnv[0:64, :], AF.Exp, lnv.r(), lnv.r(), scale=-0.5)
                    stt(P, oaf[0:64, :], po[0:64, :], on_ap, lnv[0:64, :], ALU.mult, ALU.mult, po.r() + lnv.r() + par.r(), oaf.r())
                    oas = OAS[g4 % 2]
                    tt(P, "dve", oas[:], oaf[0:64, :], G[0:64, gcs], ALU.mult, oaf.r() + G.r(g4 * 512, g4 * 512 + 512), oas.r())
                    dma(P, "sp", oT[0:64, b * T + g4 * 512: b * T + (g4 + 1) * 512], oas[:], oas.r(), (), oas.dsem)
        if os.environ.get('HG_DBG'):
            dsd2 = P.new_dsem("dbg2")
            for nm, tl, shp, dt_ in (("dKH", KH, [128, (T // 128) * 64], BF16), ("dKt", Fh, [64, T], BF16), ("dQt", Q, [64, T], BF16), ("dELC", ELC, [64, T // 64], F32), ("dSBF", SBF, [64, T], BF16)):
                dd = nc.dram_tensor(nm, shp, dt_, kind="ExternalOutput").ap()
                src = tl[:] if len(tl.h.shape) == 2 else tl[:].rearrange("p a b -> p (a b)")
                dma(P, "sp", dd, src, tl.r(), (), dsd2)
        if debug:
            dsd = P.new_dsem("dbg")
            for nm, tl, shp in (("dQ", Q, [64, T]), ("dF", Fh, [64, T]), ("dG", G, [64, T]), ("dBQ", BQ, [70, T]), ("dBK", BK, [70, T]), ("dV", VAB, [128, (T // 128) * 129])):
                dd = nc.dram_tensor(nm, shp, BF16, kind="ExternalOutput").ap()
                src = tl[:] if nm != "dV" else tl[:].rearrange("p a b -> p (a b)")
                dma(P, "sp", dd, src, tl.r(), (), dsd)
        P.replay()
    return nc


NT = 2048


def build_B():
    nc = bass.Bass("TRN2", target_bir_lowering=False)
    hT_in = nc.dram_tensor("hT_in", [1024, NT], F32, kind="ExternalInput").ap()
    oT_in = nc.dram_tensor("oT_in", [1024, NT], BF16, kind="ExternalInput").ap()
    w_out = nc.dram_tensor("w_out", [1024, 1024], F32, kind="ExternalInput").ap()
    gffn = nc.dram_tensor("gffn", [128, 8], F32, kind="ExternalInput").ap()
    w_r = nc.dram_tensor("w_r", [1024, 36], F32, kind="ExternalInput").ap()
    w_gate = nc.dram_tensor("w_gate", [32, 1024, 512], F32, kind="ExternalInput").ap()
    w_up = nc.dram_tensor("w_up", [32, 1024, 512], F32, kind="ExternalInput").ap()
    w_down = nc.dram_tensor("w_down", [32, 512, 1024], F32, kind="ExternalInput").ap()
    hT_out = nc.dram_tensor("hT_out", [1024, NT], F32, kind="ExternalOutput").ap()
    P = Prog(nc)
    hT_v = hT_in.rearrange("(kc p) t -> p kc t", p=128)
    oT_v = oT_in.rearrange("(kc p) t -> p kc t", p=128)
    ho_v = hT_out.rearrange("(kc p) t -> p kc t", p=128)
    with ExitStack() as st:
        def SB(name, shape, dt, bs=None, dsem=False):
            return TT(P, st, name, shape, dt, bs=bs, dsem=dsem)

        ones_bf = SB("ones_bf", [128, 128], BF16)
        eps_c = SB("eps_c", [128, 1], F32)
        identf = SB("identf", [128, 128], F32)
        SEL = SB("SEL", [32, 32, 128], BF16)
        gf = SB("gf", [128, 8], F32, dsem=True)
        WR = SB("WR", [128, 8, 36], F32, dsem=True)
        HM = SB("HM", [128, 8, NT], F32, bs=512, dsem=True)
        U = SB("U", [128, 8, NT], BF16, bs=512)
        WBUF = [SB("WB%d" % i, [128, 12288], BF16, dsem=True) for i in range(2)]
        OTB = [SB("OTB%d" % i, [128, 8, 512], BF16, dsem=True) for i in range(2)]
        HID = [SB("HID%d" % i, [128, 4, 512], BF16) for i in range(2)]
        SIL = [SB("SIL%d" % i, [128, 512], BF16) for i in range(2)]
        S2 = [SB("S2%d" % i, [128, 512], BF16) for i in range(2)]
        CREP = [SB("CREP%d" % i, [128, 512], BF16) for i in range(2)]
        CT = SB("CT", [32, NT], BF16, bs=128)
        LNV = SB("LNV", [128, 512], F32)
        RSTD = SB("RSTD", [128, 512], F32)
        RLA = SB("RLA", [128, NT // 128, 36], F32)
        R16 = SB("R16", [128, 8, NT // 128], F32)
        OHA = SB("OHA", [128, NT // 128, 4], F32)
        EGA = SB("EGA", [128, NT // 128, 4], F32)
        SELA = SB("SELA", [128, NT // 128, 8], F32)
        TMP8 = SB("TMP8", [128, NT // 128, 8], F32)
        E1A = SB("E1A", [128, NT // 128, 8], F32)
        E2A = SB("E2A", [128, NT // 128, 8], F32)
        COMBA = SB("COMBA", [128, NT // 128, 32], F32)
        RL = SB("RL", [128, 36], F32)
        RT = SB("RT", [128, 16], F32)
        OH = SB("OH", [128, 4], F32)
        EG = SB("EG", [128, 4], F32)
        SEL8 = SB("SEL8", [128, 8], F32)
        M8 = SB("M8", [128, 8], F32)
        WA8 = SB("WA8", [128, 8], F32)
        WB8 = SB("WB8", [128, 8], F32)
        COMB = SB("COMB", [128, 32], F32)
        PSB = [TT(P, st, "ps%d" % i, [128, 512], F32, psum=True) for i in range(8)]
        Wo = WBUF[1]
        Wo_v = Wo[:, 0:8192].rearrange("p (a b) -> p a b", b=1024)
        UFt = WBUF[0]
        UF_v = UFt.h.bitcast(F32)[:, 0:4096].rearrange("p (a b) -> p a b", b=512)
        SQt = HID[0]
        SQ = SB("SQ", [128, 8, 512], BF16)

        memset(P, "pool", ones_bf[:], 1.0, ones_bf.r())
        memset(P, "pool", eps_c[:], EPS, eps_c.r())
        memset(P, "pool", identf[:], 1.0, identf.r())
        P.op("pool", lambda e: e.affine_select(out=identf[:], in_=identf[:], pattern=[[-1, 128]], compare_op=ALU.is_equal,
                                               fill=0.0, base=0, channel_multiplier=1), identf.r(), identf.r())
        memset(P, "pool", SEL[:], 1.0, SEL.r())
        P.op("pool", lambda e: e.affine_select(out=SEL[:], in_=SEL[:], pattern=[[-1, 32], [0, 128]], compare_op=ALU.is_equal,
                                               fill=0.0, base=0, channel_multiplier=1), SEL.r(), SEL.r())
        dma(P, "sp", gf[:], gffn, (), gf.r(), gf.dsem)
        dma(P, "sp", WR[:], w_r.rearrange("(kc p) n -> p kc n", p=128), (), WR.r(), WR.dsem)
        dma(P, "pool", Wo_v, w_out.rearrange("(kc p) n -> p kc n", p=128), (), Wo.r(), Wo.dsem)
        for h2 in range(2):
            dma(P, "sp", HM[:, :, h2 * 1024:(h2 + 1) * 1024], hT_v[:, :, h2 * 1024:(h2 + 1) * 1024], (), HM.r(), HM.dsem)

        for blk in range(NT // 512):
            cs = slice(blk * 512, (blk + 1) * 512)
            ot = OTB[blk % 2]
            dma(P, "sp", ot[:], oT_v[:, :, cs], (), ot.r(), ot.dsem)
            for dc in range(8):
                ps = PSB[dc % 2]
                for kc in range(8):
                    mm(P, ps[:, :], Wo_v[:, kc, dc * 128:(dc + 1) * 128], ot[:, kc, :], kc == 0, kc == 7, Wo.r() + ot.r(), ps.r())
                lo = dc * NT + blk * 512
                tt(P, "dve", HM[:, dc, cs], HM[:, dc, cs], ps[:, :], ALU.add, HM.r(lo, lo + 512) + ps.r(), HM.r(lo, lo + 512))
            hm_blk = []
            for dc in range(8):
                hm_blk += HM.r(dc * NT + blk * 512, dc * NT + blk * 512 + 512)
            act(P, SQ[:], HM[:, :, cs], AF.Square, hm_blk, SQ.r())
            ss = PSB[2]
            for kc in range(8):
                mm(P, ss[:, :], ones_bf[:, :], SQ[:, kc, :], kc == 0, kc == 7, SQ.r() + ones_bf.r(), ss.r())
            act(P, LNV[:], ss[:, :], AF.Ln, ss.r() + eps_c.r(), LNV.r(), scale=1.0 / 1024, bias=eps_c[:])
            act(P, RSTD[:], LNV[:], AF.Exp, LNV.r(), RSTD.r(), scale=-0.5)
            u_blk = []
            for dc in range(8):
                lo = dc * NT + blk * 512
                stt(P, UF_v[:, dc, :], HM[:, dc, cs], gf[:, dc:dc + 1], RSTD[:], ALU.mult, ALU.mult,
                    HM.r(lo, lo + 512) + gf.r() + RSTD.r(), UFt.r())
                u_blk += U.r(lo, lo + 512)
            cp(P, "pool", U[:, :, cs], UF_v, UFt.r(), u_blk)
            pr = PSB[3]
            for sub in range(4):
                scs = slice(sub * 128, (sub + 1) * 128)
                for dc in range(8):
                    mm(P, pr[:, sub * 36:(sub + 1) * 36], UF_v[:, dc, scs], WR[:, dc, :], dc == 0, dc == 7, UFt.r() + WR.r(), pr.r())
            cp(P, "dve", RLA[:, blk * 4:(blk + 1) * 4, :].rearrange("p a b -> p (a b)"), pr[:, 0:144], pr.r(), RLA.r())

        S_ = NT // 128
        def b3(ap2, n):
            return ap2.unsqueeze(2).broadcast_to([128, S_, n])
        GL = RLA[:, :, 0:4]
        P.op("dve", lambda e: e.tensor_reduce(out=R16[:, 0, :], in_=GL, axis=AX.X, op=ALU.max), RLA.r(), R16.r())
        tt(P, "dve", OHA[:], GL, b3(R16[:, 0, :], 4), ALU.is_equal, RLA.r() + R16.r(), OHA.r())
        tt(P, "dve", EGA[:], GL, b3(R16[:, 0, :], 4), ALU.subtract, RLA.r() + R16.r(), EGA.r())
        act(P, EGA[:], EGA[:], AF.Exp, EGA.r(), EGA.r())
        P.op("dve", lambda e: e.tensor_reduce(out=R16[:, 1, :], in_=EGA[:], axis=AX.X, op=ALU.add), EGA.r(), R16.r())
        recip(P, R16[:, 2, :], R16[:, 1, :], R16.r(), R16.r())
        tt(P, "dve", SELA[:], RLA[:, :, 4:12], b3(OHA[:, :, 0], 8), ALU.mult, RLA.r() + OHA.r(), SELA.r())
        for g in range(1, 4):
            tt(P, "dve", TMP8[:], RLA[:, :, 4 + 8 * g:12 + 8 * g], b3(OHA[:, :, g], 8), ALU.mult, RLA.r() + OHA.r(), TMP8.r())
            tt(P, "dve", SELA[:], SELA[:], TMP8[:], ALU.add, SELA.r() + TMP8.r(), SELA.r())
        P.op("dve", lambda e: e.tensor_reduce(out=R16[:, 3, :], in_=SELA[:], axis=AX.X, op=ALU.max), SELA.r(), R16.r())
        tt(P, "dve", E1A[:], SELA[:], b3(R16[:, 3, :], 8), ALU.is_equal, SELA.r() + R16.r(), E1A.r())
        ts(P, "dve", TMP8[:], E1A[:], -1.0e30, ALU.mult, E1A.r(), TMP8.r())
        tt(P, "dve", TMP8[:], SELA[:], TMP8[:], ALU.add, SELA.r() + TMP8.r(), TMP8.r())
        P.op("dve", lambda e: e.tensor_reduce(out=R16[:, 4, :], in_=TMP8[:], axis=AX.X, op=ALU.max), TMP8.r(), R16.r())
        tt(P, "dve", E2A[:], TMP8[:], b3(R16[:, 4, :], 8), ALU.is_equal, TMP8.r() + R16.r(), E2A.r())
        tt(P, "dve", R16[:, 5, :], R16[:, 4, :], R16[:, 3, :], ALU.subtract, R16.r(), R16.r())
        act(P, R16[:, 5, :], R16[:, 5, :], AF.Exp, R16.r(), R16.r())
        ts(P, "dve", R16[:, 5, :], R16[:, 5, :], 1.0, ALU.add, R16.r(), R16.r())
        recip(P, R16[:, 6, :], R16[:, 5, :], R16.r(), R16.r())
        ts(P, "dve", R16[:, 7, :], R16[:, 6, :], -1.0, ALU.mult, R16.r(), R16.r(), 1.0, ALU.add)
        tt(P, "dve", R16[:, 6, :], R16[:, 6, :], R16[:, 2, :], ALU.mult, R16.r(), R16.r())
        tt(P, "dve", R16[:, 7, :], R16[:, 7, :], R16[:, 2, :], ALU.mult, R16.r(), R16.r())
        tt(P, "dve", E1A[:], E1A[:], b3(R16[:, 6, :], 8), ALU.mult, E1A.r() + R16.r(), E1A.r())
        tt(P, "dve", E2A[:], E2A[:], b3(R16[:, 7, :], 8), ALU.mult, E2A.r() + R16.r(), E2A.r())
        tt(P, "dve", E1A[:], E1A[:], E2A[:], ALU.add, E1A.r() + E2A.r(), E1A.r())
        tt(P, "dve", COMBA[:].rearrange("p s (g e) -> p s g e", e=8),
           E1A[:].unsqueeze(2).broadcast_to([128, S_, 4, 8]), OHA[:].unsqueeze(3).broadcast_to([128, S_, 4, 8]), ALU.mult,
           E1A.r() + OHA.r(), COMBA.r())
        for q4 in range(S_ // 4):
            pt = PSB[4 + q4 % 2]
            for sub in range(4):
                sidx = q4 * 4 + sub
                P.op("pe", lambda e, pt=pt, sub=sub, sidx=sidx: e.transpose(out=pt[0:32, sub * 128:(sub + 1) * 128], in_=COMBA[:, sidx, :], identity=identf[:, :]),
                     COMBA.r() + identf.r(), pt.r())
            cp(P, "dve", CT[0:32, q4 * 512:(q4 + 1) * 512], pt[0:32, :], pt.r(), CT.r(q4 * 512, q4 * 512 + 512))

        units = [(e, blk) for e in range(32) for blk in range(NT // 512)]
        wviews = []
        for i in range(2):
            wb = WBUF[i]
            wviews.append((wb[:, 0:4096].rearrange("p (a b) -> p a b", b=512),
                           wb[:, 4096:8192].rearrange("p (a b) -> p a b", b=512),
                           wb[:, 8192:12288].rearrange("p (a b) -> p a b", b=1024)))

        def load_w(e):
            wb = WBUF[e % 2]
            Wg, Wu, Wd = wviews[e % 2]
            dma(P, "pool", Wg, w_gate[e].rearrange("(kc p) n -> p kc n", p=128), (), wb.r(), wb.dsem)
            dma(P, "pool", Wu, w_up[e].rearrange("(kc p) n -> p kc n", p=128), (), wb.r(), wb.dsem)
            dma(P, "pool", Wd, w_down[e].rearrange("(fc p) n -> p fc n", p=128), (), wb.r(), wb.dsem)

        def GU(ui):
            e, blk = units[ui]
            cs = slice(blk * 512, (blk + 1) * 512)
            wb = WBUF[e % 2]
            Wg, Wu, Wd = wviews[e % 2]
            hid = HID[ui % 2]
            crep = CREP[ui % 2]
            pc = PSB[6]
            mm(P, pc[:, :], SEL[0:32, e, :], CT[0:32, cs], True, True, SEL.r() + CT.r(blk * 512, blk * 512 + 512), pc.r())
            cp(P, "dve", crep[:], pc[:, :], pc.r(), crep.r())
            u_blk = []
            for dc in range(8):
                u_blk += U.r(dc * NT + blk * 512, dc * NT + blk * 512 + 512)
            for fc in range(4):
                pg_ = PSB[(fc % 2) * 2]
                pu_ = PSB[(fc % 2) * 2 + 1]
                fs = slice(fc * 128, (fc + 1) * 128)
                for dc in range(8):
                    mm(P, pg_[:, :], Wg[:, dc, fs], U[:, dc, cs], dc == 0, dc == 7, wb.r() + u_blk, pg_.r())
                for dc in range(8):
                    mm(P, pu_[:, :], Wu[:, dc, fs], U[:, dc, cs], dc == 0, dc == 7, wb.r() + u_blk, pu_.r())
                sil = SIL[fc % 2]
                s2 = S2[fc % 2]
                act(P, sil[:], pg_[:, :], AF.Silu, pg_.r(), sil.r())
                tt(P, "dve", s2[:], sil[:], crep[:], ALU.mult, sil.r() + crep.r(), s2.r())
                tt(P, "dve", hid[:, fc, :], s2[:], pu_[:, :], ALU.mult, s2.r() + pu_.r(), hid.r())

        def DN(ui):
            e, blk = units[ui]
            cs = slice(blk * 512, (blk + 1) * 512)
            wb = WBUF[e % 2]
            Wg, Wu, Wd = wviews[e % 2]
            hid = HID[ui % 2]
            for dc in range(8):
                py = PSB[4 + dc % 2]
                for fc in range(4):
                    mm(P, py[:, :], Wd[:, fc, dc * 128:(dc + 1) * 128], hid[:, fc, :], fc == 0, fc == 3, wb.r() + hid.r(), py.r())
                lo = dc * NT + blk * 512
                tt(P, "dve", HM[:, dc, cs], HM[:, dc, cs], py[:, :], ALU.add, HM.r(lo, lo + 512) + py.r(), HM.r(lo, lo + 512))

        nE = int(os.environ.get("KB_NE", 32))
        units = [u_ for u_ in units if u_[0] < nE]
        load_w(0)
        for ui in range(len(units) + 1):
            if ui < len(units):
                GU(ui)
            if ui >= 1:
                DN(ui - 1)
            if ui < len(units):
                e, blk = units[ui]
                if blk == 0 and e + 1 < nE:
                    load_w(e + 1)
        for h2 in range(2):
            rr = []
            for dc in range(8):
                rr += HM.r(dc * NT + h2 * 1024, dc * NT + h2 * 1024 + 1024)
            dma(P, "sp", ho_v[:, :, h2 * 1024:(h2 + 1) * 1024], HM[:, :, h2 * 1024:(h2 + 1) * 1024], rr, (), HM.dsem)
        P.replay()
    return nc


import math

NCOL_C = 384
TWO_PI = 2.0 * math.pi
LAM_INIT = 0.8 - 0.6 * math.exp(-0.3 * 1)


def build_C(nbatch=2, debug=False):
    nc = bass.Bass("TRN2", target_bir_lowering=False)
    xT = nc.dram_tensor("xT", [1024, TOK], F32, kind="ExternalInput").ap()
    wC = nc.dram_tensor("wC", [1024, NCOL_C], F32, kind="ExternalInput").ap()
    gmix = nc.dram_tensor("gmix", [128, 8], F32, kind="ExternalInput").ap()
    pC = nc.dram_tensor("pC", [128, 8], F32, kind="ExternalInput").ap()
    posd = nc.dram_tensor("pos", [2, T], I32, kind="ExternalInput").ap()
    oT = nc.dram_tensor("oT", [128, TOK], BF16, kind="ExternalOutput").ap()
    P = Prog(nc)
    xT_v = xT.rearrange("(kc p) t -> p kc t", p=128)
    with ExitStack() as st:
        def SB(name, shape, dt, bs=None, dsem=False):
            return TT(P, st, name, shape, dt, bs=bs, dsem=dsem)

        ones_bf = SB("ones_bf", [128, 128], BF16)
        BD = SB("BD", [128, 128], BF16)
        ones_f = SB("ones_f", [128, 128], F32)
        eps_c = SB("eps_c", [128, 1], F32)
        pi_c = SB("pi_c", [128, 2], F32)
        pic2 = SB("pic2", [128, 2], F32)
        ident = SB("ident", [128, 128], BF16)
        PM = SB("PM", [128, 128], BF16)
        PM2 = SB("PM2", [128, 128], BF16)
        par = SB("par", [128, 8], F32, dsem=True)
        gm = SB("gm", [128, 8], F32, dsem=True)
        cst = SB("cst", [128, 8], F32)
        invi = SB("invi", [1, 128], I32)
        invf = SB("invf", [1, 128], F32)
        Wb = SB("Wb", [128, 8, NCOL_C], BF16, dsem=True)

        memset(P, "pool", ones_bf[:], 1.0, ones_bf.r())
        memset(P, "pool", ones_f[:], 1.0, ones_f.r())
        memset(P, "pool", eps_c[:], EPS, eps_c.r())
        memset(P, "pool", BD[:], 1.0, BD.r())
        memset(P, "pool", BD[0:64, 64:128], 0.0, BD.r())
        memset(P, "pool", BD[64:128, 0:64], 0.0, BD.r())
        memset(P, "pool", pi_c[:, 0:1], math.pi, pi_c.r())
        memset(P, "pool", pi_c[:, 1:2], -TWO_PI, pi_c.r())
        memset(P, "pool", pi_c[0:32, 0:1], -math.pi, pi_c.r())
        memset(P, "pool", pi_c[0:32, 1:2], TWO_PI, pi_c.r())
        memset(P, "pool", pi_c[64:96, 0:1], -math.pi, pi_c.r())
        memset(P, "pool", pi_c[64:96, 1:2], TWO_PI, pi_c.r())
        memset(P, "pool", pic2[:, 0:1], math.pi, pic2.r())
        memset(P, "pool", PM[:], 1.0, PM.r())
        P.op("pool", lambda e: e.affine_select(out=PM[:], in_=PM[:], pattern=[[1, 128]], compare_op=ALU.is_equal,
                                               fill=0.0, base=-32, channel_multiplier=-1), PM.r(), PM.r())
        memset(P, "pool", PM[:, 0:32], 0.0, PM.r())
        memset(P, "pool", PM[:, 64:96], 0.0, PM.r())
        memset(P, "pool", PM2[:], 1.0, PM2.r())
        P.op("pool", lambda e: e.affine_select(out=PM2[:], in_=PM2[:], pattern=[[1, 128]], compare_op=ALU.is_equal,
                                               fill=0.0, base=32, channel_multiplier=-1), PM2.r(), PM2.r())
        memset(P, "pool", PM2[:, 32:64], 0.0, PM2.r())
        memset(P, "pool", PM2[:, 96:128], 0.0, PM2.r())
        tt(P, "pool", PM[:], PM[:], PM2[:], ALU.add, PM.r() + PM2.r(), PM.r())
        P.op("pool", lambda e: e.iota(invi[:].rearrange("p (a b) -> p a b", b=32), pattern=[[0, 4], [1, 32]], base=0, channel_multiplier=0),
             (), invi.r())
        cp(P, "dve", invf[:], invi[:], invi.r(), invf.r())
        act(P, invf[:], invf[:], AF.Exp, invf.r(), invf.r(), scale=-math.log(10000.0) / 32.0)

        dma(P, "sp", par[:], pC, (), par.r(), par.dsem)
        dma(P, "sp", gm[:], gmix, (), gm.r(), gm.dsem)
        dma(P, "pool", Wb[:], wC.rearrange("(kc p) n -> p kc n", p=128), (), Wb.r(), Wb.dsem)
        for kc in range(8):
            ts(P, "dve", Wb[:, kc, :], Wb[:, kc, :], gm[:, kc:kc + 1], ALU.mult, Wb.r() + gm.r(), Wb.r())
        ts(P, "dve", cst[:, 0:1], par[:, 0:1], 0.125, ALU.mult, par.r(), cst.r())
        ts(P, "dve", cst[:, 3:4], par[:, 6:7], 1.0 - LAM_INIT, ALU.mult, par.r(), cst.r())
        PSB = [TT(P, st, "ps%d" % i, [128, 512], F32, psum=True) for i in range(8)]
        tt(P, "dve", cst[:, 4:5], par[:, 2:3], par[:, 3:4], ALU.mult, par.r(), cst.r())
        tt(P, "dve", cst[:, 5:6], par[:, 4:5], par[:, 5:6], ALU.mult, par.r(), cst.r())
        mm(P, PSB[0][:, 0:2], ones_f[:, :], cst[:, 4:6], True, True, ones_f.r() + cst.r(), PSB[0].r())
        act(P, cst[:, 6:8], PSB[0][:, 0:2], AF.Exp, PSB[0].r(), cst.r())
        tt(P, "dve", cst[:, 2:3], cst[:, 7:8], cst[:, 6:7], ALU.subtract, cst.r(), cst.r())
        ts(P, "dve", cst[:, 2:3], cst[:, 2:3], -LAM_INIT, ALU.add, cst.r(), cst.r())
        gq_ap, gk_ap, nlam_ap, gs_ap = cst[:, 0:1], par[:, 1:2], cst[:, 2:3], cst[:, 3:4]

        QT = SB("QT", [128, T], BF16, bs=512)
        KT = SB("KT", [128, T], BF16, bs=128)
        V = SB("V", [128, T // 128, 128], BF16, bs=128)
        SINT = SB("SINT", [128, T], BF16, bs=512)
        COST = SB("COST", [128, T], BF16, bs=512)
        XB = [SB("XB%d" % i, [128, 8, 512], BF16, dsem=True) for i in range(2)]
        SQ = SB("SQ", [128, 8, 512], BF16)
        LNV = SB("LNV", [128, 512], F32)
        RSTD = [SB("RSTD%d" % i, [128, 512], F32) for i in range(2)]
        RCS = SB("RCS", [128, 4], F32)
        POSI = SB("POSI", [1, 512], I32, dsem=True)
        POSF = SB("POSF", [1, 512], F32)
        RR = [SB("RR%d" % i, [128, 512], F32) for i in range(2)]
        RI = SB("RI", [128, 512], I32)
        RF = SB("RF", [128, 512], F32)
        RMK = SB("RMK", [128, 512], F32)
        TA = SB("TA", [128, 512], BF16)
        TB_ = SB("TB", [128, 512], BF16)
        PTB = [[SB("PT%d_%d" % (m, i), [128, 512], BF16) for i in range(3)] for m in range(2)]
        FT = [SB("FT%d" % i, [128, 512], F32) for i in range(4)]
        OUTS = [SB("OUTS%d" % i, [128, 512], BF16, dsem=True) for i in range(2)]
        DACC = [SB("DACC%d" % i, [128, 512], F32) for i in range(2)]
        TMPP = [[SB("TMPP%d_%d" % (m, i), [128, 512], BF16) for i in range(2)] for m in range(2)]

        groups = [(0, QT), (128, KT)]
        blk = 0
        for b in range(nbatch):
            for n in range(T // 512):
                tok0 = b * T + n * 512
                cols = (n * 512, (n + 1) * 512)
                xb = XB[blk % 2]
                rs = RSTD[blk % 2]
                dma(P, "pool", xb[:], xT_v[:, :, tok0:tok0 + 512], (), xb.r(), xb.dsem)
                act(P, SQ[:], xb[:], AF.Square, xb.r(), SQ.r())
                ss = PSB[0]
                for kc in range(8):
                    mm(P, ss[:, :], ones_bf[:, :], SQ[:, kc, :], kc == 0, kc == 7, SQ.r() + ones_bf.r(), ss.r())
                act(P, LNV[:], ss[:, :], AF.Ln, ss.r() + eps_c.r(), LNV.r(), scale=1.0 / 1024, bias=eps_c[:])
                act(P, rs[:], LNV[:], AF.Exp, LNV.r(), rs.r(), scale=-0.5)
                for gi, (c0, dest) in enumerate(groups):
                    ps = PSB[1 + gi]
                    for kc in range(8):
                        mm(P, ps[:, :], Wb[:, kc, c0:c0 + 128], xb[:, kc, :], kc == 0, kc == 7, Wb.r() + xb.r(), ps.r())
                    tt(P, "dve", dest[:, cols[0]:cols[1]], ps[:, :], rs[:, :], ALU.mult, ps.r() + rs.r(), dest.r(*cols))
                ptm = PSB[3]
                prc = PSB[4]
                for sub in range(4):
                    sc = slice(sub * 128, (sub + 1) * 128)
                    for kc in range(8):
                        mm(P, ptm[:, sc], xb[:, kc, sc], Wb[:, kc, 256:384], kc == 0, kc == 7, Wb.r() + xb.r(), ptm.r())
                    mm(P, prc[:, sub:sub + 1], rs[0:1, sc], ones_f[0:1, 0:1], True, True, rs.r() + ones_f.r(), prc.r())
                cp(P, "dve", RCS[:, 0:4], prc[:, 0:4], prc.r(), RCS.r())
                for sub in range(4):
                    ti = n * 4 + sub
                    sc = slice(sub * 128, (sub + 1) * 128)
                    ts(P, "dve", V[:, ti, :], ptm[:, sc], RCS[:, sub:sub + 1], ALU.mult, ptm.r() + RCS.r(), V.r(ti * 128, ti * 128 + 128))
                blk += 1
            for n in range(T // 512):
                cols = (n * 512, (n + 1) * 512)
                cs = slice(*cols)
                dma(P, "sp", POSI[:], posd[b:b + 1, cs], (), POSI.r(), POSI.dsem)
                cp(P, "dve", POSF[:], POSI[:], POSI.r(), POSF.r())
                pa = PSB[5]
                mm(P, pa[:, :], invf[0:1, :], POSF[0:1, :], True, True, invf.r() + POSF.r(), pa.r())
                for which, dest in ((0, SINT), (1, COST)):
                    rr = RR[which]
                    if which == 0:
                        ts(P, "dve", rr[:], pa[:, :], 1.0 / TWO_PI, ALU.mult, pa.r(), rr.r())
                    else:
                        ts(P, "dve", rr[:], pa[:, :], 1.0 / TWO_PI, ALU.mult, pa.r(), rr.r(), 0.25, ALU.add)
                    cp(P, "dve", RI[:], rr[:], rr.r(), RI.r())
                    cp(P, "act", RF[:], RI[:], RI.r(), RF.r())
                    tt(P, "dve", rr[:], rr[:], RF[:], ALU.subtract, rr.r() + RF.r(), rr.r())
                    ts(P, "dve", RMK[:], rr[:], 0.0, ALU.is_lt, rr.r(), RMK.r())
                    tt(P, "dve", rr[:], rr[:], RMK[:], ALU.add, rr.r() + RMK.r(), rr.r())
                    if which == 0:
                        P.op("act", lambda e, rr=rr, dest=dest, cs=cs: e.activation(out=dest[:, cs], in_=rr[:], func=AF.Sin, scale=pi_c[:, 1:2], bias=pi_c[:, 0:1]),
                             rr.r() + pi_c.r(), dest.r(*cols))
                    else:
                        P.op("act", lambda e, rr=rr, dest=dest, cs=cs: e.activation(out=dest[:, cs], in_=rr[:], func=AF.Sin, scale=-TWO_PI, bias=pic2[:, 0:1]),
                             rr.r() + pic2.r(), dest.r(*cols))
            for n in range(T // 512):
                cols = (n * 512, (n + 1) * 512)
                cs = slice(*cols)
                for (tl, gap, bank) in ((QT, gq_ap, 1), (KT, gk_ap, 2)):
                    ps = PSB[bank]
                    act(P, SQ[:, 0, :], tl[:, cs], AF.Square, tl.r(*cols), SQ.r())
                    mm(P, ps[:, :], BD[:, :], SQ[:, 0, :], True, True, SQ.r() + BD.r(), ps.r())
                    act(P, LNV[:], ps[:, :], AF.Ln, ps.r() + eps_c.r(), LNV.r(), scale=1.0 / 64, bias=eps_c[:])
                    act(P, LNV[:], LNV[:], AF.Exp, LNV.r(), LNV.r(), scale=-0.5)
                    stt(P, tl[:, cs], tl[:, cs], gap, LNV[:], ALU.mult, ALU.mult, tl.r(*cols) + LNV.r() + par.r() + cst.r(), tl.r(*cols))
                    pw = PSB[bank + 2]
                    mm(P, pw[:, :], PM[:, :], tl[:, cs], True, True, PM.r() + tl.r(*cols), pw.r())
                    tt(P, "dve", TA[:], tl[:, cs], COST[:, cs], ALU.mult, tl.r(*cols) + COST.r(*cols), TA.r())
                    tt(P, "dve", TB_[:], pw[:, :], SINT[:, cs], ALU.mult, pw.r() + SINT.r(*cols), TB_.r())
                    tt(P, "dve", tl[:, cs], TA[:], TB_[:], ALU.add, TA.r() + TB_.r(), tl.r(*cols))
            if debug:
                continue
            k = 0
            for j in range(T // 512):
                accs = (PSB[4], PSB[5])
                dens = (PSB[6], PSB[7])
                nt = 4 * j + 4
                items = []
                for i in range(nt):
                    m_ = i - 4 * j
                    c0 = 128 * m_ if m_ >= 0 else 0
                    items.append((i, m_, c0))

                def qk(it, k):
                    i, m_, c0 = it
                    for mp in range(2):
                        sc = PSB[(k % 2) * 2 + mp]
                        rows = slice(mp * 64, (mp + 1) * 64)
                        mm(P, sc[:, c0:512], KT[rows, i * 128:(i + 1) * 128], QT[rows, j * 512 + c0:(j + 1) * 512], True, True,
                           KT.r(i * 128, i * 128 + 128) + QT.r(j * 512, j * 512 + 512), sc.r())

                def rest(it, k):
                    i, m_, c0 = it
                    for mp in range(2):
                        sc = PSB[(k % 2) * 2 + mp]
                        pt = PTB[mp][k % 3]
                        act(P, pt[:, c0:512], sc[:, c0:512], AF.Exp, sc.r(), pt.r())
                        if m_ >= 0:
                            memset(P, "pool", pt[64:128, c0:c0 + 64], 0.0, pt.r())
                        mm(P, accs[mp][:, c0:512], V[:, i, :], pt[:, c0:512], i == 0, i == nt - 1, V.r(i * 128, i * 128 + 128) + pt.r(), accs[mp].r())
                        if mp == 0:
                            mm(P, dens[0][:, c0:512], ones_bf[:, :], pt[:, c0:512], i == 0, i == nt - 1, ones_bf.r() + pt.r(), dens[0].r())
                        elif not dinit[mp]:
                            assert c0 == 0
                            cp(P, "dve", DACC[mp][:, :], pt[:, :], pt.r(), DACC[mp].r())
                            dinit[mp] = True
                        else:
                            tt(P, "dve", DACC[mp][:, c0:512], DACC[mp][:, c0:512], pt[:, c0:512], ALU.add, DACC[mp].r() + pt.r(), DACC[mp].r())
                LOOK = 1
                dinit = [False, False]
                pend = [None, None]
                npair = [0, 0]
                for idx in range(nt + LOOK):
                    if idx < nt:
                        qk(items[idx], k + idx)
                    if idx >= LOOK:
                        rest(items[idx - LOOK], k + idx - LOOK)
                k += nt
                for mp in range(1, 2):
                    mm(P, dens[mp][:, :], ones_f[:, :], DACC[mp][:, :], True, True, ones_f.r() + DACC[mp].r(), dens[mp].r())
                recip(P, FT[0][:], dens[0][:, :], dens[0].r(), FT[0].r())
                tt(P, "dve", FT[1][:], accs[0][:, :], FT[0][:], ALU.mult, accs[0].r() + FT[0].r(), FT[1].r())
                recip(P, FT[0][:], dens[1][:, :], dens[1].r(), FT[0].r())
                tt(P, "dve", FT[2][:], accs[1][:, :], FT[0][:], ALU.mult, accs[1].r() + FT[0].r(), FT[2].r())
                stt(P, FT[3][:], FT[2][:], nlam_ap, FT[1][:], ALU.mult, ALU.add, FT[2].r() + FT[1].r() + cst.r(), FT[3].r())
                act(P, SQ[:, 0, :], FT[3][:], AF.Square, FT[3].r(), SQ.r())
                pn = PSB[0]
                mm(P, pn[:, :], ones_bf[:, :], SQ[:, 0, :], True, True, SQ.r() + ones_bf.r(), pn.r())
                act(P, LNV[:], pn[:, :], AF.Ln, pn.r() + eps_c.r(), LNV.r(), scale=1.0 / 128, bias=eps_c[:])
                act(P, LNV[:], LNV[:], AF.Exp, LNV.r(), LNV.r(), scale=-0.5)
                outs = OUTS[j % 2]
                stt(P, outs[:], FT[3][:], gs_ap, LNV[:], ALU.mult, ALU.mult, FT[3].r() + LNV.r() + cst.r(), outs.r())
                dma(P, "sp", oT[:, b * T + j * 512: b * T + (j + 1) * 512], outs[:], outs.r(), (), outs.dsem)
        if debug:
            dsd = P.new_dsem("dbg")
            for nm, tl in (("dQ", QT), ("dK", KT), ("dS", SINT), ("dC", COST)):
                dd = nc.dram_tensor(nm, [128, T], BF16, kind="ExternalOutput").ap()
                dma(P, "sp", dd, tl[:], tl.r(), (), dsd)
            dd = nc.dram_tensor("dV", [128, T], BF16, kind="ExternalOutput").ap()
            dma(P, "sp", dd, V[:].rearrange("p a b -> p (a b)"), V.r(), (), dsd)
        P.replay()
    return nc


def _prep_A(inp, xT):
    w = inp["even_w_in"][0]
    gm = np.ascontiguousarray(inp["norm_mix"][0].reshape(8, 128).T)
    maps = []
    for c in range(8):
        sl = lambda base: w[:, base + c * 64: base + (c + 1) * 64]
        wA = np.concatenate([sl(0), sl(512), sl(1536), sl(2048), sl(2560), w[:, 3584 + c:3585 + c], sl(1024), sl(3072)], axis=1)
        pA = np.zeros((64, 8), np.float32)
        pA[:, 0] = inp["hgrn_lb_logits"][0, c * 64:(c + 1) * 64]
        pA[:, 1] = inp["hgrn_lb_logits"][1, c * 64:(c + 1) * 64]
        pA[:, 2] = inp["hgrn_out_norm"][0]
        pA[:, 3] = inp["fox_q_norm"][0]
        pA[:, 4] = inp["fox_k_norm"][0]
        fb = np.empty((128, 1), np.float32)
        fb[:, 0] = inp["fox_f_bias"][0, c]
        maps.append({"xT": xT, "wA": np.ascontiguousarray(wA), "gmix": gm, "pA": pA, "fbias": fb})
    return maps


def _prep_B(inp, layer, hT_full, oT_full, w_out):
    wr = np.concatenate([inp["moe_router_group"][layer]] + [inp["moe_router_expert"][layer, g] for g in range(4)], axis=1)
    wg = np.ascontiguousarray(inp["moe_w_gate"][layer].reshape(32, 1024, 512))
    wu = np.ascontiguousarray(inp["moe_w_up"][layer].reshape(32, 1024, 512))
    wd = np.ascontiguousarray(inp["moe_w_down"][layer].reshape(32, 512, 1024))
    gf = np.ascontiguousarray(inp["norm_ffn"][layer].reshape(8, 128).T)
    wr = np.ascontiguousarray(wr)
    w_out = np.ascontiguousarray(w_out)
    maps = []
    for c in range(8):
        cs = slice(c * NT, (c + 1) * NT)
        maps.append({"hT_in": np.ascontiguousarray(hT_full[:, cs]), "oT_in": np.ascontiguousarray(oT_full[:, cs]),
                     "w_out": w_out, "gffn": gf, "w_r": wr, "w_gate": wg, "w_up": wu, "w_down": wd})
    return maps


def _prep_C(inp, hT_full):
    w = inp["odd_w_in"][0]
    gm = np.ascontiguousarray(inp["norm_mix"][1].reshape(8, 128).T)
    pos = np.ascontiguousarray(inp["positions"].astype(np.int32))
    maps = []
    for c in range(8):
        wC = np.concatenate([w[:, c * 128:(c + 1) * 128], w[:, 1024 + c * 128:1024 + (c + 1) * 128],
                             w[:, 2048 + c * 128:2048 + (c + 1) * 128]], axis=1)
        pC = np.zeros((128, 8), np.float32)
        pC[0:64, 0] = inp["diff_q_norm"][0]; pC[64:128, 0] = inp["diff_q_norm"][0]
        pC[0:64, 1] = inp["diff_k_norm"][0]; pC[64:128, 1] = inp["diff_k_norm"][0]
        pC[0:64, 2] = inp["diff_lambda_q1"][0]; pC[0:64, 3] = inp["diff_lambda_k1"][0]
        pC[0:64, 4] = inp["diff_lambda_q2"][0]; pC[0:64, 5] = inp["diff_lambda_k2"][0]
        pC[:, 6] = inp["diff_subln"][0]
        maps.append({"xT": hT_full, "wC": np.ascontiguousarray(wC), "gmix": gm, "pC": pC, "pos": pos})
    return maps


def _run(nc, maps):
    res = run_bass_kernel_spmd(nc, maps, core_ids=list(range(8)))
    return res.results


def kernel(**inputs):
    inp = {k: np.asarray(v) for k, v in inputs.items()}
    x = inp["x"].astype(np.float32, copy=False).reshape(-1, 1024)
    xT = np.ascontiguousarray(x.T)
    rA = _run(build_A(), _prep_A(inp, xT))
    oT0 = np.empty((1024, TOK), ml_dtypes.bfloat16)
    for c in range(8):
        o = np.asarray(rA[c]["oT"])
        oT0[c * 64:(c + 1) * 64] = o[0:64]
        oT0[512 + c * 64:512 + (c + 1) * 64] = o[64:128]
    rB = _run(build_B(), _prep_B(inp, 0, xT, oT0, inp["even_w_out"][0]))
    h1T = np.ascontiguousarray(np.concatenate([np.asarray(r["hT_out"]) for r in rB], axis=1))
    rC = _run(build_C(), _prep_C(inp, h1T))
    oT1 = np.ascontiguousarray(np.concatenate([np.asarray(r["oT"]) for r in rC], axis=0))
    rD = _run(build_B(), _prep_B(inp, 1, h1T, oT1, inp["odd_w_out"][0]))
    outT = np.concatenate([np.asarray(r["hT_out"]) for r in rD], axis=1)
    return np.ascontiguousarray(outT.T).reshape(2, 8192, 1024).astype(np.float32, copy=False)
```

```python
import ml_dtypes
import numpy as np
from contextlib import ExitStack
import concourse.bass as bass
import concourse.mybir as mybir
from concourse.bass_utils import run_bass_kernel_spmd

F32 = mybir.dt.float32
BF16 = mybir.dt.bfloat16
I32 = mybir.dt.int32
AF = mybir.ActivationFunctionType
ALU = mybir.AluOpType
AX = mybir.AxisListType

ENGS = ("pe", "act", "dve", "pool", "sp")


class Res:
    __slots__ = ("name", "last_w", "readers", "dsem")

    def __init__(self, name):
        self.name = name
        self.last_w = None
        self.readers = []
        self.dsem = None


class Op:
    __slots__ = ("eng", "fn", "deps", "is_dma", "sem", "semval", "signal", "count", "dma_waits", "idx")

    def __init__(self, eng, fn):
        self.eng = eng
        self.fn = fn
        self.deps = []
        self.is_dma = False
        self.sem = None
        self.semval = 0
        self.signal = False
        self.count = 0
        self.dma_waits = []
        self.idx = 0


class Prog:
    def __init__(self, nc):
        self.nc = nc
        self.ops = {e: [] for e in ENGS}
        self.all_res_reset = True
        self.esem = {e: nc.alloc_semaphore("es_" + e) for e in ENGS}
        self.dsems = []
        self.dma_cum = {}
        self.nops = 0
        self.ecount = {e: 0 for e in ENGS}
        self.waited = {e: {} for e in ENGS}

    def res(self, name, n=1):
        return [Res("%s.%d" % (name, i)) for i in range(n)]

    def new_dsem(self, name):
        s = self.nc.alloc_semaphore("ds_" + name)
        self.dma_cum[s] = 0
        return s

    def op(self, eng, fn, reads=(), writes=(), dma_sem=None):
        o = Op(eng, fn)
        o.idx = self.nops
        self.nops += 1
        deps = {}
        dma_waits = {}

        def add_dep(p):
            if p is None:
                return
            if p.is_dma:
                dma_waits[p.sem] = self.dma_cum[p.sem]
            else:
                deps[id(p)] = p

        for r in reads:
            add_dep(r.last_w)
        for r in writes:
            add_dep(r.last_w)
            for q in r.readers:
                add_dep(q)
        raw = set()
        for r in reads:
            if r.last_w is not None and not r.last_w.is_dma:
                raw.add(id(r.last_w))
        for k, p in list(deps.items()):
            if p.eng == eng and eng == "pe" and dma_sem is None:
                del deps[k]
        o.deps = list(deps.values())
        for p in o.deps:
            p.signal = True
        o.dma_waits = list(dma_waits.items())
        if dma_sem is not None:
            o.is_dma = True
            o.sem = dma_sem
            self.dma_cum[dma_sem] += 16
            o.semval = self.dma_cum[dma_sem]
        for r in reads:
            r.readers.append(o)
        for r in writes:
            r.last_w = o
            r.readers = []
        self.ops[eng].append(o)
        return o

    def finish_wait(self, eng="sp"):
        waits = [(s, v) for s, v in self.dma_cum.items() if v > 0]
        o = Op(eng, None)
        o.dma_waits = waits
        self.ops[eng].append(o)

    def replay(self):
        nc = self.nc
        for e in ENGS:
            last = None
            for o in self.ops[e]:
                if o.fn is not None and not o.is_dma:
                    last = o
            if last is not None:
                last.signal = True
            c = self.ecount[e]
            for o in self.ops[e]:
                if o.signal and not o.is_dma:
                    c += 1
                    o.count = c
            self.ecount[e] = c
        final = {self.esem[e]: self.ecount[e] for e in ENGS if self.ecount[e] > 0}
        for s_, v_ in self.dma_cum.items():
            if v_ > 0:
                final[s_] = v_
        handles = {"pe": "tensor", "act": "scalar", "dve": "vector", "pool": "gpsimd", "sp": "sync"}
        with nc.Block() as block:
            for e in ENGS:
                ops = self.ops[e]
                esem = self.esem
                my = esem[e]

                def body(engh, ops=ops, e=e, my=my):
                    waited = self.waited[e]
                    for o in ops:
                        need = {}
                        for p in o.deps:
                            s = esem[p.eng]
                            if need.get(s, 0) < p.count:
                                need[s] = p.count
                        for s, v in o.dma_waits:
                            if need.get(s, 0) < v:
                                need[s] = v
                        for s, v in need.items():
                            if waited.get(s, 0) < v:
                                engh.wait_ge(s, v)
                                waited[s] = v
                        if o.fn is None:
                            continue
                        ins = o.fn(engh)
                        if o.is_dma:
                            ins.then_inc(o.sem, 16)
                        elif o.signal:
                            ins.then_inc(my, 1)
                    for s, v in final.items():
                        if s is my:
                            continue
                        if waited.get(s, 0) < v:
                            engh.wait_ge(s, v)
                            waited[s] = v

                getattr(block, handles[e])(body)
        self.ops = {e: [] for e in ENGS}
        self.all_res_reset = True


from math import prod
import os
HG = int(os.environ.get('HG_STAGE', '9'))


class TT:
    def __init__(self, P, st, name, shape, dt, bs=None, psum=False, dsem=False):
        nc = P.nc
        self.h = st.enter_context(nc.psum_tensor(name, shape, dt) if psum else nc.sbuf_tensor(name, shape, dt))
        self.F = prod(shape[1:])
        self.bs = bs or self.F
        self.res = P.res(name, (self.F + self.bs - 1) // self.bs)
        self.dsem = P.new_dsem(name) if dsem else None

    def r(self, lo=0, hi=None):
        hi = self.F if hi is None else hi
        return self.res[lo // self.bs:(hi - 1) // self.bs + 1]

    def __getitem__(self, k):
        return self.h[k]


def mm(P, out, lhsT, rhs, start, stop, rd, wr):
    return P.op("pe", lambda e: e.matmul(out, lhsT=lhsT, rhs=rhs, start=start, stop=stop), rd, wr)


def act(P, out, in_, func, rd, wr, scale=1.0, bias=None):
    if bias is None:
        return P.op("act", lambda e: e.activation(out=out, in_=in_, func=func, scale=scale), rd, wr)
    return P.op("act", lambda e: e.activation(out=out, in_=in_, func=func, scale=scale, bias=bias), rd, wr)


def tt(P, eng, out, in0, in1, op, rd, wr):
    return P.op(eng, lambda e: e.tensor_tensor(out=out, in0=in0, in1=in1, op=op), rd, wr)


def ts(P, eng, out, in0, s1, op0, rd, wr, s2=None, op1=None):
    if op1 is None:
        return P.op(eng, lambda e: e.tensor_scalar(out=out, in0=in0, scalar1=s1, scalar2=None, op0=op0), rd, wr)
    return P.op(eng, lambda e: e.tensor_scalar(out=out, in0=in0, scalar1=s1, scalar2=s2, op0=op0, op1=op1), rd, wr)


def stt(P, out, in0, scalar, in1, op0, op1, rd, wr):
    return P.op("dve", lambda e: e.scalar_tensor_tensor(out=out, in0=in0, scalar=scalar, in1=in1, op0=op0, op1=op1), rd, wr)


def cp(P, eng, out, in_, rd, wr):
    if eng == "act":
        return P.op("act", lambda e: e.copy(out=out, in_=in_), rd, wr)
    return P.op(eng, lambda e: e.tensor_copy(out=out, in_=in_), rd, wr)


def recip(P, out, in_, rd, wr):
    return P.op("dve", lambda e: e.reciprocal(out=out, in_=in_), rd, wr)


def dma(P, q, out, in_, rd, wr, sem):
    return P.op(q, lambda e: e.dma_start(out=out, in_=in_), rd, wr, dma_sem=sem)


def memset(P, eng, ap, val, wr):
    return P.op(eng, lambda e: e.memset(ap, val), (), wr)


T = int(os.environ.get('KT', 8192))
TOK = 2 * T
NCOL_A = 449
EPS = 1e-6


def build_A(nbatch=2, do_hgrn=True, do_fox=True, debug=False):
    nc = bass.Bass("TRN2", target_bir_lowering=False)
    xT = nc.dram_tensor("xT", [1024, TOK], F32, kind="ExternalInput").ap()
    wA = nc.dram_tensor("wA", [1024, NCOL_A], F32, kind="ExternalInput").ap()
    gmix = nc.dram_tensor("gmix", [128, 8], F32, kind="ExternalInput").ap()
    pA = nc.dram_tensor("pA", [64, 8], F32, kind="ExternalInput").ap()
    fbias = nc.dram_tensor("fbias", [128, 1], F32, kind="ExternalInput").ap()
    oT = nc.dram_tensor("oT", [128, TOK], BF16, kind="ExternalOutput").ap()
    P = Prog(nc)
    xT_v = xT.rearrange("(kc p) t -> p kc t", p=128)
    with ExitStack() as st:
        def SB(name, shape, dt, bs=None, dsem=False):
            return TT(P, st, name, shape, dt, bs=bs, dsem=dsem)

        ones_bf = SB("ones_bf", [128, 128], BF16)
        ones_f = SB("ones_f", [128, 64], F32)
        eps_c = SB("eps_c", [128, 1], F32)
        one_c = SB("one_c", [128, 1], F32)
        ident = SB("ident", [128, 128], BF16)
        M2 = SB("M2", [128, 128], BF16)
        TBH = 256
        rmask = SB("rmask", [64, TBH], F32)
        par = SB("par", [64, 8], F32, dsem=True)
        gm = SB("gm", [128, 8], F32, dsem=True)
        nfb = SB("nfb", [128, 1], F32, dsem=True)
        lbc = SB("lbc", [64, 4], F32)
        Wf = None
        Wb = SB("Wb", [128, 8, NCOL_A], BF16, dsem=True)

        memset(P, "pool", ones_bf[:], 1.0, ones_bf.r())
        memset(P, "pool", ones_f[:], 1.0, ones_f.r())
        memset(P, "pool", eps_c[:], EPS, eps_c.r())
        memset(P, "pool", one_c[:], 1.0, one_c.r())
        memset(P, "pool", ident[:], 1.0, ident.r())
        P.op("pool", lambda e: e.affine_select(out=ident[:], in_=ident[:], pattern=[[-1, 128]], compare_op=ALU.is_equal,
                                               fill=0.0, base=0, channel_multiplier=1), ident.r(), ident.r())
        memset(P, "pool", M2[:], 1.0, M2.r())
        P.op("pool", lambda e: e.affine_select(out=M2[:], in_=M2[:], pattern=[[1, 128]], compare_op=ALU.is_ge,
                                               fill=0.0, base=0, channel_multiplier=-1), M2.r(), M2.r())
        memset(P, "pool", M2[0:64, 64:128], 0.0, M2.r())
        memset(P, "pool", rmask[:], 1.0, rmask.r())
        memset(P, "pool", rmask[:].rearrange("p (c l) -> p c l", l=64)[:, :, 0:1], 0.0, rmask.r())

        dma(P, "sp", par[:], pA, (), par.r(), par.dsem)
        dma(P, "sp", gm[:], gmix, (), gm.r(), gm.dsem)
        dma(P, "sp", nfb[:], fbias, (), nfb.r(), nfb.dsem)
        dma(P, "pool", Wb[:], wA.rearrange("(kc p) n -> p kc n", p=128), (), Wb.r(), Wb.dsem)
        for kc in range(8):
            ts(P, "dve", Wb[:, kc, :], Wb[:, kc, :], gm[:, kc:kc + 1], ALU.mult, Wb.r() + gm.r(), Wb.r())
        ts(P, "dve", nfb[:], nfb[:], -1.0, ALU.mult, nfb.r(), nfb.r())
        tt(P, "dve", lbc[:, 3:4], par[:, 1:2], par[:, 0:1], ALU.subtract, par.r(), lbc.r())
        act(P, lbc[:, 3:4], lbc[:, 3:4], AF.Exp, lbc.r(), lbc.r())
        ts(P, "dve", lbc[:, 3:4], lbc[:, 3:4], 1.0, ALU.add, lbc.r(), lbc.r())
        recip(P, lbc[:, 0:1], lbc[:, 3:4], lbc.r(), lbc.r())
        ts(P, "dve", lbc[:, 1:2], lbc[:, 0:1], -1.0, ALU.mult, lbc.r(), lbc.r(), 1.0, ALU.add)
        ts(P, "dve", lbc[:, 2:3], par[:, 3:4], 0.125, ALU.mult, par.r(), lbc.r())
        lb_ap, oml_ap, gq8_ap = lbc[:, 0:1], lbc[:, 1:2], lbc[:, 2:3]
        on_ap, gk_ap = par[:, 2:3], par[:, 4:5]

        Q = SB("Q", [64, T], BF16, bs=512)
        Fh = SB("Fh", [64, T], BF16, bs=512, dsem=True)
        G = SB("G", [64, T], BF16, bs=512)
        BQ = SB("BQ", [70, T], BF16, bs=512, dsem=True)
        BK = SB("BK", [70, T], BF16, bs=128, dsem=True)
        VAB = SB("VAB", [128, T // 128, 129], BF16, bs=129)
        XB = [SB("XB%d" % i, [128, 8, 512], BF16, dsem=True) for i in range(2)]
        SQ = SB("SQ", [128, 8, 512], BF16)
        LNV = SB("LNV", [128, 512], F32)
        RSTD = [SB("RSTD%d" % i, [128, 512], F32) for i in range(2)]
        ZF = SB("ZF", [128, 512], F32)
        CC = [SB("CC%d" % i, [128, 512], F32) for i in range(2)]
        R1 = SB("R1", [128, 512], F32)
        AUG = [SB("AUG0", [128, 6, 512], BF16)] * 2
        RCS = SB("RCS", [128, 4], F32)
        PSB = [TT(P, st, "ps%d" % i, [128, 512], F32, psum=True) for i in range(8)]

        HT2 = [[SB("HT%d_%d" % (k_, i), [64, TBH], F32) for i in range(5)] for k_ in range(2)]
        KHT2 = [SB("KHT%d" % k_, [64, TBH], BF16) for k_ in range(2)]
        ELC0 = SB("ELC0", [64, T // 64], F32)

        KH = SB("KH", [128, T // 128, 64], BF16, bs=64)
        ELC = SB("ELC", [64, T // 64], F32)
        SBF = SB("SBF", [64, T], BF16)
        Z64 = SB("Z64", [64, 64], BF16)
        memset(P, "pool", Z64[:], 0.0, Z64.r())
        ATS = [SB("ATS%d" % i, [128, 512], BF16) for i in range(2)]
        OAS = [SB("OAS%d" % i, [64, 512], BF16, dsem=True) for i in range(2)]
        ACCS = SB("ACCS", [65, 512], F32)
        RDEN = SB("RDEN", [64, 512], F32)
        OBS = [SB("OBS%d" % i, [64, 512], BF16, dsem=True) for i in range(2)]
        RONE = SB("RONE", [128, 512], BF16)
        memset(P, "pool", RONE[:], 1.0, RONE.r())
        memset(P, "pool", VAB[:, :, 128:129], 1.0, VAB.r())
        memset(P, "pool", BQ[64:70, :], 1.0, BQ.r())
        memset(P, "pool", BK[64:70, :], 1.0, BK.r())

        PTB = [SB("PTB%d" % i, [128, 512], BF16) for i in range(4)]
        groups = [(0, 128, Q, Fh), (128, 128, G, BQ), (256, 65, BK, None)]
        blk = 0
        for b in range(nbatch):
            for n in range(T // 512):
                tok0 = b * T + n * 512
                cols = (n * 512, (n + 1) * 512)
                xb = XB[blk % 2]
                rs = RSTD[blk % 2]
                dma(P, "pool", xb[:], xT_v[:, :, tok0:tok0 + 512], (), xb.r(), xb.dsem)
                act(P, SQ[:], xb[:], AF.Square, xb.r(), SQ.r())
                ss = PSB[0]
                for kc in range(8):
                    mm(P, ss[:, :], ones_bf[:, :], SQ[:, kc, :], kc == 0, kc == 7, SQ.r() + ones_bf.r(), ss.r())
                act(P, LNV[:], ss[:, :], AF.Ln, ss.r() + eps_c.r(), LNV.r(), scale=1.0 / 1024, bias=eps_c[:])
                act(P, rs[:], LNV[:], AF.Exp, LNV.r(), rs.r(), scale=-0.5)
                for gi, (c0, M, dest, dest2) in enumerate(groups):
                    ps = PSB[[[1, 4], [2, 2], [3, 5]][gi][blk % 2]]
                    for kc in range(8):
                        mm(P, ps[0:M, :], Wb[:, kc, c0:c0 + M], xb[:, kc, :], kc == 0, kc == 7, Wb.r() + xb.r(), ps.r())
                    tt(P, "dve", dest[0:64, cols[0]:cols[1]], ps[0:64, :], rs[0:64, :], ALU.mult,
                       ps.r() + rs.r(), dest.r(*cols))
                    if dest2 is not None:
                        stg = PTB[gi * 2 + blk % 2]
                        tt(P, "dve", stg[64:128, :], ps[64:128, :], rs[64:128, :], ALU.mult, ps.r() + rs.r(), stg.r())
                        dma(P, "sp", dest2[0:64, cols[0]:cols[1]], stg[64:128, :], stg.r(), dest2.r(*cols), dest2.dsem)
                    if M == 65:
                        cc = CC[blk % 2]
                        ccp = CC[(blk + 1) % 2]
                        aug = AUG[blk % 2]
                        r64 = slice(64, 65)
                        tt(P, "dve", ZF[r64, :], ps[r64, :], rs[r64, :], ALU.mult, ps.r() + rs.r(), ZF.r())
                        act(P, ZF[r64, :], ZF[r64, :], AF.Exp, ZF.r() + nfb.r(), ZF.r(), scale=-1.0, bias=nfb[r64, :])
                        act(P, ZF[r64, :], ZF[r64, :], AF.Ln, ZF.r() + one_c.r(), ZF.r(), scale=1.0, bias=one_c[r64, :])
                        init = 0.0 if n == 0 else ccp[r64, 511:512]
                        P.op("dve", lambda e, cc=cc, init=init: e.tensor_tensor_scan(
                            out=cc[64:65, :], data0=RONE[64:65, :],
                            data1=ZF[64:65, :], initial=init, op0=ALU.mult, op1=ALU.add),
                            ZF.r() + ccp.r() + RONE.r(), cc.r())
                        cp(P, "dve", aug[r64, 0, :], cc[r64, :], cc.r(), aug.r())
                        tt(P, "dve", R1[r64, :], cc[r64, :], aug[r64, 0, :], ALU.subtract, cc.r() + aug.r(), R1.r())
                        cp(P, "dve", aug[r64, 1, :], R1[r64, :], R1.r(), aug.r())
                        tt(P, "dve", R1[r64, :], R1[r64, :], aug[r64, 1, :], ALU.subtract, R1.r() + aug.r(), R1.r())
                        cp(P, "dve", aug[r64, 2, :], R1[r64, :], R1.r(), aug.r())
                        ts(P, "dve", aug[r64, 3:6, :], aug[r64, 0:3, :], -1.0, ALU.mult, aug.r(), aug.r())
                        for i in range(3):
                            dma(P, "sp", BK[67 + i:68 + i, cols[0]:cols[1]], aug[r64, i, :], aug.r(), BK.r(*cols), BK.dsem)
                            dma(P, "sp", BQ[64 + i:65 + i, cols[0]:cols[1]], aug[r64, 3 + i, :], aug.r(), BQ.r(*cols), BQ.dsem)
                ptm = PSB[6]
                prc = PSB[7]
                for sub in range(4):
                    sc = slice(sub * 128, (sub + 1) * 128)
                    for kc in range(8):
                        mm(P, ptm[:, sc], xb[:, kc, sc], Wb[:, kc, 321:449], kc == 0, kc == 7, Wb.r() + xb.r(), ptm.r())
                    mm(P, prc[:, sub:sub + 1], rs[0:1, sc], ones_f[0:1, 0:1], True, True, rs.r() + ones_f.r(), prc.r())
                cp(P, "dve", RCS[:, 0:4], prc[:, 0:4], prc.r(), RCS.r())
                for sub in range(4):
                    ti = n * 4 + sub
                    sc = slice(sub * 128, (sub + 1) * 128)
                    ts(P, "dve", VAB[:, ti, 0:128], ptm[:, sc], RCS[:, sub:sub + 1], ALU.mult,
                       ptm.r() + RCS.r(), VAB.r(ti * 129, ti * 129 + 129))
                blk += 1
            if do_fox:
                for n in range(T // 512):
                    cs = slice(n * 512, (n + 1) * 512)
                    for (tl, gap, bank) in ((BQ, gq8_ap, 1), (BK, gk_ap, 2)):
                        ps = PSB[bank]
                        act(P, SQ[0:64, 0, :], tl[0:64, cs], AF.Square, tl.r(n * 512, n * 512 + 512), SQ.r())
                        mm(P, ps[0:64, :], ones_bf[0:64, 0:64], SQ[0:64, 0, :], True, True, SQ.r() + ones_bf.r(), ps.r())
                        act(P, LNV[0:64, :], ps[0:64, :], AF.Ln, ps.r() + eps_c.r(), LNV.r(), scale=1.0 / 64, bias=eps_c[0:64, :])
                        act(P, LNV[0:64, :], LNV[0:64, :], AF.Exp, LNV.r(), LNV.r(), scale=-0.5)
                        stt(P, tl[0:64, cs], tl[0:64, cs], gap, LNV[0:64, :], ALU.mult, ALU.mult,
                            tl.r(n * 512, n * 512 + 512) + LNV.r() + par.r() + lbc.r(), tl.r(n * 512, n * 512 + 512))
            if do_hgrn:
                def hg_elem(par_):
                    for sbk in range(par_, T // TBH, 2):
                        c0, c1 = sbk * TBH, (sbk + 1) * TBH
                        cs = slice(c0, c1)
                        t1, t2, t3, t4, t5 = HT2[sbk % 2]
                        kht = KHT2[sbk % 2]
                        act(P, t1[:], Fh[0:64, cs], AF.Exp, Fh.r(c0, c1), t1.r(), scale=-1.0)
                        yield
                        act(P, t1[:], t1[:], AF.Identity, t1.r() + one_c.r(), t1.r(), scale=1.0, bias=one_c[0:64, :])
                        yield
                        recip(P, t1[:], t1[:], t1.r(), t1.r())
                        yield
                        ts(P, "dve", t1[:], t1[:], oml_ap, ALU.mult, t1.r() + lbc.r(), t1.r(), lb_ap, ALU.add)
                        yield
                        act(P, t2[:], t1[:], AF.Ln, t1.r(), t2.r())
                        yield
                        P.op("dve", lambda e, t3=t3, t2=t2: e.tensor_tensor_scan(out=t3[:], data0=rmask[:], data1=t2[:], initial=0.0,
                                                                                 op0=ALU.mult, op1=ALU.add), t2.r() + rmask.r(), t3.r())
                        yield
                        act(P, t1[:], t1[:], AF.Identity, t1.r() + one_c.r(), t1.r(), scale=-1.0, bias=one_c[0:64, :])
                        yield
                        act(P, t4[:], t3[:], AF.Exp, t3.r(), t4.r())
                        yield
                        act(P, t5[:], Q[0:64, cs], AF.Exp, Q.r(c0, c1), t5.r(), scale=-1.0)
                        yield
                        act(P, t5[:], t5[:], AF.Identity, t5.r() + one_c.r(), t5.r(), scale=1.0, bias=one_c[0:64, :])
                        yield
                        recip(P, t5[:], t5[:], t5.r(), t5.r())
                        yield
                        tt(P, "dve", t5[:], Q[0:64, cs], t5[:], ALU.mult, Q.r(c0, c1) + t5.r(), t5.r())
                        yield
                        tt(P, "dve", Q[0:64, cs], t5[:], t4[:], ALU.mult, t5.r() + t4.r(), Q.r(c0, c1))
                        yield
                        act(P, t4[:], t3[:], AF.Exp, t3.r(), t4.r(), scale=-1.0)
                        yield
                        tt(P, "dve", Fh[0:64, cs], t1[:], t4[:], ALU.mult, t1.r() + t4.r(), Fh.r(c0, c1))
                        yield
                        nch = TBH // 64
                        ch0 = sbk * nch
                        act(P, ELC[0:64, ch0:ch0 + nch], t3[:].rearrange("p (c l) -> p c l", l=64)[:, :, 63],
                            AF.Exp, t3.r(), ELC.r())
                        yield
                        tt(P, "dve", kht[:].rearrange("p (c l) -> p c l", l=64), Fh[0:64, cs].rearrange("p (c l) -> p c l", l=64),
                           ELC[0:64, ch0:ch0 + nch].unsqueeze(2).broadcast_to([64, nch, 64]), ALU.mult,
                           Fh.r(c0, c1) + ELC.r(), kht.r())
                        yield
                        pst = PSB[7] if sbk % 2 == 0 else PSB[0]
                        pstb = pst.h.bitcast(BF16)
                        ntl = TBH // 128
                        for j in range(ntl):
                            P.op("pe", lambda e, j=j, pstb=pstb, kht=kht: e.transpose(out=pstb[:, j * 64:(j + 1) * 64], in_=kht[0:64, j * 128:(j + 1) * 128],
                                                                  identity=ident[0:64, 0:64]), kht.r() + ident.r(), pst.r())
                            yield
                        ti0 = sbk * ntl
                        cp(P, "act", KH[:, ti0:ti0 + ntl, :], pstb[:, 0:ntl * 64].rearrange("p (a b) -> p a b", b=64),
                           pst.r(), KH.r(ti0 * 64, (ti0 + ntl) * 64))
                        yield
                        act(P, t2[:], G[0:64, cs], AF.Exp, G.r(c0, c1), t2.r(), scale=-1.0)
                        yield
                        act(P, t2[:], t2[:], AF.Identity, t2.r() + one_c.r(), t2.r(), scale=1.0, bias=one_c[0:64, :])
                        yield
                        recip(P, t2[:], t2[:], t2.r(), t2.r())
                        yield
                        tt(P, "dve", G[0:64, cs], G[0:64, cs], t2[:], ALU.mult, G.r(c0, c1) + t2.r(), G.r(c0, c1))
                        yield

                hg_gens = [hg_elem(0), hg_elem(1)]
            if do_fox:
                k = 0
                for j in range(T // 512):
                    acc = PSB[4 + j % 2]
                    nt = 4 * j + 4
                    items = []
                    for i in range(nt):
                        m = i - 4 * j
                        c0 = 128 * m if m >= 0 else 0
                        items.append((i, m, c0))
                    def qk(it, k):
                        i, m, c0 = it
                        sc = PSB[1 + k % 3]
                        mm(P, sc[:, c0:512], BK[0:70, i * 128:(i + 1) * 128], BQ[0:70, j * 512 + c0:(j + 1) * 512], True, True,
                           BK.r(i * 128, i * 128 + 128) + BQ.r(j * 512, j * 512 + 512), sc.r())
                    def rest(it, k):
                        i, m, c0 = it
                        sc = PSB[1 + k % 3]
                        pt = PTB[k % 4]
                        act(P, pt[:, c0:512], sc[:, c0:512], AF.Exp, sc.r(), pt.r())
                        if m >= 0:
                            P.op("pool", lambda e, pt=pt, c0=c0: e.affine_select(out=pt[:, c0:c0 + 128], in_=pt[:, c0:c0 + 128], pattern=[[1, 128]],
                                 compare_op=ALU.is_ge, fill=0.0, base=0, channel_multiplier=-1), pt.r(), pt.r())
                        mm(P, acc[0:65, c0:512], VAB[:, i, 64:129], pt[:, c0:512], i == 0, i == nt - 1,
                           VAB.r(i * 129, i * 129 + 129) + pt.r(), acc.r())
                    LOOK = 2
                    for idx in range(nt + LOOK):
                        if idx < nt:
                            qk(items[idx], k + idx)
                        if idx >= LOOK:
                            rest(items[idx - LOOK], k + idx - LOOK)
                            if do_hgrn and os.environ.get("HG_NOILV") is None:
                                for g_ in hg_gens:
                                    next(g_, None)
                    k += nt
                    cp(P, "dve", ACCS[0:65, :], acc[0:65, :], acc.r(), ACCS.r())
                    pden = PSB[6]
                    mm(P, pden[0:64, :], ones_f[64:65, 0:64], ACCS[64:65, :], True, True, ones_f.r() + ACCS.r(), pden.r())
                    recip(P, RDEN[:], pden[0:64, :], pden.r(), RDEN.r())
                    obs = OBS[j % 2]
                    tt(P, "pool", obs[:], ACCS[0:64, :], RDEN[:], ALU.mult, ACCS.r() + RDEN.r(), obs.r())
                    dma(P, "sp", oT[64:128, b * T + j * 512: b * T + (j + 1) * 512], obs[:], obs.r(), (), obs.dsem)
            if do_hgrn:
                for g_ in hg_gens:
                    for _ in g_:
                        pass
                DSV = BQ
                NC_ = T // 64
                for g4 in range(T // 512):
                    for c in range(8 * g4, 8 * g4 + 8):
                        ti, hh = c // 2, c % 2
                        pds = PSB[4 + (g4 % 2) * 2 + hh]
                        slot = (c % 8) * 64
                        hs = slice(hh * 64, (hh + 1) * 64)
                        mm(P, pds[0:64, slot:slot + 64], KH[hs, ti, :], VAB[hs, ti, 0:64], True, True,
                           KH.r(ti * 64, ti * 64 + 64) + VAB.r(ti * 129, ti * 129 + 129), pds.r())
                    for hh in range(2):
                        pds = PSB[4 + (g4 % 2) * 2 + hh]
                        src = pds[0:64, :].rearrange("p (a b) -> p a b", b=128)[:, :, hh * 64:(hh + 1) * 64]
                        dst = DSV[0:64, :].rearrange("p (v c) -> p c v", c=NC_)[:, 8 * g4 + hh:8 * g4 + 8:2, :]
                        cp(P, "dve" if hh == 0 else "act", dst, src, pds.r(), DSV.r())
                cp(P, "pool", ELC0[:], ELC[:], ELC.r(), ELC0.r())
                memset(P, "pool", ELC0[:, 0:1], 0.0, ELC0.r())
                D0 = BK
                cp(P, "act", D0[0:64, :].rearrange("p (v c) -> p v c", c=NC_), ELC0[:, :].unsqueeze(1).broadcast_to([64, 64, NC_]),
                   ELC0.r(), D0.r())
                P.op("dve", lambda e: e.tensor_tensor_scan(out=SBF[:, :], data0=D0[0:64, :], data1=DSV[0:64, :],
                                                           initial=0.0, op0=ALU.mult, op1=ALU.add), D0.r() + DSV.r(), SBF.r())
                for g4 in range(T // 512):
                    pat = PSB[6 + g4 % 2]
                    for j in range(4):
                        ti = g4 * 4 + j
                        tcs = slice(ti * 128, (ti + 1) * 128)
                        mm(P, pat[:, j * 128:(j + 1) * 128], Fh[0:64, tcs], Q[0:64, tcs], True, True,
                           Fh.r(ti * 128, ti * 128 + 128) + Q.r(ti * 128, ti * 128 + 128), pat.r())
                    ats = ATS[g4 % 2]
                    tt(P, "dve", ats[:].rearrange("p (a b) -> p a b", b=128), pat[:, :].rearrange("p (a b) -> p a b", b=128),
                       M2[:].unsqueeze(1).broadcast_to([128, 4, 128]), ALU.mult, pat.r() + M2.r(), ats.r())
                    po = PSB[1 + g4 % 2]
                    for jj in range(4):
                        tj = g4 * 4 + jj
                        for hh in range(2):
                            c = 2 * tj + hh
                            ccs = slice(c * 64, (c + 1) * 64)
                            oc = slice(jj * 128 + hh * 64, jj * 128 + hh * 64 + 64)
                            mm(P, po[0:64, oc], VAB[:, tj, 0:64], ats[:, oc], True, False,
                               VAB.r(tj * 129, tj * 129 + 129) + ats.r(), po.r())
                            st_ap = Z64[:, :] if c == 0 else SBF[:, :].rearrange("p (v c) -> p v c", c=NC_)[:, :, c - 1]
                            mm(P, po[0:64, oc], st_ap, Q[0:64, ccs], False, True,
                               SBF.r() + Z64.r() + Q.r(c * 64, c * 64 + 64), po.r())
                    gcs = slice(g4 * 512, (g4 + 1) * 512)
                    pn = PSB[3]
                    sq64 = SQ
                    lnv = RSTD[g4 % 2]
                    oaf = (ZF, R1)[g4 % 2]
                    act(P, sq64[0:64, g4 % 2, :], po[0:64, :], AF.Square, po.r(), sq64.r())
                    mm(P, pn[0:64, :], ones_bf[0:64, 0:64], sq64[0:64, g4 % 2, :], True, True, sq64.r() + ones_bf.r(), pn.r())
                    act(P, lnv[0:64, :], pn[0:64, :], AF.Ln, pn.r() + eps_c.r(), lnv.r(), scale=1.0 / 64, bias=eps_c[0:64, :])
                    act(P, lnv[0:64, :], lnv[0:64, :], AF.Exp, lnv.r(), lnv.r(), scale=-0.5)
                    stt(P, oaf[0:64, :], po[0:64, :], on_ap, lnv[0:64, :], ALU.mult, ALU.mult, po.r() + lnv.r() + par.r(), oaf.r())
                    oas = OAS[g4 % 2]
                    tt(P, "dve", oas[:], oaf[0:64, :], G[0:64, gcs], ALU.mult, oaf.r() + G.r(g4 * 512, g4 * 512 + 512), oas.r())
                    dma(P, "sp", oT[0:64, b * T + g4 * 512: b * T + (g4 + 1) * 512], oas[:], oas.r(), (), oas.dsem)
        if os.environ.get('HG_DBG'):
            dsd2 = P.new_dsem("dbg2")
            for nm, tl, shp, dt_ in (("dKH", KH, [128, (T // 128) * 64], BF16), ("dKt", Fh, [64, T], BF16), ("dQt", Q, [64, T], BF16), ("dELC", ELC, [64, T // 64], F32), ("dSBF", SBF, [64, T], BF16)):
                dd = nc.dram_tensor(nm, shp, dt_, kind="ExternalOutput").ap()
                src = tl[:] if len(tl.h.shape) == 2 else tl[:].rearrange("p a b -> p (a b)")
                dma(P, "sp", dd, src, tl.r(), (), dsd2)
        if debug:
            dsd = P.new_dsem("dbg")
            for nm, tl, shp in (("dQ", Q, [64, T]), ("dF", Fh, [64, T]), ("dG", G, [64, T]), ("dBQ", BQ, [70, T]), ("dBK", BK, [70, T]), ("dV", VAB, [128, (T // 128) * 129])):
                dd = nc.dram_tensor(nm, shp, BF16, kind="ExternalOutput").ap()
                src = tl[:] if nm != "dV" else tl[:].rearrange("p a b -> p (a b)")
                dma(P, "sp", dd, src, tl.r(), (), dsd)
        P.replay()
    return nc


NT = 2048


def build_B():
    nc = bass.Bass("TRN2", target_bir_lowering=False)
    hT_in = nc.dram_tensor("hT_in", [1024, NT], F32, kind="ExternalInput").ap()
    oT_in = nc.dram_tensor("oT_in", [1024, NT], BF16, kind="ExternalInput").ap()
    w_out = nc.dram_tensor("w_out", [1024, 1024], F32, kind="ExternalInput").ap()
    gffn = nc.dram_tensor("gffn", [128, 8], F32, kind="ExternalInput").ap()
    w_r = nc.dram_tensor("w_r", [1024, 36], F32, kind="ExternalInput").ap()
    w_gate = nc.dram_tensor("w_gate", [32, 1024, 512], F32, kind="ExternalInput").ap()
    w_up = nc.dram_tensor("w_up", [32, 1024, 512], F32, kind="ExternalInput").ap()
    w_down = nc.dram_tensor("w_down", [32, 512, 1024], F32, kind="ExternalInput").ap()
    hT_out = nc.dram_tensor("hT_out", [1024, NT], F32, kind="ExternalOutput").ap()
    P = Prog(nc)
    hT_v = hT_in.rearrange("(kc p) t -> p kc t", p=128)
    oT_v = oT_in.rearrange("(kc p) t -> p kc t", p=128)
    ho_v = hT_out.rearrange("(kc p) t -> p kc t", p=128)
    with ExitStack() as st:
        def SB(name, shape, dt, bs=None, dsem=False):
            return TT(P, st, name, shape, dt, bs=bs, dsem=dsem)

        ones_bf = SB("ones_bf", [128, 128], BF16)
        eps_c = SB("eps_c", [128, 1], F32)
        identf = SB("identf", [128, 128], F32)
        SEL = SB("SEL", [32, 32, 128], BF16)
        gf = SB("gf", [128, 8], F32, dsem=True)
        WR = SB("WR", [128, 8, 36], F32, dsem=True)
        HM = SB("HM", [128, 8, NT], F32, bs=512, dsem=True)
        U = SB("U", [128, 8, NT], BF16, bs=512)
        WBUF = [SB("WB%d" % i, [128, 12288], BF16, dsem=True) for i in range(2)]
        OTB = [SB("OTB%d" % i, [128, 8, 512], BF16, dsem=True) for i in range(2)]
        HID = [SB("HID%d" % i, [128, 4, 512], BF16) for i in range(2)]
        SIL = [SB("SIL%d" % i, [128, 512], BF16) for i in range(2)]
        S2 = [SB("S2%d" % i, [128, 512], BF16) for i in range(2)]
        CREP = [SB("CREP%d" % i, [128, 512], BF16) for i in range(2)]
        CT = SB("CT", [32, NT], BF16, bs=128)
        LNV = SB("LNV", [128, 512], F32)
        RSTD = SB("RSTD", [128, 512], F32)
        RLA = SB("RLA", [128, NT // 128, 36], F32)
        R16 = SB("R16", [128, 8, NT // 128], F32)
        OHA = SB("OHA", [128, NT // 128, 4], F32)
        EGA = SB("EGA", [128, NT // 128, 4], F32)
        SELA = SB("SELA", [128, NT // 128, 8], F32)
        TMP8 = SB("TMP8", [128, NT // 128, 8], F32)
        E1A = SB("E1A", [128, NT // 128, 8], F32)
        E2A = SB("E2A", [128, NT // 128, 8], F32)
        COMBA = SB("COMBA", [128, NT // 128, 32], F32)
        RL = SB("RL", [128, 36], F32)
        RT = SB("RT", [128, 16], F32)
        OH = SB("OH", [128, 4], F32)
        EG = SB("EG", [128, 4], F32)
        SEL8 = SB("SEL8", [128, 8], F32)
        M8 = SB("M8", [128, 8], F32)
        WA8 = SB("WA8", [128, 8], F32)
        WB8 = SB("WB8", [128, 8], F32)
        COMB = SB("COMB", [128, 32], F32)
        PSB = [TT(P, st, "ps%d" % i, [128, 512], F32, psum=True) for i in range(8)]
        Wo = WBUF[1]
        Wo_v = Wo[:, 0:8192].rearrange("p (a b) -> p a b", b=1024)
        UFt = WBUF[0]
        UF_v = UFt.h.bitcast(F32)[:, 0:4096].rearrange("p (a b) -> p a b", b=512)
        SQt = HID[0]
        SQ = SB("SQ", [128, 8, 512], BF16)

        memset(P, "pool", ones_bf[:], 1.0, ones_bf.r())
        memset(P, "pool", eps_c[:], EPS, eps_c.r())
        memset(P, "pool", identf[:], 1.0, identf.r())
        P.op("pool", lambda e: e.affine_select(out=identf[:], in_=identf[:], pattern=[[-1, 128]], compare_op=ALU.is_equal,
                                               fill=0.0, base=0, channel_multiplier=1), identf.r(), identf.r())
        memset(P, "pool", SEL[:], 1.0, SEL.r())
        P.op("pool", lambda e: e.affine_select(out=SEL[:], in_=SEL[:], pattern=[[-1, 32], [0, 128]], compare_op=ALU.is_equal,
                                               fill=0.0, base=0, channel_multiplier=1), SEL.r(), SEL.r())
        dma(P, "sp", gf[:], gffn, (), gf.r(), gf.dsem)
        dma(P, "sp", WR[:], w_r.rearrange("(kc p) n -> p kc n", p=128), (), WR.r(), WR.dsem)
        dma(P, "pool", Wo_v, w_out.rearrange("(kc p) n -> p kc n", p=128), (), Wo.r(), Wo.dsem)
        for h2 in range(2):
            dma(P, "sp", HM[:, :, h2 * 1024:(h2 + 1) * 1024], hT_v[:, :, h2 * 1024:(h2 + 1) * 1024], (), HM.r(), HM.dsem)

        def stage_a(blk):
            cs = slice(blk * 512, (blk + 1) * 512)
            ot = OTB[blk % 2]
            dma(P, "sp", ot[:], oT_v[:, :, cs], (), ot.r(), ot.dsem)
            for dc in range(8):
                ps = PSB[dc % 2]
                for kc in range(8):
                    mm(P, ps[:, :], Wo_v[:, kc, dc * 128:(dc + 1) * 128], ot[:, kc, :], kc == 0, kc == 7, Wo.r() + ot.r(), ps.r())
                lo = dc * NT + blk * 512
                tt(P, "dve", HM[:, dc, cs], HM[:, dc, cs], ps[:, :], ALU.add, HM.r(lo, lo + 512) + ps.r(), HM.r(lo, lo + 512))

        def stage_b(blk):
            cs = slice(blk * 512, (blk + 1) * 512)
            hm_blk = []
            for dc in range(8):
                hm_blk += HM.r(dc * NT + blk * 512, dc * NT + blk * 512 + 512)
            act(P, SQ[:], HM[:, :, cs], AF.Square, hm_blk, SQ.r())
            ss = PSB[2]
            for kc in range(8):
                mm(P, ss[:, :], ones_bf[:, :], SQ[:, kc, :], kc == 0, kc == 7, SQ.r() + ones_bf.r(), ss.r())
            act(P, LNV[:], ss[:, :], AF.Ln, ss.r() + eps_c.r(), LNV.r(), scale=1.0 / 1024, bias=eps_c[:])
            act(P, RSTD[:], LNV[:], AF.Exp, LNV.r(), RSTD.r(), scale=-0.5)
            u_blk = []
            for dc in range(8):
                lo = dc * NT + blk * 512
                stt(P, UF_v[:, dc, :], HM[:, dc, cs], gf[:, dc:dc + 1], RSTD[:], ALU.mult, ALU.mult,
                    HM.r(lo, lo + 512) + gf.r() + RSTD.r(), UFt.r())
                u_blk += U.r(lo, lo + 512)
            cp(P, "act", U[:, :, cs], UF_v, UFt.r(), u_blk)
            pr = PSB[3]
            for sub in range(4):
                scs = slice(sub * 128, (sub + 1) * 128)
                for dc in range(8):
                    mm(P, pr[:, sub * 36:(sub + 1) * 36], UF_v[:, dc, scs], WR[:, dc, :], dc == 0, dc == 7, UFt.r() + WR.r(), pr.r())
            cp(P, "dve", RLA[:, blk * 4:(blk + 1) * 4, :].rearrange("p a b -> p (a b)"), pr[:, 0:144], pr.r(), RLA.r())

        nblk_ = NT // 512
        for blk in range(nblk_ + 1):
            if blk < nblk_:
                stage_a(blk)
            if blk >= 1:
                stage_b(blk - 1)

        S_ = NT // 128
        def b3(ap2, n):
            return ap2.unsqueeze(2).broadcast_to([128, S_, n])
        GL = RLA[:, :, 0:4]
        P.op("dve", lambda e: e.tensor_reduce(out=R16[:, 0, :], in_=GL, axis=AX.X, op=ALU.max), RLA.r(), R16.r())
        tt(P, "dve", OHA[:], GL, b3(R16[:, 0, :], 4), ALU.is_equal, RLA.r() + R16.r(), OHA.r())
        tt(P, "dve", EGA[:], GL, b3(R16[:, 0, :], 4), ALU.subtract, RLA.r() + R16.r(), EGA.r())
        act(P, EGA[:], EGA[:], AF.Exp, EGA.r(), EGA.r())
        P.op("dve", lambda e: e.tensor_reduce(out=R16[:, 1, :], in_=EGA[:], axis=AX.X, op=ALU.add), EGA.r(), R16.r())
        recip(P, R16[:, 2, :], R16[:, 1, :], R16.r(), R16.r())
        tt(P, "dve", SELA[:], RLA[:, :, 4:12], b3(OHA[:, :, 0], 8), ALU.mult, RLA.r() + OHA.r(), SELA.r())
        for g in range(1, 4):
            tt(P, "dve", TMP8[:], RLA[:, :, 4 + 8 * g:12 + 8 * g], b3(OHA[:, :, g], 8), ALU.mult, RLA.r() + OHA.r(), TMP8.r())
            tt(P, "dve", SELA[:], SELA[:], TMP8[:], ALU.add, SELA.r() + TMP8.r(), SELA.r())
        P.op("dve", lambda e: e.tensor_reduce(out=R16[:, 3, :], in_=SELA[:], axis=AX.X, op=ALU.max), SELA.r(), R16.r())
        tt(P, "dve", E1A[:], SELA[:], b3(R16[:, 3, :], 8), ALU.is_equal, SELA.r() + R16.r(), E1A.r())
        ts(P, "dve", TMP8[:], E1A[:], -1.0e30, ALU.mult, E1A.r(), TMP8.r())
        tt(P, "dve", TMP8[:], SELA[:], TMP8[:], ALU.add, SELA.r() + TMP8.r(), TMP8.r())
        P.op("dve", lambda e: e.tensor_reduce(out=R16[:, 4, :], in_=TMP8[:], axis=AX.X, op=ALU.max), TMP8.r(), R16.r())
        tt(P, "dve", E2A[:], TMP8[:], b3(R16[:, 4, :], 8), ALU.is_equal, TMP8.r() + R16.r(), E2A.r())
        tt(P, "dve", R16[:, 5, :], R16[:, 4, :], R16[:, 3, :], ALU.subtract, R16.r(), R16.r())
        act(P, R16[:, 5, :], R16[:, 5, :], AF.Exp, R16.r(), R16.r())
        ts(P, "dve", R16[:, 5, :], R16[:, 5, :], 1.0, ALU.add, R16.r(), R16.r())
        recip(P, R16[:, 6, :], R16[:, 5, :], R16.r(), R16.r())
        ts(P, "dve", R16[:, 7, :], R16[:, 6, :], -1.0, ALU.mult, R16.r(), R16.r(), 1.0, ALU.add)
        tt(P, "dve", R16[:, 6, :], R16[:, 6, :], R16[:, 2, :], ALU.mult, R16.r(), R16.r())
        tt(P, "dve", R16[:, 7, :], R16[:, 7, :], R16[:, 2, :], ALU.mult, R16.r(), R16.r())
        tt(P, "dve", E1A[:], E1A[:], b3(R16[:, 6, :], 8), ALU.mult, E1A.r() + R16.r(), E1A.r())
        tt(P, "dve", E2A[:], E2A[:], b3(R16[:, 7, :], 8), ALU.mult, E2A.r() + R16.r(), E2A.r())
        tt(P, "dve", E1A[:], E1A[:], E2A[:], ALU.add, E1A.r() + E2A.r(), E1A.r())
        tt(P, "dve", COMBA[:].rearrange("p s (g e) -> p s g e", e=8),
           E1A[:].unsqueeze(2).broadcast_to([128, S_, 4, 8]), OHA[:].unsqueeze(3).broadcast_to([128, S_, 4, 8]), ALU.mult,
           E1A.r() + OHA.r(), COMBA.r())
        for q4 in range(S_ // 4):
            pt = PSB[4 + q4 % 2]
            for sub in range(4):
                sidx = q4 * 4 + sub
                P.op("pe", lambda e, pt=pt, sub=sub, sidx=sidx: e.transpose(out=pt[0:32, sub * 128:(sub + 1) * 128], in_=COMBA[:, sidx, :], identity=identf[:, :]),
                     COMBA.r() + identf.r(), pt.r())
            cp(P, "dve", CT[0:32, q4 * 512:(q4 + 1) * 512], pt[0:32, :], pt.r(), CT.r(q4 * 512, q4 * 512 + 512))

        units = [(e, blk) for e in range(32) for blk in range(NT // 512)]
        wviews = []
        for i in range(2):
            wb = WBUF[i]
            wviews.append((wb[:, 0:4096].rearrange("p (a b) -> p a b", b=512),
                           wb[:, 4096:8192].rearrange("p (a b) -> p a b", b=512),
                           wb[:, 8192:12288].rearrange("p (a b) -> p a b", b=1024)))

        def load_w(e):
            wb = WBUF[e % 2]
            Wg, Wu, Wd = wviews[e % 2]
            dma(P, "pool", Wg, w_gate[e].rearrange("(kc p) n -> p kc n", p=128), (), wb.r(), wb.dsem)
            dma(P, "pool", Wu, w_up[e].rearrange("(kc p) n -> p kc n", p=128), (), wb.r(), wb.dsem)
            dma(P, "pool", Wd, w_down[e].rearrange("(fc p) n -> p fc n", p=128), (), wb.r(), wb.dsem)

        def GU(ui):
            e, blk = units[ui]
            cs = slice(blk * 512, (blk + 1) * 512)
            wb = WBUF[e % 2]
            Wg, Wu, Wd = wviews[e % 2]
            hid = HID[ui % 2]
            crep = CREP[ui % 2]
            pc = PSB[6]
            mm(P, pc[:, :], SEL[0:32, e, :], CT[0:32, cs], True, True, SEL.r() + CT.r(blk * 512, blk * 512 + 512), pc.r())
            cp(P, "dve", crep[:], pc[:, :], pc.r(), crep.r())
            u_blk = []
            for dc in range(8):
                u_blk += U.r(dc * NT + blk * 512, dc * NT + blk * 512 + 512)
            for fc in range(4):
                pg_ = PSB[(fc % 2) * 2]
                pu_ = PSB[(fc % 2) * 2 + 1]
                fs = slice(fc * 128, (fc + 1) * 128)
                for dc in range(8):
                    mm(P, pg_[:, :], Wg[:, dc, fs], U[:, dc, cs], dc == 0, dc == 7, wb.r() + u_blk, pg_.r())
                for dc in range(8):
                    mm(P, pu_[:, :], Wu[:, dc, fs], U[:, dc, cs], dc == 0, dc == 7, wb.r() + u_blk, pu_.r())
                sil = SIL[fc % 2]
                s2 = S2[fc % 2]
                act(P, sil[:], pg_[:, :], AF.Silu, pg_.r(), sil.r())
                tt(P, "dve", s2[:], sil[:], crep[:], ALU.mult, sil.r() + crep.r(), s2.r())
                tt(P, "dve", hid[:, fc, :], s2[:], pu_[:, :], ALU.mult, s2.r() + pu_.r(), hid.r())

        def DN(ui):
            e, blk = units[ui]
            cs = slice(blk * 512, (blk + 1) * 512)
            wb = WBUF[e % 2]
            Wg, Wu, Wd = wviews[e % 2]
            hid = HID[ui % 2]
            for dc in range(8):
                py = PSB[4 + dc % 2]
                for fc in range(4):
                    mm(P, py[:, :], Wd[:, fc, dc * 128:(dc + 1) * 128], hid[:, fc, :], fc == 0, fc == 3, wb.r() + hid.r(), py.r())
                lo = dc * NT + blk * 512
                tt(P, "dve", HM[:, dc, cs], HM[:, dc, cs], py[:, :], ALU.add, HM.r(lo, lo + 512) + py.r(), HM.r(lo, lo + 512))

        nE = int(os.environ.get("KB_NE", 32))
        units = [u_ for u_ in units if u_[0] < nE]
        load_w(0)
        for ui in range(len(units) + 1):
            if ui < len(units):
                GU(ui)
            if ui >= 1:
                DN(ui - 1)
            if ui < len(units):
                e, blk = units[ui]
                if blk == 0 and e + 1 < nE:
                    load_w(e + 1)
        for h2 in range(2):
            rr = []
            for dc in range(8):
                rr += HM.r(dc * NT + h2 * 1024, dc * NT + h2 * 1024 + 1024)
            dma(P, "sp", ho_v[:, :, h2 * 1024:(h2 + 1) * 1024], HM[:, :, h2 * 1024:(h2 + 1) * 1024], rr, (), HM.dsem)
        P.replay()
    return nc


import math

NCOL_C = 384
TWO_PI = 2.0 * math.pi
LAM_INIT = 0.8 - 0.6 * math.exp(-0.3 * 1)


def build_C(nbatch=2, debug=False):
    nc = bass.Bass("TRN2", target_bir_lowering=False)
    xT = nc.dram_tensor("xT", [1024, TOK], F32, kind="ExternalInput").ap()
    wC = nc.dram_tensor("wC", [1024, NCOL_C], F32, kind="ExternalInput").ap()
    gmix = nc.dram_tensor("gmix", [128, 8], F32, kind="ExternalInput").ap()
    pC = nc.dram_tensor("pC", [128, 8], F32, kind="ExternalInput").ap()
    posd = nc.dram_tensor("pos", [2, T], I32, kind="ExternalInput").ap()
    oT = nc.dram_tensor("oT", [128, TOK], BF16, kind="ExternalOutput").ap()
    P = Prog(nc)
    xT_v = xT.rearrange("(kc p) t -> p kc t", p=128)
    with ExitStack() as st:
        def SB(name, shape, dt, bs=None, dsem=False):
            return TT(P, st, name, shape, dt, bs=bs, dsem=dsem)

        ones_bf = SB("ones_bf", [128, 128], BF16)
        BD = SB("BD", [128, 128], BF16)
        ones_f = SB("ones_f", [128, 128], F32)
        eps_c = SB("eps_c", [128, 1], F32)
        pi_c = SB("pi_c", [128, 2], F32)
        pic2 = SB("pic2", [128, 2], F32)
        ident = SB("ident", [128, 128], BF16)
        PM = SB("PM", [128, 128], BF16)
        PM2 = SB("PM2", [128, 128], BF16)
        par = SB("par", [128, 8], F32, dsem=True)
        gm = SB("gm", [128, 8], F32, dsem=True)
        cst = SB("cst", [128, 8], F32)
        invi = SB("invi", [1, 128], I32)
        invf = SB("invf", [1, 128], F32)
        Wb = SB("Wb", [128, 8, NCOL_C], BF16, dsem=True)

        memset(P, "pool", ones_bf[:], 1.0, ones_bf.r())
        memset(P, "pool", ones_f[:], 1.0, ones_f.r())
        memset(P, "pool", eps_c[:], EPS, eps_c.r())
        memset(P, "pool", BD[:], 1.0, BD.r())
        memset(P, "pool", BD[0:64, 64:128], 0.0, BD.r())
        memset(P, "pool", BD[64:128, 0:64], 0.0, BD.r())
        memset(P, "pool", pi_c[:, 0:1], math.pi, pi_c.r())
        memset(P, "pool", pi_c[:, 1:2], -TWO_PI, pi_c.r())
        memset(P, "pool", pi_c[0:32, 0:1], -math.pi, pi_c.r())
        memset(P, "pool", pi_c[0:32, 1:2], TWO_PI, pi_c.r())
        memset(P, "pool", pi_c[64:96, 0:1], -math.pi, pi_c.r())
        memset(P, "pool", pi_c[64:96, 1:2], TWO_PI, pi_c.r())
        memset(P, "pool", pic2[:, 0:1], math.pi, pic2.r())
        memset(P, "pool", PM[:], 1.0, PM.r())
        P.op("pool", lambda e: e.affine_select(out=PM[:], in_=PM[:], pattern=[[1, 128]], compare_op=ALU.is_equal,
                                               fill=0.0, base=-32, channel_multiplier=-1), PM.r(), PM.r())
        memset(P, "pool", PM[:, 0:32], 0.0, PM.r())
        memset(P, "pool", PM[:, 64:96], 0.0, PM.r())
        memset(P, "pool", PM2[:], 1.0, PM2.r())
        P.op("pool", lambda e: e.affine_select(out=PM2[:], in_=PM2[:], pattern=[[1, 128]], compare_op=ALU.is_equal,
                                               fill=0.0, base=32, channel_multiplier=-1), PM2.r(), PM2.r())
        memset(P, "pool", PM2[:, 32:64], 0.0, PM2.r())
        memset(P, "pool", PM2[:, 96:128], 0.0, PM2.r())
        tt(P, "pool", PM[:], PM[:], PM2[:], ALU.add, PM.r() + PM2.r(), PM.r())
        P.op("pool", lambda e: e.iota(invi[:].rearrange("p (a b) -> p a b", b=32), pattern=[[0, 4], [1, 32]], base=0, channel_multiplier=0),
             (), invi.r())
        cp(P, "dve", invf[:], invi[:], invi.r(), invf.r())
        act(P, invf[:], invf[:], AF.Exp, invf.r(), invf.r(), scale=-math.log(10000.0) / 32.0)

        dma(P, "sp", par[:], pC, (), par.r(), par.dsem)
        dma(P, "sp", gm[:], gmix, (), gm.r(), gm.dsem)
        dma(P, "pool", Wb[:], wC.rearrange("(kc p) n -> p kc n", p=128), (), Wb.r(), Wb.dsem)
        for kc in range(8):
            ts(P, "dve", Wb[:, kc, :], Wb[:, kc, :], gm[:, kc:kc + 1], ALU.mult, Wb.r() + gm.r(), Wb.r())
        ts(P, "dve", cst[:, 0:1], par[:, 0:1], 0.125, ALU.mult, par.r(), cst.r())
        ts(P, "dve", cst[:, 3:4], par[:, 6:7], 1.0 - LAM_INIT, ALU.mult, par.r(), cst.r())
        PSB = [TT(P, st, "ps%d" % i, [128, 512], F32, psum=True) for i in range(8)]
        tt(P, "dve", cst[:, 4:5], par[:, 2:3], par[:, 3:4], ALU.mult, par.r(), cst.r())
        tt(P, "dve", cst[:, 5:6], par[:, 4:5], par[:, 5:6], ALU.mult, par.r(), cst.r())
        mm(P, PSB[0][:, 0:2], ones_f[:, :], cst[:, 4:6], True, True, ones_f.r() + cst.r(), PSB[0].r())
        act(P, cst[:, 6:8], PSB[0][:, 0:2], AF.Exp, PSB[0].r(), cst.r())
        tt(P, "dve", cst[:, 2:3], cst[:, 7:8], cst[:, 6:7], ALU.subtract, cst.r(), cst.r())
        ts(P, "dve", cst[:, 2:3], cst[:, 2:3], -LAM_INIT, ALU.add, cst.r(), cst.r())
        gq_ap, gk_ap, nlam_ap, gs_ap = cst[:, 0:1], par[:, 1:2], cst[:, 2:3], cst[:, 3:4]

        QT = SB("QT", [128, T], BF16, bs=512)
        KT = SB("KT", [128, T], BF16, bs=128)
        V = SB("V", [128, T // 128, 128], BF16, bs=128)
        SINT = SB("SINT", [128, T], BF16, bs=512)
        COST = SB("COST", [128, T], BF16, bs=512)
        XB = [SB("XB%d" % i, [128, 8, 512], BF16, dsem=True) for i in range(2)]
        SQ = SB("SQ", [128, 8, 512], BF16)
        LNV = SB("LNV", [128, 512], F32)
        RSTD = [SB("RSTD%d" % i, [128, 512], F32) for i in range(2)]
        RCS = SB("RCS", [128, 4], F32)
        POSI = SB("POSI", [1, 512], I32, dsem=True)
        POSF = SB("POSF", [1, 512], F32)
        RR = [SB("RR%d" % i, [128, 512], F32) for i in range(2)]
        RI = SB("RI", [128, 512], I32)
        RF = SB("RF", [128, 512], F32)
        RMK = SB("RMK", [128, 512], F32)
        TA = SB("TA", [128, 512], BF16)
        TB_ = SB("TB", [128, 512], BF16)
        PTB = [[SB("PT%d_%d" % (m, i), [128, 512], BF16) for i in range(3)] for m in range(2)]
        FT = [SB("FT%d" % i, [128, 512], F32) for i in range(4)]
        OUTS = [SB("OUTS%d" % i, [128, 512], BF16, dsem=True) for i in range(2)]
        DACC = [SB("DACC%d" % i, [128, 512], F32) for i in range(2)]
        TMPP = [[SB("TMPP%d_%d" % (m, i), [128, 512], BF16) for i in range(2)] for m in range(2)]

        groups = [(0, QT), (128, KT)]
        blk = 0
        for b in range(nbatch):
            for n in range(T // 512):
                tok0 = b * T + n * 512
                cols = (n * 512, (n + 1) * 512)
                xb = XB[blk % 2]
                rs = RSTD[blk % 2]
                dma(P, "pool", xb[:], xT_v[:, :, tok0:tok0 + 512], (), xb.r(), xb.dsem)
                act(P, SQ[:], xb[:], AF.Square, xb.r(), SQ.r())
                ss = PSB[0]
                for kc in range(8):
                    mm(P, ss[:, :], ones_bf[:, :], SQ[:, kc, :], kc == 0, kc == 7, SQ.r() + ones_bf.r(), ss.r())
                act(P, LNV[:], ss[:, :], AF.Ln, ss.r() + eps_c.r(), LNV.r(), scale=1.0 / 1024, bias=eps_c[:])
                act(P, rs[:], LNV[:], AF.Exp, LNV.r(), rs.r(), scale=-0.5)
                for gi, (c0, dest) in enumerate(groups):
                    ps = PSB[1 + gi]
                    for kc in range(8):
                        mm(P, ps[:, :], Wb[:, kc, c0:c0 + 128], xb[:, kc, :], kc == 0, kc == 7, Wb.r() + xb.r(), ps.r())
                    tt(P, "dve", dest[:, cols[0]:cols[1]], ps[:, :], rs[:, :], ALU.mult, ps.r() + rs.r(), dest.r(*cols))
                ptm = PSB[3]
                prc = PSB[4]
                for sub in range(4):
                    sc = slice(sub * 128, (sub + 1) * 128)
                    for kc in range(8):
                        mm(P, ptm[:, sc], xb[:, kc, sc], Wb[:, kc, 256:384], kc == 0, kc == 7, Wb.r() + xb.r(), ptm.r())
                    mm(P, prc[:, sub:sub + 1], rs[0:1, sc], ones_f[0:1, 0:1], True, True, rs.r() + ones_f.r(), prc.r())
                cp(P, "dve", RCS[:, 0:4], prc[:, 0:4], prc.r(), RCS.r())
                for sub in range(4):
                    ti = n * 4 + sub
                    sc = slice(sub * 128, (sub + 1) * 128)
                    ts(P, "dve", V[:, ti, :], ptm[:, sc], RCS[:, sub:sub + 1], ALU.mult, ptm.r() + RCS.r(), V.r(ti * 128, ti * 128 + 128))
                blk += 1
            for n in range(T // 512):
                cols = (n * 512, (n + 1) * 512)
                cs = slice(*cols)
                dma(P, "sp", POSI[:], posd[b:b + 1, cs], (), POSI.r(), POSI.dsem)
                cp(P, "dve", POSF[:], POSI[:], POSI.r(), POSF.r())
                pa = PSB[5]
                mm(P, pa[:, :], invf[0:1, :], POSF[0:1, :], True, True, invf.r() + POSF.r(), pa.r())
                for which, dest in ((0, SINT), (1, COST)):
                    rr = RR[which]
                    if which == 0:
                        ts(P, "dve", rr[:], pa[:, :], 1.0 / TWO_PI, ALU.mult, pa.r(), rr.r())
                    else:
                        ts(P, "dve", rr[:], pa[:, :], 1.0 / TWO_PI, ALU.mult, pa.r(), rr.r(), 0.25, ALU.add)
                    cp(P, "dve", RI[:], rr[:], rr.r(), RI.r())
                    cp(P, "act", RF[:], RI[:], RI.r(), RF.r())
                    tt(P, "dve", rr[:], rr[:], RF[:], ALU.subtract, rr.r() + RF.r(), rr.r())
                    ts(P, "dve", RMK[:], rr[:], 0.0, ALU.is_lt, rr.r(), RMK.r())
                    tt(P, "dve", rr[:], rr[:], RMK[:], ALU.add, rr.r() + RMK.r(), rr.r())
                    if which == 0:
                        P.op("act", lambda e, rr=rr, dest=dest, cs=cs: e.activation(out=dest[:, cs], in_=rr[:], func=AF.Sin, scale=pi_c[:, 1:2], bias=pi_c[:, 0:1]),
                             rr.r() + pi_c.r(), dest.r(*cols))
                    else:
                        P.op("act", lambda e, rr=rr, dest=dest, cs=cs: e.activation(out=dest[:, cs], in_=rr[:], func=AF.Sin, scale=-TWO_PI, bias=pic2[:, 0:1]),
                             rr.r() + pic2.r(), dest.r(*cols))
            for n in range(T // 512):
                cols = (n * 512, (n + 1) * 512)
                cs = slice(*cols)
                for (tl, gap, bank) in ((QT, gq_ap, 1), (KT, gk_ap, 2)):
                    ps = PSB[bank]
                    act(P, SQ[:, 0, :], tl[:, cs], AF.Square, tl.r(*cols), SQ.r())
                    mm(P, ps[:, :], BD[:, :], SQ[:, 0, :], True, True, SQ.r() + BD.r(), ps.r())
                    act(P, LNV[:], ps[:, :], AF.Ln, ps.r() + eps_c.r(), LNV.r(), scale=1.0 / 64, bias=eps_c[:])
                    act(P, LNV[:], LNV[:], AF.Exp, LNV.r(), LNV.r(), scale=-0.5)
                    stt(P, tl[:, cs], tl[:, cs], gap, LNV[:], ALU.mult, ALU.mult, tl.r(*cols) + LNV.r() + par.r() + cst.r(), tl.r(*cols))
                    pw = PSB[bank + 2]
                    mm(P, pw[:, :], PM[:, :], tl[:, cs], True, True, PM.r() + tl.r(*cols), pw.r())
                    tt(P, "dve", TA[:], tl[:, cs], COST[:, cs], ALU.mult, tl.r(*cols) + COST.r(*cols), TA.r())
                    tt(P, "dve", TB_[:], pw[:, :], SINT[:, cs], ALU.mult, pw.r() + SINT.r(*cols), TB_.r())
                    tt(P, "dve", tl[:, cs], TA[:], TB_[:], ALU.add, TA.r() + TB_.r(), tl.r(*cols))
            if debug:
                continue
            k = 0
            for j in range(T // 512):
                accs = (PSB[4], PSB[5])
                dens = (PSB[6], PSB[7])
                nt = 4 * j + 4
                items = []
                for i in range(nt):
                    m_ = i - 4 * j
                    c0 = 128 * m_ if m_ >= 0 else 0
                    items.append((i, m_, c0))

                def qk(it, k):
                    i, m_, c0 = it
                    for mp in range(2):
                        sc = PSB[(k % 2) * 2 + mp]
                        rows = slice(mp * 64, (mp + 1) * 64)
                        mm(P, sc[:, c0:512], KT[rows, i * 128:(i + 1) * 128], QT[rows, j * 512 + c0:(j + 1) * 512], True, True,
                           KT.r(i * 128, i * 128 + 128) + QT.r(j * 512, j * 512 + 512), sc.r())

                def rest(it, k):
                    i, m_, c0 = it
                    for mp in range(2):
                        sc = PSB[(k % 2) * 2 + mp]
                        pt = PTB[mp][k % 3]
                        act(P, pt[:, c0:512], sc[:, c0:512], AF.Exp, sc.r(), pt.r())
                        if m_ >= 0:
                            memset(P, "pool", pt[64:128, c0:c0 + 64], 0.0, pt.r())
                        mm(P, accs[mp][:, c0:512], V[:, i, :], pt[:, c0:512], i == 0, i == nt - 1, V.r(i * 128, i * 128 + 128) + pt.r(), accs[mp].r())
                        if mp == 0:
                            mm(P, dens[0][:, c0:512], ones_bf[:, :], pt[:, c0:512], i == 0, i == nt - 1, ones_bf.r() + pt.r(), dens[0].r())
                        elif not dinit[mp]:
                            assert c0 == 0
                            cp(P, "dve", DACC[mp][:, :], pt[:, :], pt.r(), DACC[mp].r())
                            dinit[mp] = True
                        else:
                            tt(P, "dve", DACC[mp][:, c0:512], DACC[mp][:, c0:512], pt[:, c0:512], ALU.add, DACC[mp].r() + pt.r(), DACC[mp].r())
                LOOK = 1
                dinit = [False, False]
                pend = [None, None]
                npair = [0, 0]
                for idx in range(nt + LOOK):
                    if idx < nt:
                        qk(items[idx], k + idx)
                    if idx >= LOOK:
                        rest(items[idx - LOOK], k + idx - LOOK)
                k += nt
                for mp in range(1, 2):
                    mm(P, dens[mp][:, :], ones_f[:, :], DACC[mp][:, :], True, True, ones_f.r() + DACC[mp].r(), dens[mp].r())
                recip(P, FT[0][:], dens[0][:, :], dens[0].r(), FT[0].r())
                tt(P, "dve", FT[1][:], accs[0][:, :], FT[0][:], ALU.mult, accs[0].r() + FT[0].r(), FT[1].r())
                recip(P, FT[0][:], dens[1][:, :], dens[1].r(), FT[0].r())
                tt(P, "dve", FT[2][:], accs[1][:, :], FT[0][:], ALU.mult, accs[1].r() + FT[0].r(), FT[2].r())
                stt(P, FT[3][:], FT[2][:], nlam_ap, FT[1][:], ALU.mult, ALU.add, FT[2].r() + FT[1].r() + cst.r(), FT[3].r())
                act(P, SQ[:, 0, :], FT[3][:], AF.Square, FT[3].r(), SQ.r())
                pn = PSB[0]
                mm(P, pn[:, :], ones_bf[:, :], SQ[:, 0, :], True, True, SQ.r() + ones_bf.r(), pn.r())
                act(P, LNV[:], pn[:, :], AF.Ln, pn.r() + eps_c.r(), LNV.r(), scale=1.0 / 128, bias=eps_c[:])
                act(P, LNV[:], LNV[:], AF.Exp, LNV.r(), LNV.r(), scale=-0.5)
                outs = OUTS[j % 2]
                stt(P, outs[:], FT[3][:], gs_ap, LNV[:], ALU.mult, ALU.mult, FT[3].r() + LNV.r() + cst.r(), outs.r())
                dma(P, "sp", oT[:, b * T + j * 512: b * T + (j + 1) * 512], outs[:], outs.r(), (), outs.dsem)
        if debug:
            dsd = P.new_dsem("dbg")
            for nm, tl in (("dQ", QT), ("dK", KT), ("dS", SINT), ("dC", COST)):
                dd = nc.dram_tensor(nm, [128, T], BF16, kind="ExternalOutput").ap()
                dma(P, "sp", dd, tl[:], tl.r(), (), dsd)
            dd = nc.dram_tensor("dV", [128, T], BF16, kind="ExternalOutput").ap()
            dma(P, "sp", dd, V[:].rearrange("p a b -> p (a b)"), V.r(), (), dsd)
        P.replay()
    return nc


def _prep_A(inp, xT):
    w = inp["even_w_in"][0]
    gm = np.ascontiguousarray(inp["norm_mix"][0].reshape(8, 128).T)
    maps = []
    for c in range(8):
        sl = lambda base: w[:, base + c * 64: base + (c + 1) * 64]
        wA = np.concatenate([sl(0), sl(512), sl(1536), sl(2048), sl(2560), w[:, 3584 + c:3585 + c], sl(1024), sl(3072)], axis=1)
        pA = np.zeros((64, 8), np.float32)
        pA[:, 0] = inp["hgrn_lb_logits"][0, c * 64:(c + 1) * 64]
        pA[:, 1] = inp["hgrn_lb_logits"][1, c * 64:(c + 1) * 64]
        pA[:, 2] = inp["hgrn_out_norm"][0]
        pA[:, 3] = inp["fox_q_norm"][0]
        pA[:, 4] = inp["fox_k_norm"][0]
        fb = np.empty((128, 1), np.float32)
        fb[:, 0] = inp["fox_f_bias"][0, c]
        maps.append({"xT": xT, "wA": np.ascontiguousarray(wA), "gmix": gm, "pA": pA, "fbias": fb})
    return maps


def _prep_B(inp, layer, hT_full, oT_full, w_out):
    wr = np.concatenate([inp["moe_router_group"][layer]] + [inp["moe_router_expert"][layer, g] for g in range(4)], axis=1)
    wg = np.ascontiguousarray(inp["moe_w_gate"][layer].reshape(32, 1024, 512))
    wu = np.ascontiguousarray(inp["moe_w_up"][layer].reshape(32, 1024, 512))
    wd = np.ascontiguousarray(inp["moe_w_down"][layer].reshape(32, 512, 1024))
    gf = np.ascontiguousarray(inp["norm_ffn"][layer].reshape(8, 128).T)
    wr = np.ascontiguousarray(wr)
    w_out = np.ascontiguousarray(w_out)
    maps = []
    for c in range(8):
        cs = slice(c * NT, (c + 1) * NT)
        maps.append({"hT_in": np.ascontiguousarray(hT_full[:, cs]), "oT_in": np.ascontiguousarray(oT_full[:, cs]),
                     "w_out": w_out, "gffn": gf, "w_r": wr, "w_gate": wg, "w_up": wu, "w_down": wd})
    return maps


def _prep_C(inp, hT_full):
    w = inp["odd_w_in"][0]
    gm = np.ascontiguousarray(inp["norm_mix"][1].reshape(8, 128).T)
    pos = np.ascontiguousarray(inp["positions"].astype(np.int32))
    maps = []
    for c in range(8):
        wC = np.concatenate([w[:, c * 128:(c + 1) * 128], w[:, 1024 + c * 128:1024 + (c + 1) * 128],
                             w[:, 2048 + c * 128:2048 + (c + 1) * 128]], axis=1)
        pC = np.zeros((128, 8), np.float32)
        pC[0:64, 0] = inp["diff_q_norm"][0]; pC[64:128, 0] = inp["diff_q_norm"][0]
        pC[0:64, 1] = inp["diff_k_norm"][0]; pC[64:128, 1] = inp["diff_k_norm"][0]
        pC[0:64, 2] = inp["diff_lambda_q1"][0]; pC[0:64, 3] = inp["diff_lambda_k1"][0]
        pC[0:64, 4] = inp["diff_lambda_q2"][0]; pC[0:64, 5] = inp["diff_lambda_k2"][0]
        pC[:, 6] = inp["diff_subln"][0]
        maps.append({"xT": hT_full, "wC": np.ascontiguousarray(wC), "gmix": gm, "pC": pC, "pos": pos})
    return maps


def _run(nc, maps):
    res = run_bass_kernel_spmd(nc, maps, core_ids=list(range(8)))
    return res.results


def kernel(**inputs):
    inp = {k: np.asarray(v) for k, v in inputs.items()}
    x = inp["x"].astype(np.float32, copy=False).reshape(-1, 1024)
    xT = np.ascontiguousarray(x.T)
    rA = _run(build_A(), _prep_A(inp, xT))
    oT0 = np.empty((1024, TOK), ml_dtypes.bfloat16)
    for c in range(8):
        o = np.asarray(rA[c]["oT"])
        oT0[c * 64:(c + 1) * 64] = o[0:64]
        oT0[512 + c * 64:512 + (c + 1) * 64] = o[64:128]
    rB = _run(build_B(), _prep_B(inp, 0, xT, oT0, inp["even_w_out"][0]))
    h1T = np.ascontiguousarray(np.concatenate([np.asarray(r["hT_out"]) for r in rB], axis=1))
    rC = _run(build_C(), _prep_C(inp, h1T))
    oT1 = np.ascontiguousarray(np.concatenate([np.asarray(r["oT"]) for r in rC], axis=0))
    rD = _run(build_B(), _prep_B(inp, 1, h1T, oT1, inp["odd_w_out"][0]))
    outT = np.concatenate([np.asarray(r["hT_out"]) for r in rD], axis=1)
    return np.ascontiguousarray(outT.T).reshape(2, 8192, 1024).astype(np.float32, copy=False)
```

```python
import ml_dtypes
import numpy as np
from contextlib import ExitStack
import concourse.bass as bass
import concourse.mybir as mybir
from concourse.bass_utils import run_bass_kernel_spmd

F32 = mybir.dt.float32
BF16 = mybir.dt.bfloat16
I32 = mybir.dt.int32
AF = mybir.ActivationFunctionType
ALU = mybir.AluOpType
AX = mybir.AxisListType

ENGS = ("pe", "act", "dve", "pool", "sp")


class Res:
    __slots__ = ("name", "last_w", "readers", "dsem")

    def __init__(self, name):
        self.name = name
        self.last_w = None
        self.readers = []
        self.dsem = None


class Op:
    __slots__ = ("eng", "fn", "deps", "is_dma", "sem", "semval", "signal", "count", "dma_waits", "idx")

    def __init__(self, eng, fn):
        self.eng = eng
        self.fn = fn
        self.deps = []
        self.is_dma = False
        self.sem = None
        self.semval = 0
        self.signal = False
        self.count = 0
        self.dma_waits = []
        self.idx = 0


class Prog:
    def __init__(self, nc):
        self.nc = nc
        self.ops = {e: [] for e in ENGS}
        self.all_res_reset = True
        self.esem = {e: nc.alloc_semaphore("es_" + e) for e in ENGS}
        self.dsems = []
        self.dma_cum = {}
        self.nops = 0
        self.ecount = {e: 0 for e in ENGS}
        self.waited = {e: {} for e in ENGS}

    def res(self, name, n=1):
        return [Res("%s.%d" % (name, i)) for i in range(n)]

    def new_dsem(self, name):
        s = self.nc.alloc_semaphore("ds_" + name)
        self.dma_cum[s] = 0
        return s

    def op(self, eng, fn, reads=(), writes=(), dma_sem=None):
        o = Op(eng, fn)
        o.idx = self.nops
        self.nops += 1
        deps = {}
        dma_waits = {}

        def add_dep(p):
            if p is None:
                return
            if p.is_dma:
                dma_waits[p.sem] = self.dma_cum[p.sem]
            else:
                deps[id(p)] = p

        for r in reads:
            add_dep(r.last_w)
        for r in writes:
            add_dep(r.last_w)
            for q in r.readers:
                add_dep(q)
        raw = set()
        for r in reads:
            if r.last_w is not None and not r.last_w.is_dma:
                raw.add(id(r.last_w))
        for k, p in list(deps.items()):
            if p.eng == eng and eng == "pe" and dma_sem is None:
                del deps[k]
        o.deps = list(deps.values())
        for p in o.deps:
            p.signal = True
        o.dma_waits = list(dma_waits.items())
        if dma_sem is not None:
            o.is_dma = True
            o.sem = dma_sem
            self.dma_cum[dma_sem] += 16
            o.semval = self.dma_cum[dma_sem]
        for r in reads:
            r.readers.append(o)
        for r in writes:
            r.last_w = o
            r.readers = []
        self.ops[eng].append(o)
        return o

    def finish_wait(self, eng="sp"):
        waits = [(s, v) for s, v in self.dma_cum.items() if v > 0]
        o = Op(eng, None)
        o.dma_waits = waits
        self.ops[eng].append(o)

    def replay(self):
        nc = self.nc
        for e in ENGS:
            last = None
            for o in self.ops[e]:
                if o.fn is not None and not o.is_dma:
                    last = o
            if last is not None:
                last.signal = True
            c = self.ecount[e]
            for o in self.ops[e]:
                if o.signal and not o.is_dma:
                    c += 1
                    o.count = c
            self.ecount[e] = c
        final = {self.esem[e]: self.ecount[e] for e in ENGS if self.ecount[e] > 0}
        for s_, v_ in self.dma_cum.items():
            if v_ > 0:
                final[s_] = v_
        handles = {"pe": "tensor", "act": "scalar", "dve": "vector", "pool": "gpsimd", "sp": "sync"}
        with nc.Block() as block:
            for e in ENGS:
                ops = self.ops[e]
                esem = self.esem
                my = esem[e]

                def body(engh, ops=ops, e=e, my=my):
                    waited = self.waited[e]
                    for o in ops:
                        need = {}
                        for p in o.deps:
                            s = esem[p.eng]
                            if need.get(s, 0) < p.count:
                                need[s] = p.count
                        for s, v in o.dma_waits:
                            if need.get(s, 0) < v:
                                need[s] = v
                        for s, v in need.items():
                            if waited.get(s, 0) < v:
                                engh.wait_ge(s, v)
                                waited[s] = v
                        if o.fn is None:
                            continue
                        ins = o.fn(engh)
                        if o.is_dma:
                            ins.then_inc(o.sem, 16)
                        elif o.signal:
                            ins.then_inc(my, 1)
                    for s, v in final.items():
                        if s is my:
                            continue
                        if waited.get(s, 0) < v:
                            engh.wait_ge(s, v)
                            waited[s] = v

                getattr(block, handles[e])(body)
        self.ops = {e: [] for e in ENGS}
        self.all_res_reset = True


from math import prod
import os
HG = int(os.environ.get('HG_STAGE', '9'))


class TT:
    def __init__(self, P, st, name, shape, dt, bs=None, psum=False, dsem=False):
        nc = P.nc
        self.h = st.enter_context(nc.psum_tensor(name, shape, dt) if psum else nc.sbuf_tensor(name, shape, dt))
        self.F = prod(shape[1:])
        self.bs = bs or self.F
        self.res = P.res(name, (self.F + self.bs - 1) // self.bs)
        self.dsem = P.new_dsem(name) if dsem else None

    def r(self, lo=0, hi=None):
        hi = self.F if hi is None else hi
        return self.res[lo // self.bs:(hi - 1) // self.bs + 1]

    def __getitem__(self, k):
        return self.h[k]


def mm(P, out, lhsT, rhs, start, stop, rd, wr):
    return P.op("pe", lambda e: e.matmul(out, lhsT=lhsT, rhs=rhs, start=start, stop=stop), rd, wr)


def act(P, out, in_, func, rd, wr, scale=1.0, bias=None):
    if bias is None:
        return P.op("act", lambda e: e.activation(out=out, in_=in_, func=func, scale=scale), rd, wr)
    return P.op("act", lambda e: e.activation(out=out, in_=in_, func=func, scale=scale, bias=bias), rd, wr)


def tt(P, eng, out, in0, in1, op, rd, wr):
    return P.op(eng, lambda e: e.tensor_tensor(out=out, in0=in0, in1=in1, op=op), rd, wr)


def ts(P, eng, out, in0, s1, op0, rd, wr, s2=None, op1=None):
    if op1 is None:
        return P.op(eng, lambda e: e.tensor_scalar(out=out, in0=in0, scalar1=s1, scalar2=None, op0=op0), rd, wr)
    return P.op(eng, lambda e: e.tensor_scalar(out=out, in0=in0, scalar1=s1, scalar2=s2, op0=op0, op1=op1), rd, wr)


def stt(P, out, in0, scalar, in1, op0, op1, rd, wr):
    return P.op("dve", lambda e: e.scalar_tensor_tensor(out=out, in0=in0, scalar=scalar, in1=in1, op0=op0, op1=op1), rd, wr)


def cp(P, eng, out, in_, rd, wr):
    if eng == "act":
        return P.op("act", lambda e: e.copy(out=out, in_=in_), rd, wr)
    return P.op(eng, lambda e: e.tensor_copy(out=out, in_=in_), rd, wr)


def recip(P, out, in_, rd, wr):
    return P.op("dve", lambda e: e.reciprocal(out=out, in_=in_), rd, wr)


def dma(P, q, out, in_, rd, wr, sem):
    return P.op(q, lambda e: e.dma_start(out=out, in_=in_), rd, wr, dma_sem=sem)


def memset(P, eng, ap, val, wr):
    return P.op(eng, lambda e: e.memset(ap, val), (), wr)


T = int(os.environ.get('KT', 8192))
TOK = 2 * T
NCOL_A = 449
EPS = 1e-6


def build_A(nbatch=2, do_hgrn=True, do_fox=True, debug=False):
    nc = bass.Bass("TRN2", target_bir_lowering=False)
    xT = nc.dram_tensor("xT", [1024, TOK], F32, kind="ExternalInput").ap()
    wA = nc.dram_tensor("wA", [1024, NCOL_A], F32, kind="ExternalInput").ap()
    gmix = nc.dram_tensor("gmix", [128, 8], F32, kind="ExternalInput").ap()
    pA = nc.dram_tensor("pA", [64, 8], F32, kind="ExternalInput").ap()
    fbias = nc.dram_tensor("fbias", [128, 1], F32, kind="ExternalInput").ap()
    oT = nc.dram_tensor("oT", [128, TOK], BF16, kind="ExternalOutput").ap()
    P = Prog(nc)
    xT_v = xT.rearrange("(kc p) t -> p kc t", p=128)
    with ExitStack() as st:
        def SB(name, shape, dt, bs=None, dsem=False):
            return TT(P, st, name, shape, dt, bs=bs, dsem=dsem)

        ones_bf = SB("ones_bf", [128, 128], BF16)
        ones_f = SB("ones_f", [128, 64], F32)
        eps_c = SB("eps_c", [128, 1], F32)
        one_c = SB("one_c", [128, 1], F32)
        ident = SB("ident", [128, 128], BF16)
        M2 = SB("M2", [128, 128], BF16)
        TBH = 256
        rmask = SB("rmask", [64, TBH], F32)
        par = SB("par", [64, 8], F32, dsem=True)
        gm = SB("gm", [128, 8], F32, dsem=True)
        nfb = SB("nfb", [128, 1], F32, dsem=True)
        lbc = SB("lbc", [64, 4], F32)
        Wf = None
        Wb = SB("Wb", [128, 8, NCOL_A], BF16, dsem=True)

        memset(P, "pool", ones_bf[:], 1.0, ones_bf.r())
        memset(P, "pool", ones_f[:], 1.0, ones_f.r())
        memset(P, "pool", eps_c[:], EPS, eps_c.r())
        memset(P, "pool", one_c[:], 1.0, one_c.r())
        memset(P, "pool", ident[:], 1.0, ident.r())
        P.op("pool", lambda e: e.affine_select(out=ident[:], in_=ident[:], pattern=[[-1, 128]], compare_op=ALU.is_equal,
                                               fill=0.0, base=0, channel_multiplier=1), ident.r(), ident.r())
        memset(P, "pool", M2[:], 1.0, M2.r())
        P.op("pool", lambda e: e.affine_select(out=M2[:], in_=M2[:], pattern=[[1, 128]], compare_op=ALU.is_ge,
                                               fill=0.0, base=0, channel_multiplier=-1), M2.r(), M2.r())
        memset(P, "pool", M2[0:64, 64:128], 0.0, M2.r())
        memset(P, "pool", rmask[:], 1.0, rmask.r())
        memset(P, "pool", rmask[:].rearrange("p (c l) -> p c l", l=64)[:, :, 0:1], 0.0, rmask.r())

        dma(P, "sp", par[:], pA, (), par.r(), par.dsem)
        dma(P, "sp", gm[:], gmix, (), gm.r(), gm.dsem)
        dma(P, "sp", nfb[:], fbias, (), nfb.r(), nfb.dsem)
        dma(P, "pool", Wb[:], wA.rearrange("(kc p) n -> p kc n", p=128), (), Wb.r(), Wb.dsem)
        for kc in range(8):
            ts(P, "dve", Wb[:, kc, :], Wb[:, kc, :], gm[:, kc:kc + 1], ALU.mult, Wb.r() + gm.r(), Wb.r())
        ts(P, "dve", nfb[:], nfb[:], -1.0, ALU.mult, nfb.r(), nfb.r())
        tt(P, "dve", lbc[:, 3:4], par[:, 1:2], par[:, 0:1], ALU.subtract, par.r(), lbc.r())
        act(P, lbc[:, 3:4], lbc[:, 3:4], AF.Exp, lbc.r(), lbc.r())
        ts(P, "dve", lbc[:, 3:4], lbc[:, 3:4], 1.0, ALU.add, lbc.r(), lbc.r())
        recip(P, lbc[:, 0:1], lbc[:, 3:4], lbc.r(), lbc.r())
        ts(P, "dve", lbc[:, 1:2], lbc[:, 0:1], -1.0, ALU.mult, lbc.r(), lbc.r(), 1.0, ALU.add)
        ts(P, "dve", lbc[:, 2:3], par[:, 3:4], 0.125, ALU.mult, par.r(), lbc.r())
        lb_ap, oml_ap, gq8_ap = lbc[:, 0:1], lbc[:, 1:2], lbc[:, 2:3]
        on_ap, gk_ap = par[:, 2:3], par[:, 4:5]

        Q = SB("Q", [64, T], BF16, bs=512)
        Fh = SB("Fh", [64, T], BF16, bs=512, dsem=True)
        G = SB("G", [64, T], BF16, bs=512)
        BQ = SB("BQ", [70, T], BF16, bs=512, dsem=True)
        BK = SB("BK", [70, T], BF16, bs=128, dsem=True)
        VAB = SB("VAB", [128, T // 128, 129], BF16, bs=129)
        XB = [SB("XB%d" % i, [128, 8, 512], BF16, dsem=True) for i in range(2)]
        SQ = SB("SQ", [128, 8, 512], BF16)
        LNV = SB("LNV", [128, 512], F32)
        RSTD = [SB("RSTD%d" % i, [128, 512], F32) for i in range(2)]
        ZF = SB("ZF", [128, 512], F32)
        CC = [SB("CC%d" % i, [128, 512], F32) for i in range(2)]
        R1 = SB("R1", [128, 512], F32)
        AUG = [SB("AUG0", [128, 6, 512], BF16)] * 2
        RCS = SB("RCS", [128, 4], F32)
        PSB = [TT(P, st, "ps%d" % i, [128, 512], F32, psum=True) for i in range(8)]

        HT2 = [[SB("HT%d_%d" % (k_, i), [64, TBH], F32) for i in range(5)] for k_ in range(2)]
        KHT2 = [SB("KHT%d" % k_, [64, TBH], BF16) for k_ in range(2)]
        ELC0 = SB("ELC0", [64, T // 64], F32)

        KH = SB("KH", [128, T // 128, 64], BF16, bs=64)
        ELC = SB("ELC", [64, T // 64], F32)
        SBF = SB("SBF", [64, T], BF16)
        Z64 = SB("Z64", [64, 64], BF16)
        memset(P, "pool", Z64[:], 0.0, Z64.r())
        ATS = [SB("ATS%d" % i, [128, 512], BF16) for i in range(2)]
        OAS = [SB("OAS%d" % i, [64, 512], BF16, dsem=True) for i in range(2)]
        ACCS = SB("ACCS", [65, 512], F32)
        RDEN = SB("RDEN", [64, 512], F32)
        OBS = [SB("OBS%d" % i, [64, 512], BF16, dsem=True) for i in range(2)]
        RONE = SB("RONE", [128, 512], BF16)
        memset(P, "pool", RONE[:], 1.0, RONE.r())
        memset(P, "pool", VAB[:, :, 128:129], 1.0, VAB.r())
        memset(P, "pool", BQ[64:70, :], 1.0, BQ.r())
        memset(P, "pool", BK[64:70, :], 1.0, BK.r())

        PTB = [SB("PTB%d" % i, [128, 512], BF16) for i in range(4)]
        groups = [(0, 128, Q, Fh), (128, 128, G, BQ), (256, 65, BK, None)]
        blk = 0
        for b in range(nbatch):
            for n in range(T // 512):
                tok0 = b * T + n * 512
                cols = (n * 512, (n + 1) * 512)
                xb = XB[blk % 2]
                rs = RSTD[blk % 2]
                dma(P, "pool", xb[:], xT_v[:, :, tok0:tok0 + 512], (), xb.r(), xb.dsem)
                act(P, SQ[:], xb[:], AF.Square, xb.r(), SQ.r())
                ss = PSB[0]
                for kc in range(8):
                    mm(P, ss[:, :], ones_bf[:, :], SQ[:, kc, :], kc == 0, kc == 7, SQ.r() + ones_bf.r(), ss.r())
                act(P, LNV[:], ss[:, :], AF.Ln, ss.r() + eps_c.r(), LNV.r(), scale=1.0 / 1024, bias=eps_c[:])
                act(P, rs[:], LNV[:], AF.Exp, LNV.r(), rs.r(), scale=-0.5)
                for gi, (c0, M, dest, dest2) in enumerate(groups):
                    ps = PSB[[[1, 4], [2, 2], [3, 5]][gi][blk % 2]]
                    for kc in range(8):
                        mm(P, ps[0:M, :], Wb[:, kc, c0:c0 + M], xb[:, kc, :], kc == 0, kc == 7, Wb.r() + xb.r(), ps.r())
                    tt(P, "dve", dest[0:64, cols[0]:cols[1]], ps[0:64, :], rs[0:64, :], ALU.mult,
                       ps.r() + rs.r(), dest.r(*cols))
                    if dest2 is not None:
                        stg = PTB[gi * 2 + blk % 2]
                        tt(P, "dve", stg[64:128, :], ps[64:128, :], rs[64:128, :], ALU.mult, ps.r() + rs.r(), stg.r())
                        dma(P, "sp", dest2[0:64, cols[0]:cols[1]], stg[64:128, :], stg.r(), dest2.r(*cols), dest2.dsem)
                    if M == 65:
                        cc = CC[blk % 2]
                        ccp = CC[(blk + 1) % 2]
                        aug = AUG[blk % 2]
                        r64 = slice(64, 65)
                        tt(P, "dve", ZF[r64, :], ps[r64, :], rs[r64, :], ALU.mult, ps.r() + rs.r(), ZF.r())
                        act(P, ZF[r64, :], ZF[r64, :], AF.Exp, ZF.r() + nfb.r(), ZF.r(), scale=-1.0, bias=nfb[r64, :])
                        act(P, ZF[r64, :], ZF[r64, :], AF.Ln, ZF.r() + one_c.r(), ZF.r(), scale=1.0, bias=one_c[r64, :])
                        init = 0.0 if n == 0 else ccp[r64, 511:512]
                        P.op("dve", lambda e, cc=cc, init=init: e.tensor_tensor_scan(
                            out=cc[64:65, :], data0=RONE[64:65, :],
                            data1=ZF[64:65, :], initial=init, op0=ALU.mult, op1=ALU.add),
                            ZF.r() + ccp.r() + RONE.r(), cc.r())
                        cp(P, "dve", aug[r64, 0, :], cc[r64, :], cc.r(), aug.r())
                        tt(P, "dve", R1[r64, :], cc[r64, :], aug[r64, 0, :], ALU.subtract, cc.r() + aug.r(), R1.r())
                        cp(P, "dve", aug[r64, 1, :], R1[r64, :], R1.r(), aug.r())
                        tt(P, "dve", R1[r64, :], R1[r64, :], aug[r64, 1, :], ALU.subtract, R1.r() + aug.r(), R1.r())
                        cp(P, "dve", aug[r64, 2, :], R1[r64, :], R1.r(), aug.r())
                        ts(P, "dve", aug[r64, 3:6, :], aug[r64, 0:3, :], -1.0, ALU.mult, aug.r(), aug.r())
                        for i in range(3):
                            dma(P, "sp", BK[67 + i:68 + i, cols[0]:cols[1]], aug[r64, i, :], aug.r(), BK.r(*cols), BK.dsem)
                            dma(P, "sp", BQ[64 + i:65 + i, cols[0]:cols[1]], aug[r64, 3 + i, :], aug.r(), BQ.r(*cols), BQ.dsem)
                ptm = PSB[6]
                prc = PSB[7]
                for sub in range(4):
                    sc = slice(sub * 128, (sub + 1) * 128)
                    for kc in range(8):
                        mm(P, ptm[:, sc], xb[:, kc, sc], Wb[:, kc, 321:449], kc == 0, kc == 7, Wb.r() + xb.r(), ptm.r())
                    mm(P, prc[:, sub:sub + 1], rs[0:1, sc], ones_f[0:1, 0:1], True, True, rs.r() + ones_f.r(), prc.r())
                cp(P, "dve", RCS[:, 0:4], prc[:, 0:4], prc.r(), RCS.r())
                for sub in range(4):
                    ti = n * 4 + sub
                    sc = slice(sub * 128, (sub + 1) * 128)
                    ts(P, "dve", VAB[:, ti, 0:128], ptm[:, sc], RCS[:, sub:sub + 1], ALU.mult,
                       ptm.r() + RCS.r(), VAB.r(ti * 129, ti * 129 + 129))
                blk += 1
            if do_fox:
                for n in range(T // 512):
                    cs = slice(n * 512, (n + 1) * 512)
                    for (tl, gap, bank) in ((BQ, gq8_ap, 1), (BK, gk_ap, 2)):
                        ps = PSB[bank]
                        act(P, SQ[0:64, 0, :], tl[0:64, cs], AF.Square, tl.r(n * 512, n * 512 + 512), SQ.r())
                        mm(P, ps[0:64, :], ones_bf[0:64, 0:64], SQ[0:64, 0, :], True, True, SQ.r() + ones_bf.r(), ps.r())
                        act(P, LNV[0:64, :], ps[0:64, :], AF.Ln, ps.r() + eps_c.r(), LNV.r(), scale=1.0 / 64, bias=eps_c[0:64, :])
                        act(P, LNV[0:64, :], LNV[0:64, :], AF.Exp, LNV.r(), LNV.r(), scale=-0.5)
                        stt(P, tl[0:64, cs], tl[0:64, cs], gap, LNV[0:64, :], ALU.mult, ALU.mult,
                            tl.r(n * 512, n * 512 + 512) + LNV.r() + par.r() + lbc.r(), tl.r(n * 512, n * 512 + 512))
            if do_hgrn:
                def hg_elem(par_):
                    for sbk in range(par_, T // TBH, 2):
                        c0, c1 = sbk * TBH, (sbk + 1) * TBH
                        cs = slice(c0, c1)
                        t1, t2, t3, t4, t5 = HT2[sbk % 2]
                        kht = KHT2[sbk % 2]
                        act(P, t1[:], Fh[0:64, cs], AF.Exp, Fh.r(c0, c1), t1.r(), scale=-1.0)
                        yield
                        act(P, t1[:], t1[:], AF.Identity, t1.r() + one_c.r(), t1.r(), scale=1.0, bias=one_c[0:64, :])
                        yield
                        recip(P, t1[:], t1[:], t1.r(), t1.r())
                        yield
                        ts(P, "dve", t1[:], t1[:], oml_ap, ALU.mult, t1.r() + lbc.r(), t1.r(), lb_ap, ALU.add)
                        yield
                        act(P, t2[:], t1[:], AF.Ln, t1.r(), t2.r())
                        yield
                        P.op("dve", lambda e, t3=t3, t2=t2: e.tensor_tensor_scan(out=t3[:], data0=rmask[:], data1=t2[:], initial=0.0,
                                                                                 op0=ALU.mult, op1=ALU.add), t2.r() + rmask.r(), t3.r())
                        yield
                        act(P, t1[:], t1[:], AF.Identity, t1.r() + one_c.r(), t1.r(), scale=-1.0, bias=one_c[0:64, :])
                        yield
                        act(P, t4[:], t3[:], AF.Exp, t3.r(), t4.r())
                        yield
                        act(P, t5[:], Q[0:64, cs], AF.Exp, Q.r(c0, c1), t5.r(), scale=-1.0)
                        yield
                        act(P, t5[:], t5[:], AF.Identity, t5.r() + one_c.r(), t5.r(), scale=1.0, bias=one_c[0:64, :])
                        yield
                        recip(P, t5[:], t5[:], t5.r(), t5.r())
                        yield
                        tt(P, "dve", t5[:], Q[0:64, cs], t5[:], ALU.mult, Q.r(c0, c1) + t5.r(), t5.r())
                        yield
                        tt(P, "dve", Q[0:64, cs], t5[:], t4[:], ALU.mult, t5.r() + t4.r(), Q.r(c0, c1))
                        yield
                        act(P, t4[:], t3[:], AF.Exp, t3.r(), t4.r(), scale=-1.0)
                        yield
                        tt(P, "dve", Fh[0:64, cs], t1[:], t4[:], ALU.mult, t1.r() + t4.r(), Fh.r(c0, c1))
                        yield
                        nch = TBH // 64
                        ch0 = sbk * nch
                        act(P, ELC[0:64, ch0:ch0 + nch], t3[:].rearrange("p (c l) -> p c l", l=64)[:, :, 63],
                            AF.Exp, t3.r(), ELC.r())
                        yield
                        tt(P, "dve", kht[:].rearrange("p (c l) -> p c l", l=64), Fh[0:64, cs].rearrange("p (c l) -> p c l", l=64),
                           ELC[0:64, ch0:ch0 + nch].unsqueeze(2).broadcast_to([64, nch, 64]), ALU.mult,
                           Fh.r(c0, c1) + ELC.r(), kht.r())
                        yield
                        pst = PSB[7] if sbk % 2 == 0 else PSB[0]
                        pstb = pst.h.bitcast(BF16)
                        ntl = TBH // 128
                        for j in range(ntl):
                            P.op("pe", lambda e, j=j, pstb=pstb, kht=kht: e.transpose(out=pstb[:, j * 64:(j + 1) * 64], in_=kht[0:64, j * 128:(j + 1) * 128],
                                                                  identity=ident[0:64, 0:64]), kht.r() + ident.r(), pst.r())
                            yield
                        ti0 = sbk * ntl
                        cp(P, "act", KH[:, ti0:ti0 + ntl, :], pstb[:, 0:ntl * 64].rearrange("p (a b) -> p a b", b=64),
                           pst.r(), KH.r(ti0 * 64, (ti0 + ntl) * 64))
                        yield
                        act(P, t2[:], G[0:64, cs], AF.Exp, G.r(c0, c1), t2.r(), scale=-1.0)
                        yield
                        act(P, t2[:], t2[:], AF.Identity, t2.r() + one_c.r(), t2.r(), scale=1.0, bias=one_c[0:64, :])
                        yield
                        recip(P, t2[:], t2[:], t2.r(), t2.r())
                        yield
                        tt(P, "dve", G[0:64, cs], G[0:64, cs], t2[:], ALU.mult, G.r(c0, c1) + t2.r(), G.r(c0, c1))
                        yield

                hg_gens = [hg_elem(0), hg_elem(1)]
            if do_fox:
                k = 0
                for j in range(T // 512):
                    acc = PSB[4 + j % 2]
                    nt = 4 * j + 4
                    items = []
                    for i in range(nt):
                        m = i - 4 * j
                        c0 = 128 * m if m >= 0 else 0
                        items.append((i, m, c0))
                    def qk(it, k):
                        i, m, c0 = it
                        sc = PSB[1 + k % 3]
                        mm(P, sc[:, c0:512], BK[0:70, i * 128:(i + 1) * 128], BQ[0:70, j * 512 + c0:(j + 1) * 512], True, True,
                           BK.r(i * 128, i * 128 + 128) + BQ.r(j * 512, j * 512 + 512), sc.r())
                    def rest(it, k):
                        i, m, c0 = it
                        sc = PSB[1 + k % 3]
                        pt = PTB[k % 4]
                        act(P, pt[:, c0:512], sc[:, c0:512], AF.Exp, sc.r(), pt.r())
                        if m >= 0:
                            P.op("pool", lambda e, pt=pt, c0=c0: e.affine_select(out=pt[:, c0:c0 + 128], in_=pt[:, c0:c0 + 128], pattern=[[1, 128]],
                                 compare_op=ALU.is_ge, fill=0.0, base=0, channel_multiplier=-1), pt.r(), pt.r())
                        mm(P, acc[0:65, c0:512], VAB[:, i, 64:129], pt[:, c0:512], i == 0, i == nt - 1,
                           VAB.r(i * 129, i * 129 + 129) + pt.r(), acc.r())
                    LOOK = 2
                    for idx in range(nt + LOOK):
                        if idx < nt:
                            qk(items[idx], k + idx)
                        if idx >= LOOK:
                            rest(items[idx - LOOK], k + idx - LOOK)
                            if do_hgrn and os.environ.get("HG_NOILV") is None:
                                for g_ in hg_gens:
                                    next(g_, None)
                    k += nt
                    cp(P, "dve", ACCS[0:65, :], acc[0:65, :], acc.r(), ACCS.r())
                    pden = PSB[6]
                    mm(P, pden[0:64, :], ones_f[64:65, 0:64], ACCS[64:65, :], True, True, ones_f.r() + ACCS.r(), pden.r())
                    recip(P, RDEN[:], pden[0:64, :], pden.r(), RDEN.r())
                    obs = OBS[j % 2]
                    tt(P, "pool", obs[:], ACCS[0:64, :], RDEN[:], ALU.mult, ACCS.r() + RDEN.r(), obs.r())
                    dma(P, "sp", oT[64:128, b * T + j * 512: b * T + (j + 1) * 512], obs[:], obs.r(), (), obs.dsem)
            if do_hgrn:
                for g_ in hg_gens:
                    for _ in g_:
                        pass
                DSV = BQ
                NC_ = T // 64
                for g4 in range(T // 512):
                    for c in range(8 * g4, 8 * g4 + 8):
                        ti, hh = c // 2, c % 2
                        pds = PSB[4 + (g4 % 2) * 2 + hh]
                        slot = (c % 8) * 64
                        hs = slice(hh * 64, (hh + 1) * 64)
                        mm(P, pds[0:64, slot:slot + 64], KH[hs, ti, :], VAB[hs, ti, 0:64], True, True,
                           KH.r(ti * 64, ti * 64 + 64) + VAB.r(ti * 129, ti * 129 + 129), pds.r())
                    for hh in range(2):
                        pds = PSB[4 + (g4 % 2) * 2 + hh]
                        src = pds[0:64, :].rearrange("p (a b) -> p a b", b=128)[:, :, hh * 64:(hh + 1) * 64]
                        dst = DSV[0:64, :].rearrange("p (v c) -> p c v", c=NC_)[:, 8 * g4 + hh:8 * g4 + 8:2, :]
                        cp(P, "dve" if hh == 0 else "act", dst, src, pds.r(), DSV.r())
                cp(P, "pool", ELC0[:], ELC[:], ELC.r(), ELC0.r())
                memset(P, "pool", ELC0[:, 0:1], 0.0, ELC0.r())
                D0 = BK
                cp(P, "act", D0[0:64, :].rearrange("p (v c) -> p v c", c=NC_), ELC0[:, :].unsqueeze(1).broadcast_to([64, 64, NC_]),
                   ELC0.r(), D0.r())
                P.op("dve", lambda e: e.tensor_tensor_scan(out=SBF[:, :], data0=D0[0:64, :], data1=DSV[0:64, :],
                                                           initial=0.0, op0=ALU.mult, op1=ALU.add), D0.r() + DSV.r(), SBF.r())
                for g4 in range(T // 512):
                    pat = PSB[6 + g4 % 2]
                    for j in range(4):
                        ti = g4 * 4 + j
                        tcs = slice(ti * 128, (ti + 1) * 128)
                        mm(P, pat[:, j * 128:(j + 1) * 128], Fh[0:64, tcs], Q[0:64, tcs], True, True,
                           Fh.r(ti * 128, ti * 128 + 128) + Q.r(ti * 128, ti * 128 + 128), pat.r())
                    ats = ATS[g4 % 2]
                    tt(P, "dve", ats[:].rearrange("p (a b) -> p a b", b=128), pat[:, :].rearrange("p (a b) -> p a b", b=128),
                       M2[:].unsqueeze(1).broadcast_to([128, 4, 128]), ALU.mult, pat.r() + M2.r(), ats.r())
                    po = PSB[1 + g4 % 2]
                    for jj in range(4):
                        tj = g4 * 4 + jj
                        for hh in range(2):
                            c = 2 * tj + hh
                            ccs = slice(c * 64, (c + 1) * 64)
                            oc = slice(jj * 128 + hh * 64, jj * 128 + hh * 64 + 64)
                            mm(P, po[0:64, oc], VAB[:, tj, 0:64], ats[:, oc], True, False,
                               VAB.r(tj * 129, tj * 129 + 129) + ats.r(), po.r())
                            st_ap = Z64[:, :] if c == 0 else SBF[:, :].rearrange("p (v c) -> p v c", c=NC_)[:, :, c - 1]
                            mm(P, po[0:64, oc], st_ap, Q[0:64, ccs], False, True,
                               SBF.r() + Z64.r() + Q.r(c * 64, c * 64 + 64), po.r())
                    gcs = slice(g4 * 512, (g4 + 1) * 512)
                    pn = PSB[3]
                    sq64 = SQ
                    lnv = RSTD[g4 % 2]
                    oaf = (ZF, R1)[g4 % 2]
                    act(P, sq64[0:64, g4 % 2, :], po[0:64, :], AF.Square, po.r(), sq64.r())
                    mm(P, pn[0:64, :], ones_bf[0:64, 0:64], sq64[0:64, g4 % 2, :], True, True, sq64.r() + ones_bf.r(), pn.r())
                    act(P, lnv[0:64, :], pn[0:64, :], AF.Ln, pn.r() + eps_c.r(), lnv.r(), scale=1.0 / 64, bias=eps_c[0:64, :])
                    act(P, lnv[0:64, :], lnv[0:64, :], AF.Exp, lnv.r(), lnv.r(), scale=-0.5)
                    stt(P, oaf[0:64, :], po[0:64, :], on_ap, lnv[0:64, :], ALU.mult, ALU.mult, po.r() + lnv.r() + par.r(), oaf.r())
                    oas = OAS[g4 % 2]
                    tt(P, "dve", oas[:], oaf[0:64, :], G[0:64, gcs], ALU.mult, oaf.r() + G.r(g4 * 512, g4 * 512 + 512), oas.r())
                    dma(P, "sp", oT[0:64, b * T + g4 * 512: b * T + (g4 + 1) * 512], oas[:], oas.r(), (), oas.dsem)
        if os.environ.get('HG_DBG'):
            dsd2 = P.new_dsem("dbg2")
            for nm, tl, shp, dt_ in (("dKH", KH, [128, (T // 128) * 64], BF16), ("dKt", Fh, [64, T], BF16), ("dQt", Q, [64, T], BF16), ("dELC", ELC, [64, T // 64], F32), ("dSBF", SBF, [64, T], BF16)):
                dd = nc.dram_tensor(nm, shp, dt_, kind="ExternalOutput").ap()
                src = tl[:] if len(tl.h.shape) == 2 else tl[:].rearrange("p a b -> p (a b)")
                dma(P, "sp", dd, src, tl.r(), (), dsd2)
        if debug:
            dsd = P.new_dsem("dbg")
            for nm, tl, shp in (("dQ", Q, [64, T]), ("dF", Fh, [64, T]), ("dG", G, [64, T]), ("dBQ", BQ, [70, T]), ("dBK", BK, [70, T]), ("dV", VAB, [128, (T // 128) * 129])):
                dd = nc.dram_tensor(nm, shp, BF16, kind="ExternalOutput").ap()
                src = tl[:] if nm != "dV" else tl[:].rearrange("p a b -> p (a b)")
                dma(P, "sp", dd, src, tl.r(), (), dsd)
        P.replay()
    return nc


NT = 2048


def build_B():
    nc = bass.Bass("TRN2", target_bir_lowering=False)
    hT_in = nc.dram_tensor("hT_in", [1024, NT], F32, kind="ExternalInput").ap()
    oT_in = nc.dram_tensor("oT_in", [1024, NT], BF16, kind="ExternalInput").ap()
    w_out = nc.dram_tensor("w_out", [1024, 1024], F32, kind="ExternalInput").ap()
    gffn = nc.dram_tensor("gffn", [128, 8], F32, kind="ExternalInput").ap()
    w_r = nc.dram_tensor("w_r", [1024, 36], F32, kind="ExternalInput").ap()
    w_gate = nc.dram_tensor("w_gate", [32, 1024, 512], F32, kind="ExternalInput").ap()
    w_up = nc.dram_tensor("w_up", [32, 1024, 512], F32, kind="ExternalInput").ap()
    w_down = nc.dram_tensor("w_down", [32, 512, 1024], F32, kind="ExternalInput").ap()
    hT_out = nc.dram_tensor("hT_out", [1024, NT], F32, kind="ExternalOutput").ap()
    P = Prog(nc)
    hT_v = hT_in.rearrange("(kc p) t -> p kc t", p=128)
    oT_v = oT_in.rearrange("(kc p) t -> p kc t", p=128)
    ho_v = hT_out.rearrange("(kc p) t -> p kc t", p=128)
    with ExitStack() as st:
        def SB(name, shape, dt, bs=None, dsem=False):
            return TT(P, st, name, shape, dt, bs=bs, dsem=dsem)

        ones_bf = SB("ones_bf", [128, 128], BF16)
        eps_c = SB("eps_c", [128, 1], F32)
        identf = SB("identf", [128, 128], F32)
        SEL = SB("SEL", [32, 32, 128], BF16)
        gf = SB("gf", [128, 8], F32, dsem=True)
        WR = SB("WR", [128, 8, 36], F32, dsem=True)
        HM = SB("HM", [128, 8, NT], F32, bs=512, dsem=True)
        U = SB("U", [128, 8, NT], BF16, bs=512)
        WBUF = [SB("WB%d" % i, [128, 12288], BF16, dsem=True) for i in range(2)]
        OTB = [SB("OTB%d" % i, [128, 8, 512], BF16, dsem=True) for i in range(2)]
        HID = [SB("HID%d" % i, [128, 4, 512], BF16) for i in range(2)]
        SIL = [SB("SIL%d" % i, [128, 512], BF16) for i in range(2)]
        S2 = [SB("S2%d" % i, [128, 512], BF16) for i in range(2)]
        CREP = [SB("CREP%d" % i, [128, 512], BF16) for i in range(2)]
        CT = SB("CT", [32, NT], BF16, bs=128)
        LNV = SB("LNV", [128, 512], F32)
        RSTD = SB("RSTD", [128, 512], F32)
        RLA = SB("RLA", [128, NT // 128, 36], F32)
        R16 = SB("R16", [128, 8, NT // 128], F32)
        OHA = SB("OHA", [128, NT // 128, 4], F32)
        EGA = SB("EGA", [128, NT // 128, 4], F32)
        SELA = SB("SELA", [128, NT // 128, 8], F32)
        TMP8 = SB("TMP8", [128, NT // 128, 8], F32)
        E1A = SB("E1A", [128, NT // 128, 8], F32)
        E2A = SB("E2A", [128, NT // 128, 8], F32)
        COMBA = SB("COMBA", [128, NT // 128, 32], F32)
        RL = SB("RL", [128, 36], F32)
        RT = SB("RT", [128, 16], F32)
        OH = SB("OH", [128, 4], F32)
        EG = SB("EG", [128, 4], F32)
        SEL8 = SB("SEL8", [128, 8], F32)
        M8 = SB("M8", [128, 8], F32)
        WA8 = SB("WA8", [128, 8], F32)
        WB8 = SB("WB8", [128, 8], F32)
        COMB = SB("COMB", [128, 32], F32)
        PSB = [TT(P, st, "ps%d" % i, [128, 512], F32, psum=True) for i in range(8)]
        Wo = WBUF[1]
        Wo_v = Wo[:, 0:8192].rearrange("p (a b) -> p a b", b=1024)
        UFt = WBUF[0]
        UF_v = UFt.h.bitcast(F32)[:, 0:4096].rearrange("p (a b) -> p a b", b=512)
        SQt = HID[0]
        SQ = SB("SQ", [128, 8, 512], BF16)

        memset(P, "pool", ones_bf[:], 1.0, ones_bf.r())
        memset(P, "pool", eps_c[:], EPS, eps_c.r())
        memset(P, "pool", identf[:], 1.0, identf.r())
        P.op("pool", lambda e: e.affine_select(out=identf[:], in_=identf[:], pattern=[[-1, 128]], compare_op=ALU.is_equal,
                                               fill=0.0, base=0, channel_multiplier=1), identf.r(), identf.r())
        memset(P, "pool", SEL[:], 1.0, SEL.r())
        P.op("pool", lambda e: e.affine_select(out=SEL[:], in_=SEL[:], pattern=[[-1, 32], [0, 128]], compare_op=ALU.is_equal,
                                               fill=0.0, base=0, channel_multiplier=1), SEL.r(), SEL.r())
        dma(P, "sp", gf[:], gffn, (), gf.r(), gf.dsem)
        dma(P, "sp", WR[:], w_r.rearrange("(kc p) n -> p kc n", p=128), (), WR.r(), WR.dsem)
        dma(P, "pool", Wo_v, w_out.rearrange("(kc p) n -> p kc n", p=128), (), Wo.r(), Wo.dsem)
        for h2 in range(2):
            dma(P, "sp", HM[:, :, h2 * 1024:(h2 + 1) * 1024], hT_v[:, :, h2 * 1024:(h2 + 1) * 1024], (), HM.r(), HM.dsem)

        def stage_a(blk):
            cs = slice(blk * 512, (blk + 1) * 512)
            ot = OTB[blk % 2]
            dma(P, "sp", ot[:], oT_v[:, :, cs], (), ot.r(), ot.dsem)
            for dc in range(8):
                ps = PSB[dc % 2]
                for kc in range(8):
                    mm(P, ps[:, :], Wo_v[:, kc, dc * 128:(dc + 1) * 128], ot[:, kc, :], kc == 0, kc == 7, Wo.r() + ot.r(), ps.r())
                lo = dc * NT + blk * 512
                tt(P, "dve", HM[:, dc, cs], HM[:, dc, cs], ps[:, :], ALU.add, HM.r(lo, lo + 512) + ps.r(), HM.r(lo, lo + 512))

        def stage_b(blk):
            cs = slice(blk * 512, (blk + 1) * 512)
            hm_blk = []
            for dc in range(8):
                hm_blk += HM.r(dc * NT + blk * 512, dc * NT + blk * 512 + 512)
            act(P, SQ[:], HM[:, :, cs], AF.Square, hm_blk, SQ.r())
            ss = PSB[2]
            for kc in range(8):
                mm(P, ss[:, :], ones_bf[:, :], SQ[:, kc, :], kc == 0, kc == 7, SQ.r() + ones_bf.r(), ss.r())
            act(P, LNV[:], ss[:, :], AF.Ln, ss.r() + eps_c.r(), LNV.r(), scale=1.0 / 1024, bias=eps_c[:])
            act(P, RSTD[:], LNV[:], AF.Exp, LNV.r(), RSTD.r(), scale=-0.5)
            u_blk = []
            for dc in range(8):
                lo = dc * NT + blk * 512
                stt(P, UF_v[:, dc, :], HM[:, dc, cs], gf[:, dc:dc + 1], RSTD[:], ALU.mult, ALU.mult,
                    HM.r(lo, lo + 512) + gf.r() + RSTD.r(), UFt.r())
                u_blk += U.r(lo, lo + 512)
            cp(P, "act", U[:, :, cs], UF_v, UFt.r(), u_blk)
            pr = PSB[3]
            for sub in range(4):
                scs = slice(sub * 128, (sub + 1) * 128)
                for dc in range(8):
                    mm(P, pr[:, sub * 36:(sub + 1) * 36], UF_v[:, dc, scs], WR[:, dc, :], dc == 0, dc == 7, UFt.r() + WR.r(), pr.r())
            cp(P, "dve", RLA[:, blk * 4:(blk + 1) * 4, :].rearrange("p a b -> p (a b)"), pr[:, 0:144], pr.r(), RLA.r())

        nblk_ = NT // 512
        for blk in range(nblk_ + 1):
            if blk < nblk_:
                stage_a(blk)
            if blk >= 1:
                stage_b(blk - 1)

        S_ = NT // 128
        def b3(ap2, n):
            return ap2.unsqueeze(2).broadcast_to([128, S_, n])
        GL = RLA[:, :, 0:4]
        P.op("dve", lambda e: e.tensor_reduce(out=R16[:, 0, :], in_=GL, axis=AX.X, op=ALU.max), RLA.r(), R16.r())
        tt(P, "dve", OHA[:], GL, b3(R16[:, 0, :], 4), ALU.is_equal, RLA.r() + R16.r(), OHA.r())
        tt(P, "dve", EGA[:], GL, b3(R16[:, 0, :], 4), ALU.subtract, RLA.r() + R16.r(), EGA.r())
        act(P, EGA[:], EGA[:], AF.Exp, EGA.r(), EGA.r())
        P.op("dve", lambda e: e.tensor_reduce(out=R16[:, 1, :], in_=EGA[:], axis=AX.X, op=ALU.add), EGA.r(), R16.r())
        recip(P, R16[:, 2, :], R16[:, 1, :], R16.r(), R16.r())
        tt(P, "dve", SELA[:], RLA[:, :, 4:12], b3(OHA[:, :, 0], 8), ALU.mult, RLA.r() + OHA.r(), SELA.r())
        for g in range(1, 4):
            tt(P, "dve", TMP8[:], RLA[:, :, 4 + 8 * g:12 + 8 * g], b3(OHA[:, :, g], 8), ALU.mult, RLA.r() + OHA.r(), TMP8.r())
            tt(P, "dve", SELA[:], SELA[:], TMP8[:], ALU.add, SELA.r() + TMP8.r(), SELA.r())
        P.op("dve", lambda e: e.tensor_reduce(out=R16[:, 3, :], in_=SELA[:], axis=AX.X, op=ALU.max), SELA.r(), R16.r())
        tt(P, "dve", E1A[:], SELA[:], b3(R16[:, 3, :], 8), ALU.is_equal, SELA.r() + R16.r(), E1A.r())
        ts(P, "dve", TMP8[:], E1A[:], -1.0e30, ALU.mult, E1A.r(), TMP8.r())
        tt(P, "dve", TMP8[:], SELA[:], TMP8[:], ALU.add, SELA.r() + TMP8.r(), TMP8.r())
        P.op("dve", lambda e: e.tensor_reduce(out=R16[:, 4, :], in_=TMP8[:], axis=AX.X, op=ALU.max), TMP8.r(), R16.r())
        tt(P, "dve", E2A[:], TMP8[:], b3(R16[:, 4, :], 8), ALU.is_equal, TMP8.r() + R16.r(), E2A.r())
        tt(P, "dve", R16[:, 5, :], R16[:, 4, :], R16[:, 3, :], ALU.subtract, R16.r(), R16.r())
        act(P, R16[:, 5, :], R16[:, 5, :], AF.Exp, R16.r(), R16.r())
        ts(P, "dve", R16[:, 5, :], R16[:, 5, :], 1.0, ALU.add, R16.r(), R16.r())
        recip(P, R16[:, 6, :], R16[:, 5, :], R16.r(), R16.r())
        ts(P, "dve", R16[:, 7, :], R16[:, 6, :], -1.0, ALU.mult, R16.r(), R16.r(), 1.0, ALU.add)
        tt(P, "dve", R16[:, 6, :], R16[:, 6, :], R16[:, 2, :], ALU.mult, R16.r(), R16.r())
        tt(P, "dve", R16[:, 7, :], R16[:, 7, :], R16[:, 2, :], ALU.mult, R16.r(), R16.r())
        tt(P, "dve", E1A[:], E1A[:], b3(R16[:, 6, :], 8), ALU.mult, E1A.r() + R16.r(), E1A.r())
        tt(P, "dve", E2A[:], E2A[:], b3(R16[:, 7, :], 8), ALU.mult, E2A.r() + R16.r(), E2A.r())
        tt(P, "dve", E1A[:], E1A[:], E2A[:], ALU.add, E1A.r() + E2A.r(), E1A.r())
        tt(P, "dve", COMBA[:].rearrange("p s (g e) -> p s g e", e=8),
           E1A[:].unsqueeze(2).broadcast_to([128, S_, 4, 8]), OHA[:].unsqueeze(3).broadcast_to([128, S_, 4, 8]), ALU.mult,
           E1A.r() + OHA.r(), COMBA.r())
        for q4 in range(S_ // 4):
            pt = PSB[4 + q4 % 2]
            for sub in range(4):
                sidx = q4 * 4 + sub
                P.op("pe", lambda e, pt=pt, sub=sub, sidx=sidx: e.transpose(out=pt[0:32, sub * 128:(sub + 1) * 128], in_=COMBA[:, sidx, :], identity=identf[:, :]),
                     COMBA.r() + identf.r(), pt.r())
            cp(P, "dve", CT[0:32, q4 * 512:(q4 + 1) * 512], pt[0:32, :], pt.r(), CT.r(q4 * 512, q4 * 512 + 512))

        units = [(e, blk) for e in range(32) for blk in range(NT // 512)]
        wviews = []
        for i in range(2):
            wb = WBUF[i]
            wviews.append((wb[:, 0:4096].rearrange("p (a b) -> p a b", b=512),
                           wb[:, 4096:8192].rearrange("p (a b) -> p a b", b=512),
                           wb[:, 8192:12288].rearrange("p (a b) -> p a b", b=1024)))

        def load_w(e):
            wb = WBUF[(e + 1) % 2]
            Wg, Wu, Wd = wviews[(e + 1) % 2]
            dma(P, "pool", Wg, w_gate[e].rearrange("(kc p) n -> p kc n", p=128), (), wb.r(), wb.dsem)
            dma(P, "pool", Wu, w_up[e].rearrange("(kc p) n -> p kc n", p=128), (), wb.r(), wb.dsem)
            dma(P, "pool", Wd, w_down[e].rearrange("(fc p) n -> p fc n", p=128), (), wb.r(), wb.dsem)

        def GU(ui):
            e, blk = units[ui]
            cs = slice(blk * 512, (blk + 1) * 512)
            wb = WBUF[(e + 1) % 2]
            Wg, Wu, Wd = wviews[(e + 1) % 2]
            hid = HID[ui % 2]
            crep = CREP[ui % 2]
            pc = PSB[6]
            mm(P, pc[:, :], SEL[0:32, e, :], CT[0:32, cs], True, True, SEL.r() + CT.r(blk * 512, blk * 512 + 512), pc.r())
            cp(P, "dve", crep[:], pc[:, :], pc.r(), crep.r())
            u_blk = []
            for dc in range(8):
                u_blk += U.r(dc * NT + blk * 512, dc * NT + blk * 512 + 512)
            for fc in range(4):
                pg_ = PSB[(fc % 2) * 2]
                pu_ = PSB[(fc % 2) * 2 + 1]
                fs = slice(fc * 128, (fc + 1) * 128)
                for dc in range(8):
                    mm(P, pg_[:, :], Wg[:, dc, fs], U[:, dc, cs], dc == 0, dc == 7, wb.r() + u_blk, pg_.r())
                for dc in range(8):
                    mm(P, pu_[:, :], Wu[:, dc, fs], U[:, dc, cs], dc == 0, dc == 7, wb.r() + u_blk, pu_.r())
                sil = SIL[fc % 2]
                s2 = S2[fc % 2]
                act(P, sil[:], pg_[:, :], AF.Silu, pg_.r(), sil.r())
                tt(P, "dve", s2[:], sil[:], crep[:], ALU.mult, sil.r() + crep.r(), s2.r())
                tt(P, "dve", hid[:, fc, :], s2[:], pu_[:, :], ALU.mult, s2.r() + pu_.r(), hid.r())

        def DN(ui):
            e, blk = units[ui]
            cs = slice(blk * 512, (blk + 1) * 512)
            wb = WBUF[(e + 1) % 2]
            Wg, Wu, Wd = wviews[(e + 1) % 2]
            hid = HID[ui % 2]
            for dc in range(8):
                py = PSB[4 + dc % 2]
                for fc in range(4):
                    mm(P, py[:, :], Wd[:, fc, dc * 128:(dc + 1) * 128], hid[:, fc, :], fc == 0, fc == 3, wb.r() + hid.r(), py.r())
                lo = dc * NT + blk * 512
                tt(P, "dve", HM[:, dc, cs], HM[:, dc, cs], py[:, :], ALU.add, HM.r(lo, lo + 512) + py.r(), HM.r(lo, lo + 512))

        nE = int(os.environ.get("KB_NE", 32))
        units = [u_ for u_ in units if u_[0] < nE]
        load_w(0)
        for ui in range(len(units) + 1):
            if ui < len(units):
                GU(ui)
            if ui >= 1:
                DN(ui - 1)
            if ui < len(units):
                e, blk = units[ui]
                if blk == 0 and e + 1 < nE:
                    load_w(e + 1)
        for h2 in range(2):
            rr = []
            for dc in range(8):
                rr += HM.r(dc * NT + h2 * 1024, dc * NT + h2 * 1024 + 1024)
            dma(P, "sp", ho_v[:, :, h2 * 1024:(h2 + 1) * 1024], HM[:, :, h2 * 1024:(h2 + 1) * 1024], rr, (), HM.dsem)
        P.replay()
    return nc


import math

NCOL_C = 384
TWO_PI = 2.0 * math.pi
LAM_INIT = 0.8 - 0.6 * math.exp(-0.3 * 1)


def build_C(nbatch=2, debug=False):
    nc = bass.Bass("TRN2", target_bir_lowering=False)
    xT = nc.dram_tensor("xT", [1024, TOK], F32, kind="ExternalInput").ap()
    wC = nc.dram_tensor("wC", [1024, NCOL_C], F32, kind="ExternalInput").ap()
    gmix = nc.dram_tensor("gmix", [128, 8], F32, kind="ExternalInput").ap()
    pC = nc.dram_tensor("pC", [128, 8], F32, kind="ExternalInput").ap()
    posd = nc.dram_tensor("pos", [2, T], I32, kind="ExternalInput").ap()
    oT = nc.dram_tensor("oT", [128, TOK], BF16, kind="ExternalOutput").ap()
    P = Prog(nc)
    xT_v = xT.rearrange("(kc p) t -> p kc t", p=128)
    with ExitStack() as st:
        def SB(name, shape, dt, bs=None, dsem=False):
            return TT(P, st, name, shape, dt, bs=bs, dsem=dsem)

        ones_bf = SB("ones_bf", [128, 128], BF16)
        BD = SB("BD", [128, 128], BF16)
        ones_f = SB("ones_f", [128, 128], F32)
        eps_c = SB("eps_c", [128, 1], F32)
        pi_c = SB("pi_c", [128, 2], F32)
        pic2 = SB("pic2", [128, 2], F32)
        ident = SB("ident", [128, 128], BF16)
        PM = SB("PM", [128, 128], BF16)
        PM2 = SB("PM2", [128, 128], BF16)
        par = SB("par", [128, 8], F32, dsem=True)
        gm = SB("gm", [128, 8], F32, dsem=True)
        cst = SB("cst", [128, 8], F32)
        invi = SB("invi", [1, 128], I32)
        invf = SB("invf", [1, 128], F32)
        Wb = SB("Wb", [128, 8, NCOL_C], BF16, dsem=True)

        memset(P, "pool", ones_bf[:], 1.0, ones_bf.r())
        memset(P, "pool", ones_f[:], 1.0, ones_f.r())
        memset(P, "pool", eps_c[:], EPS, eps_c.r())
        memset(P, "pool", BD[:], 1.0, BD.r())
        memset(P, "pool", BD[0:64, 64:128], 0.0, BD.r())
        memset(P, "pool", BD[64:128, 0:64], 0.0, BD.r())
        memset(P, "pool", pi_c[:, 0:1], math.pi, pi_c.r())
        memset(P, "pool", pi_c[:, 1:2], -TWO_PI, pi_c.r())
        memset(P, "pool", pi_c[0:32, 0:1], -math.pi, pi_c.r())
        memset(P, "pool", pi_c[0:32, 1:2], TWO_PI, pi_c.r())
        memset(P, "pool", pi_c[64:96, 0:1], -math.pi, pi_c.r())
        memset(P, "pool", pi_c[64:96, 1:2], TWO_PI, pi_c.r())
        memset(P, "pool", pic2[:, 0:1], math.pi, pic2.r())
        memset(P, "pool", PM[:], 1.0, PM.r())
        P.op("pool", lambda e: e.affine_select(out=PM[:], in_=PM[:], pattern=[[1, 128]], compare_op=ALU.is_equal,
                                               fill=0.0, base=-32, channel_multiplier=-1), PM.r(), PM.r())
        memset(P, "pool", PM[:, 0:32], 0.0, PM.r())
        memset(P, "pool", PM[:, 64:96], 0.0, PM.r())
        memset(P, "pool", PM2[:], 1.0, PM2.r())
        P.op("pool", lambda e: e.affine_select(out=PM2[:], in_=PM2[:], pattern=[[1, 128]], compare_op=ALU.is_equal,
                                               fill=0.0, base=32, channel_multiplier=-1), PM2.r(), PM2.r())
        memset(P, "pool", PM2[:, 32:64], 0.0, PM2.r())
        memset(P, "pool", PM2[:, 96:128], 0.0, PM2.r())
        tt(P, "pool", PM[:], PM[:], PM2[:], ALU.add, PM.r() + PM2.r(), PM.r())
        P.op("pool", lambda e: e.iota(invi[:].rearrange("p (a b) -> p a b", b=32), pattern=[[0, 4], [1, 32]], base=0, channel_multiplier=0),
             (), invi.r())
        cp(P, "dve", invf[:], invi[:], invi.r(), invf.r())
        act(P, invf[:], invf[:], AF.Exp, invf.r(), invf.r(), scale=-math.log(10000.0) / 32.0)

        dma(P, "sp", par[:], pC, (), par.r(), par.dsem)
        dma(P, "sp", gm[:], gmix, (), gm.r(), gm.dsem)
        dma(P, "pool", Wb[:], wC.rearrange("(kc p) n -> p kc n", p=128), (), Wb.r(), Wb.dsem)
        for kc in range(8):
            ts(P, "dve", Wb[:, kc, :], Wb[:, kc, :], gm[:, kc:kc + 1], ALU.mult, Wb.r() + gm.r(), Wb.r())
        ts(P, "dve", cst[:, 0:1], par[:, 0:1], 0.125, ALU.mult, par.r(), cst.r())
        ts(P, "dve", cst[:, 3:4], par[:, 6:7], 1.0 - LAM_INIT, ALU.mult, par.r(), cst.r())
        PSB = [TT(P, st, "ps%d" % i, [128, 512], F32, psum=True) for i in range(8)]
        tt(P, "dve", cst[:, 4:5], par[:, 2:3], par[:, 3:4], ALU.mult, par.r(), cst.r())
        tt(P, "dve", cst[:, 5:6], par[:, 4:5], par[:, 5:6], ALU.mult, par.r(), cst.r())
        mm(P, PSB[0][:, 0:2], ones_f[:, :], cst[:, 4:6], True, True, ones_f.r() + cst.r(), PSB[0].r())
        act(P, cst[:, 6:8], PSB[0][:, 0:2], AF.Exp, PSB[0].r(), cst.r())
        tt(P, "dve", cst[:, 2:3], cst[:, 7:8], cst[:, 6:7], ALU.subtract, cst.r(), cst.r())
        ts(P, "dve", cst[:, 2:3], cst[:, 2:3], -LAM_INIT, ALU.add, cst.r(), cst.r())
        gq_ap, gk_ap, nlam_ap, gs_ap = cst[:, 0:1], par[:, 1:2], cst[:, 2:3], cst[:, 3:4]

        QT = SB("QT", [128, T], BF16, bs=512)
        KT = SB("KT", [128, T], BF16, bs=128)
        V = SB("V", [128, T // 128, 128], BF16, bs=128)
        SINT = SB("SINT", [128, T], BF16, bs=512)
        COST = SB("COST", [128, T], BF16, bs=512)
        XB = [SB("XB%d" % i, [128, 8, 512], BF16, dsem=True) for i in range(2)]
        SQ = SB("SQ", [128, 8, 512], BF16)
        LNV = SB("LNV", [128, 512], F32)
        RSTD = [SB("RSTD%d" % i, [128, 512], F32) for i in range(2)]
        RCS = SB("RCS", [128, 4], F32)
        POSI = SB("POSI", [1, 512], I32, dsem=True)
        POSF = SB("POSF", [1, 512], F32)
        RR = [SB("RR%d" % i, [128, 512], F32) for i in range(2)]
        RI = SB("RI", [128, 512], I32)
        RF = SB("RF", [128, 512], F32)
        RMK = SB("RMK", [128, 512], F32)
        TA = SB("TA", [128, 512], BF16)
        TB_ = SB("TB", [128, 512], BF16)
        PTB = [[SB("PT%d_%d" % (m, i), [128, 512], BF16) for i in range(3)] for m in range(2)]
        FT = [SB("FT%d" % i, [128, 512], F32) for i in range(4)]
        OUTS = [SB("OUTS%d" % i, [128, 512], BF16, dsem=True) for i in range(2)]
        DACC = [SB("DACC%d" % i, [128, 512], F32) for i in range(2)]
        TMPP = [[SB("TMPP%d_%d" % (m, i), [128, 512], BF16) for i in range(2)] for m in range(2)]

        groups = [(0, QT), (128, KT)]
        blk = 0
        for b in range(nbatch):
            for n in range(T // 512):
                tok0 = b * T + n * 512
                cols = (n * 512, (n + 1) * 512)
                xb = XB[blk % 2]
                rs = RSTD[blk % 2]
                dma(P, "pool", xb[:], xT_v[:, :, tok0:tok0 + 512], (), xb.r(), xb.dsem)
                act(P, SQ[:], xb[:], AF.Square, xb.r(), SQ.r())
                ss = PSB[0]
                for kc in range(8):
                    mm(P, ss[:, :], ones_bf[:, :], SQ[:, kc, :], kc == 0, kc == 7, SQ.r() + ones_bf.r(), ss.r())
                act(P, LNV[:], ss[:, :], AF.Ln, ss.r() + eps_c.r(), LNV.r(), scale=1.0 / 1024, bias=eps_c[:])
                act(P, rs[:], LNV[:], AF.Exp, LNV.r(), rs.r(), scale=-0.5)
                for gi, (c0, dest) in enumerate(groups):
                    ps = PSB[1 + gi]
                    for kc in range(8):
                        mm(P, ps[:, :], Wb[:, kc, c0:c0 + 128], xb[:, kc, :], kc == 0, kc == 7, Wb.r() + xb.r(), ps.r())
                    tt(P, "dve", dest[:, cols[0]:cols[1]], ps[:, :], rs[:, :], ALU.mult, ps.r() + rs.r(), dest.r(*cols))
                ptm = PSB[3]
                prc = PSB[4]
                for sub in range(4):
                    sc = slice(sub * 128, (sub + 1) * 128)
                    for kc in range(8):
                        mm(P, ptm[:, sc], xb[:, kc, sc], Wb[:, kc, 256:384], kc == 0, kc == 7, Wb.r() + xb.r(), ptm.r())
                    mm(P, prc[:, sub:sub + 1], rs[0:1, sc], ones_f[0:1, 0:1], True, True, rs.r() + ones_f.r(), prc.r())
                cp(P, "dve", RCS[:, 0:4], prc[:, 0:4], prc.r(), RCS.r())
                for sub in range(4):
                    ti = n * 4 + sub
                    sc = slice(sub * 128, (sub + 1) * 128)
                    ts(P, "dve", V[:, ti, :], ptm[:, sc], RCS[:, sub:sub + 1], ALU.mult, ptm.r() + RCS.r(), V.r(ti * 128, ti * 128 + 128))
                blk += 1
            for n in range(T // 512):
                cols = (n * 512, (n + 1) * 512)
                cs = slice(*cols)
                dma(P, "sp", POSI[:], posd[b:b + 1, cs], (), POSI.r(), POSI.dsem)
                cp(P, "dve", POSF[:], POSI[:], POSI.r(), POSF.r())
                pa = PSB[5]
                mm(P, pa[:, :], invf[0:1, :], POSF[0:1, :], True, True, invf.r() + POSF.r(), pa.r())
                for which, dest in ((0, SINT), (1, COST)):
                    rr = RR[which]
                    if which == 0:
                        ts(P, "dve", rr[:], pa[:, :], 1.0 / TWO_PI, ALU.mult, pa.r(), rr.r())
                    else:
                        ts(P, "dve", rr[:], pa[:, :], 1.0 / TWO_PI, ALU.mult, pa.r(), rr.r(), 0.25, ALU.add)
                    cp(P, "dve", RI[:], rr[:], rr.r(), RI.r())
                    cp(P, "act", RF[:], RI[:], RI.r(), RF.r())
                    tt(P, "dve", rr[:], rr[:], RF[:], ALU.subtract, rr.r() + RF.r(), rr.r())
                    ts(P, "dve", RMK[:], rr[:], 0.0, ALU.is_lt, rr.r(), RMK.r())
                    tt(P, "dve", rr[:], rr[:], RMK[:], ALU.add, rr.r() + RMK.r(), rr.r())
                    if which == 0:
                        P.op("act", lambda e, rr=rr, dest=dest, cs=cs: e.activation(out=dest[:, cs], in_=rr[:], func=AF.Sin, scale=pi_c[:, 1:2], bias=pi_c[:, 0:1]),
                             rr.r() + pi_c.r(), dest.r(*cols))
                    else:
                        P.op("act", lambda e, rr=rr, dest=dest, cs=cs: e.activation(out=dest[:, cs], in_=rr[:], func=AF.Sin, scale=-TWO_PI, bias=pic2[:, 0:1]),
                             rr.r() + pic2.r(), dest.r(*cols))
            for n in range(T // 512):
                cols = (n * 512, (n + 1) * 512)
                cs = slice(*cols)
                for (tl, gap, bank) in ((QT, gq_ap, 1), (KT, gk_ap, 2)):
                    ps = PSB[bank]
                    act(P, SQ[:, 0, :], tl[:, cs], AF.Square, tl.r(*cols), SQ.r())
                    mm(P, ps[:, :], BD[:, :], SQ[:, 0, :], True, True, SQ.r() + BD.r(), ps.r())
                    act(P, LNV[:], ps[:, :], AF.Ln, ps.r() + eps_c.r(), LNV.r(), scale=1.0 / 64, bias=eps_c[:])
                    act(P, LNV[:], LNV[:], AF.Exp, LNV.r(), LNV.r(), scale=-0.5)
                    stt(P, tl[:, cs], tl[:, cs], gap, LNV[:], ALU.mult, ALU.mult, tl.r(*cols) + LNV.r() + par.r() + cst.r(), tl.r(*cols))
                    pw = PSB[bank + 2]
                    mm(P, pw[:, :], PM[:, :], tl[:, cs], True, True, PM.r() + tl.r(*cols), pw.r())
                    tt(P, "dve", TA[:], tl[:, cs], COST[:, cs], ALU.mult, tl.r(*cols) + COST.r(*cols), TA.r())
                    tt(P, "dve", TB_[:], pw[:, :], SINT[:, cs], ALU.mult, pw.r() + SINT.r(*cols), TB_.r())
                    tt(P, "dve", tl[:, cs], TA[:], TB_[:], ALU.add, TA.r() + TB_.r(), tl.r(*cols))
            if debug:
                continue
            k = 0
            for j in range(T // 512):
                accs = (PSB[4], PSB[5])
                dens = (PSB[6], PSB[7])
                nt = 4 * j + 4
                items = []
                for i in range(nt):
                    m_ = i - 4 * j
                    c0 = 128 * m_ if m_ >= 0 else 0
                    items.append((i, m_, c0))

                def qk(it, k):
                    i, m_, c0 = it
                    for mp in range(2):
                        sc = PSB[(k % 2) * 2 + mp]
                        rows = slice(mp * 64, (mp + 1) * 64)
                        mm(P, sc[:, c0:512], KT[rows, i * 128:(i + 1) * 128], QT[rows, j * 512 + c0:(j + 1) * 512], True, True,
                           KT.r(i * 128, i * 128 + 128) + QT.r(j * 512, j * 512 + 512), sc.r())

                def rest(it, k):
                    i, m_, c0 = it
                    for mp in range(2):
                        sc = PSB[(k % 2) * 2 + mp]
                        pt = PTB[mp][k % 3]
                        act(P, pt[:, c0:512], sc[:, c0:512], AF.Exp, sc.r(), pt.r())
                        if m_ >= 0:
                            memset(P, "pool", pt[64:128, c0:c0 + 64], 0.0, pt.r())
                        mm(P, accs[mp][:, c0:512], V[:, i, :], pt[:, c0:512], i == 0, i == nt - 1, V.r(i * 128, i * 128 + 128) + pt.r(), accs[mp].r())
                        if mp == 0:
                            mm(P, dens[0][:, c0:512], ones_bf[:, :], pt[:, c0:512], i == 0, i == nt - 1, ones_bf.r() + pt.r(), dens[0].r())
                        elif not dinit[mp]:
                            assert c0 == 0
                            cp(P, "dve", DACC[mp][:, :], pt[:, :], pt.r(), DACC[mp].r())
                            dinit[mp] = True
                        else:
                            tt(P, "dve", DACC[mp][:, c0:512], DACC[mp][:, c0:512], pt[:, c0:512], ALU.add, DACC[mp].r() + pt.r(), DACC[mp].r())
                LOOK = 1
                dinit = [False, False]
                pend = [None, None]
                npair = [0, 0]
                for idx in range(nt + LOOK):
                    if idx < nt:
                        qk(items[idx], k + idx)
                    if idx >= LOOK:
                        rest(items[idx - LOOK], k + idx - LOOK)
                k += nt
                for mp in range(1, 2):
                    mm(P, dens[mp][:, :], ones_f[:, :], DACC[mp][:, :], True, True, ones_f.r() + DACC[mp].r(), dens[mp].r())
                recip(P, FT[0][:], dens[0][:, :], dens[0].r(), FT[0].r())
                tt(P, "dve", FT[1][:], accs[0][:, :], FT[0][:], ALU.mult, accs[0].r() + FT[0].r(), FT[1].r())
                recip(P, FT[0][:], dens[1][:, :], dens[1].r(), FT[0].r())
                tt(P, "dve", FT[2][:], accs[1][:, :], FT[0][:], ALU.mult, accs[1].r() + FT[0].r(), FT[2].r())
                stt(P, FT[3][:], FT[2][:], nlam_ap, FT[1][:], ALU.mult, ALU.add, FT[2].r() + FT[1].r() + cst.r(), FT[3].r())
                act(P, SQ[:, 0, :], FT[3][:], AF.Square, FT[3].r(), SQ.r())
                pn = PSB[0]
                mm(P, pn[:, :], ones_bf[:, :], SQ[:, 0, :], True, True, SQ.r() + ones_bf.r(), pn.r())
                act(P, LNV[:], pn[:, :], AF.Ln, pn.r() + eps_c.r(), LNV.r(), scale=1.0 / 128, bias=eps_c[:])
                act(P, LNV[:], LNV[:], AF.Exp, LNV.r(), LNV.r(), scale=-0.5)
                outs = OUTS[j % 2]
                stt(P, outs[:], FT[3][:], gs_ap, LNV[:], ALU.mult, ALU.mult, FT[3].r() + LNV.r() + cst.r(), outs.r())
                dma(P, "sp", oT[:, b * T + j * 512: b * T + (j + 1) * 512], outs[:], outs.r(), (), outs.dsem)
        if debug:
            dsd = P.new_dsem("dbg")
            for nm, tl in (("dQ", QT), ("dK", KT), ("dS", SINT), ("dC", COST)):
                dd = nc.dram_tensor(nm, [128, T], BF16, kind="ExternalOutput").ap()
                dma(P, "sp", dd, tl[:], tl.r(), (), dsd)
            dd = nc.dram_tensor("dV", [128, T], BF16, kind="ExternalOutput").ap()
            dma(P, "sp", dd, V[:].rearrange("p a b -> p (a b)"), V.r(), (), dsd)
        P.replay()
    return nc


def _prep_A(inp, xT):
    w = inp["even_w_in"][0]
    gm = np.ascontiguousarray(inp["norm_mix"][0].reshape(8, 128).T)
    maps = []
    for c in range(8):
        sl = lambda base: w[:, base + c * 64: base + (c + 1) * 64]
        wA = np.concatenate([sl(0), sl(512), sl(1536), sl(2048), sl(2560), w[:, 3584 + c:3585 + c], sl(1024), sl(3072)], axis=1)
        pA = np.zeros((64, 8), np.float32)
        pA[:, 0] = inp["hgrn_lb_logits"][0, c * 64:(c + 1) * 64]
        pA[:, 1] = inp["hgrn_lb_logits"][1, c * 64:(c + 1) * 64]
        pA[:, 2] = inp["hgrn_out_norm"][0]
        pA[:, 3] = inp["fox_q_norm"][0]
        pA[:, 4] = inp["fox_k_norm"][0]
        fb = np.empty((128, 1), np.float32)
        fb[:, 0] = inp["fox_f_bias"][0, c]
        maps.append({"xT": xT, "wA": np.ascontiguousarray(wA), "gmix": gm, "pA": pA, "fbias": fb})
    return maps


def _prep_B(inp, layer, hT_full, oT_full, w_out):
    wr = np.concatenate([inp["moe_router_group"][layer]] + [inp["moe_router_expert"][layer, g] for g in range(4)], axis=1)
    wg = np.ascontiguousarray(inp["moe_w_gate"][layer].reshape(32, 1024, 512))
    wu = np.ascontiguousarray(inp["moe_w_up"][layer].reshape(32, 1024, 512))
    wd = np.ascontiguousarray(inp["moe_w_down"][layer].reshape(32, 512, 1024))
    gf = np.ascontiguousarray(inp["norm_ffn"][layer].reshape(8, 128).T)
    wr = np.ascontiguousarray(wr)
    w_out = np.ascontiguousarray(w_out)
    maps = []
    for c in range(8):
        cs = slice(c * NT, (c + 1) * NT)
        maps.append({"hT_in": np.ascontiguousarray(hT_full[:, cs]), "oT_in": np.ascontiguousarray(oT_full[:, cs]),
                     "w_out": w_out, "gffn": gf, "w_r": wr, "w_gate": wg, "w_up": wu, "w_down": wd})
    return maps


def _prep_C(inp, hT_full):
    w = inp["odd_w_in"][0]
    gm = np.ascontiguousarray(inp["norm_mix"][1].reshape(8, 128).T)
    pos = np.ascontiguousarray(inp["positions"].astype(np.int32))
    maps = []
    for c in range(8):
        wC = np.concatenate([w[:, c * 128:(c + 1) * 128], w[:, 1024 + c * 128:1024 + (c + 1) * 128],
                             w[:, 2048 + c * 128:2048 + (c + 1) * 128]], axis=1)
        pC = np.zeros((128, 8), np.float32)
        pC[0:64, 0] = inp["diff_q_norm"][0]; pC[64:128, 0] = inp["diff_q_norm"][0]
        pC[0:64, 1] = inp["diff_k_norm"][0]; pC[64:128, 1] = inp["diff_k_norm"][0]
        pC[0:64, 2] = inp["diff_lambda_q1"][0]; pC[0:64, 3] = inp["diff_lambda_k1"][0]
        pC[0:64, 4] = inp["diff_lambda_q2"][0]; pC[0:64, 5] = inp["diff_lambda_k2"][0]
        pC[:, 6] = inp["diff_subln"][0]
        maps.append({"xT": hT_full, "wC": np.ascontiguousarray(wC), "gmix": gm, "pC": pC, "pos": pos})
    return maps


def _run(nc, maps):
    res = run_bass_kernel_spmd(nc, maps, core_ids=list(range(8)))
    return res.results


def kernel(**inputs):
    inp = {k: np.asarray(v) for k, v in inputs.items()}
    x = inp["x"].astype(np.float32, copy=False).reshape(-1, 1024)
    xT = np.ascontiguousarray(x.T)
    rA = _run(build_A(), _prep_A(inp, xT))
    oT0 = np.empty((1024, TOK), ml_dtypes.bfloat16)
    for c in range(8):
        o = np.asarray(rA[c]["oT"])
        oT0[c * 64:(c + 1) * 64] = o[0:64]
        oT0[512 + c * 64:512 + (c + 1) * 64] = o[64:128]
    rB = _run(build_B(), _prep_B(inp, 0, xT, oT0, inp["even_w_out"][0]))
    h1T = np.ascontiguousarray(np.concatenate([np.asarray(r["hT_out"]) for r in rB], axis=1))
    rC = _run(build_C(), _prep_C(inp, h1T))
    oT1 = np.ascontiguousarray(np.concatenate([np.asarray(r["oT"]) for r in rC], axis=0))
    rD = _run(build_B(), _prep_B(inp, 1, h1T, oT1, inp["odd_w_out"][0]))
    outT = np.concatenate([np.asarray(r["hT_out"]) for r in rD], axis=1)
    return np.ascontiguousarray(outT.T).reshape(2, 8192, 1024).astype(np.float32, copy=False)
```
